# Optimizing a Trainium2 kernel written in Bass

```python
import jax, jax.numpy as jnp
from jax import lax
import numpy as np

D_MODEL = 1024
BATCH = 4
SEQ = 8192
DEPTH = 1

HEAD_DIM = 64
N_SWA_HEADS = 8
N_SWA_KV = 2
WINDOW = 128
BLOCK = 128
N_DSA_HEADS = 4
N_IDX_HEADS = 4
IDX_DIM = 64
TOPK_MAX = 256
N_MEM_HEADS = 4
MEM_LEN = 256
N_BRANCH = 3
D_FF = 4 * D_MODEL
ROPE_THETA = 10000.0
EPS = 1e-6

SWA_WIDTH = N_SWA_HEADS * HEAD_DIM
SWA_KV_WIDTH = N_SWA_KV * HEAD_DIM
DSA_WIDTH = N_DSA_HEADS * HEAD_DIM
MEM_WIDTH = N_MEM_HEADS * HEAD_DIM
IN_SPLITS = (SWA_WIDTH, SWA_KV_WIDTH, SWA_KV_WIDTH,
             DSA_WIDTH, HEAD_DIM, HEAD_DIM,
             N_IDX_HEADS * IDX_DIM, IDX_DIM, N_IDX_HEADS,
             MEM_WIDTH, N_BRANCH * D_MODEL)
D_IN = sum(IN_SPLITS)

kernel_name = "hybrid_swa_dsa_memory_gated_block"


def rmsnorm(x, g):
    xf = x.astype(jnp.float32)
    y = xf * lax.rsqrt(jnp.mean(xf * xf, axis=-1, keepdims=True) + EPS)
    return (y * g.astype(jnp.float32)).astype(x.dtype)


def rope(x, pos):
    half = x.shape[-1] // 2
    inv_freq = jnp.power(ROPE_THETA, -jnp.arange(half, dtype=jnp.float32) / half)
    ang = pos.astype(jnp.float32)[..., None] * inv_freq
    cos = jnp.cos(ang)[:, :, None, :].astype(x.dtype)
    sin = jnp.sin(ang)[:, :, None, :].astype(x.dtype)
    x1, x2 = x[..., :half], x[..., half:]
    return jnp.concatenate([x1 * cos - x2 * sin, x2 * cos + x1 * sin], axis=-1)


def sliding_window_attention(q, k, v, sinks):
    B, S, Hq, D = q.shape
    Hkv = k.shape[2]
    G = Hq // Hkv
    nb = S // BLOCK
    qb = q.reshape(B, nb, BLOCK, Hkv, G, D)

    def with_prev(t):
        tb = t.reshape(B, nb, BLOCK, Hkv, D)
        prev = jnp.concatenate([jnp.zeros_like(tb[:, :1]), tb[:, :-1]], axis=1)
        return jnp.concatenate([prev, tb], axis=2)

    kw, vw = with_prev(k), with_prev(v)
    scores = jnp.einsum("bnqkgd,bnskd->bnkgqs", qb, kw).astype(jnp.float32) * (D ** -0.5)
    qi = jnp.arange(BLOCK)[:, None]
    sj = jnp.arange(2 * BLOCK)[None, :]
    dist = qi + BLOCK - sj
    blk = jnp.arange(nb)[:, None, None]
    valid = (dist >= 0) & (dist < WINDOW) & ((blk > 0) | (sj >= BLOCK))
    scores = jnp.where(valid[None, :, None, None], scores, -jnp.inf)
    sink = jnp.broadcast_to(
        sinks.reshape(Hkv, G)[None, None, :, :, None, None].astype(jnp.float32),
        scores.shape[:-1] + (1,))
    probs = jax.nn.softmax(jnp.concatenate([scores, sink], axis=-1), axis=-1)[..., :-1]
    out = jnp.einsum("bnkgqs,bnskd->bnqkgd", probs.astype(vw.dtype), vw)
    return out.reshape(B, S, Hq * D)


def dsa_attention(q, k, v, q_idx, k_idx, w_idx):
    B, S = q.shape[:2]
    nb = S // BLOCK
    top_k = min(TOPK_MAX, S // 4)
    key_pos = jnp.arange(S)
    gather = jax.vmap(lambda t, i: t[i])

    def to_blocks(t):
        return jnp.moveaxis(t.reshape((B, nb, BLOCK) + t.shape[2:]), 1, 0)

    def block_fn(args):
        qb, qib, wb, blk = args
        qpos = blk * BLOCK + jnp.arange(BLOCK)
        rel = jax.nn.relu(jnp.einsum("bqhd,bsd->bqhs", qib, k_idx).astype(jnp.float32))
        iscore = jnp.einsum("bqhs,bqh->bqs", rel, wb.astype(jnp.float32))
        iscore = jnp.where(key_pos[None, None, :] <= qpos[None, :, None], iscore, -jnp.inf)
        _, sel = lax.top_k(iscore, top_k)
        kg = gather(k, sel)
        vg = gather(v, sel)
        s = jnp.einsum("bqhd,bqkd->bhqk", qb, kg).astype(jnp.float32) * (HEAD_DIM ** -0.5)
        s = jnp.where((sel <= qpos[None, :, None])[:, None], s, -jnp.inf)
        p = jax.nn.softmax(s, axis=-1)
        return jnp.einsum("bhqk,bqkd->bqhd", p.astype(vg.dtype), vg)

    out = lax.map(block_fn, (to_blocks(q), to_blocks(q_idx), to_blocks(w_idx), jnp.arange(nb)))
    return jnp.moveaxis(out, 0, 1).reshape(B, S, -1)


def memory_cross_attention(q, mem_k, mem_v):
    B, S = q.shape[:2]
    s = jnp.einsum("bqhd,bmhd->bhqm", q, mem_k).astype(jnp.float32) * (HEAD_DIM ** -0.5)
    p = jax.nn.softmax(s, axis=-1)
    return jnp.einsum("bhqm,bmhd->bqhd", p.astype(mem_v.dtype), mem_v).reshape(B, S, -1)


def setup_inputs(seed: int = 0) -> dict:
    key = jax.random.key(seed)
    ks = jax.random.split(key, 20)
    f32 = jnp.float32

    def nrm(k, shape, scale):
        return jax.random.normal(k, shape, f32) * scale

    def gain(k, shape):
        return 1.0 + 0.02 * jax.random.normal(k, shape, f32)

    return {
        "x": nrm(ks[0], (BATCH, SEQ, D_MODEL), 1.0),
        "mem": nrm(ks[1], (BATCH, MEM_LEN, D_MODEL), 1.0),
        "positions": jnp.broadcast_to(jnp.arange(SEQ, dtype=jnp.int32)[None], (BATCH, SEQ)),
        "g_mix": gain(ks[2], (DEPTH, D_MODEL)),
        "w_in": nrm(ks[3], (DEPTH, D_MODEL, D_IN), D_MODEL ** -0.5),
        "b_gate": nrm(ks[4], (DEPTH, N_BRANCH * D_MODEL), 0.01),
        "sinks": nrm(ks[5], (DEPTH, N_SWA_HEADS), 0.5),
        "g_mem": gain(ks[6], (DEPTH, D_MODEL)),
        "w_mem_kv": nrm(ks[7], (DEPTH, D_MODEL, 2 * MEM_WIDTH), D_MODEL ** -0.5),
        "w_proj_swa": nrm(ks[8], (DEPTH, SWA_WIDTH, D_MODEL), SWA_WIDTH ** -0.5),
        "w_proj_dsa": nrm(ks[9], (DEPTH, DSA_WIDTH, D_MODEL), DSA_WIDTH ** -0.5),
        "w_proj_mem": nrm(ks[10], (DEPTH, MEM_WIDTH, D_MODEL), MEM_WIDTH ** -0.5),
        "w_out": nrm(ks[11], (DEPTH, D_MODEL, D_MODEL), D_MODEL ** -0.5),
        "g_mlp": gain(ks[12], (DEPTH, D_MODEL)),
        "w_mlp_in": nrm(ks[13], (DEPTH, D_MODEL, D_FF), D_MODEL ** -0.5),
        "w_mlp_out": nrm(ks[14], (DEPTH, D_FF, D_MODEL), D_FF ** -0.5),
        "g_final": gain(ks[15], (D_MODEL,)),
    }


def reference(x, mem, positions, g_mix, w_in, b_gate, sinks, g_mem, w_mem_kv,
              w_proj_swa, w_proj_dsa, w_proj_mem, w_out, g_mlp, w_mlp_in, w_mlp_out,
              g_final):
    B, S, _ = x.shape
    M = mem.shape[1]
    split_points = np.cumsum(IN_SPLITS)[:-1].tolist()
    idx_scale = (N_IDX_HEADS ** -0.5) * (IDX_DIM ** -0.5)
    for l in range(DEPTH):
        h = rmsnorm(x, g_mix[l])
        proj = jnp.einsum("bsd,de->bse", h, w_in[l])
        (q_s, k_s, v_s, q_d, k_d, v_d, q_i, k_i, w_i, q_m, gate_logits) = jnp.split(
            proj, split_points, axis=-1)

        q_s = rope(q_s.reshape(B, S, N_SWA_HEADS, HEAD_DIM), positions)
        k_s = rope(k_s.reshape(B, S, N_SWA_KV, HEAD_DIM), positions)
        v_s = v_s.reshape(B, S, N_SWA_KV, HEAD_DIM)
        o_swa = sliding_window_attention(q_s, k_s, v_s, sinks[l])

        q_d = rope(q_d.reshape(B, S, N_DSA_HEADS, HEAD_DIM), positions)
        k_d = rope(k_d[:, :, None, :], positions)[:, :, 0]
        q_i = rope(q_i.reshape(B, S, N_IDX_HEADS, IDX_DIM), positions)
        k_i = rope(k_i[:, :, None, :], positions)[:, :, 0]
        o_dsa = dsa_attention(q_d, k_d, v_d, q_i, k_i, w_i * idx_scale)

        m = rmsnorm(mem, g_mem[l])
        mkv = jnp.einsum("bmd,de->bme", m, w_mem_kv[l]).reshape(B, M, 2, N_MEM_HEADS, HEAD_DIM)
        o_mem = memory_cross_attention(q_m.reshape(B, S, N_MEM_HEADS, HEAD_DIM),
                                       mkv[:, :, 0], mkv[:, :, 1])

        gates = jax.nn.sigmoid(gate_logits + b_gate[l]).reshape(B, S, N_BRANCH, D_MODEL)
        merged = (gates[:, :, 0] * jnp.einsum("bse,ed->bsd", o_swa, w_proj_swa[l])
                  + gates[:, :, 1] * jnp.einsum("bse,ed->bsd", o_dsa, w_proj_dsa[l])
                  + gates[:, :, 2] * jnp.einsum("bse,ed->bsd", o_mem, w_proj_mem[l]))
        x = x + jnp.einsum("bsd,de->bse", merged, w_out[l])

        h = rmsnorm(x, g_mlp[l])
        hid = jnp.square(jax.nn.relu(jnp.einsum("bsd,df->bsf", h, w_mlp_in[l])))
        x = x + jnp.einsum("bsf,fd->bsd", hid, w_mlp_out[l])
    return rmsnorm(x, g_final)
```

```python
import math
import numpy as np
from contextlib import ExitStack
import concourse.bass as bass
import concourse.mybir as mybir
from concourse.bass_utils import run_bass_kernel_spmd

F32 = mybir.dt.float32; BF16 = mybir.dt.bfloat16; I32 = mybir.dt.int32
ALU = mybir.AluOpType; AF = mybir.ActivationFunctionType; AX = mybir.AxisListType

STAGE = 99
class _Stop(Exception):
    pass
def _ckpt(n):
    if STAGE <= n:
        raise _Stop()
NITER = 18
NEG = -1.0e30
NCHUNK = 37
TOPK = 256.0


class Reg:
    __slots__ = ("name", "w", "r", "dsem", "dcnt")
    def __init__(self, name):
        self.name = name; self.w = None; self.r = {}; self.dsem = None; self.dcnt = 0


class Eng:
    def __init__(self, name):
        self.name = name; self.q = []; self.sem = None; self.cnt = 0; self.seen = {}


class Trk:
    def __init__(self, nc, stack, nsem):
        self.nc = nc
        self.sems = [stack.enter_context(nc.semaphore(f"s{i}")) for i in range(nsem)]
        self.si = 0
        self.E = {n: Eng(n) for n in ("pe", "act", "dve", "pool", "sp")}
        for e in self.E.values():
            e.sem = self.newsem()
    def newsem(self):
        s = self.sems[self.si]; self.si += 1; return s
    def _deps(self, eng, reads, writes):
        deps = {}
        def add(ev, kind):
            if ev is None: return
            sem, val, src = ev
            if src is eng and eng.name == "pe":
                return
            k = id(sem)
            if eng.seen.get(k, 0) >= val: return
            if k not in deps or deps[k][1] < val: deps[k] = (sem, val)
        for r in reads: add(r.w, "raw")
        for w in writes:
            add(w.w, "waw")
            for ev in w.r.values(): add(ev, "war")
        out = list(deps.values())
        for sem, val in out: eng.seen[id(sem)] = val
        return out
    def op(self, en, fn, r=(), w=()):
        eng = self.E[en]
        waits = self._deps(eng, r, w)
        eng.cnt += 1
        ev = (eng.sem, eng.cnt, eng)
        eng.q.append((waits, fn, (eng.sem, 1)))
        for x in r: x.r[en] = ev
        for x in w: x.w = ev; x.r = {}
    def dma(self, en, out_ap, in_ap, slot, r=(), w=()):
        eng = self.E[en]
        waits = self._deps(eng, r, w)
        if slot.dsem is None: slot.dsem = self.newsem()
        slot.dcnt += 16
        ev = (slot.dsem, slot.dcnt, None)
        eng.q.append((waits, lambda h: h.dma_start(out=out_ap, in_=in_ap), (slot.dsem, 16)))
        for x in r: x.r["dma%d" % id(slot)] = ev
        for x in w: x.w = ev; x.r = {}
    def wait_all(self, en, regs):
        eng = self.E[en]
        waits = self._deps(eng, [], regs)
        eng.q.append((waits, None, None))
    def replay(self, block):
        def mk(en):
            q = self.E[en].q
            def body(h):
                for waits, fn, inc in q:
                    for sem, val in waits: h.wait_ge(sem, val)
                    if fn is None: continue
                    ins = fn(h)
                    ins.then_inc(inc[0], inc[1])
            return body
        block.tensor(mk("pe")); block.scalar(mk("act")); block.vector(mk("dve"))
        block.gpsimd(mk("pool")); block.sync(mk("sp"))


def f_mm(out, lhsT, rhs, start=True, stop=True):
    return lambda h: h.matmul(out, lhsT=lhsT, rhs=rhs, start=start, stop=stop)
def f_tr(out, in_, ident):
    return lambda h: h.transpose(out=out, in_=in_, identity=ident)
def f_seq(fns):
    def f(h):
        ins = None
        for fn in fns: ins = fn(h)
        return ins
    return f
def f_act(out, in_, func, bias=None, scale=None, accum=None):
    kw = {}
    if bias is not None: kw["bias"] = bias
    if scale is not None: kw["scale"] = scale
    if accum is not None: kw["accum_out"] = accum
    return lambda h: h.activation(out=out, in_=in_, func=func, **kw)
def f_ts(out, in0, s1, s2=None, op0=ALU.mult, op1=None, accum=None):
    kw = {}
    if op1 is not None: kw["op1"] = op1
    if accum is not None: kw["accum_out"] = accum
    return lambda h: h.tensor_scalar(out=out, in0=in0, scalar1=s1, scalar2=s2, op0=op0, **kw)
def f_tt(out, in0, in1, op):
    return lambda h: h.tensor_tensor(out=out, in0=in0, in1=in1, op=op)
def f_stt(out, in0, scalar, in1, op0, op1):
    return lambda h: h.scalar_tensor_tensor(out=out, in0=in0, scalar=scalar, in1=in1, op0=op0, op1=op1)
def f_cp(out, in_):
    return lambda h: h.tensor_copy(out=out, in_=in_)
def f_memset(ap, v):
    return lambda h: h.memset(ap, v)


def build_program():
    nc = bass.Bass("TRN2", target_bir_lowering=False)
    dt_in = lambda n, s, d=F32: nc.dram_tensor(n, s, d, kind="ExternalInput").ap()
    xc = dt_in("xc", [64 * 128, 1024])
    posc = dt_in("posc", [128, 64], I32)
    memc = dt_in("memc", [256, 1024])
    cmask = dt_in("cmask", [128, 5 * 128])
    invf_d = dt_in("invf", [128, 32])
    wk_d = dt_in("wk", [128, 8 * 448])
    wch_d = dt_in("wch", [NCHUNK, 128, 4096])
    wmem_d = dt_in("wmem", [128, 4096])
    gvec_d = dt_in("gvec", [128, 24])
    gfin_d = dt_in("gfin", [128, 1024])
    bgate_d = dt_in("bgate", [128, 24])
    sinkr_d = dt_in("sinkr", [64, 8 * 128])
    out_d = nc.dram_tensor("out", [32 * 128, 1024], F32, kind="ExternalOutput").ap()
    wbf_d = nc.dram_tensor("wbf", [NCHUNK, 128, 4096], BF16, kind="Internal").ap()

    with ExitStack() as st:
        T = Trk(nc, st, 48)
        def sb(name, shape, dt):
            return st.enter_context(nc.sbuf_tensor(name, shape, dt))
        BIG = sb("BIG", [128, 8192], F32)
        MSK = sb("MSK", [128, 8192], BF16)
        ISC = [Reg(f"isc{c}") for c in range(16)]
        MSKR = [Reg(f"msk{c}") for c in range(16)]
        hidT = BIG[:].bitcast(BF16).rearrange("p (f t) -> p f t", t=512)
        wk_sb = sb("wk_sb", [128, 8, 448], BF16); R_wk = Reg("wk")
        kDI = sb("kDI", [128, 8192], BF16); R_kDI = [Reg(f"kDI{i}") for i in range(64)]
        vD = sb("vD", [128, 64, 64], BF16); R_vD = [Reg(f"vD{i}") for i in range(64)]
        kST = sb("kST", [128, 16, 128], BF16); R_kST = [Reg(f"kST{i}") for i in range(16)]
        vS = sb("vS", [128, 16, 128], BF16); R_vS = [Reg(f"vS{i}") for i in range(16)]
        mkT = sb("mkT", [128, 2, 256], BF16); R_mkT = Reg("mkT")
        mv = sb("mv", [128, 2, 256], BF16); R_mv = Reg("mv")
        xg = sb("xg", [128, 4, 1024], F32); R_xg = [Reg(f"xg{j}") for j in range(4)]
        xo = sb("xo", [128, 1024], F32); R_xo = Reg("xo")
        hTg = sb("hTg", [128, 8, 512], BF16); R_hTg = [Reg(f"hTg{j}") for j in range(4)]
        hTo = sb("hTo", [128, 8, 128], BF16); R_hTo = Reg("hTo")
        oTg = sb("oTg", [64, 16, 512], BF16); R_oTg = [Reg(f"oTg{j}") for j in range(4)]
        QM = sb("QM", [128, 4096], BF16)
        qsT = QM[:, 0:2048].rearrange("p (s c) -> p s c", s=4)
        qdiT = QM[:, 2048:4096].rearrange("p (s c) -> p s c", s=4)
        mergedT = QM[:].rearrange("p (k t) -> p k t", k=8)
        R_qs = [Reg(f"qs{j}") for j in range(4)]; R_qdi = [Reg(f"qdi{j}") for j in range(4)]
        qmT = sb("qmT", [128, 4, 256], BF16); R_qm = [Reg(f"qm{j}") for j in range(4)]
        wiT = sb("wiT", [128, 4, 4], F32); R_wi = [Reg(f"wi{j}") for j in range(4)]
        NRING = 2
        ring = [sb(f"ring{i}", [128, 4096], BF16) for i in range(NRING)]
        R_ring = [Reg(f"ring{i}") for i in range(NRING)]
        ident = sb("ident", [128, 128], BF16); identf = sb("identf", [128, 128], F32); onesf = sb("onesf", [128, 128], F32)
        ones_b = sb("ones_b", [128, 64], BF16)
        R_const = Reg("const")
        cm_f = sb("cm_f", [128, 5, 128], F32)
        cm_b = sb("cm_b", [128, 3, 128], BF16)
        invf = sb("invf_sb", [128, 32], F32)
        gvec = sb("gvec_sb", [128, 24], F32)
        gfin = sb("gfin_sb", [128, 1024], F32)
        bgate = sb("bgate_sb", [128, 24], F32)
        es = sb("es_sb", [64, 8, 128], F32)
        pw2 = sb("pw2", [128, NITER], F32)
        posi = sb("posi", [128, 64], I32); posf = sb("posf", [128, 64], F32)
        cosT = sb("cosT", [128, 8, 32], F32); sinT = sb("sinT", [128, 8, 32], F32); R_cs = Reg("cs")
        rtmp = [sb(f"rtmp{i}", [128, 256], F32) for i in range(3)]; rtmpi = sb("rtmpi", [128, 256], I32); R_rt = Reg("rt")
        stat = sb("stat", [128, 16], F32); R_stat = [Reg(f"stat{i}") for i in range(4)]
        hn = [sb(f"hn{i}", [128, 1024], BF16) for i in range(2)]; R_hn = [Reg(f"hn{i}") for i in range(2)]
        tok_b = [sb(f"tok_b{i}", [128, 512], BF16) for i in range(2)]; R_tok = [Reg(f"tok{i}") for i in range(2)]
        Et = [sb(f"Et{i}", [128, 512], BF16) for i in range(3)]; R_E = [Reg(f"E{i}") for i in range(3)]
        Pt = [sb(f"Pt{i}", [128, 512], BF16) for i in range(3)]; R_P = [Reg(f"P{i}") for i in range(3)]
        rl = [sb(f"rl{i}", [128, 512], F32) for i in range(2)]; R_rl = [Reg(f"rl{i}") for i in range(2)]
        mT = [sb(f"mT{i}", [128, 512], BF16) for i in range(2)]; R_mT = [Reg(f"mT{i}") for i in range(2)]
        dn = [sb(f"dn{i}", [64, 512], F32) for i in range(1)]; R_dn = [Reg(f"dn{i}") for i in range(1)]
        mg = rl; R_mg = R_rl
        rope_t = rl[0]; rope_u = rl[1]
        gsb = [sb(f"gsb{i}", [128, 512], BF16) for i in range(3)]; R_gsb = [Reg(f"gsb{i}") for i in range(3)]
        bis = sb("bis", [128, 8 + NITER], F32); R_bis = Reg("bis")
        PB = [st.enter_context(nc.psum_tensor(f"pb{i}", [128, 512], F32)) for i in range(8)]
        R_PB = [Reg(f"pb{i}") for i in range(8)]
        def pbf(i):
            return PB[i][:].bitcast(BF16)

        cnt = {"ring": 0, "stat": 0, "hn": 0, "tok": 0, "E": 0, "P": 0, "rl": 0, "mT": 0, "dn": 0, "mg": 0}
        def rot(name, n):
            i = cnt[name] % n; cnt[name] += 1; return i

        R_cmf = Reg("cmf"); R_misc = Reg("misc")
        T.dma("sp", cm_f[:].rearrange("p a b -> p (a b)"), cmask[:, :], R_cmf, w=[R_cmf])
        for (dst, src) in ((invf, invf_d), (gvec, gvec_d), (gfin, gfin_d), (bgate, bgate_d)):
            T.dma("sp", dst[:], src[:, :], R_misc, w=[R_misc])
            T.wait_all("sp", [R_misc])
        R_es = Reg("es")
        T.dma("sp", es[:].rearrange("p a b -> p (a b)"), sinkr_d[:, :], R_es, w=[R_es])
        R_pos = Reg("pos")
        T.dma("sp", posi[:], posc[:, :], R_pos, w=[R_pos])
        T.op("pool", f_memset(onesf[:], 1.0), w=[R_const])
        T.op("pool", f_memset(identf[:], 0.0), w=[R_const])
        T.op("pool", lambda h: h.affine_select(out=identf[:], in_=onesf[:], pattern=[[-1, 128]], compare_op=ALU.is_equal,
                                                fill=0.0, base=0, channel_multiplier=1), r=[R_const], w=[R_const])
        T.op("dve", f_cp(ident[:], identf[:]), r=[R_const], w=[R_const])
        T.op("dve", f_memset(ones_b[:], 1.0), w=[R_const])
        for k in range(NITER):
            T.op("dve", f_memset(pw2[:, k:k + 1], 2.0 ** (-(k + 1))), w=[R_const])
        T.op("dve", f_cp(cm_b[:], cm_f[:, 0:3, :]), r=[R_cmf], w=[R_const])
        T.op("act", f_act(es[:], es[:], AF.Exp), r=[R_es], w=[R_es])
        T.op("dve", f_cp(posf[:], posi[:]), r=[R_pos], w=[R_pos])
        tri_cur = cm_b[:, 0, :]; tri_prev = cm_b[:, 1, :]; prev0 = cm_b[:, 2, :]
        dbias = cm_f[:, 3, :]; p63bias = cm_f[:, 4, :]

        def emit():
            def norm_T(x_ap, x_regs, dstT, dst_regs, pbank):
                si = rot("stat", 4); s = stat[:, si * 4:(si + 1) * 4]; rs = R_stat[si]
                hi = rot("hn", 2); h_ = hn[hi]; rh = R_hn[hi]
                T.op("act", f_act(h_[:], x_ap, AF.Square, accum=s[:, 0:1]), r=x_regs, w=[rh, rs])
                T.op("act", f_act(s[:, 1:2], s[:, 0:1], AF.Ln, scale=1.0 / 1024.0, bias=1e-6), r=[rs], w=[rs])
                T.op("act", f_act(s[:, 2:3], s[:, 1:2], AF.Exp, scale=-0.5), r=[rs], w=[rs])
                T.op("dve", f_ts(h_[:], x_ap, s[:, 2:3], None, ALU.mult), r=x_regs + [rs], w=[rh])
                pv = pbf(pbank)
                T.op("pe", f_seq([f_tr(pv[:, kc * 128:(kc + 1) * 128], h_[:, kc * 128:(kc + 1) * 128], ident[:]) for kc in range(8)]),
                     r=[rh, R_const], w=[R_PB[pbank]])
                T.op("act", f_act(dstT, pv[:, :].rearrange("p (k t) -> p k t", k=8), AF.Copy), r=[R_PB[pbank]], w=dst_regs)
                return s, rs

            def rope_tables(blk0, nb):
                n = nb * 32
                a0 = rtmp[0][:, 0:n]; a1 = rtmp[1][:, 0:n]; a2 = rtmp[2][:, 0:n]; ai = rtmpi[:, 0:n]
                v3 = lambda a: a.rearrange("p (b f) -> p b f", f=32)
                T.op("dve", f_tt(v3(a0), posf[:, blk0:blk0 + nb].unsqueeze(2).to_broadcast([128, nb, 32]),
                                 invf[:].unsqueeze(1).to_broadcast([128, nb, 32]), ALU.mult), r=[R_pos, R_misc], w=[R_rt])
                T.op("dve", f_ts(ai, a0, 1.0 / (2 * math.pi), None, ALU.mult), r=[R_rt], w=[R_rt])
                T.op("dve", f_cp(a1, ai), r=[R_rt], w=[R_rt])
                T.op("dve", f_stt(a2, a1, -6.28125, a0, ALU.mult, ALU.add), r=[R_rt], w=[R_rt])
                T.op("dve", f_stt(a2, a1, -0.0019353072, a2, ALU.mult, ALU.add), r=[R_rt], w=[R_rt])
                T.op("dve", f_ts(a0, a2, -3.1415925, 3.1415925, ALU.max, ALU.min), r=[R_rt], w=[R_rt])
                T.op("act", f_act(sinT[:, 0:nb, :].rearrange("p b f -> p (b f)"), a0, AF.Sin), r=[R_rt], w=[R_cs])
                T.op("dve", f_ts(a1, a2, math.pi / 2, None, ALU.add), r=[R_rt], w=[R_rt])
                T.op("dve", f_ts(a0, a1, math.pi, -2 * math.pi, ALU.is_gt, ALU.mult), r=[R_rt, R_cs], w=[R_rt])
                T.op("dve", f_tt(a0, a0, a1, ALU.add), r=[R_rt], w=[R_rt])
                T.op("dve", f_ts(a0, a0, -3.1415925, 3.1415925, ALU.max, ALU.min), r=[R_rt], w=[R_rt])
                T.op("act", f_act(cosT[:, 0:nb, :].rearrange("p b f -> p (b f)"), a0, AF.Sin), r=[R_rt], w=[R_cs])

            def rope_apply(src, src_regs, dst, dst_regs, H, cb):
                n = H * 64
                t_ = rope_t[:, 0:n]; u_ = rope_u[:, 0:n]
                cosb = cosT[:, cb:cb + 1, :]; sinb = sinT[:, cb:cb + 1, :]
                T.op("dve", f_tt(t_.rearrange("p (a f) -> p a f", f=32), src.rearrange("p (a f) -> p a f", f=32),
                                 cosb.to_broadcast([128, 2 * H, 32]), ALU.mult), r=src_regs + [R_cs], w=[R_rl[0]])
                s4 = src.rearrange("p (h e f) -> p h e f", e=2, f=32)
                u4 = u_.rearrange("p (h e f) -> p h e f", e=2, f=32)
                t4 = t_.rearrange("p (h e f) -> p h e f", e=2, f=32)
                d4 = dst.rearrange("p (h e f) -> p h e f", e=2, f=32)
                sb_ = sinb.to_broadcast([128, H, 32])
                T.op("dve", f_tt(u4[:, :, 0, :], s4[:, :, 1, :], sb_, ALU.mult), r=src_regs + [R_cs], w=[R_rl[1]])
                T.op("dve", f_tt(u4[:, :, 1, :], s4[:, :, 0, :], sb_, ALU.mult), r=src_regs + [R_cs], w=[R_rl[1]])
                T.op("dve", f_tt(d4[:, :, 0, :], t4[:, :, 0, :], u4[:, :, 0, :], ALU.subtract), r=R_rl, w=dst_regs)
                T.op("dve", f_tt(d4[:, :, 1, :], t4[:, :, 1, :], u4[:, :, 1, :], ALU.add), r=R_rl, w=dst_regs)

            R_wbf = [Reg(f"wbf{i}") for i in range(NCHUNK)]
            CH_N = [4096, 4096, 8 * 260] + [8 * 384] * 8 + [2048] * 8 + [4096] * 18
            CH_P = [128] * 11 + [64] * 8 + [128] * 18
            sched = [0, 1, 2] + [v for oc in range(8) for v in (3 + oc, 11 + oc)] + list(range(19, 37))
            ring_state = {"issued": 0, "got": 0}
            total_gets = 8 * NCHUNK
            def ring_issue():
                k = ring_state["issued"]
                if k >= total_gets: return
                c = sched[k % NCHUNK]; b = k % NRING
                T.dma("sp", ring[b][0:CH_P[c], 0:CH_N[c]], wbf_d[c, 0:CH_P[c], 0:CH_N[c]], R_ring[b], r=[R_wbf[c]], w=[R_ring[b]])
                ring_state["issued"] += 1
            def ring_get(expect):
                k = ring_state["got"]
                assert sched[k % NCHUNK] == expect, (k, expect)
                while ring_state["issued"] < min(k + NRING, total_gets):
                    ring_issue()
                ring_state["got"] += 1
                b = k % NRING
                return ring[b], R_ring[b]

            def convert(src_dram_ap, npart, nelem, ncols, goff, dst_ap, dst_regs, half):
                stg = BIG[0:npart, half * 4096: half * 4096 + nelem]
                sregs = ISC[half * 8:(half + 1) * 8]
                T.dma("pool", stg, src_dram_ap, sregs[0], w=sregs)
                if goff is None:
                    T.op("pool", f_cp(dst_ap, stg), r=sregs, w=dst_regs)
                else:
                    for kc in range(8):
                        T.op("pool", f_ts(dst_ap[:, kc * ncols:(kc + 1) * ncols], stg[:, kc * ncols:(kc + 1) * ncols],
                                          gvec[:, goff + kc:goff + kc + 1], None, ALU.mult), r=sregs + [R_misc], w=dst_regs)
            _ckpt(1)
            convert(wk_d[:, :], 128, 8 * 448, 448, 0, wk_sb[:].rearrange("p k c -> p (k c)"), [R_wk], 0)
            wmem_b = MSK[:, 4096:8192]
            convert(wmem_d[:, :], 128, 4096, 512, 16, wmem_b, MSKR[8:16], 1)

            for mc in range(2):
                T.dma("sp", xo[:], memc[mc * 128:(mc + 1) * 128, :], R_xo, w=[R_xo])
                norm_T(xo[:], [R_xo], hTo[:], [R_hTo], 0)
                T.op("pe", f_seq([f_mm(PB[1][:, :], hTo[:, kc, :], wmem_b[:, kc * 512:(kc + 1) * 512], kc == 0, kc == 7) for kc in range(8)]),
                     r=[R_hTo] + MSKR[8:16], w=[R_PB[1]])
                ti = rot("tok", 2)
                T.op("act", f_act(tok_b[ti][:, 0:256], PB[1][:, 0:256], AF.Copy), r=[R_PB[1]], w=[R_tok[ti]])
                T.op("act", f_act(mv[:, mc, :], PB[1][:, 256:512], AF.Copy), r=[R_PB[1]], w=[R_mv])
                pv = pbf(2)
                T.op("pe", f_seq([f_tr(pv[:, jj * 128:(jj + 1) * 128], tok_b[ti][:, jj * 128:(jj + 1) * 128], ident[:]) for jj in range(2)]),
                     r=[R_tok[ti], R_const], w=[R_PB[2]])
                T.op("act", f_act(mkT[:, :, mc * 128:(mc + 1) * 128], pv[:, 0:256].rearrange("p (j m) -> p j m", j=2), AF.Copy),
                     r=[R_PB[2]], w=[R_mkT])

            _ckpt(2)
            for c in range(NCHUNK):
                half = c % 2
                goff = 0 if c < 11 else (8 if 21 <= c < 29 else None)
                ncols = {0: 512, 1: 512, 2: 260}.get(c, 384 if c < 11 else 512)
                npart = CH_P[c]; nelem = CH_N[c]
                dstb = MSK[0:npart, half * 4096: half * 4096 + nelem]
                dregs = MSKR[half * 8:(half + 1) * 8]
                convert(wch_d[c, 0:npart, 0:nelem], npart, nelem, ncols, goff, dstb, dregs, half)
                T.dma("pool", wbf_d[c, 0:npart, 0:nelem], dstb, dregs[0], r=dregs, w=[R_wbf[c]])

            def kside(pos, x_ap, x_regs, hT_ap, hT_regs, cb):
                kidx = 0 if pos == 63 else pos + 1
                rg = (pos + 1) % 16
                norm_T(x_ap, x_regs, hT_ap, hT_regs, 0)
                _ckpt(3.1)
                T.op("pe", f_seq([f_mm(PB[1][:, 0:448], hT_ap[:, kc, :], wk_sb[:, kc, :], kc == 0, kc == 7) for kc in range(8)]),
                     r=hT_regs + [R_wk], w=[R_PB[1]])
                _ckpt(3.2)
                ti = rot("tok", 2)
                rope_apply(PB[1][:, 0:256], [R_PB[1]], tok_b[ti][:, 0:256], [R_tok[ti]], 4, cb)
                _ckpt(3.3)
                T.op("dve", f_cp(vS[:, rg, :], PB[1][:, 256:384]), r=[R_PB[1]], w=[R_vS[rg]])
                T.op("dve", f_cp(vD[:, kidx, :], PB[1][:, 384:448]), r=[R_PB[1]], w=[R_vD[kidx]])
                _ckpt(3.4)
                pv = pbf(2)
                T.op("pe", f_seq([f_tr(pv[:, jj * 128:(jj + 1) * 128], tok_b[ti][:, jj * 128:(jj + 1) * 128], ident[:]) for jj in range(2)]),
                     r=[R_tok[ti], R_const], w=[R_PB[2]])
                _ckpt(3.5)
                T.op("act", f_act(kST[:, rg, :], pv[:, 0:128], AF.Copy), r=[R_PB[2]], w=[R_kST[rg]])
                T.op("act", f_act(kDI[:, kidx * 128:(kidx + 1) * 128], pv[:, 128:256], AF.Copy), r=[R_PB[2]], w=[R_kDI[kidx]])

            _ckpt(3)
            rope_tables(63, 1)
            T.dma("sp", xo[:], xc[63 * 128:64 * 128, :], R_xo, w=[R_xo])
            kside(63, xo[:], [R_xo], hTo[:], [R_hTo], 0)

            def finish_attn(bO, bD, j, h0, es_ap):
                di = rot("dn", 1)
                if es_ap is not None:
                    T.op("dve", f_tt(dn[di][:], PB[bD][0:64, :], es_ap, ALU.add), r=[R_PB[bD], R_es], w=[R_dn[di]])
                    T.op("dve", lambda h, a=dn[di][:]: h.reciprocal(out=a, in_=a), r=[R_dn[di]], w=[R_dn[di]])
                else:
                    T.op("dve", f_cp(dn[di][:], PB[bD][0:64, :]), r=[R_PB[bD]], w=[R_dn[di]])
                    T.op("dve", lambda h, a=dn[di][:]: h.reciprocal(out=a, in_=a), r=[R_dn[di]], w=[R_dn[di]])
                T.op("dve", f_tt(oTg[:, h0:h0 + 4, j * 128:(j + 1) * 128], PB[bO][0:64, :].rearrange("p (h t) -> p h t", h=4),
                                 dn[di][:].rearrange("p (h t) -> p h t", h=4), ALU.mult), r=[R_PB[bO], R_dn[di]], w=[R_oTg[j]])

            _ckpt(4)
            for g in range(8):
                npos = 8 if g < 7 else 7
                rope_tables(8 * g, 8)
                for pl in range(npos):
                    pos = 8 * g + pl
                    if pl % 2 == 0:
                        j = pl // 2
                        T.dma("sp", xg[:, j, :], xc[pos * 128:(pos + 1) * 128, :], R_xg[j], w=[R_xg[j]])
                        kside(pos, xg[:, j, :], [R_xg[j]], hTg[:, :, j * 128:(j + 1) * 128], [R_hTg[j]], pl)
                    else:
                        T.dma("sp", xo[:], xc[pos * 128:(pos + 1) * 128, :], R_xo, w=[R_xo])
                        kside(pos, xo[:], [R_xo], hTo[:], [R_hTo], pl)

                _ckpt(10 * (g + 1) + 1)
                for c in range(3):
                    W, RW = ring_get(c)
                    ncols = (512, 512, 260)[c]
                    Wv = W[:, 0:8 * ncols].rearrange("p (k c) -> p k c", k=8)
                    for j in range(4):
                        pb = 3 + (j % 2)
                        T.op("pe", f_seq([f_mm(PB[pb][:, 0:ncols], hTg[:, kc, j * 128:(j + 1) * 128], Wv[:, kc, :], kc == 0, kc == 7) for kc in range(8)]),
                             r=[R_hTg[j], RW], w=[R_PB[pb]])
                        ti = rot("tok", 2)
                        if c < 2:
                            rope_apply(PB[pb][:, 0:512], [R_PB[pb]], tok_b[ti][:, 0:512], [R_tok[ti]], 8, 2 * j)
                            pv = pbf(5 + (j % 2))
                            T.op("pe", f_seq([f_tr(pv[:, q * 128:(q + 1) * 128], tok_b[ti][:, q * 128:(q + 1) * 128], ident[:]) for q in range(4)]),
                                 r=[R_tok[ti], R_const], w=[R_PB[5 + (j % 2)]])
                            dst = qsT if c == 0 else qdiT
                            T.op("act", f_act(dst[:, j, :], pv[:, 0:512], AF.Copy), r=[R_PB[5 + (j % 2)]], w=[(R_qs if c == 0 else R_qdi)[j]])
                        else:
                            T.op("act", f_act(tok_b[ti][:, 0:256], PB[pb][:, 0:256], AF.Copy), r=[R_PB[pb]], w=[R_tok[ti]])
                            T.op("act", f_act(wiT[:, j, :], PB[pb][:, 256:260], AF.Copy, scale=1.0 / 16.0), r=[R_PB[pb]], w=[R_wi[j]])
                            pv = pbf(5 + (j % 2))
                            T.op("pe", f_seq([f_tr(pv[:, q * 128:(q + 1) * 128], tok_b[ti][:, q * 128:(q + 1) * 128], ident[:]) for q in range(2)]),
                                 r=[R_tok[ti], R_const], w=[R_PB[5 + (j % 2)]])
                            T.op("act", f_act(qmT[:, j, :], pv[:, 0:256], AF.Copy), r=[R_PB[5 + (j % 2)]], w=[R_qm[j]])

                _ckpt(10 * (g + 1) + 2)
                for j in range(4):
                    i = 4 * g + j
                    tok = slice(j * 128, (j + 1) * 128)
                    rp = (2 * i) % 16; rc = (2 * i + 1) % 16
                    for kv in range(2):
                        pr = slice(kv * 64, (kv + 1) * 64)
                        bS = (2, 3) if kv == 0 else (4, 5)
                        bO, bD = (0, 1) if kv == 0 else (6, 7)
                        Ps = []
                        for which, rr_ in enumerate((rp, rc)):
                            b = bS[which]
                            T.op("pe", f_mm(PB[b][:, :], kST[pr, rr_, :], qsT[pr, j, :]), r=[R_kST[rr_], R_qs[j]], w=[R_PB[b]])
                            ei = rot("E", 3)
                            T.op("act", f_act(Et[ei][:], PB[b][:, :], AF.Exp, scale=0.125), r=[R_PB[b]], w=[R_E[ei]])
                            pi_ = rot("P", 3)
                            msk = (prev0 if i == 0 else tri_prev) if which == 0 else tri_cur
                            T.op("dve", f_tt(Pt[pi_][:].rearrange("p (h t) -> p h t", h=4), Et[ei][:].rearrange("p (h t) -> p h t", h=4),
                                             msk.unsqueeze(1).to_broadcast([128, 4, 128]), ALU.mult), r=[R_E[ei], R_const], w=[R_P[pi_]])
                            Ps.append((pi_, rr_))
                        T.op("pe", f_seq([f_mm(PB[bO][0:64, :], vS[:, rr_, pr], Pt[pi_][:], w_ == 0, w_ == 1) for w_, (pi_, rr_) in enumerate(Ps)]
                                         + [f_mm(PB[bD][0:64, :], ones_b[:], Pt[pi_][:], w_ == 0, w_ == 1) for w_, (pi_, rr_) in enumerate(Ps)]),
                             r=[R_P[p_] for p_, _ in Ps] + [R_vS[r_] for _, r_ in Ps] + [R_const], w=[R_PB[bO], R_PB[bD]])
                        finish_attn(bO, bD, j, 4 * kv, es[:, 4 * kv:4 * kv + 4, :].rearrange("p h t -> p (h t)"))
                    _ckpt(12.1)
                    Es = [rot("E", 3), rot("E", 3)]
                    fns = []
                    for e in range(2):
                        pr = slice(e * 64, (e + 1) * 64)
                        for mc in range(2):
                            for jj in range(2):
                                fns.append(f_mm(PB[2 + 2 * mc + e][:, jj * 128:(jj + 1) * 128], mkT[pr, jj, mc * 128:(mc + 1) * 128],
                                                qmT[pr, j, jj * 128:(jj + 1) * 128]))
                    T.op("pe", f_seq(fns), r=[R_mkT, R_qm[j]], w=[R_PB[2], R_PB[3], R_PB[4], R_PB[5]])
                    for mc in range(2):
                        E4 = Et[Es[mc]][:].rearrange("p (jj e t) -> p jj e t", jj=2, e=2)
                        for e in range(2):
                            b = 2 + 2 * mc + e
                            T.op("act", f_act(E4[:, :, e, :], PB[b][:, 0:256].rearrange("p (jj t) -> p jj t", jj=2), AF.Exp, scale=0.125),
                                 r=[R_PB[b]], w=[R_E[Es[mc]]])
                    _ckpt(12.15)
                    fns = []
                    for hh in range(4):
                        for mc in range(2):
                            fns.append(f_mm(PB[0][0:64, hh * 128:(hh + 1) * 128], mv[:, mc, hh * 64:(hh + 1) * 64], Et[Es[mc]][:, hh * 128:(hh + 1) * 128], mc == 0, mc == 1))
                    for mc in range(2):
                        fns.append(f_mm(PB[1][0:64, :], ones_b[:], Et[Es[mc]][:], mc == 0, mc == 1))
                    T.op("pe", f_seq(fns), r=[R_E[e_] for e_ in Es] + [R_mv, R_const], w=[R_PB[0], R_PB[1]])
                    _ckpt(12.17)
                    finish_attn(0, 1, j, 12, None)
                    _ckpt(12.2)
                    nkb = 2 * i + 2
                    nch = (nkb + 3) // 4
                    n = nkb * 128
                    for c in range(nch):
                        kb0 = 4 * c; nb = min(4, nkb - kb0); w_ = nb * 128
                        cols = slice(kb0 * 128, kb0 * 128 + w_)
                        T.op("pe", f_seq([f_mm(PB[2 + hh][:, 0:w_], qdiT[64:128, j, hh * 128:(hh + 1) * 128], kDI[64:128, cols]) for hh in range(4)]),
                             r=[R_qdi[j]] + R_kDI[kb0:kb0 + nb], w=[R_PB[2 + hh] for hh in range(4)])
                        for hh in range(4):
                            ri = rot("rl", 2)
                            T.op("act", f_act(rl[ri][:, 0:w_], PB[2 + hh][:, 0:w_], AF.Relu), r=[R_PB[2 + hh]], w=[R_rl[ri]])
                            if hh == 0:
                                T.op("pool", f_ts(BIG[:, cols], rl[ri][:, 0:w_], wiT[:, j, 0:1], None, ALU.mult), r=[R_rl[ri], R_wi[j]], w=[ISC[c]])
                            else:
                                T.op("pool", f_ts(rl[ri][:, 0:w_], rl[ri][:, 0:w_], wiT[:, j, hh:hh + 1], None, ALU.mult), r=[R_rl[ri], R_wi[j]], w=[R_rl[ri]])
                                T.op("pool", f_tt(BIG[:, cols], BIG[:, cols], rl[ri][:, 0:w_], ALU.add), r=[R_rl[ri], ISC[c]], w=[ISC[c]])
                    _ckpt(12.3)
                    IR = ISC[0:nch]; MR = MSKR[0:nch]
                    amax = bis[:, 0:1]; wtot = bis[:, 1:2]; lo = bis[:, 2:3]; mid = bis[:, 3:4]; cn = bis[:, 4:5]; dl = bis[:, 5:6]
                    wks = bis[:, 8:8 + NITER]
                    T.op("dve", lambda h, a=amax, b=BIG[:, 0:n]: h.tensor_reduce(out=a, in_=b, axis=AX.X, op=ALU.max, apply_absolute_value=True),
                         r=IR, w=[R_bis])
                    T.op("pool", f_tt(BIG[:, 0:128], BIG[:, 0:128], p63bias, ALU.add), r=[ISC[0], R_cmf, R_bis], w=[ISC[0]])
                    dcol = slice((nkb - 1) * 128, nkb * 128)
                    T.op("pool", f_tt(BIG[:, dcol], BIG[:, dcol], dbias, ALU.add), r=[ISC[nch - 1], R_cmf, R_bis], w=[ISC[nch - 1]])
                    T.op("dve", f_ts(wtot, amax, 2.0002, 2e-6, ALU.mult, ALU.add), r=[R_bis], w=[R_bis])
                    T.op("dve", f_ts(lo, amax, -1.0001, -1e-6, ALU.mult, ALU.add), r=[R_bis], w=[R_bis])
                    T.op("dve", f_ts(wks, pw2[:], wtot, None, ALU.mult), r=[R_bis, R_const], w=[R_bis])
                    for k in range(NITER):
                        T.op("dve", f_tt(mid, lo, wks[:, k:k + 1], ALU.add), r=[R_bis], w=[R_bis])
                        T.op("dve", f_ts(MSK[:, 0:n], BIG[:, 0:n], mid, 0.0, ALU.is_ge, ALU.add, accum=cn), r=IR + [R_bis], w=MR + [R_bis])
                        T.op("dve", f_stt(dl, cn, TOPK, wks[:, k:k + 1], ALU.is_ge, ALU.mult), r=[R_bis], w=[R_bis])
                        T.op("dve", f_tt(lo, lo, dl, ALU.add), r=[R_bis], w=[R_bis])
                    T.op("dve", f_ts(MSK[:, 0:n], BIG[:, 0:n], lo, None, ALU.is_ge), r=IR + [R_bis], w=MR)
                    _ckpt(12.4)
                    for c in range(nch):
                        kb0 = 4 * c; nb = min(4, nkb - kb0)
                        mi = rot("mT", 2)
                        pv = pbf(2)
                        T.op("pe", f_seq([f_tr(pv[:, q * 128:(q + 1) * 128], MSK[:, (kb0 + q) * 128:(kb0 + q + 1) * 128], ident[:]) for q in range(nb)]),
                             r=[MSKR[c], R_const], w=[R_PB[2]])
                        T.op("act", f_act(mT[mi][:, 0:nb * 128], pv[:, 0:nb * 128], AF.Copy), r=[R_PB[2]], w=[R_mT[mi]])
                        for q in range(nb):
                            kb = kb0 + q
                            b = 3 + (kb % 3)
                            T.op("pe", f_mm(PB[b][:, :], kDI[0:64, kb * 128:(kb + 1) * 128], qdiT[0:64, j, :]), r=[R_kDI[kb], R_qdi[j]], w=[R_PB[b]])
                            ei = rot("E", 3)
                            T.op("act", f_act(Et[ei][:], PB[b][:, :], AF.Exp, scale=0.125), r=[R_PB[b]], w=[R_E[ei]])
                            pi_ = rot("P", 3)
                            T.op("dve", f_tt(Pt[pi_][:].rearrange("p (h t) -> p h t", h=4), Et[ei][:].rearrange("p (h t) -> p h t", h=4),
                                             mT[mi][:, q * 128:(q + 1) * 128].unsqueeze(1).to_broadcast([128, 4, 128]), ALU.mult),
                                 r=[R_E[ei], R_mT[mi]], w=[R_P[pi_]])
                            T.op("pe", f_seq([f_mm(PB[0][0:64, :], vD[:, kb, :], Pt[pi_][:], kb == 0, kb == nkb - 1),
                                              f_mm(PB[1][0:64, :], ones_b[:], Pt[pi_][:], kb == 0, kb == nkb - 1)]),
                                 r=[R_P[pi_], R_vD[kb], R_const], w=[R_PB[0], R_PB[1]])
                    finish_attn(0, 1, j, 8, None)

                _ckpt(10 * (g + 1) + 3)
                RQ = R_qs + R_qdi
                for oc in range(8):
                    G, RG = ring_get(3 + oc)
                    Gv = G[:, 0:8 * 384].rearrange("p (k c) -> p k c", k=8)
                    for jb in range(3):
                        T.op("pe", f_seq([f_mm(PB[jb][:, :], Gv[:, kc, jb * 128:(jb + 1) * 128], hTg[:, kc, :], kc == 0, kc == 7) for kc in range(8)]),
                             r=R_hTg + [RG], w=[R_PB[jb]])
                        T.op("act", f_act(gsb[jb][:], PB[jb][:, :], AF.Sigmoid, bias=bgate[:, oc * 3 + jb:oc * 3 + jb + 1], scale=1.0),
                             r=[R_PB[jb], R_misc], w=[R_gsb[jb]])
                    Pw, RP = ring_get(11 + oc)
                    Pv = Pw[0:64, 0:2048].rearrange("p (h c) -> p h c", h=16)
                    for jb, (h0, nh) in enumerate(((0, 8), (8, 4), (12, 4))):
                        T.op("pe", f_seq([f_mm(PB[3 + jb][:, :], Pv[:, h0 + hh, :], oTg[:, h0 + hh, :], hh == 0, hh == nh - 1) for hh in range(nh)]),
                             r=R_oTg + [RP], w=[R_PB[3 + jb]])
                    T.op("dve", f_tt(mg[0][:], PB[3][:, :], gsb[0][:], ALU.mult), r=[R_PB[3], R_gsb[0]], w=[R_mg[0]])
                    T.op("dve", f_tt(mg[1][:], PB[4][:, :], gsb[1][:], ALU.mult), r=[R_PB[4], R_gsb[1]], w=[R_mg[1]])
                    T.op("dve", f_tt(mg[0][:], mg[0][:], mg[1][:], ALU.add), r=[R_mg[0], R_mg[1]], w=[R_mg[0]])
                    T.op("dve", f_tt(mg[1][:], PB[5][:, :], gsb[2][:], ALU.mult), r=[R_PB[5], R_gsb[2]], w=[R_mg[1]])
                    T.op("dve", f_tt(mergedT[:, oc, :], mg[0][:], mg[1][:], ALU.add), r=[R_mg[0], R_mg[1]], w=RQ)

                _ckpt(10 * (g + 1) + 4)
                for cc in range(2):
                    W, RW = ring_get(19 + cc)
                    Wv = W[:, :].rearrange("p (k c) -> p k c", k=8)
                    for j in range(4):
                        pb = 6 + (j % 2)
                        T.op("pe", f_seq([f_mm(PB[pb][:, :], mergedT[:, kc, j * 128:(j + 1) * 128], Wv[:, kc, :], kc == 0, kc == 7) for kc in range(8)]),
                             r=RQ + [RW], w=[R_PB[pb]])
                        T.op("dve", f_tt(xg[:, j, cc * 512:(cc + 1) * 512], PB[pb][:, :], xg[:, j, cc * 512:(cc + 1) * 512], ALU.add),
                             r=[R_PB[pb], R_xg[j]], w=[R_xg[j]])

                _ckpt(10 * (g + 1) + 5)
                for j in range(4):
                    norm_T(xg[:, j, :], [R_xg[j]], hTg[:, :, j * 128:(j + 1) * 128], [R_hTg[j]], j % 2)
                for fb in range(8):
                    W, RW = ring_get(21 + fb)
                    Wv = W[:, :].rearrange("p (k c) -> p k c", k=8)
                    for fs in range(4):
                        fc = fb * 4 + fs
                        pb = 2 + (fc % 4)
                        T.op("pe", f_seq([f_mm(PB[pb][:, :], Wv[:, kc, fs * 128:(fs + 1) * 128], hTg[:, kc, :], kc == 0, kc == 7) for kc in range(8)]),
                             r=R_hTg + [RW], w=[R_PB[pb]])
                        ri = rot("rl", 2)
                        T.op("act", f_act(rl[ri][:], PB[pb][:, :], AF.Relu), r=[R_PB[pb]], w=[R_rl[ri]])
                        T.op("dve", f_tt(hidT[:, fc, :], rl[ri][:], rl[ri][:], ALU.mult), r=[R_rl[ri]], w=[ISC[fc // 2]])
                for cc in range(2):
                    for ks in range(4):
                        W, RW = ring_get(29 + cc * 4 + ks)
                        Wv = W[:, :].rearrange("p (k c) -> p k c", k=8)
                        for j in range(4):
                            pb = 4 + j
                            T.op("pe", f_seq([f_mm(PB[pb][:, :], hidT[:, ks * 8 + fl, j * 128:(j + 1) * 128], Wv[:, fl, :],
                                                   ks == 0 and fl == 0, ks == 3 and fl == 7) for fl in range(8)]),
                                 r=ISC[ks * 4:(ks + 1) * 4] + [RW], w=[R_PB[pb]])
                    for j in range(4):
                        pb = 4 + j
                        T.op("dve", f_tt(xg[:, j, cc * 512:(cc + 1) * 512], PB[pb][:, :], xg[:, j, cc * 512:(cc + 1) * 512], ALU.add),
                             r=[R_PB[pb], R_xg[j]], w=[R_xg[j]])
                _ckpt(10 * (g + 1) + 6)
                for j in range(4):
                    i = 4 * g + j
                    si = rot("stat", 4); s = stat[:, si * 4:(si + 1) * 4]; rs = R_stat[si]
                    hi = rot("hn", 2)
                    T.op("act", f_act(hn[hi][:], xg[:, j, :], AF.Square, accum=s[:, 0:1]), r=[R_xg[j]], w=[R_hn[hi], rs])
                    T.op("act", f_act(s[:, 1:2], s[:, 0:1], AF.Ln, scale=1.0 / 1024.0, bias=1e-6), r=[rs], w=[rs])
                    T.op("act", f_act(s[:, 2:3], s[:, 1:2], AF.Exp, scale=-0.5), r=[rs], w=[rs])
                    T.op("dve", f_stt(xg[:, j, :], xg[:, j, :], s[:, 2:3], gfin[:], ALU.mult, ALU.mult), r=[R_xg[j], rs, R_misc], w=[R_xg[j]])
                    T.dma("sp", out_d[i * 128:(i + 1) * 128, :], xg[:, j, :], R_xg[j], r=[R_xg[j]])
        try:
            emit()
        except _Stop:
            T.dma("sp", out_d[0:128, :], xo[:], R_xo, r=[R_xo])
            T.wait_all("sp", [R_xo] + R_ring)
            T.wait_all("pool", [MSKR[0], MSKR[8], ISC[0], ISC[8]])
        T.wait_all("sp", R_xg)
        with nc.Block() as block:
            T.replay(block)
    return nc


OFF = dict(q_s=0, k_s=512, v_s=640, q_d=768, k_d=1024, v_d=1088, q_i=1152, k_i=1408, w_i=1472, q_m=1476, gates=1732)


def _chunked(w, cols):
    sub = w[:, cols]
    n = sub.shape[1]
    return np.ascontiguousarray(sub.reshape(8, 128, n).transpose(1, 0, 2).reshape(128, 8 * n))


def _prep_weights(w_in, w_proj_swa, w_proj_dsa, w_proj_mem, w_out, w_mlp_in, w_mlp_out):
    wch = np.zeros((NCHUNK, 128, 4096), np.float32)
    r64 = np.arange(64)
    qs_cols = np.concatenate([OFF["q_s"] + h * 64 + r64 for h in (0, 4, 1, 5, 2, 6, 3, 7)])
    qdi_cols = np.concatenate([np.concatenate([OFF["q_d"] + h * 64 + r64, OFF["q_i"] + h * 64 + r64]) for h in range(4)])
    qm_cols = np.concatenate([OFF["q_m"] + np.arange(256), OFF["w_i"] + np.arange(4)])
    wch[0] = _chunked(w_in, qs_cols)
    wch[1] = _chunked(w_in, qdi_cols)
    wch[2, :, 0:8 * 260] = _chunked(w_in, qm_cols)
    for oc in range(8):
        gc = np.concatenate([OFF["gates"] + jb * 1024 + oc * 128 + np.arange(128) for jb in range(3)])
        wch[3 + oc, :, 0:8 * 384] = _chunked(w_in, gc)
        wp = np.concatenate([w_proj_swa.reshape(8, 64, 1024), w_proj_dsa.reshape(4, 64, 1024), w_proj_mem.reshape(4, 64, 1024)], 0)
        wch[11 + oc, 0:64, 0:2048] = wp[:, :, oc * 128:(oc + 1) * 128].transpose(1, 0, 2).reshape(64, 2048)
    for cc in range(2):
        wch[19 + cc] = _chunked(w_out, cc * 512 + np.arange(512))
    for fb in range(8):
        wch[21 + fb] = _chunked(w_mlp_in, fb * 512 + np.arange(512))
    for cc in range(2):
        for ks in range(4):
            blk = w_mlp_out[ks * 1024:(ks + 1) * 1024, cc * 512:(cc + 1) * 512]
            wch[29 + cc * 4 + ks] = blk.reshape(8, 128, 512).transpose(1, 0, 2).reshape(128, 4096)
    k_cols = np.concatenate([OFF["k_s"] + np.arange(128), OFF["k_d"] + r64, OFF["k_i"] + r64, OFF["v_s"] + np.arange(128), OFF["v_d"] + r64])
    wk = _chunked(w_in, k_cols)
    return wch, wk


_NC_CACHE = {}


def kernel(x, mem, positions, g_mix, w_in, b_gate, sinks, g_mem, w_mem_kv, w_proj_swa, w_proj_dsa,
           w_proj_mem, w_out, g_mlp, w_mlp_in, w_mlp_out, g_final):
    f = lambda a: np.asarray(a, dtype=np.float32)
    x = f(x); mem = f(mem); positions = np.asarray(positions, dtype=np.int32)
    wch, wk = _prep_weights(f(w_in)[0], f(w_proj_swa)[0], f(w_proj_dsa)[0], f(w_proj_mem)[0], f(w_out)[0], f(w_mlp_in)[0], f(w_mlp_out)[0])
    wmem = _chunked(f(w_mem_kv)[0], np.arange(512))
    tr = lambda v: np.ascontiguousarray(f(v).reshape(8, 128).T)
    gvec = np.concatenate([tr(g_mix[0]), tr(g_mlp[0]), tr(g_mem[0])], 1)
    gfin = np.ascontiguousarray(np.broadcast_to(f(g_final)[None, :], (128, 1024)))
    bg = f(b_gate)[0].reshape(3, 8, 128)
    bgate = np.ascontiguousarray(bg.transpose(2, 1, 0).reshape(128, 24))
    sinkr = np.ascontiguousarray(np.broadcast_to(f(sinks)[0][None, :, None], (64, 8, 128)).reshape(64, 1024))
    half = 32
    invf = np.power(np.float32(10000.0), -np.arange(half, dtype=np.float32) / np.float32(half)).astype(np.float32)
    invf = np.ascontiguousarray(np.broadcast_to(invf[None, :], (128, 32)))
    s_ = np.arange(128)[:, None]; t_ = np.arange(128)[None, :]
    tri_cur = (t_ >= s_).astype(np.float32)
    tri_prev = (s_ > t_).astype(np.float32)
    dbias = np.where(t_ <= s_, 0.0, NEG).astype(np.float32)
    in_maps = []
    for c in range(8):
        b, par = c // 2, c % 2
        order = np.arange(64) if par == 0 else np.concatenate([np.arange(1, 64), [0]])
        xb = x[b].reshape(64, 128, 1024)[order].reshape(64 * 128, 1024)
        pb = positions[b].reshape(64, 128)[order]
        posc = np.ascontiguousarray(pb.T)
        prev0 = tri_prev * float(par)
        p63 = np.full((128, 128), 0.0 if par == 1 else NEG, np.float32)
        cmask = np.ascontiguousarray(np.stack([tri_cur, tri_prev, prev0, dbias, p63], 1).reshape(128, 5 * 128).astype(np.float32))
        in_maps.append(dict(xc=np.ascontiguousarray(xb), posc=posc, memc=np.ascontiguousarray(mem[b]), cmask=cmask, invf=invf,
                            wk=wk, wch=wch, wmem=wmem, gvec=gvec, gfin=gfin, bgate=bgate, sinkr=sinkr))
    if "nc" not in _NC_CACHE:
        _NC_CACHE["nc"] = build_program()
    nc = _NC_CACHE["nc"]
    res = run_bass_kernel_spmd(nc, in_maps, core_ids=list(range(8)))
    out = np.zeros((4, 64, 128, 1024), np.float32)
    for c in range(8):
        b, par = c // 2, c % 2
        o = np.asarray(res.results[c]["out"]).reshape(32, 128, 1024)
        out[b, par::2] = o
    return out.reshape(4, 8192, 1024)
```

```python
import math
import numpy as np
from contextlib import ExitStack
import concourse.bass as bass
import concourse.mybir as mybir
from concourse.bass_utils import run_bass_kernel_spmd

F32 = mybir.dt.float32; BF16 = mybir.dt.bfloat16; I32 = mybir.dt.int32
ALU = mybir.AluOpType; AF = mybir.ActivationFunctionType; AX = mybir.AxisListType

STAGE = 99
class _Stop(Exception):
    pass
def _ckpt(n):
    if STAGE <= n:
        raise _Stop()
NITER = 14
NEG = -1.0e30
NCHUNK = 37
TOPK = 256.0


class Reg:
    __slots__ = ("name", "w", "r", "dsem", "dcnt")
    def __init__(self, name):
        self.name = name; self.w = None; self.r = {}; self.dsem = None; self.dcnt = 0


class Eng:
    def __init__(self, name):
        self.name = name; self.q = []; self.sem = None; self.cnt = 0; self.seen = {}


class Trk:
    def __init__(self, nc, stack, nsem):
        self.nc = nc
        self.sems = [stack.enter_context(nc.semaphore(f"s{i}")) for i in range(nsem)]
        self.si = 0
        self.E = {n: Eng(n) for n in ("pe", "act", "dve", "pool", "sp")}
        for e in self.E.values():
            e.sem = self.newsem()
    def newsem(self):
        s = self.sems[self.si]; self.si += 1; return s
    def _deps(self, eng, reads, writes):
        deps = {}
        def add(ev, kind):
            if ev is None: return
            sem, val, src = ev
            if src is eng and eng.name == "pe":
                return
            k = id(sem)
            if eng.seen.get(k, 0) >= val: return
            if k not in deps or deps[k][1] < val: deps[k] = (sem, val)
        for r in reads: add(r.w, "raw")
        for w in writes:
            add(w.w, "waw")
            for ev in w.r.values(): add(ev, "war")
        out = list(deps.values())
        for sem, val in out: eng.seen[id(sem)] = val
        return out
    def op(self, en, fn, r=(), w=()):
        eng = self.E[en]
        waits = self._deps(eng, r, w)
        eng.cnt += 1
        ev = (eng.sem, eng.cnt, eng)
        eng.q.append((waits, fn, (eng.sem, 1)))
        for x in r: x.r[en] = ev
        for x in w: x.w = ev; x.r = {}
    def dma(self, en, out_ap, in_ap, slot, r=(), w=()):
        eng = self.E[en]
        waits = self._deps(eng, r, w)
        if slot.dsem is None: slot.dsem = self.newsem()
        slot.dcnt += 16
        ev = (slot.dsem, slot.dcnt, None)
        eng.q.append((waits, lambda h: h.dma_start(out=out_ap, in_=in_ap), (slot.dsem, 16)))
        for x in r: x.r["dma%d" % id(slot)] = ev
        for x in w: x.w = ev; x.r = {}
    def wait_all(self, en, regs):
        eng = self.E[en]
        waits = self._deps(eng, [], regs)
        eng.q.append((waits, None, None))
    def replay(self, block):
        def mk(en):
            q = self.E[en].q
            def body(h):
                for waits, fn, inc in q:
                    for sem, val in waits: h.wait_ge(sem, val)
                    if fn is None: continue
                    ins = fn(h)
                    ins.then_inc(inc[0], inc[1])
            return body
        block.tensor(mk("pe")); block.scalar(mk("act")); block.vector(mk("dve"))
        block.gpsimd(mk("pool")); block.sync(mk("sp"))


def f_mm(out, lhsT, rhs, start=True, stop=True):
    return lambda h: h.matmul(out, lhsT=lhsT, rhs=rhs, start=start, stop=stop)
def f_tr(out, in_, ident):
    return lambda h: h.transpose(out=out, in_=in_, identity=ident)
def f_seq(fns):
    def f(h):
        ins = None
        for fn in fns: ins = fn(h)
        return ins
    return f
def f_act(out, in_, func, bias=None, scale=None, accum=None):
    kw = {}
    if bias is not None: kw["bias"] = bias
    if scale is not None: kw["scale"] = scale
    if accum is not None: kw["accum_out"] = accum
    return lambda h: h.activation(out=out, in_=in_, func=func, **kw)
def f_ts(out, in0, s1, s2=None, op0=ALU.mult, op1=None, accum=None):
    kw = {}
    if op1 is not None: kw["op1"] = op1
    if accum is not None: kw["accum_out"] = accum
    return lambda h: h.tensor_scalar(out=out, in0=in0, scalar1=s1, scalar2=s2, op0=op0, **kw)
def f_tt(out, in0, in1, op):
    return lambda h: h.tensor_tensor(out=out, in0=in0, in1=in1, op=op)
def f_stt(out, in0, scalar, in1, op0, op1):
    return lambda h: h.scalar_tensor_tensor(out=out, in0=in0, scalar=scalar, in1=in1, op0=op0, op1=op1)
def f_cp(out, in_):
    return lambda h: h.tensor_copy(out=out, in_=in_)
def f_memset(ap, v):
    return lambda h: h.memset(ap, v)


def build_program():
    nc = bass.Bass("TRN2", target_bir_lowering=False)
    dt_in = lambda n, s, d=F32: nc.dram_tensor(n, s, d, kind="ExternalInput").ap()
    xc = dt_in("xc", [64 * 128, 1024])
    posc = dt_in("posc", [128, 64], I32)
    memc = dt_in("memc", [256, 1024])
    cmask = dt_in("cmask", [128, 5 * 128])
    invf_d = dt_in("invf", [128, 32])
    wk_d = dt_in("wk", [128, 8 * 448])
    wch_d = dt_in("wch", [NCHUNK, 128, 4096])
    wmem_d = dt_in("wmem", [128, 4096])
    gvec_d = dt_in("gvec", [128, 24])
    gfin_d = dt_in("gfin", [128, 1024])
    bgate_d = dt_in("bgate", [128, 24])
    sinkr_d = dt_in("sinkr", [64, 8 * 128])
    out_d = nc.dram_tensor("out", [32 * 128, 1024], F32, kind="ExternalOutput").ap()
    wbf_d = nc.dram_tensor("wbf", [NCHUNK, 128, 4096], BF16, kind="Internal").ap()

    with ExitStack() as st:
        T = Trk(nc, st, 48)
        def sb(name, shape, dt):
            return st.enter_context(nc.sbuf_tensor(name, shape, dt))
        BIG = sb("BIG", [128, 8192], F32)
        MSK = sb("MSK", [128, 8192], BF16)
        ISC = [Reg(f"isc{c}") for c in range(16)]
        MSKR = [Reg(f"msk{c}") for c in range(16)]
        hidT = BIG[:].bitcast(BF16).rearrange("p (f t) -> p f t", t=512)
        wk_sb = sb("wk_sb", [128, 8, 448], BF16); R_wk = Reg("wk")
        kDI = sb("kDI", [128, 8192], BF16); R_kDI = [Reg(f"kDI{i}") for i in range(64)]
        vD = sb("vD", [128, 64, 64], BF16); R_vD = [Reg(f"vD{i}") for i in range(64)]
        kST = sb("kST", [128, 16, 128], BF16); R_kST = [Reg(f"kST{i}") for i in range(16)]
        vS = sb("vS", [128, 16, 128], BF16); R_vS = [Reg(f"vS{i}") for i in range(16)]
        mkT = sb("mkT", [128, 2, 256], BF16); R_mkT = Reg("mkT")
        mv = sb("mv", [128, 2, 256], BF16); R_mv = Reg("mv")
        xg = sb("xg", [128, 4, 1024], F32); R_xg = [Reg(f"xg{j}") for j in range(4)]
        xo = sb("xo", [128, 1024], F32); R_xo = Reg("xo")
        hTg = sb("hTg", [128, 8, 512], BF16); R_hTg = [Reg(f"hTg{j}") for j in range(4)]
        hTo = sb("hTo", [128, 8, 128], BF16); R_hTo = Reg("hTo")
        oTg = sb("oTg", [64, 16, 512], BF16); R_oTg = [Reg(f"oTg{j}") for j in range(4)]
        QM = sb("QM", [128, 4096], BF16)
        qsT = QM[:, 0:2048].rearrange("p (s c) -> p s c", s=4)
        qdiT = QM[:, 2048:4096].rearrange("p (s c) -> p s c", s=4)
        mergedT = QM[:].rearrange("p (k t) -> p k t", k=8)
        R_qs = [Reg(f"qs{j}") for j in range(4)]; R_qdi = [Reg(f"qdi{j}") for j in range(4)]
        qmT = sb("qmT", [128, 4, 256], BF16); R_qm = [Reg(f"qm{j}") for j in range(4)]
        wiT = sb("wiT", [128, 4, 4], F32); R_wi = [Reg(f"wi{j}") for j in range(4)]
        NRING = 2
        ring = [sb(f"ring{i}", [128, 4096], BF16) for i in range(NRING)]
        R_ring = [Reg(f"ring{i}") for i in range(NRING)]
        ident = sb("ident", [128, 128], BF16); identf = sb("identf", [128, 128], F32); onesf = sb("onesf", [128, 128], F32)
        ones_b = sb("ones_b", [128, 64], BF16)
        R_const = Reg("const")
        cm_f = sb("cm_f", [128, 5, 128], F32)
        cm_b = sb("cm_b", [128, 3, 128], BF16)
        invf = sb("invf_sb", [128, 32], F32)
        gvec = sb("gvec_sb", [128, 24], F32)
        gfin = sb("gfin_sb", [128, 1024], F32)
        bgate = sb("bgate_sb", [128, 24], F32)
        es = sb("es_sb", [64, 8, 128], F32)
        pw2 = sb("pw2", [128, NITER], F32)
        posi = sb("posi", [128, 64], I32); posf = sb("posf", [128, 64], F32)
        cosT = sb("cosT", [128, 8, 32], F32); sinT = sb("sinT", [128, 8, 32], F32); R_cs = Reg("cs")
        rtmp = [sb(f"rtmp{i}", [128, 256], F32) for i in range(3)]; rtmpi = sb("rtmpi", [128, 256], I32); R_rt = Reg("rt")
        stat = sb("stat", [128, 16], F32); R_stat = [Reg(f"stat{i}") for i in range(4)]
        hn = [sb(f"hn{i}", [128, 1024], BF16) for i in range(2)]; R_hn = [Reg(f"hn{i}") for i in range(2)]
        tok_b = [sb(f"tok_b{i}", [128, 512], BF16) for i in range(2)]; R_tok = [Reg(f"tok{i}") for i in range(2)]
        Et = [sb(f"Et{i}", [128, 512], BF16) for i in range(3)]; R_E = [Reg(f"E{i}") for i in range(3)]
        Pt = [sb(f"Pt{i}", [128, 512], BF16) for i in range(3)]; R_P = [Reg(f"P{i}") for i in range(3)]
        rl = [sb(f"rl{i}", [128, 512], F32) for i in range(2)]; R_rl = [Reg(f"rl{i}") for i in range(2)]
        mT = [sb(f"mT{i}", [128, 512], BF16) for i in range(2)]; R_mT = [Reg(f"mT{i}") for i in range(2)]
        dn = [sb(f"dn{i}", [64, 512], F32) for i in range(1)]; R_dn = [Reg(f"dn{i}") for i in range(1)]
        mg = rl; R_mg = R_rl
        rope_t = rl[0]; rope_u = rl[1]
        gsb = [sb(f"gsb{i}", [128, 512], BF16) for i in range(3)]; R_gsb = [Reg(f"gsb{i}") for i in range(3)]
        bis = sb("bis", [128, 8 + NITER], F32); R_lo = Reg("lo"); R_mid = Reg("mid"); R_cd = Reg("cd"); R_ca = Reg("ca"); R_t = Reg("t")
        PB = [st.enter_context(nc.psum_tensor(f"pb{i}", [128, 512], F32)) for i in range(8)]
        R_PB = [Reg(f"pb{i}") for i in range(8)]
        def pbf(i):
            return PB[i][:].bitcast(BF16)

        cnt = {"ring": 0, "stat": 0, "hn": 0, "tok": 0, "E": 0, "P": 0, "rl": 0, "mT": 0, "dn": 0, "mg": 0}
        def rot(name, n):
            i = cnt[name] % n; cnt[name] += 1; return i

        R_cmf = Reg("cmf"); R_misc = Reg("misc")
        T.dma("sp", cm_f[:].rearrange("p a b -> p (a b)"), cmask[:, :], R_cmf, w=[R_cmf])
        for (dst, src) in ((invf, invf_d), (gvec, gvec_d), (gfin, gfin_d), (bgate, bgate_d)):
            T.dma("sp", dst[:], src[:, :], R_misc, w=[R_misc])
            T.wait_all("sp", [R_misc])
        R_es = Reg("es")
        T.dma("sp", es[:].rearrange("p a b -> p (a b)"), sinkr_d[:, :], R_es, w=[R_es])
        R_pos = Reg("pos")
        T.dma("sp", posi[:], posc[:, :], R_pos, w=[R_pos])
        T.op("pool", f_memset(onesf[:], 1.0), w=[R_const])
        T.op("pool", f_memset(identf[:], 0.0), w=[R_const])
        T.op("pool", lambda h: h.affine_select(out=identf[:], in_=onesf[:], pattern=[[-1, 128]], compare_op=ALU.is_equal,
                                                fill=0.0, base=0, channel_multiplier=1), r=[R_const], w=[R_const])
        T.op("dve", f_cp(ident[:], identf[:]), r=[R_const], w=[R_const])
        T.op("dve", f_memset(ones_b[:], 1.0), w=[R_const])
        for k in range(NITER):
            T.op("dve", f_memset(pw2[:, k:k + 1], 2.0 ** (-(k + 1))), w=[R_const])
        T.op("dve", f_cp(cm_b[:], cm_f[:, 0:3, :]), r=[R_cmf], w=[R_const])
        T.op("act", f_act(es[:], es[:], AF.Exp), r=[R_es], w=[R_es])
        T.op("dve", f_cp(posf[:], posi[:]), r=[R_pos], w=[R_pos])
        tri_cur = cm_b[:, 0, :]; tri_prev = cm_b[:, 1, :]; prev0 = cm_b[:, 2, :]
        dbias = cm_f[:, 3, :]; p63bias = cm_f[:, 4, :]

        def emit():
            def norm_T(x_ap, x_regs, dstT, dst_regs, pbank):
                si = rot("stat", 4); s = stat[:, si * 4:(si + 1) * 4]; rs = R_stat[si]
                hi = rot("hn", 2); h_ = hn[hi]; rh = R_hn[hi]
                T.op("act", f_act(h_[:], x_ap, AF.Square, accum=s[:, 0:1]), r=x_regs, w=[rh, rs])
                T.op("act", f_act(s[:, 1:2], s[:, 0:1], AF.Ln, scale=1.0 / 1024.0, bias=1e-6), r=[rs], w=[rs])
                T.op("act", f_act(s[:, 2:3], s[:, 1:2], AF.Exp, scale=-0.5), r=[rs], w=[rs])
                T.op("dve", f_ts(h_[:], x_ap, s[:, 2:3], None, ALU.mult), r=x_regs + [rs], w=[rh])
                pv = pbf(pbank)
                T.op("pe", f_seq([f_tr(pv[:, kc * 128:(kc + 1) * 128], h_[:, kc * 128:(kc + 1) * 128], ident[:]) for kc in range(8)]),
                     r=[rh, R_const], w=[R_PB[pbank]])
                T.op("act", f_act(dstT, pv[:, :].rearrange("p (k t) -> p k t", k=8), AF.Copy), r=[R_PB[pbank]], w=dst_regs)
                return s, rs

            def rope_tables(blk0, nb):
                n = nb * 32
                a0 = rtmp[0][:, 0:n]; a1 = rtmp[1][:, 0:n]; a2 = rtmp[2][:, 0:n]; ai = rtmpi[:, 0:n]
                v3 = lambda a: a.rearrange("p (b f) -> p b f", f=32)
                T.op("dve", f_tt(v3(a0), posf[:, blk0:blk0 + nb].unsqueeze(2).to_broadcast([128, nb, 32]),
                                 invf[:].unsqueeze(1).to_broadcast([128, nb, 32]), ALU.mult), r=[R_pos, R_misc], w=[R_rt])
                T.op("dve", f_ts(ai, a0, 1.0 / (2 * math.pi), None, ALU.mult), r=[R_rt], w=[R_rt])
                T.op("dve", f_cp(a1, ai), r=[R_rt], w=[R_rt])
                T.op("dve", f_stt(a2, a1, -6.28125, a0, ALU.mult, ALU.add), r=[R_rt], w=[R_rt])
                T.op("dve", f_stt(a2, a1, -0.0019353072, a2, ALU.mult, ALU.add), r=[R_rt], w=[R_rt])
                T.op("dve", f_ts(a0, a2, -3.1415925, 3.1415925, ALU.max, ALU.min), r=[R_rt], w=[R_rt])
                T.op("act", f_act(sinT[:, 0:nb, :].rearrange("p b f -> p (b f)"), a0, AF.Sin), r=[R_rt], w=[R_cs])
                T.op("dve", f_ts(a1, a2, math.pi / 2, None, ALU.add), r=[R_rt], w=[R_rt])
                T.op("dve", f_ts(a0, a1, math.pi, -2 * math.pi, ALU.is_gt, ALU.mult), r=[R_rt, R_cs], w=[R_rt])
                T.op("dve", f_tt(a0, a0, a1, ALU.add), r=[R_rt], w=[R_rt])
                T.op("dve", f_ts(a0, a0, -3.1415925, 3.1415925, ALU.max, ALU.min), r=[R_rt], w=[R_rt])
                T.op("act", f_act(cosT[:, 0:nb, :].rearrange("p b f -> p (b f)"), a0, AF.Sin), r=[R_rt], w=[R_cs])

            def rope_apply(src, src_regs, dst, dst_regs, H, cb):
                n = H * 64
                t_ = rope_t[:, 0:n]; u_ = rope_u[:, 0:n]
                cosb = cosT[:, cb:cb + 1, :]; sinb = sinT[:, cb:cb + 1, :]
                T.op("dve", f_tt(t_.rearrange("p (a f) -> p a f", f=32), src.rearrange("p (a f) -> p a f", f=32),
                                 cosb.to_broadcast([128, 2 * H, 32]), ALU.mult), r=src_regs + [R_cs], w=[R_rl[0]])
                s4 = src.rearrange("p (h e f) -> p h e f", e=2, f=32)
                u4 = u_.rearrange("p (h e f) -> p h e f", e=2, f=32)
                t4 = t_.rearrange("p (h e f) -> p h e f", e=2, f=32)
                d4 = dst.rearrange("p (h e f) -> p h e f", e=2, f=32)
                sb_ = sinb.to_broadcast([128, H, 32])
                T.op("dve", f_tt(u4[:, :, 0, :], s4[:, :, 1, :], sb_, ALU.mult), r=src_regs + [R_cs], w=[R_rl[1]])
                T.op("dve", f_tt(u4[:, :, 1, :], s4[:, :, 0, :], sb_, ALU.mult), r=src_regs + [R_cs], w=[R_rl[1]])
                T.op("dve", f_tt(d4[:, :, 0, :], t4[:, :, 0, :], u4[:, :, 0, :], ALU.subtract), r=R_rl, w=dst_regs)
                T.op("dve", f_tt(d4[:, :, 1, :], t4[:, :, 1, :], u4[:, :, 1, :], ALU.add), r=R_rl, w=dst_regs)

            R_wbf = [Reg(f"wbf{i}") for i in range(NCHUNK)]
            CH_N = [4096, 4096, 8 * 260] + [8 * 384] * 8 + [2048] * 8 + [4096] * 18
            CH_P = [128] * 11 + [64] * 8 + [128] * 18
            sched = [0, 1, 2] + [v for oc in range(8) for v in (3 + oc, 11 + oc)] + list(range(19, 37))
            ring_state = {"issued": 0, "got": 0}
            total_gets = 8 * NCHUNK
            def ring_issue():
                k = ring_state["issued"]
                if k >= total_gets: return
                c = sched[k % NCHUNK]; b = k % NRING
                T.dma("sp", ring[b][0:CH_P[c], 0:CH_N[c]], wbf_d[c, 0:CH_P[c], 0:CH_N[c]], R_ring[b], r=[R_wbf[c]], w=[R_ring[b]])
                ring_state["issued"] += 1
            def ring_get(expect):
                k = ring_state["got"]
                assert sched[k % NCHUNK] == expect, (k, expect)
                while ring_state["issued"] < min(k + NRING, total_gets):
                    ring_issue()
                ring_state["got"] += 1
                b = k % NRING
                return ring[b], R_ring[b]

            def convert(src_dram_ap, npart, nelem, ncols, goff, dst_ap, dst_regs, half):
                stg = BIG[0:npart, half * 4096: half * 4096 + nelem]
                sregs = ISC[half * 8:(half + 1) * 8]
                T.dma("pool", stg, src_dram_ap, sregs[0], w=sregs)
                if goff is None:
                    T.op("act", f_act(dst_ap, stg, AF.Copy), r=sregs, w=dst_regs)
                else:
                    for kc in range(8):
                        T.op("dve", f_ts(dst_ap[:, kc * ncols:(kc + 1) * ncols], stg[:, kc * ncols:(kc + 1) * ncols],
                                          gvec[:, goff + kc:goff + kc + 1], None, ALU.mult), r=sregs + [R_misc], w=dst_regs)
            _ckpt(1)
            convert(wk_d[:, :], 128, 8 * 448, 448, 0, wk_sb[:].rearrange("p k c -> p (k c)"), [R_wk], 0)
            wmem_b = MSK[:, 4096:8192]
            convert(wmem_d[:, :], 128, 4096, 512, 16, wmem_b, MSKR[8:16], 1)

            for mc in range(2):
                T.dma("sp", xo[:], memc[mc * 128:(mc + 1) * 128, :], R_xo, w=[R_xo])
                norm_T(xo[:], [R_xo], hTo[:], [R_hTo], 0)
                T.op("pe", f_seq([f_mm(PB[1][:, :], hTo[:, kc, :], wmem_b[:, kc * 512:(kc + 1) * 512], kc == 0, kc == 7) for kc in range(8)]),
                     r=[R_hTo] + MSKR[8:16], w=[R_PB[1]])
                ti = rot("tok", 2)
                T.op("act", f_act(tok_b[ti][:, 0:256], PB[1][:, 0:256], AF.Copy), r=[R_PB[1]], w=[R_tok[ti]])
                T.op("act", f_act(mv[:, mc, :], PB[1][:, 256:512], AF.Copy), r=[R_PB[1]], w=[R_mv])
                pv = pbf(2)
                T.op("pe", f_seq([f_tr(pv[:, jj * 128:(jj + 1) * 128], tok_b[ti][:, jj * 128:(jj + 1) * 128], ident[:]) for jj in range(2)]),
                     r=[R_tok[ti], R_const], w=[R_PB[2]])
                T.op("act", f_act(mkT[:, :, mc * 128:(mc + 1) * 128], pv[:, 0:256].rearrange("p (j m) -> p j m", j=2), AF.Copy),
                     r=[R_PB[2]], w=[R_mkT])

            _ckpt(2)
            for c in range(NCHUNK):
                half = c % 2
                goff = 0 if c < 11 else (8 if 21 <= c < 29 else None)
                ncols = {0: 512, 1: 512, 2: 260}.get(c, 384 if c < 11 else 512)
                npart = CH_P[c]; nelem = CH_N[c]
                dstb = MSK[0:npart, half * 4096: half * 4096 + nelem]
                dregs = MSKR[half * 8:(half + 1) * 8]
                convert(wch_d[c, 0:npart, 0:nelem], npart, nelem, ncols, goff, dstb, dregs, half)
                T.dma("pool", wbf_d[c, 0:npart, 0:nelem], dstb, dregs[0], r=dregs, w=[R_wbf[c]])

            def kside(pos, x_ap, x_regs, hT_ap, hT_regs, cb):
                kidx = 0 if pos == 63 else pos + 1
                rg = (pos + 1) % 16
                norm_T(x_ap, x_regs, hT_ap, hT_regs, 0)
                _ckpt(3.1)
                T.op("pe", f_seq([f_mm(PB[1][:, 0:448], hT_ap[:, kc, :], wk_sb[:, kc, :], kc == 0, kc == 7) for kc in range(8)]),
                     r=hT_regs + [R_wk], w=[R_PB[1]])
                _ckpt(3.2)
                ti = rot("tok", 2)
                rope_apply(PB[1][:, 0:256], [R_PB[1]], tok_b[ti][:, 0:256], [R_tok[ti]], 4, cb)
                _ckpt(3.3)
                T.op("dve", f_cp(vS[:, rg, :], PB[1][:, 256:384]), r=[R_PB[1]], w=[R_vS[rg]])
                T.op("dve", f_cp(vD[:, kidx, :], PB[1][:, 384:448]), r=[R_PB[1]], w=[R_vD[kidx]])
                _ckpt(3.4)
                pv = pbf(2)
                T.op("pe", f_seq([f_tr(pv[:, jj * 128:(jj + 1) * 128], tok_b[ti][:, jj * 128:(jj + 1) * 128], ident[:]) for jj in range(2)]),
                     r=[R_tok[ti], R_const], w=[R_PB[2]])
                _ckpt(3.5)
                T.op("act", f_act(kST[:, rg, :], pv[:, 0:128], AF.Copy), r=[R_PB[2]], w=[R_kST[rg]])
                T.op("act", f_act(kDI[:, kidx * 128:(kidx + 1) * 128], pv[:, 128:256], AF.Copy), r=[R_PB[2]], w=[R_kDI[kidx]])

            _ckpt(3)
            rope_tables(63, 1)
            T.dma("sp", xo[:], xc[63 * 128:64 * 128, :], R_xo, w=[R_xo])
            kside(63, xo[:], [R_xo], hTo[:], [R_hTo], 0)

            def finish_attn(bO, bD, j, h0, es_ap):
                di = rot("dn", 1)
                if es_ap is not None:
                    T.op("dve", f_tt(dn[di][:], PB[bD][0:64, :], es_ap, ALU.add), r=[R_PB[bD], R_es], w=[R_dn[di]])
                    T.op("dve", lambda h, a=dn[di][:]: h.reciprocal(out=a, in_=a), r=[R_dn[di]], w=[R_dn[di]])
                else:
                    T.op("dve", f_cp(dn[di][:], PB[bD][0:64, :]), r=[R_PB[bD]], w=[R_dn[di]])
                    T.op("dve", lambda h, a=dn[di][:]: h.reciprocal(out=a, in_=a), r=[R_dn[di]], w=[R_dn[di]])
                T.op("dve", f_tt(oTg[:, h0:h0 + 4, j * 128:(j + 1) * 128], PB[bO][0:64, :].rearrange("p (h t) -> p h t", h=4),
                                 dn[di][:].rearrange("p (h t) -> p h t", h=4), ALU.mult), r=[R_PB[bO], R_dn[di]], w=[R_oTg[j]])

            _ckpt(4)
            for g in range(8):
                npos = 8 if g < 7 else 7
                rope_tables(8 * g, 8)
                for pl in range(npos):
                    pos = 8 * g + pl
                    if pl % 2 == 0:
                        j = pl // 2
                        T.dma("sp", xg[:, j, :], xc[pos * 128:(pos + 1) * 128, :], R_xg[j], w=[R_xg[j]])
                        kside(pos, xg[:, j, :], [R_xg[j]], hTg[:, :, j * 128:(j + 1) * 128], [R_hTg[j]], pl)
                    else:
                        T.dma("sp", xo[:], xc[pos * 128:(pos + 1) * 128, :], R_xo, w=[R_xo])
                        kside(pos, xo[:], [R_xo], hTo[:], [R_hTo], pl)

                _ckpt(10 * (g + 1) + 1)
                for c in range(3):
                    W, RW = ring_get(c)
                    ncols = (512, 512, 260)[c]
                    Wv = W[:, 0:8 * ncols].rearrange("p (k c) -> p k c", k=8)
                    for j in range(4):
                        pb = 3 + (j % 2)
                        T.op("pe", f_seq([f_mm(PB[pb][:, 0:ncols], hTg[:, kc, j * 128:(j + 1) * 128], Wv[:, kc, :], kc == 0, kc == 7) for kc in range(8)]),
                             r=[R_hTg[j], RW], w=[R_PB[pb]])
                        ti = rot("tok", 2)
                        if c < 2:
                            rope_apply(PB[pb][:, 0:512], [R_PB[pb]], tok_b[ti][:, 0:512], [R_tok[ti]], 8, 2 * j)
                            pv = pbf(5 + (j % 2))
                            T.op("pe", f_seq([f_tr(pv[:, q * 128:(q + 1) * 128], tok_b[ti][:, q * 128:(q + 1) * 128], ident[:]) for q in range(4)]),
                                 r=[R_tok[ti], R_const], w=[R_PB[5 + (j % 2)]])
                            dst = qsT if c == 0 else qdiT
                            T.op("act", f_act(dst[:, j, :], pv[:, 0:512], AF.Copy), r=[R_PB[5 + (j % 2)]], w=[(R_qs if c == 0 else R_qdi)[j]])
                        else:
                            T.op("act", f_act(tok_b[ti][:, 0:256], PB[pb][:, 0:256], AF.Copy), r=[R_PB[pb]], w=[R_tok[ti]])
                            T.op("act", f_act(wiT[:, j, :], PB[pb][:, 256:260], AF.Copy, scale=1.0 / 16.0), r=[R_PB[pb]], w=[R_wi[j]])
                            pv = pbf(5 + (j % 2))
                            T.op("pe", f_seq([f_tr(pv[:, q * 128:(q + 1) * 128], tok_b[ti][:, q * 128:(q + 1) * 128], ident[:]) for q in range(2)]),
                                 r=[R_tok[ti], R_const], w=[R_PB[5 + (j % 2)]])
                            T.op("act", f_act(qmT[:, j, :], pv[:, 0:256], AF.Copy), r=[R_PB[5 + (j % 2)]], w=[R_qm[j]])

                _ckpt(10 * (g + 1) + 2)
                def slot_vars(j):
                    i = 4 * g + j
                    nkb = 2 * i + 2
                    return i, slice(j * 128, (j + 1) * 128), nkb, (nkb + 3) // 4, nkb * 128
                def swa_mem(j):
                    i, tok, nkb, nch, n = slot_vars(j)
                    rp = (2 * i) % 16; rc = (2 * i + 1) % 16
                    for kv in range(2):
                        pr = slice(kv * 64, (kv + 1) * 64)
                        bS = (2, 3) if kv == 0 else (4, 5)
                        bO, bD = (0, 1) if kv == 0 else (6, 7)
                        Ps = []
                        for which, rr_ in enumerate((rp, rc)):
                            b = bS[which]
                            T.op("pe", f_mm(PB[b][:, :], kST[pr, rr_, :], qsT[pr, j, :]), r=[R_kST[rr_], R_qs[j]], w=[R_PB[b]])
                            ei = rot("E", 3)
                            T.op("act", f_act(Et[ei][:], PB[b][:, :], AF.Exp, scale=0.125), r=[R_PB[b]], w=[R_E[ei]])
                            pi_ = rot("P", 3)
                            msk = (prev0 if i == 0 else tri_prev) if which == 0 else tri_cur
                            T.op("dve", f_tt(Pt[pi_][:].rearrange("p (h t) -> p h t", h=4), Et[ei][:].rearrange("p (h t) -> p h t", h=4),
                                             msk.unsqueeze(1).to_broadcast([128, 4, 128]), ALU.mult), r=[R_E[ei], R_const], w=[R_P[pi_]])
                            Ps.append((pi_, rr_))
                        T.op("pe", f_seq([f_mm(PB[bO][0:64, :], vS[:, rr_, pr], Pt[pi_][:], w_ == 0, w_ == 1) for w_, (pi_, rr_) in enumerate(Ps)]
                                         + [f_mm(PB[bD][0:64, :], ones_b[:], Pt[pi_][:], w_ == 0, w_ == 1) for w_, (pi_, rr_) in enumerate(Ps)]),
                             r=[R_P[p_] for p_, _ in Ps] + [R_vS[r_] for _, r_ in Ps] + [R_const], w=[R_PB[bO], R_PB[bD]])
                        finish_attn(bO, bD, j, 4 * kv, es[:, 4 * kv:4 * kv + 4, :].rearrange("p h t -> p (h t)"))
                    _ckpt(12.1)
                    Es = [rot("E", 3), rot("E", 3)]
                    fns = []
                    for e in range(2):
                        pr = slice(e * 64, (e + 1) * 64)
                        for mc in range(2):
                            for jj in range(2):
                                fns.append(f_mm(PB[2 + 2 * mc + e][:, jj * 128:(jj + 1) * 128], mkT[pr, jj, mc * 128:(mc + 1) * 128],
                                                qmT[pr, j, jj * 128:(jj + 1) * 128]))
                    T.op("pe", f_seq(fns), r=[R_mkT, R_qm[j]], w=[R_PB[2], R_PB[3], R_PB[4], R_PB[5]])
                    for mc in range(2):
                        E4 = Et[Es[mc]][:].rearrange("p (jj e t) -> p jj e t", jj=2, e=2)
                        for e in range(2):
                            b = 2 + 2 * mc + e
                            T.op("act", f_act(E4[:, :, e, :], PB[b][:, 0:256].rearrange("p (jj t) -> p jj t", jj=2), AF.Exp, scale=0.125),
                                 r=[R_PB[b]], w=[R_E[Es[mc]]])
                    _ckpt(12.15)
                    fns = []
                    for hh in range(4):
                        for mc in range(2):
                            fns.append(f_mm(PB[0][0:64, hh * 128:(hh + 1) * 128], mv[:, mc, hh * 64:(hh + 1) * 64], Et[Es[mc]][:, hh * 128:(hh + 1) * 128], mc == 0, mc == 1))
                    for mc in range(2):
                        fns.append(f_mm(PB[1][0:64, :], ones_b[:], Et[Es[mc]][:], mc == 0, mc == 1))
                    T.op("pe", f_seq(fns), r=[R_E[e_] for e_ in Es] + [R_mv, R_const], w=[R_PB[0], R_PB[1]])
                    _ckpt(12.17)
                    finish_attn(0, 1, j, 12, None)
                    _ckpt(12.2)
                def dsa_index(j):
                    i, tok, nkb, nch, n = slot_vars(j)
                    for c in range(nch):
                        kb0 = 4 * c; nb = min(4, nkb - kb0); w_ = nb * 128
                        cols = slice(kb0 * 128, kb0 * 128 + w_)
                        T.op("pe", f_seq([f_mm(PB[2 + hh][:, 0:w_], qdiT[64:128, j, hh * 128:(hh + 1) * 128], kDI[64:128, cols]) for hh in range(4)]),
                             r=[R_qdi[j]] + R_kDI[kb0:kb0 + nb], w=[R_PB[2 + hh] for hh in range(4)])
                        for hh in range(4):
                            ri = rot("rl", 2)
                            T.op("act", f_act(rl[ri][:, 0:w_], PB[2 + hh][:, 0:w_], AF.Relu), r=[R_PB[2 + hh]], w=[R_rl[ri]])
                            if hh == 0:
                                T.op("dve", f_ts(BIG[:, cols], rl[ri][:, 0:w_], wiT[:, j, 0:1], None, ALU.mult), r=[R_rl[ri], R_wi[j]], w=[ISC[c]])
                            else:
                                T.op("dve", f_stt(BIG[:, cols], rl[ri][:, 0:w_], wiT[:, j, hh:hh + 1], BIG[:, cols], ALU.mult, ALU.add),
                                     r=[R_rl[ri], R_wi[j], ISC[c]], w=[ISC[c]])
                def dsa_bisect(j):
                    i, tok, nkb, nch, n = slot_vars(j)
                    _ckpt(12.3)
                    IR = ISC[0:nch]; MR = MSKR[0:nch]
                    cd = nch if nch < 2 else (nch + 1) // 2
                    nd = min(n, cd * 512); na = n - nd
                    IRd, MRd, IRa, MRa = ISC[0:cd], MSKR[0:cd], ISC[cd:nch], MSKR[cd:nch]
                    amax = bis[:, 0:1]; wtot = bis[:, 1:2]; lo = bis[:, 2:3]; mid = bis[:, 3:4]; cn = bis[:, 4:5]; dl = bis[:, 5:6]
                    sa = bis[:, 6:7]; tt_ = bis[:, 7:8]
                    wks = bis[:, 8:8 + NITER]
                    T.op("dve", lambda h, a=amax, b=BIG[:, 0:n]: h.tensor_reduce(out=a, in_=b, axis=AX.X, op=ALU.max, apply_absolute_value=True),
                         r=IR, w=[R_lo])
                    T.op("pool", f_tt(BIG[:, 0:128], BIG[:, 0:128], p63bias, ALU.add), r=[ISC[0], R_cmf, R_lo], w=[ISC[0]])
                    dcol = slice((nkb - 1) * 128, nkb * 128)
                    T.op("pool", f_tt(BIG[:, dcol], BIG[:, dcol], dbias, ALU.add), r=[ISC[nch - 1], R_cmf, R_lo], w=[ISC[nch - 1]])
                    T.op("dve", f_ts(wtot, amax, 2.0002, 2e-6, ALU.mult, ALU.add), r=[R_lo], w=[R_lo])
                    T.op("dve", f_ts(lo, amax, -1.0001, -1e-6, ALU.mult, ALU.add), r=[R_lo], w=[R_lo])
                    T.op("dve", f_ts(wks, pw2[:], wtot, None, ALU.mult), r=[R_lo, R_const], w=[R_lo])
                    thrc = TOPK - na / 2.0
                    for k in range(NITER):
                        T.op("dve", f_tt(mid, lo, wks[:, k:k + 1], ALU.add), r=[R_lo], w=[R_mid])
                        if na > 0:
                            T.op("act", f_act(MSK[:, nd:n], BIG[:, nd:n], AF.Sign, bias=mid, scale=-1.0, accum=sa), r=IRa + [R_mid], w=MRa + [R_ca])
                        T.op("dve", f_ts(MSK[:, 0:nd], BIG[:, 0:nd], mid, 0.0, ALU.is_ge, ALU.add, accum=cn), r=IRd + [R_mid], w=MRd + [R_cd])
                        if na > 0:
                            T.op("dve", f_stt(tt_, sa, -0.5, cn, ALU.mult, ALU.add), r=[R_ca, R_cd], w=[R_t])
                            T.op("dve", f_stt(dl, tt_, thrc, wks[:, k:k + 1], ALU.is_ge, ALU.mult), r=[R_t, R_lo], w=[R_t])
                        else:
                            T.op("dve", f_stt(dl, cn, TOPK, wks[:, k:k + 1], ALU.is_ge, ALU.mult), r=[R_cd, R_lo], w=[R_t])
                        T.op("dve", f_tt(lo, lo, dl, ALU.add), r=[R_t], w=[R_lo])
                    T.op("dve", f_ts(MSK[:, 0:n], BIG[:, 0:n], lo, None, ALU.is_ge), r=IR + [R_lo], w=MR)
                def dsa_attn(j):
                    i, tok, nkb, nch, n = slot_vars(j)
                    _ckpt(12.4)
                    for c in range(nch):
                        kb0 = 4 * c; nb = min(4, nkb - kb0)
                        mi = rot("mT", 2)
                        pv = pbf(2)
                        T.op("pe", f_seq([f_tr(pv[:, q * 128:(q + 1) * 128], MSK[:, (kb0 + q) * 128:(kb0 + q + 1) * 128], ident[:]) for q in range(nb)]),
                             r=[MSKR[c], R_const], w=[R_PB[2]])
                        T.op("act", f_act(mT[mi][:, 0:nb * 128], pv[:, 0:nb * 128], AF.Copy), r=[R_PB[2]], w=[R_mT[mi]])
                        for q in range(nb):
                            kb = kb0 + q
                            b = 3 + (kb % 3)
                            T.op("pe", f_mm(PB[b][:, :], kDI[0:64, kb * 128:(kb + 1) * 128], qdiT[0:64, j, :]), r=[R_kDI[kb], R_qdi[j]], w=[R_PB[b]])
                            ei = rot("E", 3)
                            T.op("act", f_act(Et[ei][:], PB[b][:, :], AF.Exp, scale=0.125), r=[R_PB[b]], w=[R_E[ei]])
                            pi_ = rot("P", 3)
                            T.op("dve", f_tt(Pt[pi_][:].rearrange("p (h t) -> p h t", h=4), Et[ei][:].rearrange("p (h t) -> p h t", h=4),
                                             mT[mi][:, q * 128:(q + 1) * 128].unsqueeze(1).to_broadcast([128, 4, 128]), ALU.mult),
                                 r=[R_E[ei], R_mT[mi]], w=[R_P[pi_]])
                            T.op("pe", f_seq([f_mm(PB[0][0:64, :], vD[:, kb, :], Pt[pi_][:], kb == 0, kb == nkb - 1),
                                              f_mm(PB[1][0:64, :], ones_b[:], Pt[pi_][:], kb == 0, kb == nkb - 1)]),
                                 r=[R_P[pi_], R_vD[kb], R_const], w=[R_PB[0], R_PB[1]])
                    finish_attn(0, 1, j, 8, None)

                swa_mem(0)
                for j in range(4):
                    dsa_index(j)
                    dsa_bisect(j)
                    if j < 3:
                        swa_mem(j + 1)
                    dsa_attn(j)

                _ckpt(10 * (g + 1) + 3)
                RQ = R_qs + R_qdi
                for oc in range(8):
                    G, RG = ring_get(3 + oc)
                    Gv = G[:, 0:8 * 384].rearrange("p (k c) -> p k c", k=8)
                    for jb in range(3):
                        T.op("pe", f_seq([f_mm(PB[jb][:, :], Gv[:, kc, jb * 128:(jb + 1) * 128], hTg[:, kc, :], kc == 0, kc == 7) for kc in range(8)]),
                             r=R_hTg + [RG], w=[R_PB[jb]])
                        T.op("act", f_act(gsb[jb][:], PB[jb][:, :], AF.Sigmoid, bias=bgate[:, oc * 3 + jb:oc * 3 + jb + 1], scale=1.0),
                             r=[R_PB[jb], R_misc], w=[R_gsb[jb]])
                    Pw, RP = ring_get(11 + oc)
                    Pv = Pw[0:64, 0:2048].rearrange("p (h c) -> p h c", h=16)
                    for jb, (h0, nh) in enumerate(((0, 8), (8, 4), (12, 4))):
                        T.op("pe", f_seq([f_mm(PB[3 + jb][:, :], Pv[:, h0 + hh, :], oTg[:, h0 + hh, :], hh == 0, hh == nh - 1) for hh in range(nh)]),
                             r=R_oTg + [RP], w=[R_PB[3 + jb]])
                    T.op("dve", f_tt(mg[0][:], PB[3][:, :], gsb[0][:], ALU.mult), r=[R_PB[3], R_gsb[0]], w=[R_mg[0]])
                    T.op("dve", f_tt(mg[1][:], PB[4][:, :], gsb[1][:], ALU.mult), r=[R_PB[4], R_gsb[1]], w=[R_mg[1]])
                    T.op("dve", f_tt(mg[0][:], mg[0][:], mg[1][:], ALU.add), r=[R_mg[0], R_mg[1]], w=[R_mg[0]])
                    T.op("dve", f_tt(mg[1][:], PB[5][:, :], gsb[2][:], ALU.mult), r=[R_PB[5], R_gsb[2]], w=[R_mg[1]])
                    T.op("dve", f_tt(mergedT[:, oc, :], mg[0][:], mg[1][:], ALU.add), r=[R_mg[0], R_mg[1]], w=RQ)

                _ckpt(10 * (g + 1) + 4)
                for cc in range(2):
                    W, RW = ring_get(19 + cc)
                    Wv = W[:, :].rearrange("p (k c) -> p k c", k=8)
                    for j in range(4):
                        pb = 6 + (j % 2)
                        T.op("pe", f_seq([f_mm(PB[pb][:, :], mergedT[:, kc, j * 128:(j + 1) * 128], Wv[:, kc, :], kc == 0, kc == 7) for kc in range(8)]),
                             r=RQ + [RW], w=[R_PB[pb]])
                        T.op("dve", f_tt(xg[:, j, cc * 512:(cc + 1) * 512], PB[pb][:, :], xg[:, j, cc * 512:(cc + 1) * 512], ALU.add),
                             r=[R_PB[pb], R_xg[j]], w=[R_xg[j]])

                _ckpt(10 * (g + 1) + 5)
                for j in range(4):
                    norm_T(xg[:, j, :], [R_xg[j]], hTg[:, :, j * 128:(j + 1) * 128], [R_hTg[j]], j % 2)
                for fb in range(8):
                    W, RW = ring_get(21 + fb)
                    Wv = W[:, :].rearrange("p (k c) -> p k c", k=8)
                    for fs in range(4):
                        fc = fb * 4 + fs
                        pb = 2 + (fc % 4)
                        T.op("pe", f_seq([f_mm(PB[pb][:, :], Wv[:, kc, fs * 128:(fs + 1) * 128], hTg[:, kc, :], kc == 0, kc == 7) for kc in range(8)]),
                             r=R_hTg + [RW], w=[R_PB[pb]])
                        ri = rot("rl", 2)
                        T.op("act", f_act(rl[ri][:], PB[pb][:, :], AF.Relu), r=[R_PB[pb]], w=[R_rl[ri]])
                        T.op("dve", f_tt(hidT[:, fc, :], rl[ri][:], rl[ri][:], ALU.mult), r=[R_rl[ri]], w=[ISC[fc // 2]])
                for cc in range(2):
                    for ks in range(4):
                        W, RW = ring_get(29 + cc * 4 + ks)
                        Wv = W[:, :].rearrange("p (k c) -> p k c", k=8)
                        for j in range(4):
                            pb = 4 + j
                            T.op("pe", f_seq([f_mm(PB[pb][:, :], hidT[:, ks * 8 + fl, j * 128:(j + 1) * 128], Wv[:, fl, :],
                                                   ks == 0 and fl == 0, ks == 3 and fl == 7) for fl in range(8)]),
                                 r=ISC[ks * 4:(ks + 1) * 4] + [RW], w=[R_PB[pb]])
                    for j in range(4):
                        pb = 4 + j
                        T.op("dve", f_tt(xg[:, j, cc * 512:(cc + 1) * 512], PB[pb][:, :], xg[:, j, cc * 512:(cc + 1) * 512], ALU.add),
                             r=[R_PB[pb], R_xg[j]], w=[R_xg[j]])
                _ckpt(10 * (g + 1) + 6)
                for j in range(4):
                    i = 4 * g + j
                    si = rot("stat", 4); s = stat[:, si * 4:(si + 1) * 4]; rs = R_stat[si]
                    hi = rot("hn", 2)
                    T.op("act", f_act(hn[hi][:], xg[:, j, :], AF.Square, accum=s[:, 0:1]), r=[R_xg[j]], w=[R_hn[hi], rs])
                    T.op("act", f_act(s[:, 1:2], s[:, 0:1], AF.Ln, scale=1.0 / 1024.0, bias=1e-6), r=[rs], w=[rs])
                    T.op("act", f_act(s[:, 2:3], s[:, 1:2], AF.Exp, scale=-0.5), r=[rs], w=[rs])
                    T.op("dve", f_stt(xg[:, j, :], xg[:, j, :], s[:, 2:3], gfin[:], ALU.mult, ALU.mult), r=[R_xg[j], rs, R_misc], w=[R_xg[j]])
                    T.dma("sp", out_d[i * 128:(i + 1) * 128, :], xg[:, j, :], R_xg[j], r=[R_xg[j]])
        try:
            emit()
        except _Stop:
            T.dma("sp", out_d[0:128, :], xo[:], R_xo, r=[R_xo])
            T.wait_all("sp", [R_xo] + R_ring)
            T.wait_all("pool", [MSKR[0], MSKR[8], ISC[0], ISC[8]])
        T.wait_all("sp", R_xg)
        with nc.Block() as block:
            T.replay(block)
    return nc


OFF = dict(q_s=0, k_s=512, v_s=640, q_d=768, k_d=1024, v_d=1088, q_i=1152, k_i=1408, w_i=1472, q_m=1476, gates=1732)


def _chunked(w, cols):
    sub = w[:, cols]
    n = sub.shape[1]
    return np.ascontiguousarray(sub.reshape(8, 128, n).transpose(1, 0, 2).reshape(128, 8 * n))


def _prep_weights(w_in, w_proj_swa, w_proj_dsa, w_proj_mem, w_out, w_mlp_in, w_mlp_out):
    wch = np.zeros((NCHUNK, 128, 4096), np.float32)
    r64 = np.arange(64)
    qs_cols = np.concatenate([OFF["q_s"] + h * 64 + r64 for h in (0, 4, 1, 5, 2, 6, 3, 7)])
    qdi_cols = np.concatenate([np.concatenate([OFF["q_d"] + h * 64 + r64, OFF["q_i"] + h * 64 + r64]) for h in range(4)])
    qm_cols = np.concatenate([OFF["q_m"] + np.arange(256), OFF["w_i"] + np.arange(4)])
    wch[0] = _chunked(w_in, qs_cols)
    wch[1] = _chunked(w_in, qdi_cols)
    wch[2, :, 0:8 * 260] = _chunked(w_in, qm_cols)
    for oc in range(8):
        gc = np.concatenate([OFF["gates"] + jb * 1024 + oc * 128 + np.arange(128) for jb in range(3)])
        wch[3 + oc, :, 0:8 * 384] = _chunked(w_in, gc)
        wp = np.concatenate([w_proj_swa.reshape(8, 64, 1024), w_proj_dsa.reshape(4, 64, 1024), w_proj_mem.reshape(4, 64, 1024)], 0)
        wch[11 + oc, 0:64, 0:2048] = wp[:, :, oc * 128:(oc + 1) * 128].transpose(1, 0, 2).reshape(64, 2048)
    for cc in range(2):
        wch[19 + cc] = _chunked(w_out, cc * 512 + np.arange(512))
    for fb in range(8):
        wch[21 + fb] = _chunked(w_mlp_in, fb * 512 + np.arange(512))
    for cc in range(2):
        for ks in range(4):
            blk = w_mlp_out[ks * 1024:(ks + 1) * 1024, cc * 512:(cc + 1) * 512]
            wch[29 + cc * 4 + ks] = blk.reshape(8, 128, 512).transpose(1, 0, 2).reshape(128, 4096)
    k_cols = np.concatenate([OFF["k_s"] + np.arange(128), OFF["k_d"] + r64, OFF["k_i"] + r64, OFF["v_s"] + np.arange(128), OFF["v_d"] + r64])
    wk = _chunked(w_in, k_cols)
    return wch, wk


_NC_CACHE = {}


def kernel(x, mem, positions, g_mix, w_in, b_gate, sinks, g_mem, w_mem_kv, w_proj_swa, w_proj_dsa,
           w_proj_mem, w_out, g_mlp, w_mlp_in, w_mlp_out, g_final):
    f = lambda a: np.asarray(a, dtype=np.float32)
    x = f(x); mem = f(mem); positions = np.asarray(positions, dtype=np.int32)
    wch, wk = _prep_weights(f(w_in)[0], f(w_proj_swa)[0], f(w_proj_dsa)[0], f(w_proj_mem)[0], f(w_out)[0], f(w_mlp_in)[0], f(w_mlp_out)[0])
    wmem = _chunked(f(w_mem_kv)[0], np.arange(512))
    tr = lambda v: np.ascontiguousarray(f(v).reshape(8, 128).T)
    gvec = np.concatenate([tr(g_mix[0]), tr(g_mlp[0]), tr(g_mem[0])], 1)
    gfin = np.ascontiguousarray(np.broadcast_to(f(g_final)[None, :], (128, 1024)))
    bg = f(b_gate)[0].reshape(3, 8, 128)
    bgate = np.ascontiguousarray(bg.transpose(2, 1, 0).reshape(128, 24))
    sinkr = np.ascontiguousarray(np.broadcast_to(f(sinks)[0][None, :, None], (64, 8, 128)).reshape(64, 1024))
    half = 32
    invf = np.power(np.float32(10000.0), -np.arange(half, dtype=np.float32) / np.float32(half)).astype(np.float32)
    invf = np.ascontiguousarray(np.broadcast_to(invf[None, :], (128, 32)))
    s_ = np.arange(128)[:, None]; t_ = np.arange(128)[None, :]
    tri_cur = (t_ >= s_).astype(np.float32)
    tri_prev = (s_ > t_).astype(np.float32)
    dbias = np.where(t_ <= s_, 0.0, NEG).astype(np.float32)
    in_maps = []
    for c in range(8):
        b, par = c // 2, c % 2
        order = np.arange(64) if par == 0 else np.concatenate([np.arange(1, 64), [0]])
        xb = x[b].reshape(64, 128, 1024)[order].reshape(64 * 128, 1024)
        pb = positions[b].reshape(64, 128)[order]
        posc = np.ascontiguousarray(pb.T)
        prev0 = tri_prev * float(par)
        p63 = np.full((128, 128), 0.0 if par == 1 else NEG, np.float32)
        cmask = np.ascontiguousarray(np.stack([tri_cur, tri_prev, prev0, dbias, p63], 1).reshape(128, 5 * 128).astype(np.float32))
        in_maps.append(dict(xc=np.ascontiguousarray(xb), posc=posc, memc=np.ascontiguousarray(mem[b]), cmask=cmask, invf=invf,
                            wk=wk, wch=wch, wmem=wmem, gvec=gvec, gfin=gfin, bgate=bgate, sinkr=sinkr))
    if "nc" not in _NC_CACHE:
        _NC_CACHE["nc"] = build_program()
    nc = _NC_CACHE["nc"]
    res = run_bass_kernel_spmd(nc, in_maps, core_ids=list(range(8)))
    out = np.zeros((4, 64, 128, 1024), np.float32)
    for c in range(8):
        b, par = c // 2, c % 2
        o = np.asarray(res.results[c]["out"]).reshape(32, 128, 1024)
        out[b, par::2] = o
    return out.reshape(4, 8192, 1024)
```

```python
import math
import numpy as np
from contextlib import ExitStack
import concourse.bass as bass
import concourse.mybir as mybir
from concourse.bass_utils import run_bass_kernel_spmd

F32 = mybir.dt.float32; BF16 = mybir.dt.bfloat16; I32 = mybir.dt.int32
ALU = mybir.AluOpType; AF = mybir.ActivationFunctionType; AX = mybir.AxisListType

STAGE = 99
class _Stop(Exception):
    pass
def _ckpt(n):
    if STAGE <= n:
        raise _Stop()
NITER = 14
NEG = -1.0e30
NCHUNK = 37
TOPK = 256.0


class Reg:
    __slots__ = ("name", "w", "r", "dsem", "dcnt")
    def __init__(self, name):
        self.name = name; self.w = None; self.r = {}; self.dsem = None; self.dcnt = 0


class Eng:
    def __init__(self, name):
        self.name = name; self.q = []; self.sem = None; self.cnt = 0; self.seen = {}


class Trk:
    def __init__(self, nc, stack, nsem):
        self.nc = nc
        self.sems = [stack.enter_context(nc.semaphore(f"s{i}")) for i in range(nsem)]
        self.si = 0
        self.E = {n: Eng(n) for n in ("pe", "act", "dve", "pool", "sp")}
        for e in self.E.values():
            e.sem = self.newsem()
    def newsem(self):
        s = self.sems[self.si]; self.si += 1; return s
    def _deps(self, eng, reads, writes):
        deps = {}
        def add(ev, kind):
            if ev is None: return
            sem, val, src = ev
            if src is eng and eng.name == "pe":
                return
            k = id(sem)
            if eng.seen.get(k, 0) >= val: return
            if k not in deps or deps[k][1] < val: deps[k] = (sem, val)
        for r in reads: add(r.w, "raw")
        for w in writes:
            add(w.w, "waw")
            for ev in w.r.values(): add(ev, "war")
        out = list(deps.values())
        for sem, val in out: eng.seen[id(sem)] = val
        return out
    def op(self, en, fn, r=(), w=()):
        eng = self.E[en]
        waits = self._deps(eng, r, w)
        eng.cnt += 1
        ev = (eng.sem, eng.cnt, eng)
        eng.q.append((waits, fn, (eng.sem, 1)))
        for x in r: x.r[en] = ev
        for x in w: x.w = ev; x.r = {}
    def dma(self, en, out_ap, in_ap, slot, r=(), w=()):
        eng = self.E[en]
        waits = self._deps(eng, r, w)
        if slot.dsem is None: slot.dsem = self.newsem()
        slot.dcnt += 16
        ev = (slot.dsem, slot.dcnt, None)
        eng.q.append((waits, lambda h: h.dma_start(out=out_ap, in_=in_ap), (slot.dsem, 16)))
        for x in r: x.r["dma%d" % id(slot)] = ev
        for x in w: x.w = ev; x.r = {}
    def mark(self, label):
        self.E["pe"].q.append(([], None, label))
    def wait_all(self, en, regs):
        eng = self.E[en]
        waits = self._deps(eng, [], regs)
        eng.q.append((waits, None, None))
    def replay(self, block):
        def mk(en):
            q = self.E[en].q
            def body(h):
                for waits, fn, inc in q:
                    for sem, val in waits: h.wait_ge(sem, val)
                    if fn is None:
                        if isinstance(inc, str): MARKS.append((inc, PE_CNT[0]))
                        continue
                    ins = fn(h)
                    ins.then_inc(inc[0], inc[1])
            return body
        block.tensor(mk("pe")); block.scalar(mk("act")); block.vector(mk("dve"))
        block.gpsimd(mk("pool")); block.sync(mk("sp"))


PE_CNT = [0]
MARKS = []
def f_mm(out, lhsT, rhs, start=True, stop=True):
    def f(h):
        PE_CNT[0] += 1
        return h.matmul(out, lhsT=lhsT, rhs=rhs, start=start, stop=stop)
    return f
def f_tr(out, in_, ident):
    def f(h):
        PE_CNT[0] += 1
        return h.transpose(out=out, in_=in_, identity=ident)
    return f
def f_seq(fns):
    def f(h):
        ins = None
        for fn in fns: ins = fn(h)
        return ins
    return f
def f_act(out, in_, func, bias=None, scale=None, accum=None):
    kw = {}
    if bias is not None: kw["bias"] = bias
    if scale is not None: kw["scale"] = scale
    if accum is not None: kw["accum_out"] = accum
    return lambda h: h.activation(out=out, in_=in_, func=func, **kw)
def f_ts(out, in0, s1, s2=None, op0=ALU.mult, op1=None, accum=None):
    kw = {}
    if op1 is not None: kw["op1"] = op1
    if accum is not None: kw["accum_out"] = accum
    return lambda h: h.tensor_scalar(out=out, in0=in0, scalar1=s1, scalar2=s2, op0=op0, **kw)
def f_tt(out, in0, in1, op):
    return lambda h: h.tensor_tensor(out=out, in0=in0, in1=in1, op=op)
def f_stt(out, in0, scalar, in1, op0, op1):
    return lambda h: h.scalar_tensor_tensor(out=out, in0=in0, scalar=scalar, in1=in1, op0=op0, op1=op1)
def f_cp(out, in_):
    return lambda h: h.tensor_copy(out=out, in_=in_)
def f_memset(ap, v):
    return lambda h: h.memset(ap, v)


def build_program():
    nc = bass.Bass("TRN2", target_bir_lowering=False)
    dt_in = lambda n, s, d=F32: nc.dram_tensor(n, s, d, kind="ExternalInput").ap()
    xc = dt_in("xc", [64 * 128, 1024])
    posc = dt_in("posc", [128, 64], I32)
    memc = dt_in("memc", [256, 1024])
    cmask = dt_in("cmask", [128, 5 * 128])
    invf_d = dt_in("invf", [128, 32])
    wk_d = dt_in("wk", [128, 8 * 448])
    wch_d = dt_in("wch", [NCHUNK, 128, 4096])
    wmem_d = dt_in("wmem", [128, 4096])
    gvec_d = dt_in("gvec", [128, 24])
    gfin_d = dt_in("gfin", [128, 1024])
    bgate_d = dt_in("bgate", [128, 24])
    sinkr_d = dt_in("sinkr", [64, 8])
    out_d = nc.dram_tensor("out", [32 * 128, 1024], F32, kind="ExternalOutput").ap()
    wbf_d = nc.dram_tensor("wbf", [NCHUNK, 128, 4096], BF16, kind="Internal").ap()

    with ExitStack() as st:
        T = Trk(nc, st, 48)
        def sb(name, shape, dt):
            return st.enter_context(nc.sbuf_tensor(name, shape, dt))
        BIG = sb("BIG", [128, 8192], F32)
        MSK = sb("MSK", [128, 8192], BF16)
        ISC = [Reg(f"isc{c}") for c in range(16)]
        MSKR = [Reg(f"msk{c}") for c in range(16)]
        hidT = BIG[:].bitcast(BF16).rearrange("p (f t) -> p f t", t=512)
        wk_sb = sb("wk_sb", [128, 8, 448], BF16); R_wk = Reg("wk")
        kDI = sb("kDI", [128, 8192], BF16); R_kDI = [Reg(f"kDI{i}") for i in range(64)]
        vD = sb("vD", [128, 64, 128], BF16); R_vD = [Reg(f"vD{i}") for i in range(64)]
        kST = sb("kST", [128, 16, 128], BF16); R_kST = [Reg(f"kST{i}") for i in range(16)]
        vS = sb("vS", [128, 16, 128], BF16); R_vS = [Reg(f"vS{i}") for i in range(16)]
        mkT = sb("mkT", [128, 2, 256], BF16); R_mkT = Reg("mkT")
        mv = sb("mv", [128, 2, 256], BF16); R_mv = Reg("mv")
        xg = sb("xg", [128, 4, 1024], F32); R_xg = [Reg(f"xg{j}") for j in range(4)]
        hTg = sb("hTg", [128, 8, 512], BF16); R_hTg = [Reg(f"hTg{j}") for j in range(4)]
        hTo = sb("hTo", [128, 8, 128], BF16); R_hTo = Reg("hTo")
        oTg = sb("oTg", [64, 16, 512], BF16); R_oTg = [Reg(f"oTg{j}") for j in range(4)]
        QM = sb("QM", [128, 4096], BF16)
        qsT = QM[:, 0:2048].rearrange("p (s c) -> p s c", s=4)
        qdiT = QM[:, 2048:4096].rearrange("p (s c) -> p s c", s=4)
        mergedT = QM[:].rearrange("p (k t) -> p k t", k=8)
        R_qs = [Reg(f"qs{j}") for j in range(4)]; R_qdi = [Reg(f"qdi{j}") for j in range(4)]
        qmT = sb("qmT", [128, 4, 256], BF16); R_qm = [Reg(f"qm{j}") for j in range(4)]
        wiT = sb("wiT", [128, 4, 4], F32); R_wi = [Reg(f"wi{j}") for j in range(4)]
        NRING = 2
        ring = [sb(f"ring{i}", [128, 4096], BF16) for i in range(NRING)]
        R_ring = [Reg(f"ring{i}") for i in range(NRING)]
        ident = sb("ident", [128, 128], BF16); identf = sb("identf", [128, 128], F32); onesf = sb("onesf", [128, 128], F32)
        ones_b = sb("ones_b", [128, 64], BF16)
        qd0 = sb("qd0", [128, 512], BF16); R_qd0 = Reg("qd0")
        R_const = Reg("const")
        cm_f = sb("cm_f", [128, 5, 128], F32)
        cm_b = sb("cm_b", [128, 3, 128], BF16)
        invf = sb("invf_sb", [128, 32], F32)
        gvec = sb("gvec_sb", [128, 24], F32)
        gfin = sb("gfin_sb", [128, 1024], F32)
        bgate = sb("bgate_sb", [128, 24], F32)
        es = sb("es_sb", [64, 8], F32)
        pw2 = sb("pw2", [128, NITER], F32)
        posi = sb("posi", [128, 64], I32); posf = sb("posf", [128, 64], F32)
        cosT = sb("cosT", [128, 8, 32], F32); sinT = sb("sinT", [128, 8, 32], F32); R_cs = Reg("cs")
        rtmp = [sb(f"rtmp{i}", [128, 256], F32) for i in range(3)]; rtmpi = sb("rtmpi", [128, 256], I32); R_rt = Reg("rt")
        stat = sb("stat", [128, 16], F32); R_stat = [Reg(f"stat{i}") for i in range(4)]
        hn = [sb(f"hn{i}", [128, 1024], BF16) for i in range(1)]; R_hn = [Reg(f"hn{i}") for i in range(1)]
        tok_b = [sb(f"tok_b{i}", [128, 512], BF16) for i in range(2)]; R_tok = [Reg(f"tok{i}") for i in range(2)]
        Et = [sb(f"Et{i}", [128, 512], BF16) for i in range(5)]; R_E = [Reg(f"E{i}") for i in range(5)]
        Pt = [sb(f"Pt{i}", [128, 512], BF16) for i in range(2)]; R_P = [Reg(f"P{i}") for i in range(2)]
        RLX = sb("RLX", [128, 1024], F32); rl = [RLX[:, 0:512], RLX[:, 512:1024]]; R_rl = [Reg(f"rl{i}") for i in range(2)]
        xo = RLX; R_xo = R_rl[0]
        mT = [sb(f"mT{i}", [128, 512], BF16) for i in range(2)]; R_mT = [Reg(f"mT{i}") for i in range(2)]
        dn = [sb(f"dn{i}", [128, 512], F32) for i in range(1)]; R_dn = [Reg(f"dn{i}") for i in range(1)]
        mg = rl; R_mg = R_rl
        rope_t = rl[0]; rope_u = rl[1]
        gsb = [sb(f"gsb{i}", [128, 512], BF16) for i in range(3)]; R_gsb = [Reg(f"gsb{i}") for i in range(3)]
        bis = sb("bis", [128, 8 + NITER], F32); R_lo = Reg("lo"); R_mid = Reg("mid"); R_cd = Reg("cd"); R_ca = Reg("ca"); R_t = Reg("t")
        PB = [st.enter_context(nc.psum_tensor(f"pb{i}", [128, 512], F32)) for i in range(8)]
        R_PB = [Reg(f"pb{i}") for i in range(8)]
        def pbf(i):
            return PB[i][:].bitcast(BF16)

        cnt = {"ring": 0, "stat": 0, "hn": 0, "tok": 0, "E": 0, "P": 0, "rl": 0, "mT": 0, "dn": 0, "mg": 0}
        def rot(name, n):
            i = cnt[name] % n; cnt[name] += 1; return i

        R_cmf = Reg("cmf"); R_misc = Reg("misc")
        T.dma("sp", cm_f[:].rearrange("p a b -> p (a b)"), cmask[:, :], R_cmf, w=[R_cmf])
        for (dst, src) in ((invf, invf_d), (gvec, gvec_d), (gfin, gfin_d), (bgate, bgate_d)):
            T.dma("sp", dst[:], src[:, :], R_misc, w=[R_misc])
            T.wait_all("sp", [R_misc])
        R_es = Reg("es")
        T.dma("sp", es[:], sinkr_d[:, :], R_es, w=[R_es])
        R_pos = Reg("pos")
        T.dma("sp", posi[:], posc[:, :], R_pos, w=[R_pos])
        T.op("pool", f_memset(onesf[:], 1.0), w=[R_const])
        T.op("pool", f_memset(identf[:], 0.0), w=[R_const])
        T.op("pool", lambda h: h.affine_select(out=identf[:], in_=onesf[:], pattern=[[-1, 128]], compare_op=ALU.is_equal,
                                                fill=0.0, base=0, channel_multiplier=1), r=[R_const], w=[R_const])
        T.op("dve", f_cp(ident[:], identf[:]), r=[R_const], w=[R_const])
        T.op("dve", f_memset(ones_b[:], 1.0), w=[R_const])
        T.op("pool", f_memset(vD[:].rearrange("p a b -> p (a b)"), 1.0), w=R_vD)
        T.op("dve", f_memset(dn[0][:], 0.0), w=[R_dn[0]])
        T.op("dve", f_memset(qd0[:], 0.0), w=[R_qd0])
        for k in range(NITER):
            T.op("dve", f_memset(pw2[:, k:k + 1], 2.0 ** (-(k + 1))), w=[R_const])
        T.op("dve", f_cp(cm_b[:], cm_f[:, 0:3, :]), r=[R_cmf], w=[R_const])
        T.op("act", f_act(es[:], es[:], AF.Exp), r=[R_es], w=[R_es])
        T.op("dve", f_cp(posf[:], posi[:]), r=[R_pos], w=[R_pos])
        tri_cur = cm_b[:, 0, :]; tri_prev = cm_b[:, 1, :]; prev0 = cm_b[:, 2, :]
        dbias = cm_f[:, 3, :]; p63bias = cm_f[:, 4, :]

        def emit():
            def norm_T(x_ap, x_regs, dstT, dst_regs, pbank):
                si = rot("stat", 4); s = stat[:, si * 4:(si + 1) * 4]; rs = R_stat[si]
                hi = rot("hn", 1); h_ = hn[hi]; rh = R_hn[hi]
                T.op("act", f_act(h_[:], x_ap, AF.Square, accum=s[:, 0:1]), r=x_regs, w=[rh, rs])
                T.op("act", f_act(s[:, 1:2], s[:, 0:1], AF.Ln, scale=1.0 / 1024.0, bias=1e-6), r=[rs], w=[rs])
                T.op("act", f_act(s[:, 2:3], s[:, 1:2], AF.Exp, scale=-0.5), r=[rs], w=[rs])
                T.op("dve", f_ts(h_[:], x_ap, s[:, 2:3], None, ALU.mult), r=x_regs + [rs], w=[rh])
                pv = pbf(pbank)
                T.op("pe", f_seq([f_tr(pv[:, kc * 128:(kc + 1) * 128], h_[:, kc * 128:(kc + 1) * 128], ident[:]) for kc in range(8)]),
                     r=[rh, R_const], w=[R_PB[pbank]])
                T.op("act", f_act(dstT, pv[:, :].rearrange("p (k t) -> p k t", k=8), AF.Copy), r=[R_PB[pbank]], w=dst_regs)
                return s, rs

            def rope_tables(blk0, nb):
                n = nb * 32
                a0 = rtmp[0][:, 0:n]; a1 = rtmp[1][:, 0:n]; a2 = rtmp[2][:, 0:n]; ai = rtmpi[:, 0:n]
                v3 = lambda a: a.rearrange("p (b f) -> p b f", f=32)
                T.op("dve", f_tt(v3(a0), posf[:, blk0:blk0 + nb].unsqueeze(2).to_broadcast([128, nb, 32]),
                                 invf[:].unsqueeze(1).to_broadcast([128, nb, 32]), ALU.mult), r=[R_pos, R_misc], w=[R_rt])
                T.op("dve", f_ts(ai, a0, 1.0 / (2 * math.pi), None, ALU.mult), r=[R_rt], w=[R_rt])
                T.op("dve", f_cp(a1, ai), r=[R_rt], w=[R_rt])
                T.op("dve", f_stt(a2, a1, -6.28125, a0, ALU.mult, ALU.add), r=[R_rt], w=[R_rt])
                T.op("dve", f_stt(a2, a1, -0.0019353072, a2, ALU.mult, ALU.add), r=[R_rt], w=[R_rt])
                T.op("dve", f_ts(a0, a2, -3.1415925, 3.1415925, ALU.max, ALU.min), r=[R_rt], w=[R_rt])
                T.op("act", f_act(sinT[:, 0:nb, :].rearrange("p b f -> p (b f)"), a0, AF.Sin), r=[R_rt], w=[R_cs])
                T.op("dve", f_ts(a1, a2, math.pi / 2, None, ALU.add), r=[R_rt], w=[R_rt])
                T.op("dve", f_ts(a0, a1, math.pi, -2 * math.pi, ALU.is_gt, ALU.mult), r=[R_rt, R_cs], w=[R_rt])
                T.op("dve", f_tt(a0, a0, a1, ALU.add), r=[R_rt], w=[R_rt])
                T.op("dve", f_ts(a0, a0, -3.1415925, 3.1415925, ALU.max, ALU.min), r=[R_rt], w=[R_rt])
                T.op("act", f_act(cosT[:, 0:nb, :].rearrange("p b f -> p (b f)"), a0, AF.Sin), r=[R_rt], w=[R_cs])

            def rope_apply(src, src_regs, dst, dst_regs, H, cb):
                n = H * 64
                t_ = rope_t[:, 0:n]; u_ = rope_u[:, 0:n]
                cosb = cosT[:, cb:cb + 1, :]; sinb = sinT[:, cb:cb + 1, :]
                T.op("dve", f_tt(t_.rearrange("p (a f) -> p a f", f=32), src.rearrange("p (a f) -> p a f", f=32),
                                 cosb.to_broadcast([128, 2 * H, 32]), ALU.mult), r=src_regs + [R_cs], w=[R_rl[0]])
                s4 = src.rearrange("p (h e f) -> p h e f", e=2, f=32)
                u4 = u_.rearrange("p (h e f) -> p h e f", e=2, f=32)
                t4 = t_.rearrange("p (h e f) -> p h e f", e=2, f=32)
                d4 = dst.rearrange("p (h e f) -> p h e f", e=2, f=32)
                sb_ = sinb.to_broadcast([128, H, 32])
                T.op("dve", f_tt(u4[:, :, 0, :], s4[:, :, 1, :], sb_, ALU.mult), r=src_regs + [R_cs], w=[R_rl[1]])
                T.op("dve", f_tt(u4[:, :, 1, :], s4[:, :, 0, :], sb_, ALU.mult), r=src_regs + [R_cs], w=[R_rl[1]])
                T.op("dve", f_tt(d4[:, :, 0, :], t4[:, :, 0, :], u4[:, :, 0, :], ALU.subtract), r=R_rl, w=dst_regs)
                T.op("dve", f_tt(d4[:, :, 1, :], t4[:, :, 1, :], u4[:, :, 1, :], ALU.add), r=R_rl, w=dst_regs)

            R_wbf = [Reg(f"wbf{i}") for i in range(NCHUNK)]
            CH_N = [4096, 4096, 8 * 260] + [8 * 384] * 8 + [2048] * 8 + [4096] * 18
            CH_P = [128] * 11 + [64] * 8 + [128] * 18
            sched = [0, 1, 2] + [v for oc in range(8) for v in (3 + oc, 11 + oc)] + list(range(19, 37))
            ring_state = {"issued": 0, "got": 0}
            total_gets = 8 * NCHUNK
            def ring_issue():
                k = ring_state["issued"]
                if k >= total_gets: return
                c = sched[k % NCHUNK]; b = k % NRING
                T.dma("sp", ring[b][0:CH_P[c], 0:CH_N[c]], wbf_d[c, 0:CH_P[c], 0:CH_N[c]], R_ring[b], r=[R_wbf[c]], w=[R_ring[b]])
                ring_state["issued"] += 1
            def ring_get(expect):
                k = ring_state["got"]
                assert sched[k % NCHUNK] == expect, (k, expect)
                while ring_state["issued"] < min(k + NRING, total_gets):
                    ring_issue()
                ring_state["got"] += 1
                b = k % NRING
                return ring[b], R_ring[b]

            def convert(src_dram_ap, npart, nelem, ncols, goff, dst_ap, dst_regs, half):
                stg = BIG[0:npart, half * 4096: half * 4096 + nelem]
                sregs = ISC[half * 8:(half + 1) * 8]
                T.dma("pool", stg, src_dram_ap, sregs[0], w=sregs)
                if goff is None:
                    T.op("act", f_act(dst_ap, stg, AF.Copy), r=sregs, w=dst_regs)
                else:
                    for kc in range(8):
                        T.op("dve", f_ts(dst_ap[:, kc * ncols:(kc + 1) * ncols], stg[:, kc * ncols:(kc + 1) * ncols],
                                          gvec[:, goff + kc:goff + kc + 1], None, ALU.mult), r=sregs + [R_misc], w=dst_regs)
            _ckpt(1)
            convert(wk_d[:, :], 128, 8 * 448, 448, 0, wk_sb[:].rearrange("p k c -> p (k c)"), [R_wk], 0)
            wmem_b = MSK[:, 4096:8192]
            convert(wmem_d[:, :], 128, 4096, 512, 16, wmem_b, MSKR[8:16], 1)

            for mc in range(2):
                T.dma("sp", xo[:], memc[mc * 128:(mc + 1) * 128, :], R_rl[0], w=R_rl)
                norm_T(xo[:], R_rl, hTo[:], [R_hTo], 0)
                T.op("pe", f_seq([f_mm(PB[1][:, :], hTo[:, kc, :], wmem_b[:, kc * 512:(kc + 1) * 512], kc == 0, kc == 7) for kc in range(8)]),
                     r=[R_hTo] + MSKR[8:16], w=[R_PB[1]])
                ti = rot("tok", 2)
                T.op("act", f_act(tok_b[ti][:, 0:256], PB[1][:, 0:256], AF.Copy), r=[R_PB[1]], w=[R_tok[ti]])
                T.op("act", f_act(mv[:, mc, :], PB[1][:, 256:512], AF.Copy), r=[R_PB[1]], w=[R_mv])
                pv = pbf(2)
                T.op("pe", f_seq([f_tr(pv[:, jj * 128:(jj + 1) * 128], tok_b[ti][:, jj * 128:(jj + 1) * 128], ident[:]) for jj in range(2)]),
                     r=[R_tok[ti], R_const], w=[R_PB[2]])
                T.op("act", f_act(mkT[:, :, mc * 128:(mc + 1) * 128], pv[:, 0:256].rearrange("p (j m) -> p j m", j=2), AF.Copy),
                     r=[R_PB[2]], w=[R_mkT])

            _ckpt(2)
            for c in range(NCHUNK):
                half = c % 2
                goff = 0 if c < 11 else (8 if 21 <= c < 29 else None)
                ncols = {0: 512, 1: 512, 2: 260}.get(c, 384 if c < 11 else 512)
                npart = CH_P[c]; nelem = CH_N[c]
                dstb = MSK[0:npart, half * 4096: half * 4096 + nelem]
                dregs = MSKR[half * 8:(half + 1) * 8]
                convert(wch_d[c, 0:npart, 0:nelem], npart, nelem, ncols, goff, dstb, dregs, half)
                T.dma("pool", wbf_d[c, 0:npart, 0:nelem], dstb, dregs[0], r=dregs, w=[R_wbf[c]])

            def kside(pos, x_ap, x_regs, hT_ap, hT_regs, cb):
                kidx = 0 if pos == 63 else pos + 1
                rg = (pos + 1) % 16
                norm_T(x_ap, x_regs, hT_ap, hT_regs, 0)
                _ckpt(3.1)
                T.op("pe", f_seq([f_mm(PB[1][:, 0:448], hT_ap[:, kc, :], wk_sb[:, kc, :], kc == 0, kc == 7) for kc in range(8)]),
                     r=hT_regs + [R_wk], w=[R_PB[1]])
                _ckpt(3.2)
                ti = rot("tok", 2)
                rope_apply(PB[1][:, 0:256], [R_PB[1]], tok_b[ti][:, 0:256], [R_tok[ti]], 4, cb)
                _ckpt(3.3)
                T.op("dve", f_cp(vS[:, rg, :], PB[1][:, 256:384]), r=[R_PB[1]], w=[R_vS[rg]])
                T.op("dve", f_cp(vD[:, kidx, 0:64], PB[1][:, 384:448]), r=[R_PB[1]], w=[R_vD[kidx]])
                _ckpt(3.4)
                pv = pbf(2)
                T.op("pe", f_seq([f_tr(pv[:, jj * 128:(jj + 1) * 128], tok_b[ti][:, jj * 128:(jj + 1) * 128], ident[:]) for jj in range(2)]),
                     r=[R_tok[ti], R_const], w=[R_PB[2]])
                _ckpt(3.5)
                T.op("act", f_act(kST[:, rg, :], pv[:, 0:128], AF.Copy), r=[R_PB[2]], w=[R_kST[rg]])
                T.op("act", f_act(kDI[:, kidx * 128:(kidx + 1) * 128], pv[:, 128:256], AF.Copy), r=[R_PB[2]], w=[R_kDI[kidx]])

            _ckpt(3)
            rope_tables(63, 1)
            T.dma("sp", xo[:], xc[63 * 128:64 * 128, :], R_rl[0], w=R_rl)
            kside(63, xo[:], R_rl, hTo[:], [R_hTo], 0)

            def finish_attn(bO, bD, j, h0, es_ap):
                di = rot("dn", 1)
                d_ = dn[di][0:64, :]
                if es_ap is not None:
                    T.op("dve", f_tt(d_.rearrange("p (h t) -> p h t", h=4), PB[bD][0:64, :].rearrange("p (h t) -> p h t", h=4), es_ap, ALU.add),
                         r=[R_PB[bD], R_es], w=[R_dn[di]])
                    T.op("act", f_act(d_, d_, AF.Ln), r=[R_dn[di]], w=[R_dn[di]])
                else:
                    T.op("act", f_act(d_, PB[bD][0:64, :], AF.Ln), r=[R_PB[bD]], w=[R_dn[di]])
                T.op("act", f_act(d_, d_, AF.Exp, scale=-1.0), r=[R_dn[di]], w=[R_dn[di]])
                T.op("dve", f_tt(oTg[:, h0:h0 + 4, j * 128:(j + 1) * 128], PB[bO][0:64, :].rearrange("p (h t) -> p h t", h=4),
                                 d_.rearrange("p (h t) -> p h t", h=4), ALU.mult), r=[R_PB[bO], R_dn[di]], w=[R_oTg[j]])

            for g in range(8):
                T.mark(f"g{g} kside")
                npos = 8 if g < 7 else 7
                rope_tables(8 * g, 8)
                for pl in range(npos):
                    pos = 8 * g + pl
                    if pl % 2 == 0:
                        j = pl // 2
                        T.dma("sp", xg[:, j, :], xc[pos * 128:(pos + 1) * 128, :], R_xg[j], w=[R_xg[j]])
                        kside(pos, xg[:, j, :], [R_xg[j]], hTg[:, :, j * 128:(j + 1) * 128], [R_hTg[j]], pl)
                    else:
                        T.dma("sp", xo[:], xc[pos * 128:(pos + 1) * 128, :], R_rl[0], w=R_rl)
                        kside(pos, xo[:], R_rl, hTo[:], [R_hTo], pl)

                _ckpt(10 * (g + 1) + 1)
                T.mark(f"g{g} qproj")
                for c in range(3):
                    W, RW = ring_get(c)
                    ncols = (512, 512, 260)[c]
                    Wv = W[:, 0:8 * ncols].rearrange("p (k c) -> p k c", k=8)
                    for j in range(4):
                        pb = 3 + (j % 2)
                        T.op("pe", f_seq([f_mm(PB[pb][:, 0:ncols], hTg[:, kc, j * 128:(j + 1) * 128], Wv[:, kc, :], kc == 0, kc == 7) for kc in range(8)]),
                             r=[R_hTg[j], RW], w=[R_PB[pb]])
                        ti = rot("tok", 2)
                        if c < 2:
                            rope_apply(PB[pb][:, 0:512], [R_PB[pb]], tok_b[ti][:, 0:512], [R_tok[ti]], 8, 2 * j)
                            pv = pbf(5 + (j % 2))
                            T.op("pe", f_seq([f_tr(pv[:, q * 128:(q + 1) * 128], tok_b[ti][:, q * 128:(q + 1) * 128], ident[:]) for q in range(4)]),
                                 r=[R_tok[ti], R_const], w=[R_PB[5 + (j % 2)]])
                            dst = qsT if c == 0 else qdiT
                            T.op("act", f_act(dst[:, j, :], pv[:, 0:512], AF.Copy), r=[R_PB[5 + (j % 2)]], w=[(R_qs if c == 0 else R_qdi)[j]])
                        else:
                            T.op("act", f_act(tok_b[ti][:, 0:256], PB[pb][:, 0:256], AF.Copy), r=[R_PB[pb]], w=[R_tok[ti]])
                            T.op("act", f_act(wiT[:, j, :], PB[pb][:, 256:260], AF.Copy, scale=1.0 / 16.0), r=[R_PB[pb]], w=[R_wi[j]])
                            pv = pbf(5 + (j % 2))
                            T.op("pe", f_seq([f_tr(pv[:, q * 128:(q + 1) * 128], tok_b[ti][:, q * 128:(q + 1) * 128], ident[:]) for q in range(2)]),
                                 r=[R_tok[ti], R_const], w=[R_PB[5 + (j % 2)]])
                            T.op("act", f_act(qmT[:, j, :], pv[:, 0:256], AF.Copy), r=[R_PB[5 + (j % 2)]], w=[R_qm[j]])

                _ckpt(10 * (g + 1) + 2)
                T.mark(f"g{g} attn")
                def slot_vars(j):
                    i = 4 * g + j
                    nkb = 2 * i + 2
                    return i, slice(j * 128, (j + 1) * 128), nkb, (nkb + 3) // 4, nkb * 128
                def swa_mem(j):
                    i, tok, nkb, nch, n = slot_vars(j)
                    rp = (2 * i) % 16; rc = (2 * i + 1) % 16
                    for kv in range(2):
                        pr = slice(kv * 64, (kv + 1) * 64)
                        bS = (2, 3) if kv == 0 else (4, 5)
                        bO, bD = (0, 1) if kv == 0 else (6, 7)
                        Ps = []
                        for which, rr_ in enumerate((rp, rc)):
                            b = bS[which]
                            T.op("pe", f_mm(PB[b][:, :], kST[pr, rr_, :], qsT[pr, j, :]), r=[R_kST[rr_], R_qs[j]], w=[R_PB[b]])
                            ei = rot("E", 5)
                            T.op("act", f_act(Et[ei][:], PB[b][:, :], AF.Exp, scale=0.125), r=[R_PB[b]], w=[R_E[ei]])
                            pi_ = rot("P", 2)
                            msk = (prev0 if i == 0 else tri_prev) if which == 0 else tri_cur
                            T.op("dve", f_tt(Pt[pi_][:].rearrange("p (h t) -> p h t", h=4), Et[ei][:].rearrange("p (h t) -> p h t", h=4),
                                             msk.unsqueeze(1).to_broadcast([128, 4, 128]), ALU.mult), r=[R_E[ei], R_const], w=[R_P[pi_]])
                            Ps.append((pi_, rr_))
                        T.op("pe", f_seq([f_mm(PB[bO][0:64, :], vS[:, rr_, pr], Pt[pi_][:], w_ == 0, w_ == 1) for w_, (pi_, rr_) in enumerate(Ps)]
                                         + [f_mm(PB[bD][0:64, :], ones_b[:], Pt[pi_][:], w_ == 0, w_ == 1) for w_, (pi_, rr_) in enumerate(Ps)]),
                             r=[R_P[p_] for p_, _ in Ps] + [R_vS[r_] for _, r_ in Ps] + [R_const], w=[R_PB[bO], R_PB[bD]])
                        finish_attn(bO, bD, j, 4 * kv, es[:, 4 * kv:4 * kv + 4].unsqueeze(2).to_broadcast([64, 4, 128]))
                    _ckpt(12.1)
                    Es = [rot("E", 5), rot("E", 5)]
                    fns = []
                    for e in range(2):
                        pr = slice(e * 64, (e + 1) * 64)
                        for mc in range(2):
                            for jj in range(2):
                                fns.append(f_mm(PB[2 + 2 * mc + e][:, jj * 128:(jj + 1) * 128], mkT[pr, jj, mc * 128:(mc + 1) * 128],
                                                qmT[pr, j, jj * 128:(jj + 1) * 128]))
                    T.op("pe", f_seq(fns), r=[R_mkT, R_qm[j]], w=[R_PB[2], R_PB[3], R_PB[4], R_PB[5]])
                    for mc in range(2):
                        E4 = Et[Es[mc]][:].rearrange("p (jj e t) -> p jj e t", jj=2, e=2)
                        for e in range(2):
                            b = 2 + 2 * mc + e
                            T.op("act", f_act(E4[:, :, e, :], PB[b][:, 0:256].rearrange("p (jj t) -> p jj t", jj=2), AF.Exp, scale=0.125),
                                 r=[R_PB[b]], w=[R_E[Es[mc]]])
                    _ckpt(12.15)
                    fns = []
                    for hh in range(4):
                        for mc in range(2):
                            fns.append(f_mm(PB[0][0:64, hh * 128:(hh + 1) * 128], mv[:, mc, hh * 64:(hh + 1) * 64], Et[Es[mc]][:, hh * 128:(hh + 1) * 128], mc == 0, mc == 1))
                    for mc in range(2):
                        fns.append(f_mm(PB[1][0:64, :], ones_b[:], Et[Es[mc]][:], mc == 0, mc == 1))
                    T.op("pe", f_seq(fns), r=[R_E[e_] for e_ in Es] + [R_mv, R_const], w=[R_PB[0], R_PB[1]])
                    _ckpt(12.17)
                    finish_attn(0, 1, j, 12, None)
                    _ckpt(12.2)
                def dsa_index(j):
                    i, tok, nkb, nch, n = slot_vars(j)
                    for c in range(nch):
                        kb0 = 4 * c; nb = min(4, nkb - kb0); w_ = nb * 128
                        cols = slice(kb0 * 128, kb0 * 128 + w_)
                        bb = 4 * (c % 2)
                        T.op("pe", f_seq([f_mm(PB[bb + hh][:, 0:w_], qdiT[64:128, j, hh * 128:(hh + 1) * 128], kDI[64:128, cols]) for hh in range(4)]),
                             r=[R_qdi[j]] + R_kDI[kb0:kb0 + nb], w=[R_PB[bb + hh] for hh in range(4)])
                        for hh in range(4):
                            ri = rot("rl", 2)
                            T.op("act", f_act(rl[ri][:, 0:w_], PB[bb + hh][:, 0:w_], AF.Relu), r=[R_PB[bb + hh]], w=[R_rl[ri]])
                            if hh == 0:
                                T.op("dve", f_ts(BIG[:, cols], rl[ri][:, 0:w_], wiT[:, j, 0:1], None, ALU.mult), r=[R_rl[ri], R_wi[j]], w=[ISC[c]])
                            else:
                                T.op("dve", f_stt(BIG[:, cols], rl[ri][:, 0:w_], wiT[:, j, hh:hh + 1], BIG[:, cols], ALU.mult, ALU.add),
                                     r=[R_rl[ri], R_wi[j], ISC[c]], w=[ISC[c]])
                def dsa_bisect(j):
                    i, tok, nkb, nch, n = slot_vars(j)
                    _ckpt(12.3)
                    IR = ISC[0:nch]; MR = MSKR[0:nch]
                    cd = nch if nch < 2 else (nch + 1) // 2
                    nd = min(n, cd * 512); na = n - nd
                    IRd, MRd, IRa, MRa = ISC[0:cd], MSKR[0:cd], ISC[cd:nch], MSKR[cd:nch]
                    amax = bis[:, 0:1]; wtot = bis[:, 1:2]; lo = bis[:, 2:3]; mid = bis[:, 3:4]; cn = bis[:, 4:5]; dl = bis[:, 5:6]
                    sa = bis[:, 6:7]; tt_ = bis[:, 7:8]
                    wks = bis[:, 8:8 + NITER]
                    T.op("dve", lambda h, a=amax, b=BIG[:, 0:n]: h.tensor_reduce(out=a, in_=b, axis=AX.X, op=ALU.max, apply_absolute_value=True),
                         r=IR, w=[R_lo])
                    T.op("pool", f_tt(BIG[:, 0:128], BIG[:, 0:128], p63bias, ALU.add), r=[ISC[0], R_cmf, R_lo], w=[ISC[0]])
                    dcol = slice((nkb - 1) * 128, nkb * 128)
                    T.op("pool", f_tt(BIG[:, dcol], BIG[:, dcol], dbias, ALU.add), r=[ISC[nch - 1], R_cmf, R_lo], w=[ISC[nch - 1]])
                    T.op("dve", f_ts(wtot, amax, 2.0002, 2e-6, ALU.mult, ALU.add), r=[R_lo], w=[R_lo])
                    T.op("dve", f_ts(lo, amax, -1.0001, -1e-6, ALU.mult, ALU.add), r=[R_lo], w=[R_lo])
                    T.op("dve", f_ts(wks, pw2[:], wtot, None, ALU.mult), r=[R_lo, R_const], w=[R_lo])
                    thrc = TOPK - na / 2.0
                    for k in range(NITER):
                        T.op("dve", f_tt(mid, lo, wks[:, k:k + 1], ALU.add), r=[R_lo], w=[R_mid])
                        if na > 0:
                            T.op("act", f_act(MSK[:, nd:n], BIG[:, nd:n], AF.Sign, bias=mid, scale=-1.0, accum=sa), r=IRa + [R_mid], w=MRa + [R_ca])
                        T.op("dve", f_ts(MSK[:, 0:nd], BIG[:, 0:nd], mid, 0.0, ALU.is_ge, ALU.add, accum=cn), r=IRd + [R_mid], w=MRd + [R_cd])
                        if na > 0:
                            T.op("dve", f_stt(tt_, sa, -0.5, cn, ALU.mult, ALU.add), r=[R_ca, R_cd], w=[R_t])
                            T.op("dve", f_stt(dl, tt_, thrc, wks[:, k:k + 1], ALU.is_ge, ALU.mult), r=[R_t, R_lo], w=[R_t])
                        else:
                            T.op("dve", f_stt(dl, cn, TOPK, wks[:, k:k + 1], ALU.is_ge, ALU.mult), r=[R_cd, R_lo], w=[R_t])
                        T.op("dve", f_tt(lo, lo, dl, ALU.add), r=[R_t], w=[R_lo])
                    T.op("dve", f_ts(MSK[:, 0:n], BIG[:, 0:n], lo, None, ALU.is_ge), r=IR + [R_lo], w=MR)
                def dsa_attn(j):
                    i, tok, nkb, nch, n = slot_vars(j)
                    _ckpt(12.4)
                    LA = 3
                    st_ = {}
                    T.op("dve", f_cp(qd0[0:64, :], qdiT[0:64, j, :]), r=[R_qdi[j]], w=[R_qd0])
                    def front(kb):
                        c, q = kb // 4, kb % 4
                        if q == 0:
                            nb = min(4, nkb - kb)
                            mi = rot("mT", 2)
                            pv = pbf(2)
                            T.op("pe", f_seq([f_tr(pv[:, q2 * 128:(q2 + 1) * 128], MSK[:, (kb + q2) * 128:(kb + q2 + 1) * 128], ident[:]) for q2 in range(nb)]),
                                 r=[MSKR[c], R_const], w=[R_PB[2]])
                            T.op("act", f_act(mT[mi][:, 0:nb * 128], pv[:, 0:nb * 128], AF.Identity, scale=30000.0, bias=-30000.0), r=[R_PB[2]], w=[R_mT[mi]])
                            st_["mi"] = mi
                        mi = st_["mi"]
                        b = 3 + (kb % 5)
                        T.op("pe", f_seq([f_mm(PB[b][:, :], kDI[:, kb * 128:(kb + 1) * 128], qd0[:, :], True, False)]
                                         + [f_mm(PB[b][:, hh * 128:(hh + 1) * 128], ident[:], mT[mi][:, q * 128:(q + 1) * 128], False, hh == 3) for hh in range(4)]),
                             r=[R_kDI[kb], R_qd0, R_mT[mi], R_const], w=[R_PB[b]])
                        ei = rot("E", 5)
                        T.op("act", f_act(Et[ei][:], PB[b][:, :], AF.Exp, scale=0.125), r=[R_PB[b]], w=[R_E[ei]])
                        st_[kb] = ei
                    def back(kb):
                        ei = st_.pop(kb)
                        T.op("pe", f_mm(PB[0][:, :], vD[:, kb, :], Et[ei][:], kb == 0, kb == nkb - 1),
                             r=[R_E[ei], R_vD[kb]], w=[R_PB[0]])
                    for t in range(nkb + LA):
                        if t < nkb: front(t)
                        if t - LA >= 0: back(t - LA)
                    T.op("act", f_act(dn[0][64:128, :], PB[0][64:128, :], AF.Copy), r=[R_PB[0]], w=[R_dn[0]])
                    T.op("pe", f_mm(PB[1][0:64, :], identf[:, 64:128], dn[0][:, :]), r=[R_dn[0], R_const], w=[R_PB[1]])
                    finish_attn(0, 1, j, 8, None)

                swa_mem(0)
                for j in range(4):
                    T.mark(f"g{g} s{j} idx")
                    dsa_index(j)
                    T.mark(f"g{g} s{j} bis")
                    dsa_bisect(j)
                    if j < 3:
                        swa_mem(j + 1)
                    T.mark(f"g{g} s{j} dattn")
                    dsa_attn(j)

                _ckpt(10 * (g + 1) + 3)
                T.mark(f"g{g} pass2")
                RQ = R_qs + R_qdi
                for oc in range(8):
                    G, RG = ring_get(3 + oc)
                    Gv = G[:, 0:8 * 384].rearrange("p (k c) -> p k c", k=8)
                    for jb in range(3):
                        T.op("pe", f_seq([f_mm(PB[jb][:, :], Gv[:, kc, jb * 128:(jb + 1) * 128], hTg[:, kc, :], kc == 0, kc == 7) for kc in range(8)]),
                             r=R_hTg + [RG], w=[R_PB[jb]])
                        T.op("act", f_act(gsb[jb][:], PB[jb][:, :], AF.Sigmoid, bias=bgate[:, oc * 3 + jb:oc * 3 + jb + 1], scale=1.0),
                             r=[R_PB[jb], R_misc], w=[R_gsb[jb]])
                    Pw, RP = ring_get(11 + oc)
                    Pv = Pw[0:64, 0:2048].rearrange("p (h c) -> p h c", h=16)
                    for jb, (h0, nh) in enumerate(((0, 8), (8, 4), (12, 4))):
                        T.op("pe", f_seq([f_mm(PB[3 + jb][:, :], Pv[:, h0 + hh, :], oTg[:, h0 + hh, :], hh == 0, hh == nh - 1) for hh in range(nh)]),
                             r=R_oTg + [RP], w=[R_PB[3 + jb]])
                    T.op("dve", f_tt(mg[0][:], PB[3][:, :], gsb[0][:], ALU.mult), r=[R_PB[3], R_gsb[0]], w=[R_mg[0]])
                    T.op("dve", f_tt(mg[1][:], PB[4][:, :], gsb[1][:], ALU.mult), r=[R_PB[4], R_gsb[1]], w=[R_mg[1]])
                    T.op("dve", f_tt(mg[0][:], mg[0][:], mg[1][:], ALU.add), r=[R_mg[0], R_mg[1]], w=[R_mg[0]])
                    T.op("dve", f_tt(mg[1][:], PB[5][:, :], gsb[2][:], ALU.mult), r=[R_PB[5], R_gsb[2]], w=[R_mg[1]])
                    T.op("dve", f_tt(mergedT[:, oc, :], mg[0][:], mg[1][:], ALU.add), r=[R_mg[0], R_mg[1]], w=RQ)

                _ckpt(10 * (g + 1) + 4)
                T.mark(f"g{g} wout")
                for cc in range(2):
                    W, RW = ring_get(19 + cc)
                    Wv = W[:, :].rearrange("p (k c) -> p k c", k=8)
                    for j in range(4):
                        pb = 6 + (j % 2)
                        T.op("pe", f_seq([f_mm(PB[pb][:, :], mergedT[:, kc, j * 128:(j + 1) * 128], Wv[:, kc, :], kc == 0, kc == 7) for kc in range(8)]),
                             r=RQ + [RW], w=[R_PB[pb]])
                        T.op("dve", f_tt(xg[:, j, cc * 512:(cc + 1) * 512], PB[pb][:, :], xg[:, j, cc * 512:(cc + 1) * 512], ALU.add),
                             r=[R_PB[pb], R_xg[j]], w=[R_xg[j]])

                _ckpt(10 * (g + 1) + 5)
                T.mark(f"g{g} mlp")
                for j in range(4):
                    norm_T(xg[:, j, :], [R_xg[j]], hTg[:, :, j * 128:(j + 1) * 128], [R_hTg[j]], j % 2)
                for fb in range(8):
                    W, RW = ring_get(21 + fb)
                    Wv = W[:, :].rearrange("p (k c) -> p k c", k=8)
                    for fs in range(4):
                        fc = fb * 4 + fs
                        pb = 2 + (fc % 4)
                        T.op("pe", f_seq([f_mm(PB[pb][:, :], Wv[:, kc, fs * 128:(fs + 1) * 128], hTg[:, kc, :], kc == 0, kc == 7) for kc in range(8)]),
                             r=R_hTg + [RW], w=[R_PB[pb]])
                        ri = rot("rl", 2)
                        T.op("act", f_act(rl[ri][:], PB[pb][:, :], AF.Relu), r=[R_PB[pb]], w=[R_rl[ri]])
                        T.op("dve", f_tt(hidT[:, fc, :], rl[ri][:], rl[ri][:], ALU.mult), r=[R_rl[ri]], w=[ISC[fc // 2]])
                for cc in range(2):
                    for ks in range(4):
                        W, RW = ring_get(29 + cc * 4 + ks)
                        Wv = W[:, :].rearrange("p (k c) -> p k c", k=8)
                        for j in range(4):
                            pb = 4 + j
                            T.op("pe", f_seq([f_mm(PB[pb][:, :], hidT[:, ks * 8 + fl, j * 128:(j + 1) * 128], Wv[:, fl, :],
                                                   ks == 0 and fl == 0, ks == 3 and fl == 7) for fl in range(8)]),
                                 r=ISC[ks * 4:(ks + 1) * 4] + [RW], w=[R_PB[pb]])
                    for j in range(4):
                        pb = 4 + j
                        T.op("dve", f_tt(xg[:, j, cc * 512:(cc + 1) * 512], PB[pb][:, :], xg[:, j, cc * 512:(cc + 1) * 512], ALU.add),
                             r=[R_PB[pb], R_xg[j]], w=[R_xg[j]])
                _ckpt(10 * (g + 1) + 6)
                T.mark(f"g{g} final")
                for j in range(4):
                    i = 4 * g + j
                    si = rot("stat", 4); s = stat[:, si * 4:(si + 1) * 4]; rs = R_stat[si]
                    hi = rot("hn", 1)
                    T.op("act", f_act(hn[hi][:], xg[:, j, :], AF.Square, accum=s[:, 0:1]), r=[R_xg[j]], w=[R_hn[hi], rs])
                    T.op("act", f_act(s[:, 1:2], s[:, 0:1], AF.Ln, scale=1.0 / 1024.0, bias=1e-6), r=[rs], w=[rs])
                    T.op("act", f_act(s[:, 2:3], s[:, 1:2], AF.Exp, scale=-0.5), r=[rs], w=[rs])
                    T.op("dve", f_stt(xg[:, j, :], xg[:, j, :], s[:, 2:3], gfin[:], ALU.mult, ALU.mult), r=[R_xg[j], rs, R_misc], w=[R_xg[j]])
                    T.dma("sp", out_d[i * 128:(i + 1) * 128, :], xg[:, j, :], R_xg[j], r=[R_xg[j]])
        try:
            emit()
        except _Stop:
            T.dma("sp", out_d[0:128, :], xo[:], R_rl[0], r=R_rl)
            T.wait_all("sp", R_rl + R_ring)
            T.wait_all("pool", [MSKR[0], MSKR[8], ISC[0], ISC[8]])
        T.wait_all("sp", R_xg)
        with nc.Block() as block:
            T.replay(block)
    return nc


OFF = dict(q_s=0, k_s=512, v_s=640, q_d=768, k_d=1024, v_d=1088, q_i=1152, k_i=1408, w_i=1472, q_m=1476, gates=1732)


def _chunked(w, cols):
    sub = w[:, cols]
    n = sub.shape[1]
    return np.ascontiguousarray(sub.reshape(8, 128, n).transpose(1, 0, 2).reshape(128, 8 * n))


def _prep_weights(w_in, w_proj_swa, w_proj_dsa, w_proj_mem, w_out, w_mlp_in, w_mlp_out):
    wch = np.zeros((NCHUNK, 128, 4096), np.float32)
    r64 = np.arange(64)
    qs_cols = np.concatenate([OFF["q_s"] + h * 64 + r64 for h in (0, 4, 1, 5, 2, 6, 3, 7)])
    qdi_cols = np.concatenate([np.concatenate([OFF["q_d"] + h * 64 + r64, OFF["q_i"] + h * 64 + r64]) for h in range(4)])
    qm_cols = np.concatenate([OFF["q_m"] + np.arange(256), OFF["w_i"] + np.arange(4)])
    wch[0] = _chunked(w_in, qs_cols)
    wch[1] = _chunked(w_in, qdi_cols)
    wch[2, :, 0:8 * 260] = _chunked(w_in, qm_cols)
    for oc in range(8):
        gc = np.concatenate([OFF["gates"] + jb * 1024 + oc * 128 + np.arange(128) for jb in range(3)])
        wch[3 + oc, :, 0:8 * 384] = _chunked(w_in, gc)
        wp = np.concatenate([w_proj_swa.reshape(8, 64, 1024), w_proj_dsa.reshape(4, 64, 1024), w_proj_mem.reshape(4, 64, 1024)], 0)
        wch[11 + oc, 0:64, 0:2048] = wp[:, :, oc * 128:(oc + 1) * 128].transpose(1, 0, 2).reshape(64, 2048)
    for cc in range(2):
        wch[19 + cc] = _chunked(w_out, cc * 512 + np.arange(512))
    for fb in range(8):
        wch[21 + fb] = _chunked(w_mlp_in, fb * 512 + np.arange(512))
    for cc in range(2):
        for ks in range(4):
            blk = w_mlp_out[ks * 1024:(ks + 1) * 1024, cc * 512:(cc + 1) * 512]
            wch[29 + cc * 4 + ks] = blk.reshape(8, 128, 512).transpose(1, 0, 2).reshape(128, 4096)
    k_cols = np.concatenate([OFF["k_s"] + np.arange(128), OFF["k_d"] + r64, OFF["k_i"] + r64, OFF["v_s"] + np.arange(128), OFF["v_d"] + r64])
    wk = _chunked(w_in, k_cols)
    return wch, wk


_NC_CACHE = {}


def kernel(x, mem, positions, g_mix, w_in, b_gate, sinks, g_mem, w_mem_kv, w_proj_swa, w_proj_dsa,
           w_proj_mem, w_out, g_mlp, w_mlp_in, w_mlp_out, g_final):
    f = lambda a: np.asarray(a, dtype=np.float32)
    x = f(x); mem = f(mem); positions = np.asarray(positions, dtype=np.int32)
    wch, wk = _prep_weights(f(w_in)[0], f(w_proj_swa)[0], f(w_proj_dsa)[0], f(w_proj_mem)[0], f(w_out)[0], f(w_mlp_in)[0], f(w_mlp_out)[0])
    wmem = _chunked(f(w_mem_kv)[0], np.arange(512))
    tr = lambda v: np.ascontiguousarray(f(v).reshape(8, 128).T)
    gvec = np.concatenate([tr(g_mix[0]), tr(g_mlp[0]), tr(g_mem[0])], 1)
    gfin = np.ascontiguousarray(np.broadcast_to(f(g_final)[None, :], (128, 1024)))
    bg = f(b_gate)[0].reshape(3, 8, 128)
    bgate = np.ascontiguousarray(bg.transpose(2, 1, 0).reshape(128, 24))
    sinkr = np.ascontiguousarray(np.broadcast_to(f(sinks)[0][None, :], (64, 8)))
    half = 32
    invf = np.power(np.float32(10000.0), -np.arange(half, dtype=np.float32) / np.float32(half)).astype(np.float32)
    invf = np.ascontiguousarray(np.broadcast_to(invf[None, :], (128, 32)))
    s_ = np.arange(128)[:, None]; t_ = np.arange(128)[None, :]
    tri_cur = (t_ >= s_).astype(np.float32)
    tri_prev = (s_ > t_).astype(np.float32)
    dbias = np.where(t_ <= s_, 0.0, NEG).astype(np.float32)
    in_maps = []
    for c in range(8):
        b, par = c // 2, c % 2
        order = np.arange(64) if par == 0 else np.concatenate([np.arange(1, 64), [0]])
        xb = x[b].reshape(64, 128, 1024)[order].reshape(64 * 128, 1024)
        pb = positions[b].reshape(64, 128)[order]
        posc = np.ascontiguousarray(pb.T)
        prev0 = tri_prev * float(par)
        p63 = np.full((128, 128), 0.0 if par == 1 else NEG, np.float32)
        cmask = np.ascontiguousarray(np.stack([tri_cur, tri_prev, prev0, dbias, p63], 1).reshape(128, 5 * 128).astype(np.float32))
        in_maps.append(dict(xc=np.ascontiguousarray(xb), posc=posc, memc=np.ascontiguousarray(mem[b]), cmask=cmask, invf=invf,
                            wk=wk, wch=wch, wmem=wmem, gvec=gvec, gfin=gfin, bgate=bgate, sinkr=sinkr))
    if "nc" not in _NC_CACHE:
        _NC_CACHE["nc"] = build_program()
    nc = _NC_CACHE["nc"]
    res = run_bass_kernel_spmd(nc, in_maps, core_ids=list(range(8)))
    out = np.zeros((4, 64, 128, 1024), np.float32)
    for c in range(8):
        b, par = c // 2, c % 2
        o = np.asarray(res.results[c]["out"]).reshape(32, 128, 1024)
        out[b, par::2] = o
    return out.reshape(4, 8192, 1024)
```

```python
import math
import numpy as np
from contextlib import ExitStack
import concourse.bass as bass
import concourse.mybir as mybir
from concourse.bass_utils import run_bass_kernel_spmd

F32 = mybir.dt.float32; BF16 = mybir.dt.bfloat16; I32 = mybir.dt.int32
ALU = mybir.AluOpType; AF = mybir.ActivationFunctionType; AX = mybir.AxisListType

STAGE = 99
class _Stop(Exception):
    pass
def _ckpt(n):
    if STAGE <= n:
        raise _Stop()
NITER = 14
NEG = -1.0e30
NCHUNK = 37
TOPK = 256.0


class Reg:
    __slots__ = ("name", "w", "r", "dsem", "dcnt")
    def __init__(self, name):
        self.name = name; self.w = None; self.r = {}; self.dsem = None; self.dcnt = 0


class Eng:
    def __init__(self, name):
        self.name = name; self.q = []; self.sem = None; self.cnt = 0; self.seen = {}


class Trk:
    def __init__(self, nc, stack, nsem):
        self.nc = nc
        self.sems = [stack.enter_context(nc.semaphore(f"s{i}")) for i in range(nsem)]
        self.si = 0
        self.E = {n: Eng(n) for n in ("pe", "act", "dve", "pool", "sp")}
        for e in self.E.values():
            e.sem = self.newsem()
    def newsem(self):
        s = self.sems[self.si]; self.si += 1; return s
    def _deps(self, eng, reads, writes):
        deps = {}
        def add(ev, kind):
            if ev is None: return
            sem, val, src = ev
            if src is eng and eng.name == "pe":
                return
            k = id(sem)
            if eng.seen.get(k, 0) >= val: return
            if k not in deps or deps[k][1] < val: deps[k] = (sem, val)
        for r in reads: add(r.w, "raw")
        for w in writes:
            add(w.w, "waw")
            for ev in w.r.values(): add(ev, "war")
        out = list(deps.values())
        for sem, val in out: eng.seen[id(sem)] = val
        return out
    def op(self, en, fn, r=(), w=()):
        eng = self.E[en]
        waits = self._deps(eng, r, w)
        eng.cnt += 1
        ev = (eng.sem, eng.cnt, eng)
        eng.q.append((waits, fn, (eng.sem, 1)))
        for x in r: x.r[en] = ev
        for x in w: x.w = ev; x.r = {}
    def dma(self, en, out_ap, in_ap, slot, r=(), w=()):
        eng = self.E[en]
        waits = self._deps(eng, r, w)
        if slot.dsem is None: slot.dsem = self.newsem()
        slot.dcnt += 16
        ev = (slot.dsem, slot.dcnt, None)
        eng.q.append((waits, lambda h: h.dma_start(out=out_ap, in_=in_ap), (slot.dsem, 16)))
        for x in r: x.r["dma%d" % id(slot)] = ev
        for x in w: x.w = ev; x.r = {}
    def mark(self, label):
        self.E["pe"].q.append(([], None, label))
    def wait_all(self, en, regs):
        eng = self.E[en]
        waits = self._deps(eng, [], regs)
        eng.q.append((waits, None, None))
    def replay(self, block):
        def mk(en):
            q = self.E[en].q
            def body(h):
                for waits, fn, inc in q:
                    for sem, val in waits: h.wait_ge(sem, val)
                    if fn is None:
                        if isinstance(inc, str): MARKS.append((inc, PE_CNT[0]))
                        continue
                    ins = fn(h)
                    ins.then_inc(inc[0], inc[1])
            return body
        block.tensor(mk("pe")); block.scalar(mk("act")); block.vector(mk("dve"))
        block.gpsimd(mk("pool")); block.sync(mk("sp"))


PE_CNT = [0]
MARKS = []
def f_mm(out, lhsT, rhs, start=True, stop=True):
    def f(h):
        PE_CNT[0] += 1
        return h.matmul(out, lhsT=lhsT, rhs=rhs, start=start, stop=stop)
    return f
def f_tr(out, in_, ident):
    def f(h):
        PE_CNT[0] += 1
        return h.transpose(out=out, in_=in_, identity=ident)
    return f
def f_seq(fns):
    def f(h):
        ins = None
        for fn in fns: ins = fn(h)
        return ins
    return f
def f_act(out, in_, func, bias=None, scale=None, accum=None):
    kw = {}
    if bias is not None: kw["bias"] = bias
    if scale is not None: kw["scale"] = scale
    if accum is not None: kw["accum_out"] = accum
    return lambda h: h.activation(out=out, in_=in_, func=func, **kw)
def f_ts(out, in0, s1, s2=None, op0=ALU.mult, op1=None, accum=None):
    kw = {}
    if op1 is not None: kw["op1"] = op1
    if accum is not None: kw["accum_out"] = accum
    return lambda h: h.tensor_scalar(out=out, in0=in0, scalar1=s1, scalar2=s2, op0=op0, **kw)
def f_tt(out, in0, in1, op):
    return lambda h: h.tensor_tensor(out=out, in0=in0, in1=in1, op=op)
def f_stt(out, in0, scalar, in1, op0, op1):
    return lambda h: h.scalar_tensor_tensor(out=out, in0=in0, scalar=scalar, in1=in1, op0=op0, op1=op1)
def f_cp(out, in_):
    return lambda h: h.tensor_copy(out=out, in_=in_)
def f_memset(ap, v):
    return lambda h: h.memset(ap, v)


def build_program():
    nc = bass.Bass("TRN2", target_bir_lowering=False)
    dt_in = lambda n, s, d=F32: nc.dram_tensor(n, s, d, kind="ExternalInput").ap()
    xc = dt_in("xc", [64 * 128, 1024])
    posc = dt_in("posc", [128, 64], I32)
    memc = dt_in("memc", [256, 1024])
    cmask = dt_in("cmask", [128, 5 * 128])
    invf_d = dt_in("invf", [128, 32])
    wk_d = dt_in("wk", [128, 8 * 448])
    wch_d = dt_in("wch", [NCHUNK, 128, 4096])
    wmem_d = dt_in("wmem", [128, 4096])
    gvec_d = dt_in("gvec", [128, 24])
    gfin_d = dt_in("gfin", [128, 1024])
    bgate_d = dt_in("bgate", [128, 24])
    sinkr_d = dt_in("sinkr", [64, 8])
    out_d = nc.dram_tensor("out", [32 * 128, 1024], F32, kind="ExternalOutput").ap()
    wbf_d = nc.dram_tensor("wbf", [NCHUNK, 128, 4096], BF16, kind="Internal").ap()

    with ExitStack() as st:
        T = Trk(nc, st, 48)
        def sb(name, shape, dt):
            return st.enter_context(nc.sbuf_tensor(name, shape, dt))
        BIG = sb("BIG", [128, 8192], F32)
        MSK = sb("MSK", [128, 8192], BF16)
        ISC = [Reg(f"isc{c}") for c in range(16)]
        MSKR = [Reg(f"msk{c}") for c in range(16)]
        hidT = BIG[:].bitcast(BF16).rearrange("p (f t) -> p f t", t=512)
        wk_sb = sb("wk_sb", [128, 8, 448], BF16); R_wk = Reg("wk")
        kDI = sb("kDI", [128, 8192], BF16); R_kDI = [Reg(f"kDI{i}") for i in range(64)]
        vD = sb("vD", [128, 64, 128], BF16); R_vD = [Reg(f"vD{i}") for i in range(64)]
        kST = sb("kST", [128, 16, 128], BF16); R_kST = [Reg(f"kST{i}") for i in range(16)]
        vS = sb("vS", [128, 16, 128], BF16); R_vS = [Reg(f"vS{i}") for i in range(16)]
        mkT = sb("mkT", [128, 2, 256], BF16); R_mkT = Reg("mkT")
        mv = sb("mv", [128, 2, 256], BF16); R_mv = Reg("mv")
        xg = sb("xg", [128, 4, 1024], F32); R_xg = [Reg(f"xg{j}") for j in range(4)]
        hTg = sb("hTg", [128, 8, 512], BF16); R_hTg = [Reg(f"hTg{j}") for j in range(4)]
        hTo = sb("hTo", [128, 8, 128], BF16); R_hTo = Reg("hTo")
        oTg = sb("oTg", [64, 16, 512], BF16); R_oTg = [Reg(f"oTg{j}") for j in range(4)]
        QM = sb("QM", [128, 4096], BF16)
        qsT = QM[:, 0:2048].rearrange("p (s c) -> p s c", s=4)
        qdiT = QM[:, 2048:4096].rearrange("p (s c) -> p s c", s=4)
        mergedT = QM[:].rearrange("p (k t) -> p k t", k=8)
        R_qs = [Reg(f"qs{j}") for j in range(4)]; R_qdi = [Reg(f"qdi{j}") for j in range(4)]
        qmT = sb("qmT", [128, 4, 256], BF16); R_qm = [Reg(f"qm{j}") for j in range(4)]
        wiT = sb("wiT", [128, 4, 4], F32); R_wi = [Reg(f"wi{j}") for j in range(4)]
        NRING = 2
        ring = [sb(f"ring{i}", [128, 4096], BF16) for i in range(NRING)]
        R_ring = [Reg(f"ring{i}") for i in range(NRING)]
        ident = sb("ident", [128, 128], BF16); identf = sb("identf", [128, 128], F32); onesf = sb("onesf", [128, 128], F32)
        ones_b = sb("ones_b", [128, 64], BF16)
        qd0 = sb("qd0", [128, 512], BF16); R_qd0 = Reg("qd0")
        R_const = Reg("const")
        cm_f = sb("cm_f", [128, 5, 128], F32)
        cm_b = sb("cm_b", [128, 3, 128], BF16)
        invf = sb("invf_sb", [128, 32], F32)
        gvec = sb("gvec_sb", [128, 24], F32)
        gfin = sb("gfin_sb", [128, 1024], F32)
        bgate = sb("bgate_sb", [128, 24], F32)
        es = sb("es_sb", [64, 8], F32)
        pw2 = sb("pw2", [128, NITER], F32)
        posi = sb("posi", [128, 64], I32); posf = sb("posf", [128, 64], F32)
        cosT = sb("cosT", [128, 8, 32], F32); sinT = sb("sinT", [128, 8, 32], F32); R_cs = Reg("cs")
        rtmp = [sb(f"rtmp{i}", [128, 256], F32) for i in range(3)]; rtmpi = sb("rtmpi", [128, 256], I32); R_rt = Reg("rt")
        stat = sb("stat", [128, 16], F32); R_stat = [Reg(f"stat{i}") for i in range(4)]
        hn = [sb(f"hn{i}", [128, 1024], BF16) for i in range(1)]; R_hn = [Reg(f"hn{i}") for i in range(1)]
        tok_b = [sb(f"tok_b{i}", [128, 512], BF16) for i in range(2)]; R_tok = [Reg(f"tok{i}") for i in range(2)]
        Et = [sb(f"Et{i}", [128, 512], BF16) for i in range(5)]; R_E = [Reg(f"E{i}") for i in range(5)]
        Pt = [sb(f"Pt{i}", [128, 512], BF16) for i in range(2)]; R_P = [Reg(f"P{i}") for i in range(2)]
        RLX = sb("RLX", [128, 1024], F32); rl = [RLX[:, 0:512], RLX[:, 512:1024]]; R_rl = [Reg(f"rl{i}") for i in range(2)]
        xo = RLX; R_xo = R_rl[0]
        mT = [sb(f"mT{i}", [128, 512], BF16) for i in range(2)]; R_mT = [Reg(f"mT{i}") for i in range(2)]
        dn = [sb(f"dn{i}", [128, 512], F32) for i in range(1)]; R_dn = [Reg(f"dn{i}") for i in range(1)]
        mg = rl; R_mg = R_rl
        RTU = sb("RTU", [128, 1024], F32); rope_t = RTU[:, 0:512]; rope_u = RTU[:, 512:1024]; R_rtu = [Reg("rt_"), Reg("ru_")]
        gsb = [sb(f"gsb{i}", [128, 512], BF16) for i in range(3)]; R_gsb = [Reg(f"gsb{i}") for i in range(3)]
        bis = sb("bis", [128, 8 + NITER], F32); R_lo = Reg("lo"); R_mid = Reg("mid"); R_cd = Reg("cd"); R_ca = Reg("ca"); R_t = Reg("t")
        PB = [st.enter_context(nc.psum_tensor(f"pb{i}", [128, 512], F32)) for i in range(8)]
        R_PB = [Reg(f"pb{i}") for i in range(8)]
        def pbf(i):
            return PB[i][:].bitcast(BF16)

        cnt = {"ring": 0, "stat": 0, "hn": 0, "tok": 0, "E": 0, "P": 0, "rl": 0, "mT": 0, "dn": 0, "mg": 0}
        def rot(name, n):
            i = cnt[name] % n; cnt[name] += 1; return i

        R_cmf = Reg("cmf"); R_misc = Reg("misc")
        T.dma("sp", cm_f[:].rearrange("p a b -> p (a b)"), cmask[:, :], R_cmf, w=[R_cmf])
        for (dst, src) in ((invf, invf_d), (gvec, gvec_d), (gfin, gfin_d), (bgate, bgate_d)):
            T.dma("sp", dst[:], src[:, :], R_misc, w=[R_misc])
            T.wait_all("sp", [R_misc])
        R_es = Reg("es")
        T.dma("sp", es[:], sinkr_d[:, :], R_es, w=[R_es])
        R_pos = Reg("pos")
        T.dma("sp", posi[:], posc[:, :], R_pos, w=[R_pos])
        T.op("pool", f_memset(onesf[:], 1.0), w=[R_const])
        T.op("pool", f_memset(identf[:], 0.0), w=[R_const])
        T.op("pool", lambda h: h.affine_select(out=identf[:], in_=onesf[:], pattern=[[-1, 128]], compare_op=ALU.is_equal,
                                                fill=0.0, base=0, channel_multiplier=1), r=[R_const], w=[R_const])
        T.op("dve", f_cp(ident[:], identf[:]), r=[R_const], w=[R_const])
        T.op("dve", f_memset(ones_b[:], 1.0), w=[R_const])
        T.op("pool", f_memset(vD[:].rearrange("p a b -> p (a b)"), 1.0), w=R_vD)
        T.op("dve", f_memset(dn[0][:], 0.0), w=[R_dn[0]])
        T.op("dve", f_memset(qd0[:], 0.0), w=[R_qd0])
        for k in range(NITER):
            T.op("dve", f_memset(pw2[:, k:k + 1], 2.0 ** (-(k + 1))), w=[R_const])
        T.op("dve", f_cp(cm_b[:], cm_f[:, 0:3, :]), r=[R_cmf], w=[R_const])
        T.op("act", f_act(es[:], es[:], AF.Exp), r=[R_es], w=[R_es])
        T.op("dve", f_cp(posf[:], posi[:]), r=[R_pos], w=[R_pos])
        tri_cur = cm_b[:, 0, :]; tri_prev = cm_b[:, 1, :]; prev0 = cm_b[:, 2, :]
        dbias = cm_f[:, 3, :]; p63bias = cm_f[:, 4, :]

        def emit():
            def norm_T(x_ap, x_regs, dstT, dst_regs, pbank):
                si = rot("stat", 4); s = stat[:, si * 4:(si + 1) * 4]; rs = R_stat[si]
                hi = rot("hn", 1); h_ = hn[hi]; rh = R_hn[hi]
                T.op("act", f_act(h_[:], x_ap, AF.Square, accum=s[:, 0:1]), r=x_regs, w=[rh, rs])
                T.op("act", f_act(s[:, 1:2], s[:, 0:1], AF.Ln, scale=1.0 / 1024.0, bias=1e-6), r=[rs], w=[rs])
                T.op("act", f_act(s[:, 2:3], s[:, 1:2], AF.Exp, scale=-0.5), r=[rs], w=[rs])
                T.op("dve", f_ts(h_[:], x_ap, s[:, 2:3], None, ALU.mult), r=x_regs + [rs], w=[rh])
                pv = pbf(pbank)
                T.op("pe", f_seq([f_tr(pv[:, kc * 128:(kc + 1) * 128], h_[:, kc * 128:(kc + 1) * 128], ident[:]) for kc in range(8)]),
                     r=[rh, R_const], w=[R_PB[pbank]])
                T.op("act", f_act(dstT, pv[:, :].rearrange("p (k t) -> p k t", k=8), AF.Copy), r=[R_PB[pbank]], w=dst_regs)
                return s, rs

            def rope_tables(blk0, nb):
                n = nb * 32
                a0 = rtmp[0][:, 0:n]; a1 = rtmp[1][:, 0:n]; a2 = rtmp[2][:, 0:n]; ai = rtmpi[:, 0:n]
                v3 = lambda a: a.rearrange("p (b f) -> p b f", f=32)
                T.op("dve", f_tt(v3(a0), posf[:, blk0:blk0 + nb].unsqueeze(2).to_broadcast([128, nb, 32]),
                                 invf[:].unsqueeze(1).to_broadcast([128, nb, 32]), ALU.mult), r=[R_pos, R_misc], w=[R_rt])
                T.op("dve", f_ts(ai, a0, 1.0 / (2 * math.pi), None, ALU.mult), r=[R_rt], w=[R_rt])
                T.op("dve", f_cp(a1, ai), r=[R_rt], w=[R_rt])
                T.op("dve", f_stt(a2, a1, -6.28125, a0, ALU.mult, ALU.add), r=[R_rt], w=[R_rt])
                T.op("dve", f_stt(a2, a1, -0.0019353072, a2, ALU.mult, ALU.add), r=[R_rt], w=[R_rt])
                T.op("dve", f_ts(a0, a2, -3.1415925, 3.1415925, ALU.max, ALU.min), r=[R_rt], w=[R_rt])
                T.op("act", f_act(sinT[:, 0:nb, :].rearrange("p b f -> p (b f)"), a0, AF.Sin), r=[R_rt], w=[R_cs])
                T.op("dve", f_ts(a1, a2, math.pi / 2, None, ALU.add), r=[R_rt], w=[R_rt])
                T.op("dve", f_ts(a0, a1, math.pi, -2 * math.pi, ALU.is_gt, ALU.mult), r=[R_rt, R_cs], w=[R_rt])
                T.op("dve", f_tt(a0, a0, a1, ALU.add), r=[R_rt], w=[R_rt])
                T.op("dve", f_ts(a0, a0, -3.1415925, 3.1415925, ALU.max, ALU.min), r=[R_rt], w=[R_rt])
                T.op("act", f_act(cosT[:, 0:nb, :].rearrange("p b f -> p (b f)"), a0, AF.Sin), r=[R_rt], w=[R_cs])

            def rope_apply(src, src_regs, dst, dst_regs, H, cb):
                n = H * 64
                t_ = rope_t[:, 0:n]; u_ = rope_u[:, 0:n]
                cosb = cosT[:, cb:cb + 1, :]; sinb = sinT[:, cb:cb + 1, :]
                T.op("dve", f_tt(t_.rearrange("p (a f) -> p a f", f=32), src.rearrange("p (a f) -> p a f", f=32),
                                 cosb.to_broadcast([128, 2 * H, 32]), ALU.mult), r=src_regs + [R_cs], w=[R_rtu[0]])
                s4 = src.rearrange("p (h e f) -> p h e f", e=2, f=32)
                u4 = u_.rearrange("p (h e f) -> p h e f", e=2, f=32)
                t4 = t_.rearrange("p (h e f) -> p h e f", e=2, f=32)
                d4 = dst.rearrange("p (h e f) -> p h e f", e=2, f=32)
                sb_ = sinb.to_broadcast([128, H, 32])
                T.op("dve", f_tt(u4[:, :, 0, :], s4[:, :, 1, :], sb_, ALU.mult), r=src_regs + [R_cs], w=[R_rtu[1]])
                T.op("dve", f_tt(u4[:, :, 1, :], s4[:, :, 0, :], sb_, ALU.mult), r=src_regs + [R_cs], w=[R_rtu[1]])
                T.op("dve", f_tt(d4[:, :, 0, :], t4[:, :, 0, :], u4[:, :, 0, :], ALU.subtract), r=R_rtu, w=dst_regs)
                T.op("dve", f_tt(d4[:, :, 1, :], t4[:, :, 1, :], u4[:, :, 1, :], ALU.add), r=R_rtu, w=dst_regs)

            R_wbf = [Reg(f"wbf{i}") for i in range(NCHUNK)]
            CH_N = [4096, 4096, 8 * 260] + [8 * 384] * 8 + [2048] * 8 + [4096] * 18
            CH_P = [128] * 11 + [64] * 8 + [128] * 18
            sched = [0, 1, 2] + [v for oc in range(8) for v in (3 + oc, 11 + oc)] + list(range(19, 37))
            ring_state = {"issued": 0, "got": 0}
            total_gets = 8 * NCHUNK
            def ring_issue():
                k = ring_state["issued"]
                if k >= total_gets: return
                c = sched[k % NCHUNK]; b = k % NRING
                T.dma("sp", ring[b][0:CH_P[c], 0:CH_N[c]], wbf_d[c, 0:CH_P[c], 0:CH_N[c]], R_ring[b], r=[R_wbf[c]], w=[R_ring[b]])
                ring_state["issued"] += 1
            def ring_get(expect):
                k = ring_state["got"]
                assert sched[k % NCHUNK] == expect, (k, expect)
                while ring_state["issued"] < min(k + NRING, total_gets):
                    ring_issue()
                ring_state["got"] += 1
                b = k % NRING
                return ring[b], R_ring[b]

            def convert(src_dram_ap, npart, nelem, ncols, goff, dst_ap, dst_regs, half):
                stg = BIG[0:npart, half * 4096: half * 4096 + nelem]
                sregs = ISC[half * 8:(half + 1) * 8]
                T.dma("pool", stg, src_dram_ap, sregs[0], w=sregs)
                if goff is None:
                    T.op("act", f_act(dst_ap, stg, AF.Copy), r=sregs, w=dst_regs)
                else:
                    for kc in range(8):
                        T.op("dve", f_ts(dst_ap[:, kc * ncols:(kc + 1) * ncols], stg[:, kc * ncols:(kc + 1) * ncols],
                                          gvec[:, goff + kc:goff + kc + 1], None, ALU.mult), r=sregs + [R_misc], w=dst_regs)
            _ckpt(1)
            convert(wk_d[:, :], 128, 8 * 448, 448, 0, wk_sb[:].rearrange("p k c -> p (k c)"), [R_wk], 0)
            wmem_b = MSK[:, 4096:8192]
            convert(wmem_d[:, :], 128, 4096, 512, 16, wmem_b, MSKR[8:16], 1)

            for mc in range(2):
                T.dma("sp", xo[:], memc[mc * 128:(mc + 1) * 128, :], R_rl[0], w=R_rl)
                norm_T(xo[:], R_rl, hTo[:], [R_hTo], 0)
                T.op("pe", f_seq([f_mm(PB[1][:, :], hTo[:, kc, :], wmem_b[:, kc * 512:(kc + 1) * 512], kc == 0, kc == 7) for kc in range(8)]),
                     r=[R_hTo] + MSKR[8:16], w=[R_PB[1]])
                ti = rot("tok", 2)
                T.op("act", f_act(tok_b[ti][:, 0:256], PB[1][:, 0:256], AF.Copy), r=[R_PB[1]], w=[R_tok[ti]])
                T.op("act", f_act(mv[:, mc, :], PB[1][:, 256:512], AF.Copy), r=[R_PB[1]], w=[R_mv])
                pv = pbf(2)
                T.op("pe", f_seq([f_tr(pv[:, jj * 128:(jj + 1) * 128], tok_b[ti][:, jj * 128:(jj + 1) * 128], ident[:]) for jj in range(2)]),
                     r=[R_tok[ti], R_const], w=[R_PB[2]])
                T.op("act", f_act(mkT[:, :, mc * 128:(mc + 1) * 128], pv[:, 0:256].rearrange("p (j m) -> p j m", j=2), AF.Copy),
                     r=[R_PB[2]], w=[R_mkT])

            _ckpt(2)
            for c in range(NCHUNK):
                half = c % 2
                goff = 0 if c < 11 else (8 if 21 <= c < 29 else None)
                ncols = {0: 512, 1: 512, 2: 260}.get(c, 384 if c < 11 else 512)
                npart = CH_P[c]; nelem = CH_N[c]
                dstb = MSK[0:npart, half * 4096: half * 4096 + nelem]
                dregs = MSKR[half * 8:(half + 1) * 8]
                convert(wch_d[c, 0:npart, 0:nelem], npart, nelem, ncols, goff, dstb, dregs, half)
                T.dma("pool", wbf_d[c, 0:npart, 0:nelem], dstb, dregs[0], r=dregs, w=[R_wbf[c]])

            def kside(pos, x_ap, x_regs, hT_ap, hT_regs, cb):
                kidx = 0 if pos == 63 else pos + 1
                rg = (pos + 1) % 16
                norm_T(x_ap, x_regs, hT_ap, hT_regs, 0)
                _ckpt(3.1)
                T.op("pe", f_seq([f_mm(PB[1][:, 0:448], hT_ap[:, kc, :], wk_sb[:, kc, :], kc == 0, kc == 7) for kc in range(8)]),
                     r=hT_regs + [R_wk], w=[R_PB[1]])
                _ckpt(3.2)
                ti = rot("tok", 2)
                rope_apply(PB[1][:, 0:256], [R_PB[1]], tok_b[ti][:, 0:256], [R_tok[ti]], 4, cb)
                _ckpt(3.3)
                T.op("dve", f_cp(vS[:, rg, :], PB[1][:, 256:384]), r=[R_PB[1]], w=[R_vS[rg]])
                T.op("dve", f_cp(vD[:, kidx, 0:64], PB[1][:, 384:448]), r=[R_PB[1]], w=[R_vD[kidx]])
                _ckpt(3.4)
                pv = pbf(2)
                T.op("pe", f_seq([f_tr(pv[:, jj * 128:(jj + 1) * 128], tok_b[ti][:, jj * 128:(jj + 1) * 128], ident[:]) for jj in range(2)]),
                     r=[R_tok[ti], R_const], w=[R_PB[2]])
                _ckpt(3.5)
                T.op("act", f_act(kST[:, rg, :], pv[:, 0:128], AF.Copy), r=[R_PB[2]], w=[R_kST[rg]])
                T.op("act", f_act(kDI[:, kidx * 128:(kidx + 1) * 128], pv[:, 128:256], AF.Copy), r=[R_PB[2]], w=[R_kDI[kidx]])

            _ckpt(3)
            rope_tables(63, 1)
            T.dma("sp", xo[:], xc[63 * 128:64 * 128, :], R_rl[0], w=R_rl)
            kside(63, xo[:], R_rl, hTo[:], [R_hTo], 0)

            def finish_attn(bO, bD, j, h0, es_ap):
                di = rot("dn", 1)
                d_ = dn[di][0:64, :]
                if es_ap is not None:
                    T.op("dve", f_tt(d_.rearrange("p (h t) -> p h t", h=4), PB[bD][0:64, :].rearrange("p (h t) -> p h t", h=4), es_ap, ALU.add),
                         r=[R_PB[bD], R_es], w=[R_dn[di]])
                    T.op("act", f_act(d_, d_, AF.Ln), r=[R_dn[di]], w=[R_dn[di]])
                else:
                    T.op("act", f_act(d_, PB[bD][0:64, :], AF.Ln), r=[R_PB[bD]], w=[R_dn[di]])
                T.op("act", f_act(d_, d_, AF.Exp, scale=-1.0), r=[R_dn[di]], w=[R_dn[di]])
                T.op("dve", f_tt(oTg[:, h0:h0 + 4, j * 128:(j + 1) * 128], PB[bO][0:64, :].rearrange("p (h t) -> p h t", h=4),
                                 d_.rearrange("p (h t) -> p h t", h=4), ALU.mult), r=[R_PB[bO], R_dn[di]], w=[R_oTg[j]])

            for g in range(8):
                T.mark(f"g{g} kside")
                npos = 8 if g < 7 else 7
                rope_tables(8 * g, 8)
                for pl in range(npos):
                    pos = 8 * g + pl
                    if pl % 2 == 0:
                        j = pl // 2
                        T.dma("sp", xg[:, j, :], xc[pos * 128:(pos + 1) * 128, :], R_xg[j], w=[R_xg[j]])
                        kside(pos, xg[:, j, :], [R_xg[j]], hTg[:, :, j * 128:(j + 1) * 128], [R_hTg[j]], pl)
                    else:
                        T.dma("sp", xo[:], xc[pos * 128:(pos + 1) * 128, :], R_rl[0], w=R_rl)
                        kside(pos, xo[:], R_rl, hTo[:], [R_hTo], pl)

                _ckpt(10 * (g + 1) + 1)
                T.mark(f"g{g} qproj")
                for c in range(3):
                    W, RW = ring_get(c)
                    ncols = (512, 512, 260)[c]
                    Wv = W[:, 0:8 * ncols].rearrange("p (k c) -> p k c", k=8)
                    for j in range(4):
                        pb = 3 + (j % 2)
                        T.op("pe", f_seq([f_mm(PB[pb][:, 0:ncols], hTg[:, kc, j * 128:(j + 1) * 128], Wv[:, kc, :], kc == 0, kc == 7) for kc in range(8)]),
                             r=[R_hTg[j], RW], w=[R_PB[pb]])
                        ti = rot("tok", 2)
                        if c < 2:
                            rope_apply(PB[pb][:, 0:512], [R_PB[pb]], tok_b[ti][:, 0:512], [R_tok[ti]], 8, 2 * j)
                            pv = pbf(5 + (j % 2))
                            T.op("pe", f_seq([f_tr(pv[:, q * 128:(q + 1) * 128], tok_b[ti][:, q * 128:(q + 1) * 128], ident[:]) for q in range(4)]),
                                 r=[R_tok[ti], R_const], w=[R_PB[5 + (j % 2)]])
                            dst = qsT if c == 0 else qdiT
                            T.op("act", f_act(dst[:, j, :], pv[:, 0:512], AF.Copy), r=[R_PB[5 + (j % 2)]], w=[(R_qs if c == 0 else R_qdi)[j]])
                        else:
                            T.op("act", f_act(tok_b[ti][:, 0:256], PB[pb][:, 0:256], AF.Copy), r=[R_PB[pb]], w=[R_tok[ti]])
                            T.op("act", f_act(wiT[:, j, :], PB[pb][:, 256:260], AF.Copy, scale=1.0 / 16.0), r=[R_PB[pb]], w=[R_wi[j]])
                            pv = pbf(5 + (j % 2))
                            T.op("pe", f_seq([f_tr(pv[:, q * 128:(q + 1) * 128], tok_b[ti][:, q * 128:(q + 1) * 128], ident[:]) for q in range(2)]),
                                 r=[R_tok[ti], R_const], w=[R_PB[5 + (j % 2)]])
                            T.op("act", f_act(qmT[:, j, :], pv[:, 0:256], AF.Copy), r=[R_PB[5 + (j % 2)]], w=[R_qm[j]])

                _ckpt(10 * (g + 1) + 2)
                T.mark(f"g{g} attn")
                def slot_vars(j):
                    i = 4 * g + j
                    nkb = 2 * i + 2
                    return i, slice(j * 128, (j + 1) * 128), nkb, (nkb + 3) // 4, nkb * 128
                def swa_mem(j):
                    i, tok, nkb, nch, n = slot_vars(j)
                    rp = (2 * i) % 16; rc = (2 * i + 1) % 16
                    for kv in range(2):
                        pr = slice(kv * 64, (kv + 1) * 64)
                        bS = (2, 3) if kv == 0 else (4, 5)
                        bO, bD = (0, 1) if kv == 0 else (6, 7)
                        Ps = []
                        for which, rr_ in enumerate((rp, rc)):
                            b = bS[which]
                            T.op("pe", f_mm(PB[b][:, :], kST[pr, rr_, :], qsT[pr, j, :]), r=[R_kST[rr_], R_qs[j]], w=[R_PB[b]])
                            ei = rot("E", 5)
                            T.op("act", f_act(Et[ei][:], PB[b][:, :], AF.Exp, scale=0.125), r=[R_PB[b]], w=[R_E[ei]])
                            pi_ = rot("P", 2)
                            msk = (prev0 if i == 0 else tri_prev) if which == 0 else tri_cur
                            T.op("dve", f_tt(Pt[pi_][:].rearrange("p (h t) -> p h t", h=4), Et[ei][:].rearrange("p (h t) -> p h t", h=4),
                                             msk.unsqueeze(1).to_broadcast([128, 4, 128]), ALU.mult), r=[R_E[ei], R_const], w=[R_P[pi_]])
                            Ps.append((pi_, rr_))
                        T.op("pe", f_seq([f_mm(PB[bO][0:64, :], vS[:, rr_, pr], Pt[pi_][:], w_ == 0, w_ == 1) for w_, (pi_, rr_) in enumerate(Ps)]
                                         + [f_mm(PB[bD][0:64, :], ones_b[:], Pt[pi_][:], w_ == 0, w_ == 1) for w_, (pi_, rr_) in enumerate(Ps)]),
                             r=[R_P[p_] for p_, _ in Ps] + [R_vS[r_] for _, r_ in Ps] + [R_const], w=[R_PB[bO], R_PB[bD]])
                        finish_attn(bO, bD, j, 4 * kv, es[:, 4 * kv:4 * kv + 4].unsqueeze(2).to_broadcast([64, 4, 128]))
                    _ckpt(12.1)
                    Es = [rot("E", 5), rot("E", 5)]
                    fns = []
                    for e in range(2):
                        pr = slice(e * 64, (e + 1) * 64)
                        for mc in range(2):
                            for jj in range(2):
                                fns.append(f_mm(PB[2 + 2 * mc + e][:, jj * 128:(jj + 1) * 128], mkT[pr, jj, mc * 128:(mc + 1) * 128],
                                                qmT[pr, j, jj * 128:(jj + 1) * 128]))
                    T.op("pe", f_seq(fns), r=[R_mkT, R_qm[j]], w=[R_PB[2], R_PB[3], R_PB[4], R_PB[5]])
                    for mc in range(2):
                        E4 = Et[Es[mc]][:].rearrange("p (jj e t) -> p jj e t", jj=2, e=2)
                        for e in range(2):
                            b = 2 + 2 * mc + e
                            T.op("act", f_act(E4[:, :, e, :], PB[b][:, 0:256].rearrange("p (jj t) -> p jj t", jj=2), AF.Exp, scale=0.125),
                                 r=[R_PB[b]], w=[R_E[Es[mc]]])
                    _ckpt(12.15)
                    fns = []
                    for hh in range(4):
                        for mc in range(2):
                            fns.append(f_mm(PB[0][0:64, hh * 128:(hh + 1) * 128], mv[:, mc, hh * 64:(hh + 1) * 64], Et[Es[mc]][:, hh * 128:(hh + 1) * 128], mc == 0, mc == 1))
                    for mc in range(2):
                        fns.append(f_mm(PB[1][0:64, :], ones_b[:], Et[Es[mc]][:], mc == 0, mc == 1))
                    T.op("pe", f_seq(fns), r=[R_E[e_] for e_ in Es] + [R_mv, R_const], w=[R_PB[0], R_PB[1]])
                    _ckpt(12.17)
                    finish_attn(0, 1, j, 12, None)
                    _ckpt(12.2)
                def dsa_index(j):
                    i, tok, nkb, nch, n = slot_vars(j)
                    for c in range(nch):
                        kb0 = 4 * c; nb = min(4, nkb - kb0); w_ = nb * 128
                        cols = slice(kb0 * 128, kb0 * 128 + w_)
                        bb = 4 * (c % 2)
                        T.op("pe", f_seq([f_mm(PB[bb + hh][:, 0:w_], qdiT[64:128, j, hh * 128:(hh + 1) * 128], kDI[64:128, cols]) for hh in range(4)]),
                             r=[R_qdi[j]] + R_kDI[kb0:kb0 + nb], w=[R_PB[bb + hh] for hh in range(4)])
                        for hh in range(4):
                            ri = rot("rl", 2)
                            T.op("act", f_act(rl[ri][:, 0:w_], PB[bb + hh][:, 0:w_], AF.Relu), r=[R_PB[bb + hh]], w=[R_rl[ri]])
                            if hh == 0:
                                T.op("dve", f_ts(BIG[:, cols], rl[ri][:, 0:w_], wiT[:, j, 0:1], None, ALU.mult), r=[R_rl[ri], R_wi[j]], w=[ISC[c]])
                            else:
                                T.op("dve", f_stt(BIG[:, cols], rl[ri][:, 0:w_], wiT[:, j, hh:hh + 1], BIG[:, cols], ALU.mult, ALU.add),
                                     r=[R_rl[ri], R_wi[j], ISC[c]], w=[ISC[c]])
                def dsa_bisect(j):
                    i, tok, nkb, nch, n = slot_vars(j)
                    _ckpt(12.3)
                    IR = ISC[0:nch]; MR = MSKR[0:nch]
                    cd = nch if nch < 2 else max(1, int(round(0.42 * nch)))
                    nd = min(n, cd * 512); na = n - nd
                    IRd, MRd, IRa, MRa = ISC[0:cd], MSKR[0:cd], ISC[cd:nch], MSKR[cd:nch]
                    amax = bis[:, 0:1]; wtot = bis[:, 1:2]; lo = bis[:, 2:3]; mid = bis[:, 3:4]; cn = bis[:, 4:5]; dl = bis[:, 5:6]
                    sa = bis[:, 6:7]; tt_ = bis[:, 7:8]
                    wks = bis[:, 8:8 + NITER]
                    T.op("dve", lambda h, a=amax, b=BIG[:, 0:n]: h.tensor_reduce(out=a, in_=b, axis=AX.X, op=ALU.max, apply_absolute_value=True),
                         r=IR, w=[R_lo])
                    T.op("pool", f_tt(BIG[:, 0:128], BIG[:, 0:128], p63bias, ALU.add), r=[ISC[0], R_cmf, R_lo], w=[ISC[0]])
                    dcol = slice((nkb - 1) * 128, nkb * 128)
                    T.op("pool", f_tt(BIG[:, dcol], BIG[:, dcol], dbias, ALU.add), r=[ISC[nch - 1], R_cmf, R_lo], w=[ISC[nch - 1]])
                    T.op("dve", f_ts(wtot, amax, 2.0002, 2e-6, ALU.mult, ALU.add), r=[R_lo], w=[R_lo])
                    T.op("dve", f_ts(lo, amax, -1.0001, -1e-6, ALU.mult, ALU.add), r=[R_lo], w=[R_lo])
                    T.op("dve", f_ts(wks, pw2[:], wtot, None, ALU.mult), r=[R_lo, R_const], w=[R_lo])
                    thrc = TOPK - na / 2.0
                    for k in range(NITER):
                        T.op("dve", f_tt(mid, lo, wks[:, k:k + 1], ALU.add), r=[R_lo], w=[R_mid])
                        if na > 0:
                            T.op("act", f_act(MSK[:, nd:n], BIG[:, nd:n], AF.Sign, bias=mid, scale=-1.0, accum=sa), r=IRa + [R_mid], w=MRa + [R_ca])
                        T.op("dve", f_ts(MSK[:, 0:nd], BIG[:, 0:nd], mid, 0.0, ALU.is_ge, ALU.add, accum=cn), r=IRd + [R_mid], w=MRd + [R_cd])
                        if na > 0:
                            T.op("dve", f_stt(tt_, sa, -0.5, cn, ALU.mult, ALU.add), r=[R_ca, R_cd], w=[R_t])
                            T.op("dve", f_stt(dl, tt_, thrc, wks[:, k:k + 1], ALU.is_ge, ALU.mult), r=[R_t, R_lo], w=[R_t])
                        else:
                            T.op("dve", f_stt(dl, cn, TOPK, wks[:, k:k + 1], ALU.is_ge, ALU.mult), r=[R_cd, R_lo], w=[R_t])
                        T.op("dve", f_tt(lo, lo, dl, ALU.add), r=[R_t], w=[R_lo])
                    T.op("dve", f_ts(MSK[:, 0:n], BIG[:, 0:n], lo, None, ALU.is_ge), r=IR + [R_lo], w=MR)
                def dsa_attn(j):
                    i, tok, nkb, nch, n = slot_vars(j)
                    _ckpt(12.4)
                    LA = 3
                    st_ = {}
                    T.op("dve", f_cp(qd0[0:64, :], qdiT[0:64, j, :]), r=[R_qdi[j]], w=[R_qd0])
                    def prep(c):
                        kb_ = 4 * c
                        nb = min(4, nkb - kb_)
                        mi = c % 2
                        pv = pbf(2)
                        T.op("pe", f_seq([f_tr(pv[:, q2 * 128:(q2 + 1) * 128], MSK[:, (kb_ + q2) * 128:(kb_ + q2 + 1) * 128], ident[:]) for q2 in range(nb)]),
                             r=[MSKR[c], R_const], w=[R_PB[2]])
                        T.op("act", f_act(mT[mi][:, 0:nb * 128], pv[:, 0:nb * 128], AF.Identity, scale=30000.0, bias=-30000.0), r=[R_PB[2]], w=[R_mT[mi]])
                    prep(0)
                    def front(kb):
                        c, q = kb // 4, kb % 4
                        if q == 0 and c + 1 < nch:
                            prep(c + 1)
                        mi = c % 2
                        b = 3 + (kb % 5)
                        T.op("pe", f_seq([f_mm(PB[b][:, :], kDI[:, kb * 128:(kb + 1) * 128], qd0[:, :], True, False)]
                                         + [f_mm(PB[b][:, hh * 128:(hh + 1) * 128], ident[:], mT[mi][:, q * 128:(q + 1) * 128], False, hh == 3) for hh in range(4)]),
                             r=[R_kDI[kb], R_qd0, R_mT[mi], R_const], w=[R_PB[b]])
                        ei = rot("E", 5)
                        T.op("act", f_act(Et[ei][:], PB[b][:, :], AF.Exp, scale=0.125), r=[R_PB[b]], w=[R_E[ei]])
                        st_[kb] = ei
                    def back(kb):
                        ei = st_.pop(kb)
                        T.op("pe", f_mm(PB[0][:, :], vD[:, kb, :], Et[ei][:], kb == 0, kb == nkb - 1),
                             r=[R_E[ei], R_vD[kb]], w=[R_PB[0]])
                    for t in range(nkb + LA):
                        if t < nkb: front(t)
                        if t - LA >= 0: back(t - LA)
                    T.op("act", f_act(dn[0][64:128, :], PB[0][64:128, :], AF.Copy), r=[R_PB[0]], w=[R_dn[0]])
                    T.op("pe", f_mm(PB[1][0:64, :], identf[:, 64:128], dn[0][:, :]), r=[R_dn[0], R_const], w=[R_PB[1]])
                    finish_attn(0, 1, j, 8, None)

                swa_mem(0)
                for j in range(4):
                    T.mark(f"g{g} s{j} idx")
                    dsa_index(j)
                    T.mark(f"g{g} s{j} bis")
                    dsa_bisect(j)
                    if j < 3:
                        swa_mem(j + 1)
                    T.mark(f"g{g} s{j} dattn")
                    dsa_attn(j)

                _ckpt(10 * (g + 1) + 3)
                T.mark(f"g{g} pass2")
                RQ = R_qs + R_qdi
                for oc in range(8):
                    G, RG = ring_get(3 + oc)
                    Gv = G[:, 0:8 * 384].rearrange("p (k c) -> p k c", k=8)
                    for jb in range(3):
                        T.op("pe", f_seq([f_mm(PB[jb][:, :], Gv[:, kc, jb * 128:(jb + 1) * 128], hTg[:, kc, :], kc == 0, kc == 7) for kc in range(8)]),
                             r=R_hTg + [RG], w=[R_PB[jb]])
                        T.op("act", f_act(gsb[jb][:], PB[jb][:, :], AF.Sigmoid, bias=bgate[:, oc * 3 + jb:oc * 3 + jb + 1], scale=1.0),
                             r=[R_PB[jb], R_misc], w=[R_gsb[jb]])
                    Pw, RP = ring_get(11 + oc)
                    Pv = Pw[0:64, 0:2048].rearrange("p (h c) -> p h c", h=16)
                    for jb, (h0, nh) in enumerate(((0, 8), (8, 4), (12, 4))):
                        T.op("pe", f_seq([f_mm(PB[3 + jb][:, :], Pv[:, h0 + hh, :], oTg[:, h0 + hh, :], hh == 0, hh == nh - 1) for hh in range(nh)]),
                             r=R_oTg + [RP], w=[R_PB[3 + jb]])
                    T.op("dve", f_tt(mg[0][:], PB[3][:, :], gsb[0][:], ALU.mult), r=[R_PB[3], R_gsb[0]], w=[R_mg[0]])
                    T.op("dve", f_tt(mg[1][:], PB[4][:, :], gsb[1][:], ALU.mult), r=[R_PB[4], R_gsb[1]], w=[R_mg[1]])
                    T.op("dve", f_tt(mg[0][:], mg[0][:], mg[1][:], ALU.add), r=[R_mg[0], R_mg[1]], w=[R_mg[0]])
                    T.op("dve", f_tt(mg[1][:], PB[5][:, :], gsb[2][:], ALU.mult), r=[R_PB[5], R_gsb[2]], w=[R_mg[1]])
                    T.op("dve", f_tt(mergedT[:, oc, :], mg[0][:], mg[1][:], ALU.add), r=[R_mg[0], R_mg[1]], w=RQ)

                _ckpt(10 * (g + 1) + 4)
                T.mark(f"g{g} wout")
                for cc in range(2):
                    W, RW = ring_get(19 + cc)
                    Wv = W[:, :].rearrange("p (k c) -> p k c", k=8)
                    for j in range(4):
                        pb = 6 + (j % 2)
                        T.op("pe", f_seq([f_mm(PB[pb][:, :], mergedT[:, kc, j * 128:(j + 1) * 128], Wv[:, kc, :], kc == 0, kc == 7) for kc in range(8)]),
                             r=RQ + [RW], w=[R_PB[pb]])
                        T.op("dve", f_tt(xg[:, j, cc * 512:(cc + 1) * 512], PB[pb][:, :], xg[:, j, cc * 512:(cc + 1) * 512], ALU.add),
                             r=[R_PB[pb], R_xg[j]], w=[R_xg[j]])

                _ckpt(10 * (g + 1) + 5)
                T.mark(f"g{g} mlp")
                for j in range(4):
                    norm_T(xg[:, j, :], [R_xg[j]], hTg[:, :, j * 128:(j + 1) * 128], [R_hTg[j]], j % 2)
                for fb in range(8):
                    W, RW = ring_get(21 + fb)
                    Wv = W[:, :].rearrange("p (k c) -> p k c", k=8)
                    for fs in range(4):
                        fc = fb * 4 + fs
                        pb = 2 + (fc % 4)
                        T.op("pe", f_seq([f_mm(PB[pb][:, :], Wv[:, kc, fs * 128:(fs + 1) * 128], hTg[:, kc, :], kc == 0, kc == 7) for kc in range(8)]),
                             r=R_hTg + [RW], w=[R_PB[pb]])
                        ri = rot("rl", 2)
                        T.op("act", f_act(rl[ri][:], PB[pb][:, :], AF.Relu), r=[R_PB[pb]], w=[R_rl[ri]])
                        T.op("dve", f_tt(hidT[:, fc, :], rl[ri][:], rl[ri][:], ALU.mult), r=[R_rl[ri]], w=[ISC[fc // 2]])
                for cc in range(2):
                    for ks in range(4):
                        W, RW = ring_get(29 + cc * 4 + ks)
                        Wv = W[:, :].rearrange("p (k c) -> p k c", k=8)
                        for j in range(4):
                            pb = 4 + j
                            T.op("pe", f_seq([f_mm(PB[pb][:, :], hidT[:, ks * 8 + fl, j * 128:(j + 1) * 128], Wv[:, fl, :],
                                                   ks == 0 and fl == 0, ks == 3 and fl == 7) for fl in range(8)]),
                                 r=ISC[ks * 4:(ks + 1) * 4] + [RW], w=[R_PB[pb]])
                    for j in range(4):
                        pb = 4 + j
                        T.op("dve", f_tt(xg[:, j, cc * 512:(cc + 1) * 512], PB[pb][:, :], xg[:, j, cc * 512:(cc + 1) * 512], ALU.add),
                             r=[R_PB[pb], R_xg[j]], w=[R_xg[j]])
                _ckpt(10 * (g + 1) + 6)
                T.mark(f"g{g} final")
                for j in range(4):
                    i = 4 * g + j
                    si = rot("stat", 4); s = stat[:, si * 4:(si + 1) * 4]; rs = R_stat[si]
                    hi = rot("hn", 1)
                    T.op("act", f_act(hn[hi][:], xg[:, j, :], AF.Square, accum=s[:, 0:1]), r=[R_xg[j]], w=[R_hn[hi], rs])
                    T.op("act", f_act(s[:, 1:2], s[:, 0:1], AF.Ln, scale=1.0 / 1024.0, bias=1e-6), r=[rs], w=[rs])
                    T.op("act", f_act(s[:, 2:3], s[:, 1:2], AF.Exp, scale=-0.5), r=[rs], w=[rs])
                    T.op("dve", f_stt(xg[:, j, :], xg[:, j, :], s[:, 2:3], gfin[:], ALU.mult, ALU.mult), r=[R_xg[j], rs, R_misc], w=[R_xg[j]])
                    T.dma("sp", out_d[i * 128:(i + 1) * 128, :], xg[:, j, :], R_xg[j], r=[R_xg[j]])
        try:
            emit()
        except _Stop:
            T.dma("sp", out_d[0:128, :], xo[:], R_rl[0], r=R_rl)
            T.wait_all("sp", R_rl + R_ring)
            T.wait_all("pool", [MSKR[0], MSKR[8], ISC[0], ISC[8]])
        T.wait_all("sp", R_xg)
        with nc.Block() as block:
            T.replay(block)
    return nc


OFF = dict(q_s=0, k_s=512, v_s=640, q_d=768, k_d=1024, v_d=1088, q_i=1152, k_i=1408, w_i=1472, q_m=1476, gates=1732)


def _chunked(w, cols):
    sub = w[:, cols]
    n = sub.shape[1]
    return np.ascontiguousarray(sub.reshape(8, 128, n).transpose(1, 0, 2).reshape(128, 8 * n))


def _prep_weights(w_in, w_proj_swa, w_proj_dsa, w_proj_mem, w_out, w_mlp_in, w_mlp_out):
    wch = np.zeros((NCHUNK, 128, 4096), np.float32)
    r64 = np.arange(64)
    qs_cols = np.concatenate([OFF["q_s"] + h * 64 + r64 for h in (0, 4, 1, 5, 2, 6, 3, 7)])
    qdi_cols = np.concatenate([np.concatenate([OFF["q_d"] + h * 64 + r64, OFF["q_i"] + h * 64 + r64]) for h in range(4)])
    qm_cols = np.concatenate([OFF["q_m"] + np.arange(256), OFF["w_i"] + np.arange(4)])
    wch[0] = _chunked(w_in, qs_cols)
    wch[1] = _chunked(w_in, qdi_cols)
    wch[2, :, 0:8 * 260] = _chunked(w_in, qm_cols)
    for oc in range(8):
        gc = np.concatenate([OFF["gates"] + jb * 1024 + oc * 128 + np.arange(128) for jb in range(3)])
        wch[3 + oc, :, 0:8 * 384] = _chunked(w_in, gc)
        wp = np.concatenate([w_proj_swa.reshape(8, 64, 1024), w_proj_dsa.reshape(4, 64, 1024), w_proj_mem.reshape(4, 64, 1024)], 0)
        wch[11 + oc, 0:64, 0:2048] = wp[:, :, oc * 128:(oc + 1) * 128].transpose(1, 0, 2).reshape(64, 2048)
    for cc in range(2):
        wch[19 + cc] = _chunked(w_out, cc * 512 + np.arange(512))
    for fb in range(8):
        wch[21 + fb] = _chunked(w_mlp_in, fb * 512 + np.arange(512))
    for cc in range(2):
        for ks in range(4):
            blk = w_mlp_out[ks * 1024:(ks + 1) * 1024, cc * 512:(cc + 1) * 512]
            wch[29 + cc * 4 + ks] = blk.reshape(8, 128, 512).transpose(1, 0, 2).reshape(128, 4096)
    k_cols = np.concatenate([OFF["k_s"] + np.arange(128), OFF["k_d"] + r64, OFF["k_i"] + r64, OFF["v_s"] + np.arange(128), OFF["v_d"] + r64])
    wk = _chunked(w_in, k_cols)
    return wch, wk


_NC_CACHE = {}


def kernel(x, mem, positions, g_mix, w_in, b_gate, sinks, g_mem, w_mem_kv, w_proj_swa, w_proj_dsa,
           w_proj_mem, w_out, g_mlp, w_mlp_in, w_mlp_out, g_final):
    f = lambda a: np.asarray(a, dtype=np.float32)
    x = f(x); mem = f(mem); positions = np.asarray(positions, dtype=np.int32)
    wch, wk = _prep_weights(f(w_in)[0], f(w_proj_swa)[0], f(w_proj_dsa)[0], f(w_proj_mem)[0], f(w_out)[0], f(w_mlp_in)[0], f(w_mlp_out)[0])
    wmem = _chunked(f(w_mem_kv)[0], np.arange(512))
    tr = lambda v: np.ascontiguousarray(f(v).reshape(8, 128).T)
    gvec = np.concatenate([tr(g_mix[0]), tr(g_mlp[0]), tr(g_mem[0])], 1)
    gfin = np.ascontiguousarray(np.broadcast_to(f(g_final)[None, :], (128, 1024)))
    bg = f(b_gate)[0].reshape(3, 8, 128)
    bgate = np.ascontiguousarray(bg.transpose(2, 1, 0).reshape(128, 24))
    sinkr = np.ascontiguousarray(np.broadcast_to(f(sinks)[0][None, :], (64, 8)))
    half = 32
    invf = np.power(np.float32(10000.0), -np.arange(half, dtype=np.float32) / np.float32(half)).astype(np.float32)
    invf = np.ascontiguousarray(np.broadcast_to(invf[None, :], (128, 32)))
    s_ = np.arange(128)[:, None]; t_ = np.arange(128)[None, :]
    tri_cur = (t_ >= s_).astype(np.float32)
    tri_prev = (s_ > t_).astype(np.float32)
    dbias = np.where(t_ <= s_, 0.0, NEG).astype(np.float32)
    in_maps = []
    for c in range(8):
        b, par = c // 2, c % 2
        order = np.arange(64) if par == 0 else np.concatenate([np.arange(1, 64), [0]])
        xb = x[b].reshape(64, 128, 1024)[order].reshape(64 * 128, 1024)
        pb = positions[b].reshape(64, 128)[order]
        posc = np.ascontiguousarray(pb.T)
        prev0 = tri_prev * float(par)
        p63 = np.full((128, 128), 0.0 if par == 1 else NEG, np.float32)
        cmask = np.ascontiguousarray(np.stack([tri_cur, tri_prev, prev0, dbias, p63], 1).reshape(128, 5 * 128).astype(np.float32))
        in_maps.append(dict(xc=np.ascontiguousarray(xb), posc=posc, memc=np.ascontiguousarray(mem[b]), cmask=cmask, invf=invf,
                            wk=wk, wch=wch, wmem=wmem, gvec=gvec, gfin=gfin, bgate=bgate, sinkr=sinkr))
    if "nc" not in _NC_CACHE:
        _NC_CACHE["nc"] = build_program()
    nc = _NC_CACHE["nc"]
    res = run_bass_kernel_spmd(nc, in_maps, core_ids=list(range(8)))
    out = np.zeros((4, 64, 128, 1024), np.float32)
    for c in range(8):
        b, par = c // 2, c % 2
        o = np.asarray(res.results[c]["out"]).reshape(32, 128, 1024)
        out[b, par::2] = o
    return out.reshape(4, 8192, 1024)
```

```python
import math
import numpy as np
from contextlib import ExitStack
import concourse.bass as bass
import concourse.mybir as mybir
from concourse.bass_utils import run_bass_kernel_spmd

F32 = mybir.dt.float32; BF16 = mybir.dt.bfloat16; I32 = mybir.dt.int32
ALU = mybir.AluOpType; AF = mybir.ActivationFunctionType; AX = mybir.AxisListType

STAGE = 99
class _Stop(Exception):
    pass
def _ckpt(n):
    if STAGE <= n:
        raise _Stop()
NITER = 14
NEG = -1.0e30
NCHUNK = 37
TOPK = 256.0


class Reg:
    __slots__ = ("name", "w", "r", "dsem", "dcnt")
    def __init__(self, name):
        self.name = name; self.w = None; self.r = {}; self.dsem = None; self.dcnt = 0


class Eng:
    def __init__(self, name):
        self.name = name; self.q = []; self.sem = None; self.cnt = 0; self.seen = {}


class Trk:
    def __init__(self, nc, stack, nsem):
        self.nc = nc
        self.sems = [stack.enter_context(nc.semaphore(f"s{i}")) for i in range(nsem)]
        self.si = 0
        self.E = {n: Eng(n) for n in ("pe", "act", "dve", "pool", "sp")}
        for e in self.E.values():
            e.sem = self.newsem()
    def newsem(self):
        s = self.sems[self.si]; self.si += 1; return s
    def _deps(self, eng, reads, writes):
        deps = {}
        def add(ev, kind):
            if ev is None: return
            sem, val, src = ev
            if src is eng and eng.name == "pe":
                return
            k = id(sem)
            if eng.seen.get(k, 0) >= val: return
            if k not in deps or deps[k][1] < val: deps[k] = (sem, val)
        for r in reads: add(r.w, "raw")
        for w in writes:
            add(w.w, "waw")
            for ev in w.r.values(): add(ev, "war")
        out = list(deps.values())
        for sem, val in out: eng.seen[id(sem)] = val
        return out
    def op(self, en, fn, r=(), w=()):
        eng = self.E[en]
        waits = self._deps(eng, r, w)
        eng.cnt += 1
        ev = (eng.sem, eng.cnt, eng)
        eng.q.append((waits, fn, (eng.sem, 1)))
        for x in r: x.r[en] = ev
        for x in w: x.w = ev; x.r = {}
    def dma(self, en, out_ap, in_ap, slot, r=(), w=()):
        eng = self.E[en]
        waits = self._deps(eng, r, w)
        if slot.dsem is None: slot.dsem = self.newsem()
        slot.dcnt += 16
        ev = (slot.dsem, slot.dcnt, None)
        eng.q.append((waits, lambda h: h.dma_start(out=out_ap, in_=in_ap), (slot.dsem, 16)))
        for x in r: x.r["dma%d" % id(slot)] = ev
        for x in w: x.w = ev; x.r = {}
    def mark(self, label):
        self.E["pe"].q.append(([], None, label))
    def wait_all(self, en, regs):
        eng = self.E[en]
        waits = self._deps(eng, [], regs)
        eng.q.append((waits, None, None))
    def replay(self, block):
        def mk(en):
            q = self.E[en].q
            def body(h):
                for waits, fn, inc in q:
                    for sem, val in waits: h.wait_ge(sem, val)
                    if fn is None:
                        if isinstance(inc, str): MARKS.append((inc, PE_CNT[0]))
                        continue
                    ins = fn(h)
                    ins.then_inc(inc[0], inc[1])
            return body
        block.tensor(mk("pe")); block.scalar(mk("act")); block.vector(mk("dve"))
        block.gpsimd(mk("pool")); block.sync(mk("sp"))


PE_CNT = [0]
MARKS = []
def f_mm(out, lhsT, rhs, start=True, stop=True):
    def f(h):
        PE_CNT[0] += 1
        return h.matmul(out, lhsT=lhsT, rhs=rhs, start=start, stop=stop)
    return f
def f_tr(out, in_, ident):
    def f(h):
        PE_CNT[0] += 1
        return h.transpose(out=out, in_=in_, identity=ident)
    return f
def f_seq(fns):
    def f(h):
        ins = None
        for fn in fns: ins = fn(h)
        return ins
    return f
def f_act(out, in_, func, bias=None, scale=None, accum=None):
    kw = {}
    if bias is not None: kw["bias"] = bias
    if scale is not None: kw["scale"] = scale
    if accum is not None: kw["accum_out"] = accum
    return lambda h: h.activation(out=out, in_=in_, func=func, **kw)
def f_ts(out, in0, s1, s2=None, op0=ALU.mult, op1=None, accum=None):
    kw = {}
    if op1 is not None: kw["op1"] = op1
    if accum is not None: kw["accum_out"] = accum
    return lambda h: h.tensor_scalar(out=out, in0=in0, scalar1=s1, scalar2=s2, op0=op0, **kw)
def f_tt(out, in0, in1, op):
    return lambda h: h.tensor_tensor(out=out, in0=in0, in1=in1, op=op)
def f_stt(out, in0, scalar, in1, op0, op1):
    return lambda h: h.scalar_tensor_tensor(out=out, in0=in0, scalar=scalar, in1=in1, op0=op0, op1=op1)
def f_cp(out, in_):
    return lambda h: h.tensor_copy(out=out, in_=in_)
def f_memset(ap, v):
    return lambda h: h.memset(ap, v)


def build_program():
    nc = bass.Bass("TRN2", target_bir_lowering=False)
    dt_in = lambda n, s, d=F32: nc.dram_tensor(n, s, d, kind="ExternalInput").ap()
    xc = dt_in("xc", [64 * 128, 1024])
    posc = dt_in("posc", [128, 64], I32)
    memc = dt_in("memc", [256, 1024])
    cmask = dt_in("cmask", [128, 5 * 128])
    invf_d = dt_in("invf", [128, 32])
    wk_d = dt_in("wk", [128, 8 * 448])
    wch_d = dt_in("wch", [NCHUNK, 128, 4096])
    wmem_d = dt_in("wmem", [128, 4096])
    gvec_d = dt_in("gvec", [128, 24])
    gfin_d = dt_in("gfin", [128, 1024])
    bgate_d = dt_in("bgate", [128, 24])
    sinkr_d = dt_in("sinkr", [64, 8])
    out_d = nc.dram_tensor("out", [32 * 128, 1024], F32, kind="ExternalOutput").ap()
    wbf_d = nc.dram_tensor("wbf", [NCHUNK, 128, 4096], BF16, kind="Internal").ap()

    with ExitStack() as st:
        T = Trk(nc, st, 48)
        def sb(name, shape, dt):
            return st.enter_context(nc.sbuf_tensor(name, shape, dt))
        BIG = sb("BIG", [128, 8192], F32)
        MSK = sb("MSK", [128, 8192], BF16)
        ISC = [Reg(f"isc{c}") for c in range(16)]
        MSKR = [Reg(f"msk{c}") for c in range(16)]
        hidT = BIG[:].bitcast(BF16).rearrange("p (f t) -> p f t", t=512)
        wk_sb = sb("wk_sb", [128, 8, 448], BF16); R_wk = Reg("wk")
        kDI = sb("kDI", [128, 8192], BF16); R_kDI = [Reg(f"kDI{i}") for i in range(64)]
        vD = sb("vD", [128, 64, 128], BF16); R_vD = [Reg(f"vD{i}") for i in range(64)]
        kST = sb("kST", [128, 16, 128], BF16); R_kST = [Reg(f"kST{i}") for i in range(16)]
        vS = sb("vS", [128, 16, 128], BF16); R_vS = [Reg(f"vS{i}") for i in range(16)]
        mkT = sb("mkT", [128, 2, 256], BF16); R_mkT = Reg("mkT")
        mv = sb("mv", [128, 2, 256], BF16); R_mv = Reg("mv")
        xg = sb("xg", [128, 4, 1024], F32); R_xg = [Reg(f"xg{j}") for j in range(4)]
        hTg = sb("hTg", [128, 8, 512], BF16); R_hTg = [Reg(f"hTg{j}") for j in range(4)]
        hTo = sb("hTo", [128, 8, 128], BF16); R_hTo = Reg("hTo")
        oTg = sb("oTg", [64, 16, 512], BF16); R_oTg = [Reg(f"oTg{j}") for j in range(4)]
        QM = sb("QM", [128, 4096], BF16)
        qsT = QM[:, 0:2048].rearrange("p (s c) -> p s c", s=4)
        qdiT = QM[:, 2048:4096].rearrange("p (s c) -> p s c", s=4)
        mergedT = QM[:].rearrange("p (k t) -> p k t", k=8)
        R_qs = [Reg(f"qs{j}") for j in range(4)]; R_qdi = [Reg(f"qdi{j}") for j in range(4)]
        qmT = sb("qmT", [128, 4, 256], BF16); R_qm = [Reg(f"qm{j}") for j in range(4)]
        wiT = sb("wiT", [128, 4, 4], F32); R_wi = [Reg(f"wi{j}") for j in range(4)]
        NRING = 2
        ring = [sb(f"ring{i}", [128, 4096], BF16) for i in range(NRING)]
        R_ring = [Reg(f"ring{i}") for i in range(NRING)]
        ident = sb("ident", [128, 128], BF16); identf = sb("identf", [128, 128], F32); onesf = sb("onesf", [128, 128], F32)
        ones_b = sb("ones_b", [128, 64], BF16)
        qd0 = sb("qd0", [128, 512], BF16); R_qd0 = Reg("qd0")
        R_const = Reg("const")
        cm_f = sb("cm_f", [128, 5, 128], F32)
        cm_b = sb("cm_b", [128, 3, 128], BF16)
        invf = sb("invf_sb", [128, 32], F32)
        gvec = sb("gvec_sb", [128, 24], F32)
        gfin = sb("gfin_sb", [128, 1024], F32)
        bgate = sb("bgate_sb", [128, 24], F32)
        es = sb("es_sb", [64, 8], F32)
        pw2 = sb("pw2", [128, NITER], F32)
        posi = sb("posi", [128, 64], I32); posf = sb("posf", [128, 64], F32)
        cosT = sb("cosT", [128, 8, 32], F32); sinT = sb("sinT", [128, 8, 32], F32); R_cs = Reg("cs")
        rtmp = [sb(f"rtmp{i}", [128, 256], F32) for i in range(3)]; rtmpi = sb("rtmpi", [128, 256], I32); R_rt = Reg("rt")
        stat = sb("stat", [128, 16], F32); R_stat = [Reg(f"stat{i}") for i in range(4)]
        hn = [sb(f"hn{i}", [128, 1024], BF16) for i in range(1)]; R_hn = [Reg(f"hn{i}") for i in range(1)]
        tok_b = [sb(f"tok_b{i}", [128, 512], BF16) for i in range(2)]; R_tok = [Reg(f"tok{i}") for i in range(2)]
        Et = [sb(f"Et{i}", [128, 512], BF16) for i in range(5)]; R_E = [Reg(f"E{i}") for i in range(5)]
        Pt = [sb(f"Pt{i}", [128, 512], BF16) for i in range(2)]; R_P = [Reg(f"P{i}") for i in range(2)]
        RLX = sb("RLX", [128, 1024], F32); rl = [RLX[:, 0:512], RLX[:, 512:1024]]; R_rl = [Reg(f"rl{i}") for i in range(2)]
        xo = RLX; R_xo = R_rl[0]
        mT = [sb(f"mT{i}", [128, 512], BF16) for i in range(2)]; R_mT = [Reg(f"mT{i}") for i in range(2)]
        dn = [sb(f"dn{i}", [128, 512], F32) for i in range(1)]; R_dn = [Reg(f"dn{i}") for i in range(1)]
        mg = rl; R_mg = R_rl
        RTU = sb("RTU", [128, 1024], F32); rope_t = RTU[:, 0:512]; rope_u = RTU[:, 512:1024]; R_rtu = [Reg("rt_"), Reg("ru_")]
        gsb = [sb(f"gsb{i}", [128, 512], BF16) for i in range(3)]; R_gsb = [Reg(f"gsb{i}") for i in range(3)]
        bis = sb("bis", [128, 8 + NITER], F32); R_lo = Reg("lo"); R_mid = Reg("mid"); R_cd = Reg("cd"); R_ca = Reg("ca"); R_t = Reg("t")
        PB = [st.enter_context(nc.psum_tensor(f"pb{i}", [128, 512], F32)) for i in range(8)]
        R_PB = [Reg(f"pb{i}") for i in range(8)]
        def pbf(i):
            return PB[i][:].bitcast(BF16)

        cnt = {"ring": 0, "stat": 0, "hn": 0, "tok": 0, "E": 0, "P": 0, "rl": 0, "mT": 0, "dn": 0, "mg": 0}
        def rot(name, n):
            i = cnt[name] % n; cnt[name] += 1; return i

        R_cmf = Reg("cmf"); R_misc = Reg("misc")
        T.dma("sp", cm_f[:].rearrange("p a b -> p (a b)"), cmask[:, :], R_cmf, w=[R_cmf])
        for (dst, src) in ((invf, invf_d), (gvec, gvec_d), (gfin, gfin_d), (bgate, bgate_d)):
            T.dma("sp", dst[:], src[:, :], R_misc, w=[R_misc])
            T.wait_all("sp", [R_misc])
        R_es = Reg("es")
        T.dma("sp", es[:], sinkr_d[:, :], R_es, w=[R_es])
        R_pos = Reg("pos")
        T.dma("sp", posi[:], posc[:, :], R_pos, w=[R_pos])
        T.op("pool", f_memset(onesf[:], 1.0), w=[R_const])
        T.op("pool", f_memset(identf[:], 0.0), w=[R_const])
        T.op("pool", lambda h: h.affine_select(out=identf[:], in_=onesf[:], pattern=[[-1, 128]], compare_op=ALU.is_equal,
                                                fill=0.0, base=0, channel_multiplier=1), r=[R_const], w=[R_const])
        T.op("dve", f_cp(ident[:], identf[:]), r=[R_const], w=[R_const])
        T.op("dve", f_memset(ones_b[:], 1.0), w=[R_const])
        T.op("pool", f_memset(vD[:].rearrange("p a b -> p (a b)"), 1.0), w=R_vD)
        T.op("dve", f_memset(dn[0][:], 0.0), w=[R_dn[0]])
        T.op("dve", f_memset(qd0[:], 0.0), w=[R_qd0])
        for k in range(NITER):
            T.op("dve", f_memset(pw2[:, k:k + 1], 2.0 ** (-(k + 1))), w=[R_const])
        T.op("dve", f_cp(cm_b[:], cm_f[:, 0:3, :]), r=[R_cmf], w=[R_const])
        T.op("act", f_act(es[:], es[:], AF.Exp), r=[R_es], w=[R_es])
        T.op("dve", f_cp(posf[:], posi[:]), r=[R_pos], w=[R_pos])
        tri_cur = cm_b[:, 0, :]; tri_prev = cm_b[:, 1, :]; prev0 = cm_b[:, 2, :]
        dbias = cm_f[:, 3, :]; p63bias = cm_f[:, 4, :]

        def emit():
            def norm_T(x_ap, x_regs, dstT, dst_regs, pbank):
                si = rot("stat", 4); s = stat[:, si * 4:(si + 1) * 4]; rs = R_stat[si]
                hi = rot("hn", 1); h_ = hn[hi]; rh = R_hn[hi]
                T.op("act", f_act(h_[:], x_ap, AF.Square, accum=s[:, 0:1]), r=x_regs, w=[rh, rs])
                T.op("act", f_act(s[:, 1:2], s[:, 0:1], AF.Ln, scale=1.0 / 1024.0, bias=1e-6), r=[rs], w=[rs])
                T.op("act", f_act(s[:, 2:3], s[:, 1:2], AF.Exp, scale=-0.5), r=[rs], w=[rs])
                T.op("dve", f_ts(h_[:], x_ap, s[:, 2:3], None, ALU.mult), r=x_regs + [rs], w=[rh])
                pv = pbf(pbank)
                T.op("pe", f_seq([f_tr(pv[:, kc * 128:(kc + 1) * 128], h_[:, kc * 128:(kc + 1) * 128], ident[:]) for kc in range(8)]),
                     r=[rh, R_const], w=[R_PB[pbank]])
                T.op("act", f_act(dstT, pv[:, :].rearrange("p (k t) -> p k t", k=8), AF.Copy), r=[R_PB[pbank]], w=dst_regs)
                return s, rs

            def rope_tables(blk0, nb):
                n = nb * 32
                a0 = rtmp[0][:, 0:n]; a1 = rtmp[1][:, 0:n]; a2 = rtmp[2][:, 0:n]; ai = rtmpi[:, 0:n]
                v3 = lambda a: a.rearrange("p (b f) -> p b f", f=32)
                T.op("dve", f_tt(v3(a0), posf[:, blk0:blk0 + nb].unsqueeze(2).to_broadcast([128, nb, 32]),
                                 invf[:].unsqueeze(1).to_broadcast([128, nb, 32]), ALU.mult), r=[R_pos, R_misc], w=[R_rt])
                T.op("dve", f_ts(ai, a0, 1.0 / (2 * math.pi), None, ALU.mult), r=[R_rt], w=[R_rt])
                T.op("dve", f_cp(a1, ai), r=[R_rt], w=[R_rt])
                T.op("dve", f_stt(a2, a1, -6.28125, a0, ALU.mult, ALU.add), r=[R_rt], w=[R_rt])
                T.op("dve", f_stt(a2, a1, -0.0019353072, a2, ALU.mult, ALU.add), r=[R_rt], w=[R_rt])
                T.op("dve", f_ts(a0, a2, -3.1415925, 3.1415925, ALU.max, ALU.min), r=[R_rt], w=[R_rt])
                T.op("act", f_act(sinT[:, 0:nb, :].rearrange("p b f -> p (b f)"), a0, AF.Sin), r=[R_rt], w=[R_cs])
                T.op("dve", f_ts(a1, a2, math.pi / 2, None, ALU.add), r=[R_rt], w=[R_rt])
                T.op("dve", f_ts(a0, a1, math.pi, -2 * math.pi, ALU.is_gt, ALU.mult), r=[R_rt, R_cs], w=[R_rt])
                T.op("dve", f_tt(a0, a0, a1, ALU.add), r=[R_rt], w=[R_rt])
                T.op("dve", f_ts(a0, a0, -3.1415925, 3.1415925, ALU.max, ALU.min), r=[R_rt], w=[R_rt])
                T.op("act", f_act(cosT[:, 0:nb, :].rearrange("p b f -> p (b f)"), a0, AF.Sin), r=[R_rt], w=[R_cs])

            def rope_apply(src, src_regs, dst, dst_regs, H, cb):
                n = H * 64
                t_ = rope_t[:, 0:n]; u_ = rope_u[:, 0:n]
                cosb = cosT[:, cb:cb + 1, :]; sinb = sinT[:, cb:cb + 1, :]
                T.op("dve", f_tt(t_.rearrange("p (a f) -> p a f", f=32), src.rearrange("p (a f) -> p a f", f=32),
                                 cosb.to_broadcast([128, 2 * H, 32]), ALU.mult), r=src_regs + [R_cs], w=[R_rtu[0]])
                s4 = src.rearrange("p (h e f) -> p h e f", e=2, f=32)
                u4 = u_.rearrange("p (h e f) -> p h e f", e=2, f=32)
                t4 = t_.rearrange("p (h e f) -> p h e f", e=2, f=32)
                d4 = dst.rearrange("p (h e f) -> p h e f", e=2, f=32)
                sb_ = sinb.to_broadcast([128, H, 32])
                T.op("dve", f_tt(u4[:, :, 0, :], s4[:, :, 1, :], sb_, ALU.mult), r=src_regs + [R_cs], w=[R_rtu[1]])
                T.op("dve", f_tt(u4[:, :, 1, :], s4[:, :, 0, :], sb_, ALU.mult), r=src_regs + [R_cs], w=[R_rtu[1]])
                T.op("dve", f_tt(d4[:, :, 0, :], t4[:, :, 0, :], u4[:, :, 0, :], ALU.subtract), r=R_rtu, w=dst_regs)
                T.op("dve", f_tt(d4[:, :, 1, :], t4[:, :, 1, :], u4[:, :, 1, :], ALU.add), r=R_rtu, w=dst_regs)

            R_wbf = [Reg(f"wbf{i}") for i in range(NCHUNK)]
            CH_N = [4096, 4096, 8 * 260] + [8 * 384] * 8 + [2048] * 8 + [4096] * 18
            CH_P = [128] * 11 + [64] * 8 + [128] * 18
            sched = [0, 1, 2] + [v for oc in range(8) for v in (3 + oc, 11 + oc)] + list(range(19, 37))
            ring_state = {"issued": 0, "got": 0}
            total_gets = 8 * NCHUNK
            def ring_issue():
                k = ring_state["issued"]
                if k >= total_gets: return
                c = sched[k % NCHUNK]; b = k % NRING
                T.dma("sp", ring[b][0:CH_P[c], 0:CH_N[c]], wbf_d[c, 0:CH_P[c], 0:CH_N[c]], R_ring[b], r=[R_wbf[c]], w=[R_ring[b]])
                ring_state["issued"] += 1
            def ring_get(expect):
                k = ring_state["got"]
                assert sched[k % NCHUNK] == expect, (k, expect)
                while ring_state["issued"] < min(k + NRING, total_gets):
                    ring_issue()
                ring_state["got"] += 1
                b = k % NRING
                return ring[b], R_ring[b]

            def convert(src_dram_ap, npart, nelem, ncols, goff, dst_ap, dst_regs, half):
                stg = BIG[0:npart, half * 4096: half * 4096 + nelem]
                sregs = ISC[half * 8:(half + 1) * 8]
                T.dma("pool", stg, src_dram_ap, sregs[0], w=sregs)
                if goff is None:
                    T.op("act", f_act(dst_ap, stg, AF.Copy), r=sregs, w=dst_regs)
                else:
                    for kc in range(8):
                        T.op("dve", f_ts(dst_ap[:, kc * ncols:(kc + 1) * ncols], stg[:, kc * ncols:(kc + 1) * ncols],
                                          gvec[:, goff + kc:goff + kc + 1], None, ALU.mult), r=sregs + [R_misc], w=dst_regs)
            _ckpt(1)
            convert(wk_d[:, :], 128, 8 * 448, 448, 0, wk_sb[:].rearrange("p k c -> p (k c)"), [R_wk], 0)
            wmem_b = MSK[:, 4096:8192]
            convert(wmem_d[:, :], 128, 4096, 512, 16, wmem_b, MSKR[8:16], 1)

            for mc in range(2):
                T.dma("sp", xo[:], memc[mc * 128:(mc + 1) * 128, :], R_rl[0], w=R_rl)
                norm_T(xo[:], R_rl, hTo[:], [R_hTo], 0)
                T.op("pe", f_seq([f_mm(PB[1][:, :], hTo[:, kc, :], wmem_b[:, kc * 512:(kc + 1) * 512], kc == 0, kc == 7) for kc in range(8)]),
                     r=[R_hTo] + MSKR[8:16], w=[R_PB[1]])
                ti = rot("tok", 2)
                T.op("act", f_act(tok_b[ti][:, 0:256], PB[1][:, 0:256], AF.Copy), r=[R_PB[1]], w=[R_tok[ti]])
                T.op("act", f_act(mv[:, mc, :], PB[1][:, 256:512], AF.Copy), r=[R_PB[1]], w=[R_mv])
                pv = pbf(2)
                T.op("pe", f_seq([f_tr(pv[:, jj * 128:(jj + 1) * 128], tok_b[ti][:, jj * 128:(jj + 1) * 128], ident[:]) for jj in range(2)]),
                     r=[R_tok[ti], R_const], w=[R_PB[2]])
                T.op("act", f_act(mkT[:, :, mc * 128:(mc + 1) * 128], pv[:, 0:256].rearrange("p (j m) -> p j m", j=2), AF.Copy),
                     r=[R_PB[2]], w=[R_mkT])

            _ckpt(2)
            for c in range(NCHUNK):
                half = c % 2
                goff = 0 if c < 11 else (8 if 21 <= c < 29 else None)
                ncols = {0: 512, 1: 512, 2: 260}.get(c, 384 if c < 11 else 512)
                npart = CH_P[c]; nelem = CH_N[c]
                dstb = MSK[0:npart, half * 4096: half * 4096 + nelem]
                dregs = MSKR[half * 8:(half + 1) * 8]
                convert(wch_d[c, 0:npart, 0:nelem], npart, nelem, ncols, goff, dstb, dregs, half)
                T.dma("pool", wbf_d[c, 0:npart, 0:nelem], dstb, dregs[0], r=dregs, w=[R_wbf[c]])

            def kside(pos, x_ap, x_regs, hT_ap, hT_regs, cb, stage="AB"):
                kidx = 0 if pos == 63 else pos + 1
                rg = (pos + 1) % 16
                if "A" in stage:
                    norm_T(x_ap, x_regs, hT_ap, hT_regs, 0)
                if "B" not in stage:
                    return
                _ckpt(3.1)
                T.op("pe", f_seq([f_mm(PB[1][:, 0:448], hT_ap[:, kc, :], wk_sb[:, kc, :], kc == 0, kc == 7) for kc in range(8)]),
                     r=hT_regs + [R_wk], w=[R_PB[1]])
                _ckpt(3.2)
                ti = rot("tok", 2)
                rope_apply(PB[1][:, 0:256], [R_PB[1]], tok_b[ti][:, 0:256], [R_tok[ti]], 4, cb)
                _ckpt(3.3)
                T.op("dve", f_cp(vS[:, rg, :], PB[1][:, 256:384]), r=[R_PB[1]], w=[R_vS[rg]])
                T.op("dve", f_cp(vD[:, kidx, 0:64], PB[1][:, 384:448]), r=[R_PB[1]], w=[R_vD[kidx]])
                _ckpt(3.4)
                pv = pbf(2)
                T.op("pe", f_seq([f_tr(pv[:, jj * 128:(jj + 1) * 128], tok_b[ti][:, jj * 128:(jj + 1) * 128], ident[:]) for jj in range(2)]),
                     r=[R_tok[ti], R_const], w=[R_PB[2]])
                _ckpt(3.5)
                T.op("act", f_act(kST[:, rg, :], pv[:, 0:128], AF.Copy), r=[R_PB[2]], w=[R_kST[rg]])
                T.op("act", f_act(kDI[:, kidx * 128:(kidx + 1) * 128], pv[:, 128:256], AF.Copy), r=[R_PB[2]], w=[R_kDI[kidx]])

            _ckpt(3)
            rope_tables(63, 1)
            T.dma("sp", xo[:], xc[63 * 128:64 * 128, :], R_rl[0], w=R_rl)
            kside(63, xo[:], R_rl, hTo[:], [R_hTo], 0)

            def finish_attn(bO, bD, j, h0, es_ap):
                di = rot("dn", 1)
                d_ = dn[di][0:64, :]
                if es_ap is not None:
                    T.op("dve", f_tt(d_.rearrange("p (h t) -> p h t", h=4), PB[bD][0:64, :].rearrange("p (h t) -> p h t", h=4), es_ap, ALU.add),
                         r=[R_PB[bD], R_es], w=[R_dn[di]])
                    T.op("act", f_act(d_, d_, AF.Ln), r=[R_dn[di]], w=[R_dn[di]])
                else:
                    T.op("act", f_act(d_, PB[bD][0:64, :], AF.Ln), r=[R_PB[bD]], w=[R_dn[di]])
                T.op("act", f_act(d_, d_, AF.Exp, scale=-1.0), r=[R_dn[di]], w=[R_dn[di]])
                T.op("dve", f_tt(oTg[:, h0:h0 + 4, j * 128:(j + 1) * 128], PB[bO][0:64, :].rearrange("p (h t) -> p h t", h=4),
                                 d_.rearrange("p (h t) -> p h t", h=4), ALU.mult), r=[R_PB[bO], R_dn[di]], w=[R_oTg[j]])

            for g in range(8):
                T.mark(f"g{g} kside")
                npos = 8 if g < 7 else 7
                rope_tables(8 * g, 8)
                def kblock(pl, stage):
                    pos = 8 * g + pl
                    if pl % 2 == 0:
                        j = pl // 2
                        if "A" in stage:
                            T.dma("sp", xg[:, j, :], xc[pos * 128:(pos + 1) * 128, :], R_xg[j], w=[R_xg[j]])
                        kside(pos, xg[:, j, :], [R_xg[j]], hTg[:, :, j * 128:(j + 1) * 128], [R_hTg[j]], pl, stage)
                    else:
                        if "A" in stage:
                            T.dma("sp", xo[:], xc[pos * 128:(pos + 1) * 128, :], R_rl[0], w=R_rl)
                        kside(pos, xo[:], R_rl, hTo[:], [R_hTo], pl, stage)
                kblock(0, "A")
                for pl in range(npos):
                    if pl + 1 < npos:
                        kblock(pl + 1, "A")
                    kblock(pl, "B")

                _ckpt(10 * (g + 1) + 1)
                T.mark(f"g{g} qproj")
                units = [(c, j) for c in range(3) for j in range(4)]
                qW = {}
                def q_mm(u):
                    c, j = units[u]
                    if j == 0:
                        W, RW = ring_get(c)
                        qW[c] = (W, RW)
                    W, RW = qW[c]
                    ncols = (512, 512, 260)[c]
                    Wv = W[:, 0:8 * ncols].rearrange("p (k c) -> p k c", k=8)
                    pb = 3 + (u % 2)
                    T.op("pe", f_seq([f_mm(PB[pb][:, 0:ncols], hTg[:, kc, j * 128:(j + 1) * 128], Wv[:, kc, :], kc == 0, kc == 7) for kc in range(8)]),
                         r=[R_hTg[j], RW], w=[R_PB[pb]])
                def q_post(u):
                    c, j = units[u]
                    pb = 3 + (u % 2); pt = 5 + (u % 2)
                    ti = rot("tok", 2)
                    if c < 2:
                        rope_apply(PB[pb][:, 0:512], [R_PB[pb]], tok_b[ti][:, 0:512], [R_tok[ti]], 8, 2 * j)
                        pv = pbf(pt)
                        T.op("pe", f_seq([f_tr(pv[:, q * 128:(q + 1) * 128], tok_b[ti][:, q * 128:(q + 1) * 128], ident[:]) for q in range(4)]),
                             r=[R_tok[ti], R_const], w=[R_PB[pt]])
                        dst = qsT if c == 0 else qdiT
                        T.op("act", f_act(dst[:, j, :], pv[:, 0:512], AF.Copy), r=[R_PB[pt]], w=[(R_qs if c == 0 else R_qdi)[j]])
                    else:
                        T.op("act", f_act(tok_b[ti][:, 0:256], PB[pb][:, 0:256], AF.Copy), r=[R_PB[pb]], w=[R_tok[ti]])
                        T.op("act", f_act(wiT[:, j, :], PB[pb][:, 256:260], AF.Copy, scale=1.0 / 16.0), r=[R_PB[pb]], w=[R_wi[j]])
                        pv = pbf(pt)
                        T.op("pe", f_seq([f_tr(pv[:, q * 128:(q + 1) * 128], tok_b[ti][:, q * 128:(q + 1) * 128], ident[:]) for q in range(2)]),
                             r=[R_tok[ti], R_const], w=[R_PB[pt]])
                        T.op("act", f_act(qmT[:, j, :], pv[:, 0:256], AF.Copy), r=[R_PB[pt]], w=[R_qm[j]])
                q_mm(0)
                for u in range(len(units)):
                    if u + 1 < len(units):
                        q_mm(u + 1)
                    q_post(u)

                _ckpt(10 * (g + 1) + 2)
                T.mark(f"g{g} attn")
                def slot_vars(j):
                    i = 4 * g + j
                    nkb = 2 * i + 2
                    return i, slice(j * 128, (j + 1) * 128), nkb, (nkb + 3) // 4, nkb * 128
                def swa_mem(j):
                    i, tok, nkb, nch, n = slot_vars(j)
                    rp = (2 * i) % 16; rc = (2 * i + 1) % 16
                    for kv in range(2):
                        pr = slice(kv * 64, (kv + 1) * 64)
                        bS = (2, 3) if kv == 0 else (4, 5)
                        bO, bD = (0, 1) if kv == 0 else (6, 7)
                        Ps = []
                        for which, rr_ in enumerate((rp, rc)):
                            b = bS[which]
                            T.op("pe", f_mm(PB[b][:, :], kST[pr, rr_, :], qsT[pr, j, :]), r=[R_kST[rr_], R_qs[j]], w=[R_PB[b]])
                            ei = rot("E", 5)
                            T.op("act", f_act(Et[ei][:], PB[b][:, :], AF.Exp, scale=0.125), r=[R_PB[b]], w=[R_E[ei]])
                            pi_ = rot("P", 2)
                            msk = (prev0 if i == 0 else tri_prev) if which == 0 else tri_cur
                            T.op("dve", f_tt(Pt[pi_][:].rearrange("p (h t) -> p h t", h=4), Et[ei][:].rearrange("p (h t) -> p h t", h=4),
                                             msk.unsqueeze(1).to_broadcast([128, 4, 128]), ALU.mult), r=[R_E[ei], R_const], w=[R_P[pi_]])
                            Ps.append((pi_, rr_))
                        T.op("pe", f_seq([f_mm(PB[bO][0:64, :], vS[:, rr_, pr], Pt[pi_][:], w_ == 0, w_ == 1) for w_, (pi_, rr_) in enumerate(Ps)]
                                         + [f_mm(PB[bD][0:64, :], ones_b[:], Pt[pi_][:], w_ == 0, w_ == 1) for w_, (pi_, rr_) in enumerate(Ps)]),
                             r=[R_P[p_] for p_, _ in Ps] + [R_vS[r_] for _, r_ in Ps] + [R_const], w=[R_PB[bO], R_PB[bD]])
                        finish_attn(bO, bD, j, 4 * kv, es[:, 4 * kv:4 * kv + 4].unsqueeze(2).to_broadcast([64, 4, 128]))
                    _ckpt(12.1)
                    Es = [rot("E", 5), rot("E", 5)]
                    fns = []
                    for e in range(2):
                        pr = slice(e * 64, (e + 1) * 64)
                        for mc in range(2):
                            for jj in range(2):
                                fns.append(f_mm(PB[2 + 2 * mc + e][:, jj * 128:(jj + 1) * 128], mkT[pr, jj, mc * 128:(mc + 1) * 128],
                                                qmT[pr, j, jj * 128:(jj + 1) * 128]))
                    T.op("pe", f_seq(fns), r=[R_mkT, R_qm[j]], w=[R_PB[2], R_PB[3], R_PB[4], R_PB[5]])
                    for mc in range(2):
                        E4 = Et[Es[mc]][:].rearrange("p (jj e t) -> p jj e t", jj=2, e=2)
                        for e in range(2):
                            b = 2 + 2 * mc + e
                            T.op("act", f_act(E4[:, :, e, :], PB[b][:, 0:256].rearrange("p (jj t) -> p jj t", jj=2), AF.Exp, scale=0.125),
                                 r=[R_PB[b]], w=[R_E[Es[mc]]])
                    _ckpt(12.15)
                    fns = []
                    for hh in range(4):
                        for mc in range(2):
                            fns.append(f_mm(PB[0][0:64, hh * 128:(hh + 1) * 128], mv[:, mc, hh * 64:(hh + 1) * 64], Et[Es[mc]][:, hh * 128:(hh + 1) * 128], mc == 0, mc == 1))
                    for mc in range(2):
                        fns.append(f_mm(PB[1][0:64, :], ones_b[:], Et[Es[mc]][:], mc == 0, mc == 1))
                    T.op("pe", f_seq(fns), r=[R_E[e_] for e_ in Es] + [R_mv, R_const], w=[R_PB[0], R_PB[1]])
                    _ckpt(12.17)
                    finish_attn(0, 1, j, 12, None)
                    _ckpt(12.2)
                def dsa_index(j):
                    i, tok, nkb, nch, n = slot_vars(j)
                    for c in range(nch):
                        kb0 = 4 * c; nb = min(4, nkb - kb0); w_ = nb * 128
                        cols = slice(kb0 * 128, kb0 * 128 + w_)
                        bb = 4 * (c % 2)
                        T.op("pe", f_seq([f_mm(PB[bb + hh][:, 0:w_], qdiT[64:128, j, hh * 128:(hh + 1) * 128], kDI[64:128, cols]) for hh in range(4)]),
                             r=[R_qdi[j]] + R_kDI[kb0:kb0 + nb], w=[R_PB[bb + hh] for hh in range(4)])
                        for hh in range(4):
                            ri = rot("rl", 2)
                            T.op("act", f_act(rl[ri][:, 0:w_], PB[bb + hh][:, 0:w_], AF.Relu), r=[R_PB[bb + hh]], w=[R_rl[ri]])
                            if hh == 0:
                                T.op("dve", f_ts(BIG[:, cols], rl[ri][:, 0:w_], wiT[:, j, 0:1], None, ALU.mult), r=[R_rl[ri], R_wi[j]], w=[ISC[c]])
                            else:
                                T.op("dve", f_stt(BIG[:, cols], rl[ri][:, 0:w_], wiT[:, j, hh:hh + 1], BIG[:, cols], ALU.mult, ALU.add),
                                     r=[R_rl[ri], R_wi[j], ISC[c]], w=[ISC[c]])
                def dsa_bisect(j):
                    i, tok, nkb, nch, n = slot_vars(j)
                    _ckpt(12.3)
                    IR = ISC[0:nch]; MR = MSKR[0:nch]
                    cd = nch if nch < 2 else max(1, int(round(0.42 * nch)))
                    nd = min(n, cd * 512); na = n - nd
                    IRd, MRd, IRa, MRa = ISC[0:cd], MSKR[0:cd], ISC[cd:nch], MSKR[cd:nch]
                    amax = bis[:, 0:1]; wtot = bis[:, 1:2]; lo = bis[:, 2:3]; mid = bis[:, 3:4]; cn = bis[:, 4:5]; dl = bis[:, 5:6]
                    sa = bis[:, 6:7]; tt_ = bis[:, 7:8]
                    wks = bis[:, 8:8 + NITER]
                    T.op("dve", lambda h, a=amax, b=BIG[:, 0:n]: h.tensor_reduce(out=a, in_=b, axis=AX.X, op=ALU.max, apply_absolute_value=True),
                         r=IR, w=[R_lo])
                    T.op("pool", f_tt(BIG[:, 0:128], BIG[:, 0:128], p63bias, ALU.add), r=[ISC[0], R_cmf, R_lo], w=[ISC[0]])
                    dcol = slice((nkb - 1) * 128, nkb * 128)
                    T.op("pool", f_tt(BIG[:, dcol], BIG[:, dcol], dbias, ALU.add), r=[ISC[nch - 1], R_cmf, R_lo], w=[ISC[nch - 1]])
                    T.op("dve", f_ts(wtot, amax, 2.0002, 2e-6, ALU.mult, ALU.add), r=[R_lo], w=[R_lo])
                    T.op("dve", f_ts(lo, amax, -1.0001, -1e-6, ALU.mult, ALU.add), r=[R_lo], w=[R_lo])
                    T.op("dve", f_ts(wks, pw2[:], wtot, None, ALU.mult), r=[R_lo, R_const], w=[R_lo])
                    thrc = TOPK - na / 2.0
                    for k in range(NITER):
                        T.op("dve", f_tt(mid, lo, wks[:, k:k + 1], ALU.add), r=[R_lo], w=[R_mid])
                        if na > 0:
                            T.op("act", f_act(MSK[:, nd:n], BIG[:, nd:n], AF.Sign, bias=mid, scale=-1.0, accum=sa), r=IRa + [R_mid], w=MRa + [R_ca])
                        T.op("dve", f_ts(MSK[:, 0:nd], BIG[:, 0:nd], mid, 0.0, ALU.is_ge, ALU.add, accum=cn), r=IRd + [R_mid], w=MRd + [R_cd])
                        if na > 0:
                            T.op("dve", f_stt(tt_, sa, -0.5, cn, ALU.mult, ALU.add), r=[R_ca, R_cd], w=[R_t])
                            T.op("dve", f_stt(dl, tt_, thrc, wks[:, k:k + 1], ALU.is_ge, ALU.mult), r=[R_t, R_lo], w=[R_t])
                        else:
                            T.op("dve", f_stt(dl, cn, TOPK, wks[:, k:k + 1], ALU.is_ge, ALU.mult), r=[R_cd, R_lo], w=[R_t])
                        T.op("dve", f_tt(lo, lo, dl, ALU.add), r=[R_t], w=[R_lo])
                    T.op("dve", f_ts(MSK[:, 0:n], BIG[:, 0:n], lo, None, ALU.is_ge), r=IR + [R_lo], w=MR)
                def dsa_attn(j):
                    i, tok, nkb, nch, n = slot_vars(j)
                    _ckpt(12.4)
                    LA = 3
                    st_ = {}
                    T.op("dve", f_cp(qd0[0:64, :], qdiT[0:64, j, :]), r=[R_qdi[j]], w=[R_qd0])
                    def prep(c):
                        kb_ = 4 * c
                        nb = min(4, nkb - kb_)
                        mi = c % 2
                        pv = pbf(2)
                        T.op("pe", f_seq([f_tr(pv[:, q2 * 128:(q2 + 1) * 128], MSK[:, (kb_ + q2) * 128:(kb_ + q2 + 1) * 128], ident[:]) for q2 in range(nb)]),
                             r=[MSKR[c], R_const], w=[R_PB[2]])
                        T.op("act", f_act(mT[mi][:, 0:nb * 128], pv[:, 0:nb * 128], AF.Identity, scale=30000.0, bias=-30000.0), r=[R_PB[2]], w=[R_mT[mi]])
                    prep(0)
                    def front(kb):
                        c, q = kb // 4, kb % 4
                        if q == 0 and c + 1 < nch:
                            prep(c + 1)
                        mi = c % 2
                        b = 3 + (kb % 5)
                        T.op("pe", f_seq([f_mm(PB[b][:, :], kDI[:, kb * 128:(kb + 1) * 128], qd0[:, :], True, False)]
                                         + [f_mm(PB[b][:, hh * 128:(hh + 1) * 128], ident[:], mT[mi][:, q * 128:(q + 1) * 128], False, hh == 3) for hh in range(4)]),
                             r=[R_kDI[kb], R_qd0, R_mT[mi], R_const], w=[R_PB[b]])
                        ei = rot("E", 5)
                        T.op("act", f_act(Et[ei][:], PB[b][:, :], AF.Exp, scale=0.125), r=[R_PB[b]], w=[R_E[ei]])
                        st_[kb] = ei
                    def back(kb):
                        ei = st_.pop(kb)
                        T.op("pe", f_mm(PB[0][:, :], vD[:, kb, :], Et[ei][:], kb == 0, kb == nkb - 1),
                             r=[R_E[ei], R_vD[kb]], w=[R_PB[0]])
                    for t in range(nkb + LA):
                        if t < nkb: front(t)
                        if t - LA >= 0: back(t - LA)
                    T.op("act", f_act(dn[0][64:128, :], PB[0][64:128, :], AF.Copy), r=[R_PB[0]], w=[R_dn[0]])
                    T.op("pe", f_mm(PB[1][0:64, :], identf[:, 64:128], dn[0][:, :]), r=[R_dn[0], R_const], w=[R_PB[1]])
                    finish_attn(0, 1, j, 8, None)

                swa_mem(0)
                for j in range(4):
                    T.mark(f"g{g} s{j} idx")
                    dsa_index(j)
                    T.mark(f"g{g} s{j} bis")
                    dsa_bisect(j)
                    if j < 3:
                        swa_mem(j + 1)
                    T.mark(f"g{g} s{j} dattn")
                    dsa_attn(j)

                _ckpt(10 * (g + 1) + 3)
                T.mark(f"g{g} pass2")
                RQ = R_qs + R_qdi
                for oc in range(8):
                    G, RG = ring_get(3 + oc)
                    Gv = G[:, 0:8 * 384].rearrange("p (k c) -> p k c", k=8)
                    for jb in range(3):
                        T.op("pe", f_seq([f_mm(PB[jb][:, :], Gv[:, kc, jb * 128:(jb + 1) * 128], hTg[:, kc, :], kc == 0, kc == 7) for kc in range(8)]),
                             r=R_hTg + [RG], w=[R_PB[jb]])
                        T.op("act", f_act(gsb[jb][:], PB[jb][:, :], AF.Sigmoid, bias=bgate[:, oc * 3 + jb:oc * 3 + jb + 1], scale=1.0),
                             r=[R_PB[jb], R_misc], w=[R_gsb[jb]])
                    Pw, RP = ring_get(11 + oc)
                    Pv = Pw[0:64, 0:2048].rearrange("p (h c) -> p h c", h=16)
                    for jb, (h0, nh) in enumerate(((0, 8), (8, 4), (12, 4))):
                        T.op("pe", f_seq([f_mm(PB[3 + jb][:, :], Pv[:, h0 + hh, :], oTg[:, h0 + hh, :], hh == 0, hh == nh - 1) for hh in range(nh)]),
                             r=R_oTg + [RP], w=[R_PB[3 + jb]])
                    T.op("dve", f_tt(mg[0][:], PB[3][:, :], gsb[0][:], ALU.mult), r=[R_PB[3], R_gsb[0]], w=[R_mg[0]])
                    T.op("dve", f_tt(mg[1][:], PB[4][:, :], gsb[1][:], ALU.mult), r=[R_PB[4], R_gsb[1]], w=[R_mg[1]])
                    T.op("dve", f_tt(mg[0][:], mg[0][:], mg[1][:], ALU.add), r=[R_mg[0], R_mg[1]], w=[R_mg[0]])
                    T.op("dve", f_tt(mg[1][:], PB[5][:, :], gsb[2][:], ALU.mult), r=[R_PB[5], R_gsb[2]], w=[R_mg[1]])
                    T.op("dve", f_tt(mergedT[:, oc, :], mg[0][:], mg[1][:], ALU.add), r=[R_mg[0], R_mg[1]], w=RQ)

                _ckpt(10 * (g + 1) + 4)
                T.mark(f"g{g} wout")
                for cc in range(2):
                    W, RW = ring_get(19 + cc)
                    Wv = W[:, :].rearrange("p (k c) -> p k c", k=8)
                    for j in range(4):
                        pb = 6 + (j % 2)
                        T.op("pe", f_seq([f_mm(PB[pb][:, :], mergedT[:, kc, j * 128:(j + 1) * 128], Wv[:, kc, :], kc == 0, kc == 7) for kc in range(8)]),
                             r=RQ + [RW], w=[R_PB[pb]])
                        T.op("dve", f_tt(xg[:, j, cc * 512:(cc + 1) * 512], PB[pb][:, :], xg[:, j, cc * 512:(cc + 1) * 512], ALU.add),
                             r=[R_PB[pb], R_xg[j]], w=[R_xg[j]])

                _ckpt(10 * (g + 1) + 5)
                T.mark(f"g{g} mlp")
                for j in range(4):
                    norm_T(xg[:, j, :], [R_xg[j]], hTg[:, :, j * 128:(j + 1) * 128], [R_hTg[j]], j % 2)
                for fb in range(8):
                    W, RW = ring_get(21 + fb)
                    Wv = W[:, :].rearrange("p (k c) -> p k c", k=8)
                    for fs in range(4):
                        fc = fb * 4 + fs
                        pb = 2 + (fc % 4)
                        T.op("pe", f_seq([f_mm(PB[pb][:, :], Wv[:, kc, fs * 128:(fs + 1) * 128], hTg[:, kc, :], kc == 0, kc == 7) for kc in range(8)]),
                             r=R_hTg + [RW], w=[R_PB[pb]])
                        ri = rot("rl", 2)
                        T.op("act", f_act(rl[ri][:], PB[pb][:, :], AF.Relu), r=[R_PB[pb]], w=[R_rl[ri]])
                        T.op("dve", f_tt(hidT[:, fc, :], rl[ri][:], rl[ri][:], ALU.mult), r=[R_rl[ri]], w=[ISC[fc // 2]])
                for cc in range(2):
                    for ks in range(4):
                        W, RW = ring_get(29 + cc * 4 + ks)
                        Wv = W[:, :].rearrange("p (k c) -> p k c", k=8)
                        for j in range(4):
                            pb = 4 + j
                            T.op("pe", f_seq([f_mm(PB[pb][:, :], hidT[:, ks * 8 + fl, j * 128:(j + 1) * 128], Wv[:, fl, :],
                                                   ks == 0 and fl == 0, ks == 3 and fl == 7) for fl in range(8)]),
                                 r=ISC[ks * 4:(ks + 1) * 4] + [RW], w=[R_PB[pb]])
                    for j in range(4):
                        pb = 4 + j
                        T.op("dve", f_tt(xg[:, j, cc * 512:(cc + 1) * 512], PB[pb][:, :], xg[:, j, cc * 512:(cc + 1) * 512], ALU.add),
                             r=[R_PB[pb], R_xg[j]], w=[R_xg[j]])
                _ckpt(10 * (g + 1) + 6)
                T.mark(f"g{g} final")
                for j in range(4):
                    i = 4 * g + j
                    si = rot("stat", 4); s = stat[:, si * 4:(si + 1) * 4]; rs = R_stat[si]
                    hi = rot("hn", 1)
                    T.op("act", f_act(hn[hi][:], xg[:, j, :], AF.Square, accum=s[:, 0:1]), r=[R_xg[j]], w=[R_hn[hi], rs])
                    T.op("act", f_act(s[:, 1:2], s[:, 0:1], AF.Ln, scale=1.0 / 1024.0, bias=1e-6), r=[rs], w=[rs])
                    T.op("act", f_act(s[:, 2:3], s[:, 1:2], AF.Exp, scale=-0.5), r=[rs], w=[rs])
                    T.op("dve", f_stt(xg[:, j, :], xg[:, j, :], s[:, 2:3], gfin[:], ALU.mult, ALU.mult), r=[R_xg[j], rs, R_misc], w=[R_xg[j]])
                    T.dma("sp", out_d[i * 128:(i + 1) * 128, :], xg[:, j, :], R_xg[j], r=[R_xg[j]])
        try:
            emit()
        except _Stop:
            T.dma("sp", out_d[0:128, :], xo[:], R_rl[0], r=R_rl)
            T.wait_all("sp", R_rl + R_ring)
            T.wait_all("pool", [MSKR[0], MSKR[8], ISC[0], ISC[8]])
        T.wait_all("sp", R_xg)
        with nc.Block() as block:
            T.replay(block)
    return nc


OFF = dict(q_s=0, k_s=512, v_s=640, q_d=768, k_d=1024, v_d=1088, q_i=1152, k_i=1408, w_i=1472, q_m=1476, gates=1732)


def _chunked(w, cols):
    sub = w[:, cols]
    n = sub.shape[1]
    return np.ascontiguousarray(sub.reshape(8, 128, n).transpose(1, 0, 2).reshape(128, 8 * n))


def _prep_weights(w_in, w_proj_swa, w_proj_dsa, w_proj_mem, w_out, w_mlp_in, w_mlp_out):
    wch = np.zeros((NCHUNK, 128, 4096), np.float32)
    r64 = np.arange(64)
    qs_cols = np.concatenate([OFF["q_s"] + h * 64 + r64 for h in (0, 4, 1, 5, 2, 6, 3, 7)])
    qdi_cols = np.concatenate([np.concatenate([OFF["q_d"] + h * 64 + r64, OFF["q_i"] + h * 64 + r64]) for h in range(4)])
    qm_cols = np.concatenate([OFF["q_m"] + np.arange(256), OFF["w_i"] + np.arange(4)])
    wch[0] = _chunked(w_in, qs_cols)
    wch[1] = _chunked(w_in, qdi_cols)
    wch[2, :, 0:8 * 260] = _chunked(w_in, qm_cols)
    for oc in range(8):
        gc = np.concatenate([OFF["gates"] + jb * 1024 + oc * 128 + np.arange(128) for jb in range(3)])
        wch[3 + oc, :, 0:8 * 384] = _chunked(w_in, gc)
        wp = np.concatenate([w_proj_swa.reshape(8, 64, 1024), w_proj_dsa.reshape(4, 64, 1024), w_proj_mem.reshape(4, 64, 1024)], 0)
        wch[11 + oc, 0:64, 0:2048] = wp[:, :, oc * 128:(oc + 1) * 128].transpose(1, 0, 2).reshape(64, 2048)
    for cc in range(2):
        wch[19 + cc] = _chunked(w_out, cc * 512 + np.arange(512))
    for fb in range(8):
        wch[21 + fb] = _chunked(w_mlp_in, fb * 512 + np.arange(512))
    for cc in range(2):
        for ks in range(4):
            blk = w_mlp_out[ks * 1024:(ks + 1) * 1024, cc * 512:(cc + 1) * 512]
            wch[29 + cc * 4 + ks] = blk.reshape(8, 128, 512).transpose(1, 0, 2).reshape(128, 4096)
    k_cols = np.concatenate([OFF["k_s"] + np.arange(128), OFF["k_d"] + r64, OFF["k_i"] + r64, OFF["v_s"] + np.arange(128), OFF["v_d"] + r64])
    wk = _chunked(w_in, k_cols)
    return wch, wk


_NC_CACHE = {}


def kernel(x, mem, positions, g_mix, w_in, b_gate, sinks, g_mem, w_mem_kv, w_proj_swa, w_proj_dsa,
           w_proj_mem, w_out, g_mlp, w_mlp_in, w_mlp_out, g_final):
    f = lambda a: np.asarray(a, dtype=np.float32)
    x = f(x); mem = f(mem); positions = np.asarray(positions, dtype=np.int32)
    wch, wk = _prep_weights(f(w_in)[0], f(w_proj_swa)[0], f(w_proj_dsa)[0], f(w_proj_mem)[0], f(w_out)[0], f(w_mlp_in)[0], f(w_mlp_out)[0])
    wmem = _chunked(f(w_mem_kv)[0], np.arange(512))
    tr = lambda v: np.ascontiguousarray(f(v).reshape(8, 128).T)
    gvec = np.concatenate([tr(g_mix[0]), tr(g_mlp[0]), tr(g_mem[0])], 1)
    gfin = np.ascontiguousarray(np.broadcast_to(f(g_final)[None, :], (128, 1024)))
    bg = f(b_gate)[0].reshape(3, 8, 128)
    bgate = np.ascontiguousarray(bg.transpose(2, 1, 0).reshape(128, 24))
    sinkr = np.ascontiguousarray(np.broadcast_to(f(sinks)[0][None, :], (64, 8)))
    half = 32
    invf = np.power(np.float32(10000.0), -np.arange(half, dtype=np.float32) / np.float32(half)).astype(np.float32)
    invf = np.ascontiguousarray(np.broadcast_to(invf[None, :], (128, 32)))
    s_ = np.arange(128)[:, None]; t_ = np.arange(128)[None, :]
    tri_cur = (t_ >= s_).astype(np.float32)
    tri_prev = (s_ > t_).astype(np.float32)
    dbias = np.where(t_ <= s_, 0.0, NEG).astype(np.float32)
    in_maps = []
    for c in range(8):
        b, par = c // 2, c % 2
        order = np.arange(64) if par == 0 else np.concatenate([np.arange(1, 64), [0]])
        xb = x[b].reshape(64, 128, 1024)[order].reshape(64 * 128, 1024)
        pb = positions[b].reshape(64, 128)[order]
        posc = np.ascontiguousarray(pb.T)
        prev0 = tri_prev * float(par)
        p63 = np.full((128, 128), 0.0 if par == 1 else NEG, np.float32)
        cmask = np.ascontiguousarray(np.stack([tri_cur, tri_prev, prev0, dbias, p63], 1).reshape(128, 5 * 128).astype(np.float32))
        in_maps.append(dict(xc=np.ascontiguousarray(xb), posc=posc, memc=np.ascontiguousarray(mem[b]), cmask=cmask, invf=invf,
                            wk=wk, wch=wch, wmem=wmem, gvec=gvec, gfin=gfin, bgate=bgate, sinkr=sinkr))
    if "nc" not in _NC_CACHE:
        _NC_CACHE["nc"] = build_program()
    nc = _NC_CACHE["nc"]
    res = run_bass_kernel_spmd(nc, in_maps, core_ids=list(range(8)))
    out = np.zeros((4, 64, 128, 1024), np.float32)
    for c in range(8):
        b, par = c // 2, c % 2
        o = np.asarray(res.results[c]["out"]).reshape(32, 128, 1024)
        out[b, par::2] = o
    return out.reshape(4, 8192, 1024)
```

```python
import math
import numpy as np
from contextlib import ExitStack
import concourse.bass as bass
import concourse.mybir as mybir
from concourse.bass_utils import run_bass_kernel_spmd

F32 = mybir.dt.float32; BF16 = mybir.dt.bfloat16; I32 = mybir.dt.int32
ALU = mybir.AluOpType; AF = mybir.ActivationFunctionType; AX = mybir.AxisListType

STAGE = 99
class _Stop(Exception):
    pass
def _ckpt(n):
    if STAGE <= n:
        raise _Stop()
NITER = 14
NEG = -1.0e30
NCHUNK = 37
TOPK = 256.0


class Reg:
    __slots__ = ("name", "w", "r", "dsem", "dcnt")
    def __init__(self, name):
        self.name = name; self.w = None; self.r = {}; self.dsem = None; self.dcnt = 0


class Eng:
    def __init__(self, name):
        self.name = name; self.q = []; self.sem = None; self.cnt = 0; self.seen = {}


class Trk:
    def __init__(self, nc, stack, nsem):
        self.nc = nc
        self.sems = [stack.enter_context(nc.semaphore(f"s{i}")) for i in range(nsem)]
        self.si = 0
        self.E = {n: Eng(n) for n in ("pe", "act", "dve", "pool", "sp")}
        for e in self.E.values():
            e.sem = self.newsem()
    def newsem(self):
        s = self.sems[self.si]; self.si += 1; return s
    def _deps(self, eng, reads, writes):
        deps = {}
        def add(ev, kind):
            if ev is None: return
            sem, val, src = ev
            if src is eng and eng.name == "pe":
                return
            k = id(sem)
            if eng.seen.get(k, 0) >= val: return
            if k not in deps or deps[k][1] < val: deps[k] = (sem, val)
        for r in reads: add(r.w, "raw")
        for w in writes:
            add(w.w, "waw")
            for ev in w.r.values(): add(ev, "war")
        out = list(deps.values())
        for sem, val in out: eng.seen[id(sem)] = val
        return out
    def op(self, en, fn, r=(), w=()):
        eng = self.E[en]
        waits = self._deps(eng, r, w)
        eng.cnt += 1
        ev = (eng.sem, eng.cnt, eng)
        eng.q.append((waits, fn, (eng.sem, 1)))
        for x in r: x.r[en] = ev
        for x in w: x.w = ev; x.r = {}
    def dma(self, en, out_ap, in_ap, slot, r=(), w=()):
        eng = self.E[en]
        waits = self._deps(eng, r, w)
        if slot.dsem is None: slot.dsem = self.newsem()
        slot.dcnt += 16
        ev = (slot.dsem, slot.dcnt, None)
        eng.q.append((waits, lambda h: h.dma_start(out=out_ap, in_=in_ap), (slot.dsem, 16)))
        for x in r: x.r["dma%d" % id(slot)] = ev
        for x in w: x.w = ev; x.r = {}
    def mark(self, label):
        self.E["pe"].q.append(([], None, label))
    def wait_all(self, en, regs):
        eng = self.E[en]
        waits = self._deps(eng, [], regs)
        eng.q.append((waits, None, None))
    def replay(self, block):
        def mk(en):
            q = self.E[en].q
            def body(h):
                for waits, fn, inc in q:
                    for sem, val in waits: h.wait_ge(sem, val)
                    if fn is None:
                        if isinstance(inc, str): MARKS.append((inc, PE_CNT[0]))
                        continue
                    ins = fn(h)
                    ins.then_inc(inc[0], inc[1])
            return body
        block.tensor(mk("pe")); block.scalar(mk("act")); block.vector(mk("dve"))
        block.gpsimd(mk("pool")); block.sync(mk("sp"))


PE_CNT = [0]
MARKS = []
def f_mm(out, lhsT, rhs, start=True, stop=True):
    def f(h):
        PE_CNT[0] += 1
        return h.matmul(out, lhsT=lhsT, rhs=rhs, start=start, stop=stop)
    return f
def f_tr(out, in_, ident):
    def f(h):
        PE_CNT[0] += 1
        return h.transpose(out=out, in_=in_, identity=ident)
    return f
def f_seq(fns):
    def f(h):
        ins = None
        for fn in fns: ins = fn(h)
        return ins
    return f
def f_act(out, in_, func, bias=None, scale=None, accum=None):
    kw = {}
    if bias is not None: kw["bias"] = bias
    if scale is not None: kw["scale"] = scale
    if accum is not None: kw["accum_out"] = accum
    return lambda h: h.activation(out=out, in_=in_, func=func, **kw)
def f_ts(out, in0, s1, s2=None, op0=ALU.mult, op1=None, accum=None):
    kw = {}
    if op1 is not None: kw["op1"] = op1
    if accum is not None: kw["accum_out"] = accum
    return lambda h: h.tensor_scalar(out=out, in0=in0, scalar1=s1, scalar2=s2, op0=op0, **kw)
def f_tt(out, in0, in1, op):
    return lambda h: h.tensor_tensor(out=out, in0=in0, in1=in1, op=op)
def f_stt(out, in0, scalar, in1, op0, op1):
    return lambda h: h.scalar_tensor_tensor(out=out, in0=in0, scalar=scalar, in1=in1, op0=op0, op1=op1)
def f_cp(out, in_):
    return lambda h: h.tensor_copy(out=out, in_=in_)
def f_memset(ap, v):
    return lambda h: h.memset(ap, v)


def build_program():
    nc = bass.Bass("TRN2", target_bir_lowering=False)
    dt_in = lambda n, s, d=F32: nc.dram_tensor(n, s, d, kind="ExternalInput").ap()
    xc = dt_in("xc", [64 * 128, 1024])
    posc = dt_in("posc", [128, 64], I32)
    memc = dt_in("memc", [256, 1024])
    cmask = dt_in("cmask", [128, 5 * 128])
    invf_d = dt_in("invf", [128, 32])
    wk_d = dt_in("wk", [128, 8 * 448])
    wch_d = dt_in("wch", [NCHUNK, 128, 4096])
    wmem_d = dt_in("wmem", [128, 4096])
    gvec_d = dt_in("gvec", [128, 24])
    gfin_d = dt_in("gfin", [128, 1024])
    bgate_d = dt_in("bgate", [128, 24])
    sinkr_d = dt_in("sinkr", [64, 8])
    out_d = nc.dram_tensor("out", [32 * 128, 1024], F32, kind="ExternalOutput").ap()
    wbf_d = nc.dram_tensor("wbf", [NCHUNK, 128, 4096], BF16, kind="Internal").ap()

    with ExitStack() as st:
        T = Trk(nc, st, 48)
        def sb(name, shape, dt):
            return st.enter_context(nc.sbuf_tensor(name, shape, dt))
        BIG = sb("BIG", [128, 8192], F32)
        MSK = sb("MSK", [128, 8192], BF16)
        ISC = [Reg(f"isc{c}") for c in range(16)]
        MSKR = [Reg(f"msk{c}") for c in range(16)]
        hidT = BIG[:].bitcast(BF16).rearrange("p (f t) -> p f t", t=512)
        wk_sb = sb("wk_sb", [128, 8, 448], BF16); R_wk = Reg("wk")
        kDI = sb("kDI", [128, 8192], BF16); R_kDI = [Reg(f"kDI{i}") for i in range(64)]
        vD = sb("vD", [128, 64, 128], BF16); R_vD = [Reg(f"vD{i}") for i in range(64)]
        kST = sb("kST", [128, 16, 128], BF16); R_kST = [Reg(f"kST{i}") for i in range(16)]
        vS = sb("vS", [128, 16, 128], BF16); R_vS = [Reg(f"vS{i}") for i in range(16)]
        mkT = sb("mkT", [128, 2, 256], BF16); R_mkT = Reg("mkT")
        mv = sb("mv", [128, 2, 256], BF16); R_mv = Reg("mv")
        xg = sb("xg", [128, 4, 1024], F32); R_xg = [Reg(f"xg{j}") for j in range(4)]
        hTg = sb("hTg", [128, 8, 512], BF16); R_hTg = [Reg(f"hTg{j}") for j in range(4)]
        hTo = sb("hTo", [128, 8, 128], BF16); R_hTo = Reg("hTo")
        oTg = sb("oTg", [64, 16, 512], BF16); R_oTg = [Reg(f"oTg{j}") for j in range(4)]
        QM = sb("QM", [128, 4096], BF16)
        qsT = QM[:, 0:2048].rearrange("p (s c) -> p s c", s=4)
        qdiT = QM[:, 2048:4096].rearrange("p (s c) -> p s c", s=4)
        mergedT = QM[:].rearrange("p (k t) -> p k t", k=8)
        R_qs = [Reg(f"qs{j}") for j in range(4)]; R_qdi = [Reg(f"qdi{j}") for j in range(4)]
        qmT = sb("qmT", [128, 4, 256], BF16); R_qm = [Reg(f"qm{j}") for j in range(4)]
        wiT = sb("wiT", [128, 4, 4], F32); R_wi = [Reg(f"wi{j}") for j in range(4)]
        NRING = 2
        ring = [sb(f"ring{i}", [128, 4096], BF16) for i in range(NRING)]
        R_ring = [Reg(f"ring{i}") for i in range(NRING)]
        ident = sb("ident", [128, 128], BF16); identf = sb("identf", [128, 128], F32); onesf = sb("onesf", [128, 128], F32)
        ones_b = sb("ones_b", [128, 64], BF16)
        qd0 = sb("qd0", [128, 512], BF16); R_qd0 = Reg("qd0")
        R_const = Reg("const")
        cm_f = sb("cm_f", [128, 5, 128], F32)
        cm_b = sb("cm_b", [128, 3, 128], BF16)
        invf = sb("invf_sb", [128, 32], F32)
        gvec = sb("gvec_sb", [128, 24], F32)
        gfin = sb("gfin_sb", [128, 1024], F32)
        bgate = sb("bgate_sb", [128, 24], F32)
        es = sb("es_sb", [64, 8], F32)
        pw2 = sb("pw2", [128, NITER], F32)
        posi = sb("posi", [128, 64], I32); posf = sb("posf", [128, 64], F32)
        cosT = sb("cosT", [128, 8, 32], F32); sinT = sb("sinT", [128, 8, 32], F32); R_cs = Reg("cs")
        rtmp = [sb(f"rtmp{i}", [128, 256], F32) for i in range(3)]; rtmpi = sb("rtmpi", [128, 256], I32); R_rt = Reg("rt")
        stat = sb("stat", [128, 16], F32); R_stat = [Reg(f"stat{i}") for i in range(4)]
        hn = [sb(f"hn{i}", [128, 1024], BF16) for i in range(1)]; R_hn = [Reg(f"hn{i}") for i in range(1)]
        tok_b = [sb(f"tok_b{i}", [128, 512], BF16) for i in range(2)]; R_tok = [Reg(f"tok{i}") for i in range(2)]
        Et = [sb(f"Et{i}", [128, 512], BF16) for i in range(5)]; R_E = [Reg(f"E{i}") for i in range(5)]
        Pt = [sb(f"Pt{i}", [128, 512], BF16) for i in range(2)]; R_P = [Reg(f"P{i}") for i in range(2)]
        RLX = sb("RLX", [128, 1024], F32); rl = [RLX[:, 0:512], RLX[:, 512:1024]]; R_rl = [Reg(f"rl{i}") for i in range(2)]
        xo = RLX; R_xo = R_rl[0]
        mT = [sb(f"mT{i}", [128, 512], BF16) for i in range(2)]; R_mT = [Reg(f"mT{i}") for i in range(2)]
        dn = [sb(f"dn{i}", [128, 512], F32) for i in range(1)]; R_dn = [Reg(f"dn{i}") for i in range(1)]
        mg = rl; R_mg = R_rl
        RTU = sb("RTU", [128, 1024], F32); rope_t = RTU[:, 0:512]; rope_u = RTU[:, 512:1024]; R_rtu = [Reg("rt_"), Reg("ru_")]
        gsb = [sb(f"gsb{i}", [128, 512], BF16) for i in range(3)]; R_gsb = [Reg(f"gsb{i}") for i in range(3)]
        bis = sb("bis", [128, 8 + NITER], F32); R_lo = Reg("lo"); R_mid = Reg("mid"); R_cd = Reg("cd"); R_ca = Reg("ca"); R_t = Reg("t")
        PB = [st.enter_context(nc.psum_tensor(f"pb{i}", [128, 512], F32)) for i in range(8)]
        R_PB = [Reg(f"pb{i}") for i in range(8)]
        def pbf(i):
            return PB[i][:].bitcast(BF16)

        cnt = {"ring": 0, "stat": 0, "hn": 0, "tok": 0, "E": 0, "P": 0, "rl": 0, "mT": 0, "dn": 0, "mg": 0}
        def rot(name, n):
            i = cnt[name] % n; cnt[name] += 1; return i

        R_cmf = Reg("cmf"); R_misc = Reg("misc")
        T.dma("sp", cm_f[:].rearrange("p a b -> p (a b)"), cmask[:, :], R_cmf, w=[R_cmf])
        for (dst, src) in ((invf, invf_d), (gvec, gvec_d), (gfin, gfin_d), (bgate, bgate_d)):
            T.dma("sp", dst[:], src[:, :], R_misc, w=[R_misc])
            T.wait_all("sp", [R_misc])
        R_es = Reg("es")
        T.dma("sp", es[:], sinkr_d[:, :], R_es, w=[R_es])
        R_pos = Reg("pos")
        T.dma("sp", posi[:], posc[:, :], R_pos, w=[R_pos])
        T.op("pool", f_memset(onesf[:], 1.0), w=[R_const])
        T.op("pool", f_memset(identf[:], 0.0), w=[R_const])
        T.op("pool", lambda h: h.affine_select(out=identf[:], in_=onesf[:], pattern=[[-1, 128]], compare_op=ALU.is_equal,
                                                fill=0.0, base=0, channel_multiplier=1), r=[R_const], w=[R_const])
        T.op("dve", f_cp(ident[:], identf[:]), r=[R_const], w=[R_const])
        T.op("dve", f_memset(ones_b[:], 1.0), w=[R_const])
        T.op("pool", f_memset(vD[:].rearrange("p a b -> p (a b)"), 1.0), w=R_vD)
        T.op("dve", f_memset(dn[0][:], 0.0), w=[R_dn[0]])
        T.op("dve", f_memset(qd0[:], 0.0), w=[R_qd0])
        for k in range(NITER):
            T.op("dve", f_memset(pw2[:, k:k + 1], 2.0 ** (-(k + 1))), w=[R_const])
        T.op("dve", f_cp(cm_b[:], cm_f[:, 0:3, :]), r=[R_cmf], w=[R_const])
        T.op("act", f_act(es[:], es[:], AF.Exp), r=[R_es], w=[R_es])
        T.op("dve", f_cp(posf[:], posi[:]), r=[R_pos], w=[R_pos])
        tri_cur = cm_b[:, 0, :]; tri_prev = cm_b[:, 1, :]; prev0 = cm_b[:, 2, :]
        dbias = cm_f[:, 3, :]; p63bias = cm_f[:, 4, :]

        def emit():
            def norm_T(x_ap, x_regs, dstT, dst_regs, pbank):
                si = rot("stat", 4); s = stat[:, si * 4:(si + 1) * 4]; rs = R_stat[si]
                hi = rot("hn", 1); h_ = hn[hi]; rh = R_hn[hi]
                T.op("act", f_act(h_[:], x_ap, AF.Square, accum=s[:, 0:1]), r=x_regs, w=[rh, rs])
                T.op("act", f_act(s[:, 1:2], s[:, 0:1], AF.Ln, scale=1.0 / 1024.0, bias=1e-6), r=[rs], w=[rs])
                T.op("act", f_act(s[:, 2:3], s[:, 1:2], AF.Exp, scale=-0.5), r=[rs], w=[rs])
                T.op("dve", f_ts(h_[:], x_ap, s[:, 2:3], None, ALU.mult), r=x_regs + [rs], w=[rh])
                pv = pbf(pbank)
                T.op("pe", f_seq([f_tr(pv[:, kc * 128:(kc + 1) * 128], h_[:, kc * 128:(kc + 1) * 128], ident[:]) for kc in range(8)]),
                     r=[rh, R_const], w=[R_PB[pbank]])
                T.op("act", f_act(dstT, pv[:, :].rearrange("p (k t) -> p k t", k=8), AF.Copy), r=[R_PB[pbank]], w=dst_regs)
                return s, rs

            def rope_tables(blk0, nb):
                n = nb * 32
                a0 = rtmp[0][:, 0:n]; a1 = rtmp[1][:, 0:n]; a2 = rtmp[2][:, 0:n]; ai = rtmpi[:, 0:n]
                v3 = lambda a: a.rearrange("p (b f) -> p b f", f=32)
                T.op("dve", f_tt(v3(a0), posf[:, blk0:blk0 + nb].unsqueeze(2).to_broadcast([128, nb, 32]),
                                 invf[:].unsqueeze(1).to_broadcast([128, nb, 32]), ALU.mult), r=[R_pos, R_misc], w=[R_rt])
                T.op("dve", f_ts(ai, a0, 1.0 / (2 * math.pi), None, ALU.mult), r=[R_rt], w=[R_rt])
                T.op("dve", f_cp(a1, ai), r=[R_rt], w=[R_rt])
                T.op("dve", f_stt(a2, a1, -6.28125, a0, ALU.mult, ALU.add), r=[R_rt], w=[R_rt])
                T.op("dve", f_stt(a2, a1, -0.0019353072, a2, ALU.mult, ALU.add), r=[R_rt], w=[R_rt])
                T.op("dve", f_ts(a0, a2, -3.1415925, 3.1415925, ALU.max, ALU.min), r=[R_rt], w=[R_rt])
                T.op("act", f_act(sinT[:, 0:nb, :].rearrange("p b f -> p (b f)"), a0, AF.Sin), r=[R_rt], w=[R_cs])
                T.op("dve", f_ts(a1, a2, math.pi / 2, None, ALU.add), r=[R_rt], w=[R_rt])
                T.op("dve", f_ts(a0, a1, math.pi, -2 * math.pi, ALU.is_gt, ALU.mult), r=[R_rt, R_cs], w=[R_rt])
                T.op("dve", f_tt(a0, a0, a1, ALU.add), r=[R_rt], w=[R_rt])
                T.op("dve", f_ts(a0, a0, -3.1415925, 3.1415925, ALU.max, ALU.min), r=[R_rt], w=[R_rt])
                T.op("act", f_act(cosT[:, 0:nb, :].rearrange("p b f -> p (b f)"), a0, AF.Sin), r=[R_rt], w=[R_cs])

            def rope_apply(src, src_regs, dst, dst_regs, H, cb):
                n = H * 64
                t_ = rope_t[:, 0:n]; u_ = rope_u[:, 0:n]
                cosb = cosT[:, cb:cb + 1, :]; sinb = sinT[:, cb:cb + 1, :]
                T.op("dve", f_tt(t_.rearrange("p (a f) -> p a f", f=32), src.rearrange("p (a f) -> p a f", f=32),
                                 cosb.to_broadcast([128, 2 * H, 32]), ALU.mult), r=src_regs + [R_cs], w=[R_rtu[0]])
                s4 = src.rearrange("p (h e f) -> p h e f", e=2, f=32)
                u4 = u_.rearrange("p (h e f) -> p h e f", e=2, f=32)
                t4 = t_.rearrange("p (h e f) -> p h e f", e=2, f=32)
                d4 = dst.rearrange("p (h e f) -> p h e f", e=2, f=32)
                sb_ = sinb.to_broadcast([128, H, 32])
                T.op("dve", f_tt(u4[:, :, 0, :], s4[:, :, 1, :], sb_, ALU.mult), r=src_regs + [R_cs], w=[R_rtu[1]])
                T.op("dve", f_tt(u4[:, :, 1, :], s4[:, :, 0, :], sb_, ALU.mult), r=src_regs + [R_cs], w=[R_rtu[1]])
                T.op("dve", f_tt(d4[:, :, 0, :], t4[:, :, 0, :], u4[:, :, 0, :], ALU.subtract), r=R_rtu, w=dst_regs)
                T.op("dve", f_tt(d4[:, :, 1, :], t4[:, :, 1, :], u4[:, :, 1, :], ALU.add), r=R_rtu, w=dst_regs)

            R_wbf = [Reg(f"wbf{i}") for i in range(NCHUNK)]
            CH_N = [4096, 4096, 8 * 260] + [8 * 384] * 8 + [2048] * 8 + [4096] * 18
            CH_P = [128] * 11 + [64] * 8 + [128] * 18
            sched = [0, 1, 2] + [v for oc in range(8) for v in (3 + oc, 11 + oc)] + list(range(19, 37))
            ring_state = {"issued": 0, "got": 0}
            total_gets = 8 * NCHUNK
            def ring_issue():
                k = ring_state["issued"]
                if k >= total_gets: return
                c = sched[k % NCHUNK]; b = k % NRING
                T.dma("sp", ring[b][0:CH_P[c], 0:CH_N[c]], wbf_d[c, 0:CH_P[c], 0:CH_N[c]], R_ring[b], r=[R_wbf[c]], w=[R_ring[b]])
                ring_state["issued"] += 1
            def ring_get(expect):
                k = ring_state["got"]
                assert sched[k % NCHUNK] == expect, (k, expect)
                while ring_state["issued"] < min(k + NRING, total_gets):
                    ring_issue()
                ring_state["got"] += 1
                b = k % NRING
                return ring[b], R_ring[b]

            def convert(src_dram_ap, npart, nelem, ncols, goff, dst_ap, dst_regs, half):
                stg = BIG[0:npart, half * 4096: half * 4096 + nelem]
                sregs = ISC[half * 8:(half + 1) * 8]
                T.dma("pool", stg, src_dram_ap, sregs[0], w=sregs)
                if goff is None:
                    T.op("act", f_act(dst_ap, stg, AF.Copy), r=sregs, w=dst_regs)
                else:
                    for kc in range(8):
                        T.op("dve", f_ts(dst_ap[:, kc * ncols:(kc + 1) * ncols], stg[:, kc * ncols:(kc + 1) * ncols],
                                          gvec[:, goff + kc:goff + kc + 1], None, ALU.mult), r=sregs + [R_misc], w=dst_regs)
            _ckpt(1)
            convert(wk_d[:, :], 128, 8 * 448, 448, 0, wk_sb[:].rearrange("p k c -> p (k c)"), [R_wk], 0)
            wmem_b = MSK[:, 4096:8192]
            convert(wmem_d[:, :], 128, 4096, 512, 16, wmem_b, MSKR[8:16], 1)

            for mc in range(2):
                T.dma("sp", xo[:], memc[mc * 128:(mc + 1) * 128, :], R_rl[0], w=R_rl)
                norm_T(xo[:], R_rl, hTo[:], [R_hTo], 0)
                T.op("pe", f_seq([f_mm(PB[1][:, :], hTo[:, kc, :], wmem_b[:, kc * 512:(kc + 1) * 512], kc == 0, kc == 7) for kc in range(8)]),
                     r=[R_hTo] + MSKR[8:16], w=[R_PB[1]])
                ti = rot("tok", 2)
                T.op("act", f_act(tok_b[ti][:, 0:256], PB[1][:, 0:256], AF.Copy), r=[R_PB[1]], w=[R_tok[ti]])
                T.op("act", f_act(mv[:, mc, :], PB[1][:, 256:512], AF.Copy), r=[R_PB[1]], w=[R_mv])
                pv = pbf(2)
                T.op("pe", f_seq([f_tr(pv[:, jj * 128:(jj + 1) * 128], tok_b[ti][:, jj * 128:(jj + 1) * 128], ident[:]) for jj in range(2)]),
                     r=[R_tok[ti], R_const], w=[R_PB[2]])
                T.op("act", f_act(mkT[:, :, mc * 128:(mc + 1) * 128], pv[:, 0:256].rearrange("p (j m) -> p j m", j=2), AF.Copy),
                     r=[R_PB[2]], w=[R_mkT])

            _ckpt(2)
            for c in range(NCHUNK):
                half = c % 2
                goff = 0 if c < 11 else (8 if 21 <= c < 29 else None)
                ncols = {0: 512, 1: 512, 2: 260}.get(c, 384 if c < 11 else 512)
                npart = CH_P[c]; nelem = CH_N[c]
                dstb = MSK[0:npart, half * 4096: half * 4096 + nelem]
                dregs = MSKR[half * 8:(half + 1) * 8]
                convert(wch_d[c, 0:npart, 0:nelem], npart, nelem, ncols, goff, dstb, dregs, half)
                T.dma("pool", wbf_d[c, 0:npart, 0:nelem], dstb, dregs[0], r=dregs, w=[R_wbf[c]])

            def kside(pos, x_ap, x_regs, hT_ap, hT_regs, cb, stage="AB"):
                kidx = 0 if pos == 63 else pos + 1
                rg = (pos + 1) % 16
                if "A" in stage:
                    norm_T(x_ap, x_regs, hT_ap, hT_regs, 0)
                if "B" in stage or "M" in stage:
                    T.op("pe", f_seq([f_mm(PB[1][:, 0:448], hT_ap[:, kc, :], wk_sb[:, kc, :], kc == 0, kc == 7) for kc in range(8)]),
                         r=hT_regs + [R_wk], w=[R_PB[1]])
                if "B" not in stage and "R" not in stage:
                    return
                ti = rot("tok", 2)
                rope_apply(PB[1][:, 0:256], [R_PB[1]], tok_b[ti][:, 0:256], [R_tok[ti]], 4, cb)
                _ckpt(3.3)
                T.op("dve", f_cp(vS[:, rg, :], PB[1][:, 256:384]), r=[R_PB[1]], w=[R_vS[rg]])
                T.op("dve", f_cp(vD[:, kidx, 0:64], PB[1][:, 384:448]), r=[R_PB[1]], w=[R_vD[kidx]])
                _ckpt(3.4)
                pv = pbf(2)
                T.op("pe", f_seq([f_tr(pv[:, jj * 128:(jj + 1) * 128], tok_b[ti][:, jj * 128:(jj + 1) * 128], ident[:]) for jj in range(2)]),
                     r=[R_tok[ti], R_const], w=[R_PB[2]])
                _ckpt(3.5)
                T.op("act", f_act(kST[:, rg, :], pv[:, 0:128], AF.Copy), r=[R_PB[2]], w=[R_kST[rg]])
                T.op("act", f_act(kDI[:, kidx * 128:(kidx + 1) * 128], pv[:, 128:256], AF.Copy), r=[R_PB[2]], w=[R_kDI[kidx]])

            _ckpt(3)
            rope_tables(63, 1)
            T.dma("sp", xo[:], xc[63 * 128:64 * 128, :], R_rl[0], w=R_rl)
            kside(63, xo[:], R_rl, hTo[:], [R_hTo], 0)

            def finish_attn(bO, bD, j, h0, es_ap):
                di = rot("dn", 1)
                d_ = dn[di][0:64, :]
                if es_ap is not None:
                    T.op("dve", f_tt(d_.rearrange("p (h t) -> p h t", h=4), PB[bD][0:64, :].rearrange("p (h t) -> p h t", h=4), es_ap, ALU.add),
                         r=[R_PB[bD], R_es], w=[R_dn[di]])
                    T.op("act", f_act(d_, d_, AF.Ln), r=[R_dn[di]], w=[R_dn[di]])
                else:
                    T.op("act", f_act(d_, PB[bD][0:64, :], AF.Ln), r=[R_PB[bD]], w=[R_dn[di]])
                T.op("act", f_act(d_, d_, AF.Exp, scale=-1.0), r=[R_dn[di]], w=[R_dn[di]])
                T.op("dve", f_tt(oTg[:, h0:h0 + 4, j * 128:(j + 1) * 128], PB[bO][0:64, :].rearrange("p (h t) -> p h t", h=4),
                                 d_.rearrange("p (h t) -> p h t", h=4), ALU.mult), r=[R_PB[bO], R_dn[di]], w=[R_oTg[j]])

            for g in range(8):
                T.mark(f"g{g} kside")
                npos = 8 if g < 7 else 7
                rope_tables(8 * g, 8)
                def kblock(pl, stage):
                    pos = 8 * g + pl
                    if pl % 2 == 0:
                        j = pl // 2
                        if "A" in stage:
                            T.dma("sp", xg[:, j, :], xc[pos * 128:(pos + 1) * 128, :], R_xg[j], w=[R_xg[j]])
                        kside(pos, xg[:, j, :], [R_xg[j]], hTg[:, :, j * 128:(j + 1) * 128], [R_hTg[j]], pl, stage)
                    else:
                        if "A" in stage:
                            T.dma("sp", xo[:], xc[pos * 128:(pos + 1) * 128, :], R_rl[0], w=R_rl)
                        kside(pos, xo[:], R_rl, hTo[:], [R_hTo], pl, stage)
                kblock(0, "A")
                for pl in range(npos):
                    kblock(pl, "M")
                    if pl + 1 < npos:
                        kblock(pl + 1, "A")
                    kblock(pl, "R")

                _ckpt(10 * (g + 1) + 1)
                T.mark(f"g{g} qproj")
                units = [(c, j) for c in range(3) for j in range(4)]
                qW = {}
                def q_mm(u):
                    c, j = units[u]
                    if j == 0:
                        W, RW = ring_get(c)
                        qW[c] = (W, RW)
                    W, RW = qW[c]
                    ncols = (512, 512, 260)[c]
                    Wv = W[:, 0:8 * ncols].rearrange("p (k c) -> p k c", k=8)
                    pb = 3 + (u % 2)
                    T.op("pe", f_seq([f_mm(PB[pb][:, 0:ncols], hTg[:, kc, j * 128:(j + 1) * 128], Wv[:, kc, :], kc == 0, kc == 7) for kc in range(8)]),
                         r=[R_hTg[j], RW], w=[R_PB[pb]])
                def q_post(u):
                    c, j = units[u]
                    pb = 3 + (u % 2); pt = 5 + (u % 2)
                    ti = rot("tok", 2)
                    if c < 2:
                        rope_apply(PB[pb][:, 0:512], [R_PB[pb]], tok_b[ti][:, 0:512], [R_tok[ti]], 8, 2 * j)
                        pv = pbf(pt)
                        T.op("pe", f_seq([f_tr(pv[:, q * 128:(q + 1) * 128], tok_b[ti][:, q * 128:(q + 1) * 128], ident[:]) for q in range(4)]),
                             r=[R_tok[ti], R_const], w=[R_PB[pt]])
                        dst = qsT if c == 0 else qdiT
                        T.op("act", f_act(dst[:, j, :], pv[:, 0:512], AF.Copy), r=[R_PB[pt]], w=[(R_qs if c == 0 else R_qdi)[j]])
                    else:
                        T.op("act", f_act(tok_b[ti][:, 0:256], PB[pb][:, 0:256], AF.Copy), r=[R_PB[pb]], w=[R_tok[ti]])
                        T.op("act", f_act(wiT[:, j, :], PB[pb][:, 256:260], AF.Copy, scale=1.0 / 16.0), r=[R_PB[pb]], w=[R_wi[j]])
                        pv = pbf(pt)
                        T.op("pe", f_seq([f_tr(pv[:, q * 128:(q + 1) * 128], tok_b[ti][:, q * 128:(q + 1) * 128], ident[:]) for q in range(2)]),
                             r=[R_tok[ti], R_const], w=[R_PB[pt]])
                        T.op("act", f_act(qmT[:, j, :], pv[:, 0:256], AF.Copy), r=[R_PB[pt]], w=[R_qm[j]])
                q_mm(0)
                for u in range(len(units)):
                    if u + 1 < len(units):
                        q_mm(u + 1)
                    q_post(u)

                _ckpt(10 * (g + 1) + 2)
                T.mark(f"g{g} attn")
                def slot_vars(j):
                    i = 4 * g + j
                    nkb = 2 * i + 2
                    return i, slice(j * 128, (j + 1) * 128), nkb, (nkb + 3) // 4, nkb * 128
                def swa_mem(j):
                    i, tok, nkb, nch, n = slot_vars(j)
                    rp = (2 * i) % 16; rc = (2 * i + 1) % 16
                    for kv in range(2):
                        pr = slice(kv * 64, (kv + 1) * 64)
                        bS = (2, 3) if kv == 0 else (4, 5)
                        bO, bD = (0, 1) if kv == 0 else (6, 7)
                        Ps = []
                        for which, rr_ in enumerate((rp, rc)):
                            b = bS[which]
                            T.op("pe", f_mm(PB[b][:, :], kST[pr, rr_, :], qsT[pr, j, :]), r=[R_kST[rr_], R_qs[j]], w=[R_PB[b]])
                            ei = rot("E", 5)
                            T.op("act", f_act(Et[ei][:], PB[b][:, :], AF.Exp, scale=0.125), r=[R_PB[b]], w=[R_E[ei]])
                            pi_ = rot("P", 2)
                            msk = (prev0 if i == 0 else tri_prev) if which == 0 else tri_cur
                            T.op("dve", f_tt(Pt[pi_][:].rearrange("p (h t) -> p h t", h=4), Et[ei][:].rearrange("p (h t) -> p h t", h=4),
                                             msk.unsqueeze(1).to_broadcast([128, 4, 128]), ALU.mult), r=[R_E[ei], R_const], w=[R_P[pi_]])
                            Ps.append((pi_, rr_))
                        T.op("pe", f_seq([f_mm(PB[bO][0:64, :], vS[:, rr_, pr], Pt[pi_][:], w_ == 0, w_ == 1) for w_, (pi_, rr_) in enumerate(Ps)]
                                         + [f_mm(PB[bD][0:64, :], ones_b[:], Pt[pi_][:], w_ == 0, w_ == 1) for w_, (pi_, rr_) in enumerate(Ps)]),
                             r=[R_P[p_] for p_, _ in Ps] + [R_vS[r_] for _, r_ in Ps] + [R_const], w=[R_PB[bO], R_PB[bD]])
                        finish_attn(bO, bD, j, 4 * kv, es[:, 4 * kv:4 * kv + 4].unsqueeze(2).to_broadcast([64, 4, 128]))
                    _ckpt(12.1)
                    Es = [rot("E", 5), rot("E", 5)]
                    fns = []
                    for e in range(2):
                        pr = slice(e * 64, (e + 1) * 64)
                        for mc in range(2):
                            for jj in range(2):
                                fns.append(f_mm(PB[2 + 2 * mc + e][:, jj * 128:(jj + 1) * 128], mkT[pr, jj, mc * 128:(mc + 1) * 128],
                                                qmT[pr, j, jj * 128:(jj + 1) * 128]))
                    T.op("pe", f_seq(fns), r=[R_mkT, R_qm[j]], w=[R_PB[2], R_PB[3], R_PB[4], R_PB[5]])
                    for mc in range(2):
                        E4 = Et[Es[mc]][:].rearrange("p (jj e t) -> p jj e t", jj=2, e=2)
                        for e in range(2):
                            b = 2 + 2 * mc + e
                            T.op("act", f_act(E4[:, :, e, :], PB[b][:, 0:256].rearrange("p (jj t) -> p jj t", jj=2), AF.Exp, scale=0.125),
                                 r=[R_PB[b]], w=[R_E[Es[mc]]])
                    _ckpt(12.15)
                    fns = []
                    for hh in range(4):
                        for mc in range(2):
                            fns.append(f_mm(PB[0][0:64, hh * 128:(hh + 1) * 128], mv[:, mc, hh * 64:(hh + 1) * 64], Et[Es[mc]][:, hh * 128:(hh + 1) * 128], mc == 0, mc == 1))
                    for mc in range(2):
                        fns.append(f_mm(PB[1][0:64, :], ones_b[:], Et[Es[mc]][:], mc == 0, mc == 1))
                    T.op("pe", f_seq(fns), r=[R_E[e_] for e_ in Es] + [R_mv, R_const], w=[R_PB[0], R_PB[1]])
                    _ckpt(12.17)
                    finish_attn(0, 1, j, 12, None)
                    _ckpt(12.2)
                def dsa_index(j):
                    i, tok, nkb, nch, n = slot_vars(j)
                    for c in range(nch):
                        kb0 = 4 * c; nb = min(4, nkb - kb0); w_ = nb * 128
                        cols = slice(kb0 * 128, kb0 * 128 + w_)
                        bb = 4 * (c % 2)
                        T.op("pe", f_seq([f_mm(PB[bb + hh][:, 0:w_], qdiT[64:128, j, hh * 128:(hh + 1) * 128], kDI[64:128, cols]) for hh in range(4)]),
                             r=[R_qdi[j]] + R_kDI[kb0:kb0 + nb], w=[R_PB[bb + hh] for hh in range(4)])
                        for hh in range(4):
                            ri = rot("rl", 2)
                            T.op("act", f_act(rl[ri][:, 0:w_], PB[bb + hh][:, 0:w_], AF.Relu), r=[R_PB[bb + hh]], w=[R_rl[ri]])
                            if hh == 0:
                                T.op("dve", f_ts(BIG[:, cols], rl[ri][:, 0:w_], wiT[:, j, 0:1], None, ALU.mult), r=[R_rl[ri], R_wi[j]], w=[ISC[c]])
                            else:
                                T.op("dve", f_stt(BIG[:, cols], rl[ri][:, 0:w_], wiT[:, j, hh:hh + 1], BIG[:, cols], ALU.mult, ALU.add),
                                     r=[R_rl[ri], R_wi[j], ISC[c]], w=[ISC[c]])
                def dsa_bisect(j):
                    i, tok, nkb, nch, n = slot_vars(j)
                    _ckpt(12.3)
                    IR = ISC[0:nch]; MR = MSKR[0:nch]
                    cd = nch if nch < 2 else max(1, int(round(0.42 * nch)))
                    nd = min(n, cd * 512); na = n - nd
                    IRd, MRd, IRa, MRa = ISC[0:cd], MSKR[0:cd], ISC[cd:nch], MSKR[cd:nch]
                    amax = bis[:, 0:1]; wtot = bis[:, 1:2]; lo = bis[:, 2:3]; mid = bis[:, 3:4]; cn = bis[:, 4:5]; dl = bis[:, 5:6]
                    sa = bis[:, 6:7]; tt_ = bis[:, 7:8]
                    wks = bis[:, 8:8 + NITER]
                    T.op("dve", lambda h, a=amax, b=BIG[:, 0:n]: h.tensor_reduce(out=a, in_=b, axis=AX.X, op=ALU.max, apply_absolute_value=True),
                         r=IR, w=[R_lo])
                    T.op("pool", f_tt(BIG[:, 0:128], BIG[:, 0:128], p63bias, ALU.add), r=[ISC[0], R_cmf, R_lo], w=[ISC[0]])
                    dcol = slice((nkb - 1) * 128, nkb * 128)
                    T.op("pool", f_tt(BIG[:, dcol], BIG[:, dcol], dbias, ALU.add), r=[ISC[nch - 1], R_cmf, R_lo], w=[ISC[nch - 1]])
                    T.op("dve", f_ts(wtot, amax, 2.0002, 2e-6, ALU.mult, ALU.add), r=[R_lo], w=[R_lo])
                    T.op("dve", f_ts(lo, amax, -1.0001, -1e-6, ALU.mult, ALU.add), r=[R_lo], w=[R_lo])
                    T.op("dve", f_ts(wks, pw2[:], wtot, None, ALU.mult), r=[R_lo, R_const], w=[R_lo])
                    thrc = TOPK - na / 2.0
                    for k in range(NITER):
                        T.op("dve", f_tt(mid, lo, wks[:, k:k + 1], ALU.add), r=[R_lo], w=[R_mid])
                        if na > 0:
                            T.op("act", f_act(MSK[:, nd:n], BIG[:, nd:n], AF.Sign, bias=mid, scale=-1.0, accum=sa), r=IRa + [R_mid], w=MRa + [R_ca])
                        T.op("dve", f_ts(MSK[:, 0:nd], BIG[:, 0:nd], mid, 0.0, ALU.is_ge, ALU.add, accum=cn), r=IRd + [R_mid], w=MRd + [R_cd])
                        if na > 0:
                            T.op("dve", f_stt(tt_, sa, -0.5, cn, ALU.mult, ALU.add), r=[R_ca, R_cd], w=[R_t])
                            T.op("dve", f_stt(dl, tt_, thrc, wks[:, k:k + 1], ALU.is_ge, ALU.mult), r=[R_t, R_lo], w=[R_t])
                        else:
                            T.op("dve", f_stt(dl, cn, TOPK, wks[:, k:k + 1], ALU.is_ge, ALU.mult), r=[R_cd, R_lo], w=[R_t])
                        T.op("dve", f_tt(lo, lo, dl, ALU.add), r=[R_t], w=[R_lo])
                    T.op("dve", f_ts(MSK[:, 0:n], BIG[:, 0:n], lo, None, ALU.is_ge), r=IR + [R_lo], w=MR)
                def dsa_attn(j):
                    i, tok, nkb, nch, n = slot_vars(j)
                    _ckpt(12.4)
                    LA = 3
                    st_ = {}
                    T.op("dve", f_cp(qd0[0:64, :], qdiT[0:64, j, :]), r=[R_qdi[j]], w=[R_qd0])
                    def prep(c):
                        kb_ = 4 * c
                        nb = min(4, nkb - kb_)
                        mi = c % 2
                        pv = pbf(2)
                        T.op("pe", f_seq([f_tr(pv[:, q2 * 128:(q2 + 1) * 128], MSK[:, (kb_ + q2) * 128:(kb_ + q2 + 1) * 128], ident[:]) for q2 in range(nb)]),
                             r=[MSKR[c], R_const], w=[R_PB[2]])
                        T.op("act", f_act(mT[mi][:, 0:nb * 128], pv[:, 0:nb * 128], AF.Identity, scale=30000.0, bias=-30000.0), r=[R_PB[2]], w=[R_mT[mi]])
                    prep(0)
                    def front(kb):
                        c, q = kb // 4, kb % 4
                        if q == 0 and c + 1 < nch:
                            prep(c + 1)
                        mi = c % 2
                        b = 3 + (kb % 5)
                        T.op("pe", f_seq([f_mm(PB[b][:, :], kDI[:, kb * 128:(kb + 1) * 128], qd0[:, :], True, False)]
                                         + [f_mm(PB[b][:, hh * 128:(hh + 1) * 128], ident[:], mT[mi][:, q * 128:(q + 1) * 128], False, hh == 3) for hh in range(4)]),
                             r=[R_kDI[kb], R_qd0, R_mT[mi], R_const], w=[R_PB[b]])
                        ei = rot("E", 5)
                        T.op("act", f_act(Et[ei][:], PB[b][:, :], AF.Exp, scale=0.125), r=[R_PB[b]], w=[R_E[ei]])
                        st_[kb] = ei
                    def back(kb):
                        ei = st_.pop(kb)
                        T.op("pe", f_mm(PB[0][:, :], vD[:, kb, :], Et[ei][:], kb == 0, kb == nkb - 1),
                             r=[R_E[ei], R_vD[kb]], w=[R_PB[0]])
                    for t in range(nkb + LA):
                        if t < nkb: front(t)
                        if t - LA >= 0: back(t - LA)
                    T.op("act", f_act(dn[0][64:128, :], PB[0][64:128, :], AF.Copy), r=[R_PB[0]], w=[R_dn[0]])
                    T.op("pe", f_mm(PB[1][0:64, :], identf[:, 64:128], dn[0][:, :]), r=[R_dn[0], R_const], w=[R_PB[1]])
                    finish_attn(0, 1, j, 8, None)

                swa_mem(0)
                for j in range(4):
                    T.mark(f"g{g} s{j} idx")
                    dsa_index(j)
                    T.mark(f"g{g} s{j} bis")
                    dsa_bisect(j)
                    if j < 3:
                        swa_mem(j + 1)
                    T.mark(f"g{g} s{j} dattn")
                    dsa_attn(j)

                _ckpt(10 * (g + 1) + 3)
                T.mark(f"g{g} pass2")
                RQ = R_qs + R_qdi
                for oc in range(8):
                    G, RG = ring_get(3 + oc)
                    Gv = G[:, 0:8 * 384].rearrange("p (k c) -> p k c", k=8)
                    for jb in range(3):
                        T.op("pe", f_seq([f_mm(PB[jb][:, :], Gv[:, kc, jb * 128:(jb + 1) * 128], hTg[:, kc, :], kc == 0, kc == 7) for kc in range(8)]),
                             r=R_hTg + [RG], w=[R_PB[jb]])
                        T.op("act", f_act(gsb[jb][:], PB[jb][:, :], AF.Sigmoid, bias=bgate[:, oc * 3 + jb:oc * 3 + jb + 1], scale=1.0),
                             r=[R_PB[jb], R_misc], w=[R_gsb[jb]])
                    Pw, RP = ring_get(11 + oc)
                    Pv = Pw[0:64, 0:2048].rearrange("p (h c) -> p h c", h=16)
                    for jb, (h0, nh) in enumerate(((0, 8), (8, 4), (12, 4))):
                        T.op("pe", f_seq([f_mm(PB[3 + jb][:, :], Pv[:, h0 + hh, :], oTg[:, h0 + hh, :], hh == 0, hh == nh - 1) for hh in range(nh)]),
                             r=R_oTg + [RP], w=[R_PB[3 + jb]])
                    T.op("dve", f_tt(mg[0][:], PB[3][:, :], gsb[0][:], ALU.mult), r=[R_PB[3], R_gsb[0]], w=[R_mg[0]])
                    T.op("dve", f_tt(mg[1][:], PB[4][:, :], gsb[1][:], ALU.mult), r=[R_PB[4], R_gsb[1]], w=[R_mg[1]])
                    T.op("dve", f_tt(mg[0][:], mg[0][:], mg[1][:], ALU.add), r=[R_mg[0], R_mg[1]], w=[R_mg[0]])
                    T.op("dve", f_tt(mg[1][:], PB[5][:, :], gsb[2][:], ALU.mult), r=[R_PB[5], R_gsb[2]], w=[R_mg[1]])
                    T.op("dve", f_tt(mergedT[:, oc, :], mg[0][:], mg[1][:], ALU.add), r=[R_mg[0], R_mg[1]], w=RQ)

                _ckpt(10 * (g + 1) + 4)
                T.mark(f"g{g} wout")
                for cc in range(2):
                    W, RW = ring_get(19 + cc)
                    Wv = W[:, :].rearrange("p (k c) -> p k c", k=8)
                    for j in range(4):
                        pb = 6 + (j % 2)
                        T.op("pe", f_seq([f_mm(PB[pb][:, :], mergedT[:, kc, j * 128:(j + 1) * 128], Wv[:, kc, :], kc == 0, kc == 7) for kc in range(8)]),
                             r=RQ + [RW], w=[R_PB[pb]])
                        T.op("dve", f_tt(xg[:, j, cc * 512:(cc + 1) * 512], PB[pb][:, :], xg[:, j, cc * 512:(cc + 1) * 512], ALU.add),
                             r=[R_PB[pb], R_xg[j]], w=[R_xg[j]])

                _ckpt(10 * (g + 1) + 5)
                T.mark(f"g{g} mlp")
                for j in range(4):
                    norm_T(xg[:, j, :], [R_xg[j]], hTg[:, :, j * 128:(j + 1) * 128], [R_hTg[j]], j % 2)
                for fb in range(8):
                    W, RW = ring_get(21 + fb)
                    Wv = W[:, :].rearrange("p (k c) -> p k c", k=8)
                    for fs in range(4):
                        fc = fb * 4 + fs
                        pb = 2 + (fc % 4)
                        T.op("pe", f_seq([f_mm(PB[pb][:, :], Wv[:, kc, fs * 128:(fs + 1) * 128], hTg[:, kc, :], kc == 0, kc == 7) for kc in range(8)]),
                             r=R_hTg + [RW], w=[R_PB[pb]])
                        ri = rot("rl", 2)
                        T.op("act", f_act(rl[ri][:], PB[pb][:, :], AF.Relu), r=[R_PB[pb]], w=[R_rl[ri]])
                        T.op("dve", f_tt(hidT[:, fc, :], rl[ri][:], rl[ri][:], ALU.mult), r=[R_rl[ri]], w=[ISC[fc // 2]])
                for cc in range(2):
                    for ks in range(4):
                        W, RW = ring_get(29 + cc * 4 + ks)
                        Wv = W[:, :].rearrange("p (k c) -> p k c", k=8)
                        for j in range(4):
                            pb = 4 + j
                            T.op("pe", f_seq([f_mm(PB[pb][:, :], hidT[:, ks * 8 + fl, j * 128:(j + 1) * 128], Wv[:, fl, :],
                                                   ks == 0 and fl == 0, ks == 3 and fl == 7) for fl in range(8)]),
                                 r=ISC[ks * 4:(ks + 1) * 4] + [RW], w=[R_PB[pb]])
                    for j in range(4):
                        pb = 4 + j
                        T.op("dve", f_tt(xg[:, j, cc * 512:(cc + 1) * 512], PB[pb][:, :], xg[:, j, cc * 512:(cc + 1) * 512], ALU.add),
                             r=[R_PB[pb], R_xg[j]], w=[R_xg[j]])
                _ckpt(10 * (g + 1) + 6)
                T.mark(f"g{g} final")
                for j in range(4):
                    i = 4 * g + j
                    si = rot("stat", 4); s = stat[:, si * 4:(si + 1) * 4]; rs = R_stat[si]
                    hi = rot("hn", 1)
                    T.op("act", f_act(hn[hi][:], xg[:, j, :], AF.Square, accum=s[:, 0:1]), r=[R_xg[j]], w=[R_hn[hi], rs])
                    T.op("act", f_act(s[:, 1:2], s[:, 0:1], AF.Ln, scale=1.0 / 1024.0, bias=1e-6), r=[rs], w=[rs])
                    T.op("act", f_act(s[:, 2:3], s[:, 1:2], AF.Exp, scale=-0.5), r=[rs], w=[rs])
                    T.op("dve", f_stt(xg[:, j, :], xg[:, j, :], s[:, 2:3], gfin[:], ALU.mult, ALU.mult), r=[R_xg[j], rs, R_misc], w=[R_xg[j]])
                    T.dma("sp", out_d[i * 128:(i + 1) * 128, :], xg[:, j, :], R_xg[j], r=[R_xg[j]])
        try:
            emit()
        except _Stop:
            T.dma("sp", out_d[0:128, :], xo[:], R_rl[0], r=R_rl)
            T.wait_all("sp", R_rl + R_ring)
            T.wait_all("pool", [MSKR[0], MSKR[8], ISC[0], ISC[8]])
        T.wait_all("sp", R_xg)
        with nc.Block() as block:
            T.replay(block)
    return nc


OFF = dict(q_s=0, k_s=512, v_s=640, q_d=768, k_d=1024, v_d=1088, q_i=1152, k_i=1408, w_i=1472, q_m=1476, gates=1732)


def _chunked(w, cols):
    sub = w[:, cols]
    n = sub.shape[1]
    return np.ascontiguousarray(sub.reshape(8, 128, n).transpose(1, 0, 2).reshape(128, 8 * n))


def _prep_weights(w_in, w_proj_swa, w_proj_dsa, w_proj_mem, w_out, w_mlp_in, w_mlp_out):
    wch = np.zeros((NCHUNK, 128, 4096), np.float32)
    r64 = np.arange(64)
    qs_cols = np.concatenate([OFF["q_s"] + h * 64 + r64 for h in (0, 4, 1, 5, 2, 6, 3, 7)])
    qdi_cols = np.concatenate([np.concatenate([OFF["q_d"] + h * 64 + r64, OFF["q_i"] + h * 64 + r64]) for h in range(4)])
    qm_cols = np.concatenate([OFF["q_m"] + np.arange(256), OFF["w_i"] + np.arange(4)])
    wch[0] = _chunked(w_in, qs_cols)
    wch[1] = _chunked(w_in, qdi_cols)
    wch[2, :, 0:8 * 260] = _chunked(w_in, qm_cols)
    for oc in range(8):
        gc = np.concatenate([OFF["gates"] + jb * 1024 + oc * 128 + np.arange(128) for jb in range(3)])
        wch[3 + oc, :, 0:8 * 384] = _chunked(w_in, gc)
        wp = np.concatenate([w_proj_swa.reshape(8, 64, 1024), w_proj_dsa.reshape(4, 64, 1024), w_proj_mem.reshape(4, 64, 1024)], 0)
        wch[11 + oc, 0:64, 0:2048] = wp[:, :, oc * 128:(oc + 1) * 128].transpose(1, 0, 2).reshape(64, 2048)
    for cc in range(2):
        wch[19 + cc] = _chunked(w_out, cc * 512 + np.arange(512))
    for fb in range(8):
        wch[21 + fb] = _chunked(w_mlp_in, fb * 512 + np.arange(512))
    for cc in range(2):
        for ks in range(4):
            blk = w_mlp_out[ks * 1024:(ks + 1) * 1024, cc * 512:(cc + 1) * 512]
            wch[29 + cc * 4 + ks] = blk.reshape(8, 128, 512).transpose(1, 0, 2).reshape(128, 4096)
    k_cols = np.concatenate([OFF["k_s"] + np.arange(128), OFF["k_d"] + r64, OFF["k_i"] + r64, OFF["v_s"] + np.arange(128), OFF["v_d"] + r64])
    wk = _chunked(w_in, k_cols)
    return wch, wk


_NC_CACHE = {}


def kernel(x, mem, positions, g_mix, w_in, b_gate, sinks, g_mem, w_mem_kv, w_proj_swa, w_proj_dsa,
           w_proj_mem, w_out, g_mlp, w_mlp_in, w_mlp_out, g_final):
    f = lambda a: np.asarray(a, dtype=np.float32)
    x = f(x); mem = f(mem); positions = np.asarray(positions, dtype=np.int32)
    wch, wk = _prep_weights(f(w_in)[0], f(w_proj_swa)[0], f(w_proj_dsa)[0], f(w_proj_mem)[0], f(w_out)[0], f(w_mlp_in)[0], f(w_mlp_out)[0])
    wmem = _chunked(f(w_mem_kv)[0], np.arange(512))
    tr = lambda v: np.ascontiguousarray(f(v).reshape(8, 128).T)
    gvec = np.concatenate([tr(g_mix[0]), tr(g_mlp[0]), tr(g_mem[0])], 1)
    gfin = np.ascontiguousarray(np.broadcast_to(f(g_final)[None, :], (128, 1024)))
    bg = f(b_gate)[0].reshape(3, 8, 128)
    bgate = np.ascontiguousarray(bg.transpose(2, 1, 0).reshape(128, 24))
    sinkr = np.ascontiguousarray(np.broadcast_to(f(sinks)[0][None, :], (64, 8)))
    half = 32
    invf = np.power(np.float32(10000.0), -np.arange(half, dtype=np.float32) / np.float32(half)).astype(np.float32)
    invf = np.ascontiguousarray(np.broadcast_to(invf[None, :], (128, 32)))
    s_ = np.arange(128)[:, None]; t_ = np.arange(128)[None, :]
    tri_cur = (t_ >= s_).astype(np.float32)
    tri_prev = (s_ > t_).astype(np.float32)
    dbias = np.where(t_ <= s_, 0.0, NEG).astype(np.float32)
    in_maps = []
    for c in range(8):
        b, par = c // 2, c % 2
        order = np.arange(64) if par == 0 else np.concatenate([np.arange(1, 64), [0]])
        xb = x[b].reshape(64, 128, 1024)[order].reshape(64 * 128, 1024)
        pb = positions[b].reshape(64, 128)[order]
        posc = np.ascontiguousarray(pb.T)
        prev0 = tri_prev * float(par)
        p63 = np.full((128, 128), 0.0 if par == 1 else NEG, np.float32)
        cmask = np.ascontiguousarray(np.stack([tri_cur, tri_prev, prev0, dbias, p63], 1).reshape(128, 5 * 128).astype(np.float32))
        in_maps.append(dict(xc=np.ascontiguousarray(xb), posc=posc, memc=np.ascontiguousarray(mem[b]), cmask=cmask, invf=invf,
                            wk=wk, wch=wch, wmem=wmem, gvec=gvec, gfin=gfin, bgate=bgate, sinkr=sinkr))
    if "nc" not in _NC_CACHE:
        _NC_CACHE["nc"] = build_program()
    nc = _NC_CACHE["nc"]
    res = run_bass_kernel_spmd(nc, in_maps, core_ids=list(range(8)))
    out = np.zeros((4, 64, 128, 1024), np.float32)
    for c in range(8):
        b, par = c // 2, c % 2
        o = np.asarray(res.results[c]["out"]).reshape(32, 128, 1024)
        out[b, par::2] = o
    return out.reshape(4, 8192, 1024)
```

```python
import math
import numpy as np
from contextlib import ExitStack
import concourse.bass as bass
import concourse.mybir as mybir
from concourse.bass_utils import run_bass_kernel_spmd

F32 = mybir.dt.float32; BF16 = mybir.dt.bfloat16; I32 = mybir.dt.int32
ALU = mybir.AluOpType; AF = mybir.ActivationFunctionType; AX = mybir.AxisListType

STAGE = 99
class _Stop(Exception):
    pass
def _ckpt(n):
    if STAGE <= n:
        raise _Stop()
NITER = 14
NEG = -1.0e30
NCHUNK = 37
TOPK = 256.0


class Reg:
    __slots__ = ("name", "w", "r", "dsem", "dcnt")
    def __init__(self, name):
        self.name = name; self.w = None; self.r = {}; self.dsem = None; self.dcnt = 0


class Eng:
    def __init__(self, name):
        self.name = name; self.q = []; self.sem = None; self.cnt = 0; self.seen = {}


class Trk:
    def __init__(self, nc, stack, nsem):
        self.nc = nc
        self.sems = [stack.enter_context(nc.semaphore(f"s{i}")) for i in range(nsem)]
        self.si = 0
        self.E = {n: Eng(n) for n in ("pe", "act", "dve", "pool", "sp")}
        for e in self.E.values():
            e.sem = self.newsem()
    def newsem(self):
        s = self.sems[self.si]; self.si += 1; return s
    def _deps(self, eng, reads, writes):
        deps = {}
        def add(ev, kind):
            if ev is None: return
            sem, val, src = ev
            if src is eng and eng.name == "pe":
                return
            k = id(sem)
            if eng.seen.get(k, 0) >= val: return
            if k not in deps or deps[k][1] < val: deps[k] = (sem, val)
        for r in reads: add(r.w, "raw")
        for w in writes:
            add(w.w, "waw")
            for ev in w.r.values(): add(ev, "war")
        out = list(deps.values())
        for sem, val in out: eng.seen[id(sem)] = val
        return out
    def op(self, en, fn, r=(), w=()):
        eng = self.E[en]
        waits = self._deps(eng, r, w)
        eng.cnt += 1
        ev = (eng.sem, eng.cnt, eng)
        eng.q.append((waits, fn, (eng.sem, 1)))
        for x in r: x.r[en] = ev
        for x in w: x.w = ev; x.r = {}
    def dma(self, en, out_ap, in_ap, slot, r=(), w=()):
        eng = self.E[en]
        waits = self._deps(eng, r, w)
        if slot.dsem is None: slot.dsem = self.newsem()
        slot.dcnt += 16
        ev = (slot.dsem, slot.dcnt, None)
        eng.q.append((waits, lambda h: h.dma_start(out=out_ap, in_=in_ap), (slot.dsem, 16)))
        for x in r: x.r["dma%d" % id(slot)] = ev
        for x in w: x.w = ev; x.r = {}
    def mark(self, label):
        self.E["pe"].q.append(([], None, label))
    def wait_all(self, en, regs):
        eng = self.E[en]
        waits = self._deps(eng, [], regs)
        eng.q.append((waits, None, None))
    def replay(self, block):
        def mk(en):
            q = self.E[en].q
            def body(h):
                for waits, fn, inc in q:
                    for sem, val in waits: h.wait_ge(sem, val)
                    if fn is None:
                        if isinstance(inc, str): MARKS.append((inc, PE_CNT[0]))
                        continue
                    ins = fn(h)
                    ins.then_inc(inc[0], inc[1])
            return body
        block.tensor(mk("pe")); block.scalar(mk("act")); block.vector(mk("dve"))
        block.gpsimd(mk("pool")); block.sync(mk("sp"))


PE_CNT = [0]
MARKS = []
def f_mm(out, lhsT, rhs, start=True, stop=True):
    def f(h):
        PE_CNT[0] += 1
        return h.matmul(out, lhsT=lhsT, rhs=rhs, start=start, stop=stop)
    return f
def f_tr(out, in_, ident):
    def f(h):
        PE_CNT[0] += 1
        return h.transpose(out=out, in_=in_, identity=ident)
    return f
def f_seq(fns):
    def f(h):
        ins = None
        for fn in fns: ins = fn(h)
        return ins
    return f
def f_act(out, in_, func, bias=None, scale=None, accum=None):
    kw = {}
    if bias is not None: kw["bias"] = bias
    if scale is not None: kw["scale"] = scale
    if accum is not None: kw["accum_out"] = accum
    return lambda h: h.activation(out=out, in_=in_, func=func, **kw)
def f_ts(out, in0, s1, s2=None, op0=ALU.mult, op1=None, accum=None):
    kw = {}
    if op1 is not None: kw["op1"] = op1
    if accum is not None: kw["accum_out"] = accum
    return lambda h: h.tensor_scalar(out=out, in0=in0, scalar1=s1, scalar2=s2, op0=op0, **kw)
def f_tt(out, in0, in1, op):
    return lambda h: h.tensor_tensor(out=out, in0=in0, in1=in1, op=op)
def f_stt(out, in0, scalar, in1, op0, op1):
    return lambda h: h.scalar_tensor_tensor(out=out, in0=in0, scalar=scalar, in1=in1, op0=op0, op1=op1)
def f_cp(out, in_):
    return lambda h: h.tensor_copy(out=out, in_=in_)
def f_memset(ap, v):
    return lambda h: h.memset(ap, v)


def build_program():
    nc = bass.Bass("TRN2", target_bir_lowering=False)
    dt_in = lambda n, s, d=F32: nc.dram_tensor(n, s, d, kind="ExternalInput").ap()
    xc = dt_in("xc", [64 * 128, 1024])
    posc = dt_in("posc", [128, 64], I32)
    memc = dt_in("memc", [256, 1024])
    cmask = dt_in("cmask", [128, 5 * 128])
    invf_d = dt_in("invf", [128, 32])
    wk_d = dt_in("wk", [128, 8 * 448])
    wch_d = dt_in("wch", [NCHUNK, 128, 4096])
    wmem_d = dt_in("wmem", [128, 4096])
    gvec_d = dt_in("gvec", [128, 24])
    gfin_d = dt_in("gfin", [128, 1024])
    bgate_d = dt_in("bgate", [128, 24])
    sinkr_d = dt_in("sinkr", [64, 8])
    out_d = nc.dram_tensor("out", [32 * 128, 1024], F32, kind="ExternalOutput").ap()
    wbf_d = nc.dram_tensor("wbf", [NCHUNK, 128, 4096], BF16, kind="Internal").ap()

    with ExitStack() as st:
        T = Trk(nc, st, 48)
        def sb(name, shape, dt):
            return st.enter_context(nc.sbuf_tensor(name, shape, dt))
        BIG = sb("BIG", [128, 8192], F32)
        MSK = sb("MSK", [128, 8192], BF16)
        ISC = [Reg(f"isc{c}") for c in range(16)]
        MSKR = [Reg(f"msk{c}") for c in range(16)]
        hidT = BIG[:].bitcast(BF16).rearrange("p (f t) -> p f t", t=512)
        wk_sb = sb("wk_sb", [128, 8, 448], BF16); R_wk = Reg("wk")
        kDI = sb("kDI", [128, 8192], BF16); R_kDI = [Reg(f"kDI{i}") for i in range(64)]
        vD = sb("vD", [128, 64, 128], BF16); R_vD = [Reg(f"vD{i}") for i in range(64)]
        kST = sb("kST", [128, 16, 128], BF16); R_kST = [Reg(f"kST{i}") for i in range(16)]
        vS = sb("vS", [128, 16, 128], BF16); R_vS = [Reg(f"vS{i}") for i in range(16)]
        mkT = sb("mkT", [128, 2, 256], BF16); R_mkT = Reg("mkT")
        mv = sb("mv", [128, 2, 256], BF16); R_mv = Reg("mv")
        xg = sb("xg", [128, 4, 1024], F32); R_xg = [Reg(f"xg{j}") for j in range(4)]
        hTg = sb("hTg", [128, 8, 512], BF16); R_hTg = [Reg(f"hTg{j}") for j in range(4)]
        hTo = sb("hTo", [128, 8, 128], BF16); R_hTo = Reg("hTo")
        oTg = sb("oTg", [64, 16, 512], BF16); R_oTg = [Reg(f"oTg{j}") for j in range(4)]
        QM = sb("QM", [128, 4096], BF16)
        qsT = QM[:, 0:2048].rearrange("p (s c) -> p s c", s=4)
        qdiT = QM[:, 2048:4096].rearrange("p (s c) -> p s c", s=4)
        mergedT = QM[:].rearrange("p (k t) -> p k t", k=8)
        R_qs = [Reg(f"qs{j}") for j in range(4)]; R_qdi = [Reg(f"qdi{j}") for j in range(4)]
        qmT = sb("qmT", [128, 4, 256], BF16); R_qm = [Reg(f"qm{j}") for j in range(4)]
        wiT = sb("wiT", [128, 4, 4], F32); R_wi = [Reg(f"wi{j}") for j in range(4)]
        NRING = 2
        ring = [sb(f"ring{i}", [128, 4096], BF16) for i in range(NRING)]
        R_ring = [Reg(f"ring{i}") for i in range(NRING)]
        ident = sb("ident", [128, 128], BF16); identf = sb("identf", [128, 128], F32); onesf = sb("onesf", [128, 128], F32)
        ones_b = sb("ones_b", [128, 64], BF16)
        qd0 = sb("qd0", [128, 512], BF16); R_qd0 = Reg("qd0")
        R_const = Reg("const")
        cm_f = sb("cm_f", [128, 5, 128], F32)
        cm_b = sb("cm_b", [128, 3, 128], BF16)
        invf = sb("invf_sb", [128, 32], F32)
        gvec = sb("gvec_sb", [128, 24], F32)
        gfin = sb("gfin_sb", [128, 1024], F32)
        bgate = sb("bgate_sb", [128, 24], F32)
        es = sb("es_sb", [64, 8], F32)
        pw2 = sb("pw2", [128, NITER], F32)
        posi = sb("posi", [128, 64], I32); posf = sb("posf", [128, 64], F32)
        cosT = sb("cosT", [128, 8, 32], F32); sinT = sb("sinT", [128, 8, 32], F32); R_cs = Reg("cs")
        rtmp = [sb(f"rtmp{i}", [128, 256], F32) for i in range(3)]; rtmpi = sb("rtmpi", [128, 256], I32); R_rt = Reg("rt")
        stat = sb("stat", [128, 16], F32); R_stat = [Reg(f"stat{i}") for i in range(4)]
        hn = [sb(f"hn{i}", [128, 1024], BF16) for i in range(1)]; R_hn = [Reg(f"hn{i}") for i in range(1)]
        tok_b = [sb(f"tok_b{i}", [128, 512], BF16) for i in range(2)]; R_tok = [Reg(f"tok{i}") for i in range(2)]
        Et = [sb(f"Et{i}", [128, 512], BF16) for i in range(5)]; R_E = [Reg(f"E{i}") for i in range(5)]
        Pt = [sb(f"Pt{i}", [128, 512], BF16) for i in range(2)]; R_P = [Reg(f"P{i}") for i in range(2)]
        RLX = sb("RLX", [128, 1024], F32); rl = [RLX[:, 0:512], RLX[:, 512:1024]]; R_rl = [Reg(f"rl{i}") for i in range(2)]
        xo = RLX; R_xo = R_rl[0]
        mT = [sb(f"mT{i}", [128, 512], BF16) for i in range(2)]; R_mT = [Reg(f"mT{i}") for i in range(2)]
        dn = [sb(f"dn{i}", [128, 512], F32) for i in range(1)]; R_dn = [Reg(f"dn{i}") for i in range(1)]
        mg = rl; R_mg = R_rl
        RTU = sb("RTU", [128, 1024], F32); rope_t = RTU[:, 0:512]; rope_u = RTU[:, 512:1024]; R_rtu = [Reg("rt_"), Reg("ru_")]
        gsb = [sb(f"gsb{i}", [128, 512], BF16) for i in range(3)]; R_gsb = [Reg(f"gsb{i}") for i in range(3)]
        bis = sb("bis", [128, 8 + NITER], F32); R_lo = Reg("lo"); R_mid = Reg("mid"); R_cd = Reg("cd"); R_ca = Reg("ca"); R_t = Reg("t")
        PB = [st.enter_context(nc.psum_tensor(f"pb{i}", [128, 512], F32)) for i in range(8)]
        R_PB = [Reg(f"pb{i}") for i in range(8)]
        def pbf(i):
            return PB[i][:].bitcast(BF16)

        cnt = {"ring": 0, "stat": 0, "hn": 0, "tok": 0, "E": 0, "P": 0, "rl": 0, "mT": 0, "dn": 0, "mg": 0}
        def rot(name, n):
            i = cnt[name] % n; cnt[name] += 1; return i

        R_cmf = Reg("cmf"); R_misc = Reg("misc")
        T.dma("sp", cm_f[:].rearrange("p a b -> p (a b)"), cmask[:, :], R_cmf, w=[R_cmf])
        for (dst, src) in ((invf, invf_d), (gvec, gvec_d), (gfin, gfin_d), (bgate, bgate_d)):
            T.dma("sp", dst[:], src[:, :], R_misc, w=[R_misc])
            T.wait_all("sp", [R_misc])
        R_es = Reg("es")
        T.dma("sp", es[:], sinkr_d[:, :], R_es, w=[R_es])
        R_pos = Reg("pos")
        T.dma("sp", posi[:], posc[:, :], R_pos, w=[R_pos])
        T.op("pool", f_memset(onesf[:], 1.0), w=[R_const])
        T.op("pool", f_memset(identf[:], 0.0), w=[R_const])
        T.op("pool", lambda h: h.affine_select(out=identf[:], in_=onesf[:], pattern=[[-1, 128]], compare_op=ALU.is_equal,
                                                fill=0.0, base=0, channel_multiplier=1), r=[R_const], w=[R_const])
        T.op("dve", f_cp(ident[:], identf[:]), r=[R_const], w=[R_const])
        T.op("dve", f_memset(ones_b[:], 1.0), w=[R_const])
        T.op("pool", f_memset(vD[:].rearrange("p a b -> p (a b)"), 1.0), w=R_vD)
        T.op("dve", f_memset(dn[0][:], 0.0), w=[R_dn[0]])
        T.op("dve", f_memset(qd0[:], 0.0), w=[R_qd0])
        for k in range(NITER):
            T.op("dve", f_memset(pw2[:, k:k + 1], 2.0 ** (-(k + 1))), w=[R_const])
        T.op("dve", f_cp(cm_b[:], cm_f[:, 0:3, :]), r=[R_cmf], w=[R_const])
        T.op("act", f_act(es[:], es[:], AF.Exp), r=[R_es], w=[R_es])
        T.op("dve", f_cp(posf[:], posi[:]), r=[R_pos], w=[R_pos])
        tri_cur = cm_b[:, 0, :]; tri_prev = cm_b[:, 1, :]; prev0 = cm_b[:, 2, :]
        dbias = cm_f[:, 3, :]; p63bias = cm_f[:, 4, :]

        def emit():
            def norm_T(x_ap, x_regs, dstT, dst_regs, pbank):
                si = rot("stat", 4); s = stat[:, si * 4:(si + 1) * 4]; rs = R_stat[si]
                hi = rot("hn", 1); h_ = hn[hi]; rh = R_hn[hi]
                T.op("act", f_act(h_[:], x_ap, AF.Square, accum=s[:, 0:1]), r=x_regs, w=[rh, rs])
                T.op("act", f_act(s[:, 1:2], s[:, 0:1], AF.Ln, scale=1.0 / 1024.0, bias=1e-6), r=[rs], w=[rs])
                T.op("act", f_act(s[:, 2:3], s[:, 1:2], AF.Exp, scale=-0.5), r=[rs], w=[rs])
                T.op("dve", f_ts(h_[:], x_ap, s[:, 2:3], None, ALU.mult), r=x_regs + [rs], w=[rh])
                pv = pbf(pbank)
                T.op("pe", f_seq([f_tr(pv[:, kc * 128:(kc + 1) * 128], h_[:, kc * 128:(kc + 1) * 128], ident[:]) for kc in range(8)]),
                     r=[rh, R_const], w=[R_PB[pbank]])
                T.op("act", f_act(dstT, pv[:, :].rearrange("p (k t) -> p k t", k=8), AF.Copy), r=[R_PB[pbank]], w=dst_regs)
                return s, rs

            def rope_tables(blk0, nb):
                n = nb * 32
                a0 = rtmp[0][:, 0:n]; a1 = rtmp[1][:, 0:n]; a2 = rtmp[2][:, 0:n]; ai = rtmpi[:, 0:n]
                v3 = lambda a: a.rearrange("p (b f) -> p b f", f=32)
                T.op("dve", f_tt(v3(a0), posf[:, blk0:blk0 + nb].unsqueeze(2).to_broadcast([128, nb, 32]),
                                 invf[:].unsqueeze(1).to_broadcast([128, nb, 32]), ALU.mult), r=[R_pos, R_misc], w=[R_rt])
                T.op("dve", f_ts(ai, a0, 1.0 / (2 * math.pi), None, ALU.mult), r=[R_rt], w=[R_rt])
                T.op("dve", f_cp(a1, ai), r=[R_rt], w=[R_rt])
                T.op("dve", f_stt(a2, a1, -6.28125, a0, ALU.mult, ALU.add), r=[R_rt], w=[R_rt])
                T.op("dve", f_stt(a2, a1, -0.0019353072, a2, ALU.mult, ALU.add), r=[R_rt], w=[R_rt])
                T.op("dve", f_ts(a0, a2, -3.1415925, 3.1415925, ALU.max, ALU.min), r=[R_rt], w=[R_rt])
                T.op("act", f_act(sinT[:, 0:nb, :].rearrange("p b f -> p (b f)"), a0, AF.Sin), r=[R_rt], w=[R_cs])
                T.op("dve", f_ts(a1, a2, math.pi / 2, None, ALU.add), r=[R_rt], w=[R_rt])
                T.op("dve", f_ts(a0, a1, math.pi, -2 * math.pi, ALU.is_gt, ALU.mult), r=[R_rt, R_cs], w=[R_rt])
                T.op("dve", f_tt(a0, a0, a1, ALU.add), r=[R_rt], w=[R_rt])
                T.op("dve", f_ts(a0, a0, -3.1415925, 3.1415925, ALU.max, ALU.min), r=[R_rt], w=[R_rt])
                T.op("act", f_act(cosT[:, 0:nb, :].rearrange("p b f -> p (b f)"), a0, AF.Sin), r=[R_rt], w=[R_cs])

            def rope_apply(src, src_regs, dst, dst_regs, H, cb):
                n = H * 64
                t_ = rope_t[:, 0:n]; u_ = rope_u[:, 0:n]
                cosb = cosT[:, cb:cb + 1, :]; sinb = sinT[:, cb:cb + 1, :]
                T.op("dve", f_tt(t_.rearrange("p (a f) -> p a f", f=32), src.rearrange("p (a f) -> p a f", f=32),
                                 cosb.to_broadcast([128, 2 * H, 32]), ALU.mult), r=src_regs + [R_cs], w=[R_rtu[0]])
                s4 = src.rearrange("p (h e f) -> p h e f", e=2, f=32)
                u4 = u_.rearrange("p (h e f) -> p h e f", e=2, f=32)
                t4 = t_.rearrange("p (h e f) -> p h e f", e=2, f=32)
                d4 = dst.rearrange("p (h e f) -> p h e f", e=2, f=32)
                sb_ = sinb.to_broadcast([128, H, 32])
                T.op("dve", f_tt(u4[:, :, 0, :], s4[:, :, 1, :], sb_, ALU.mult), r=src_regs + [R_cs], w=[R_rtu[1]])
                T.op("dve", f_tt(u4[:, :, 1, :], s4[:, :, 0, :], sb_, ALU.mult), r=src_regs + [R_cs], w=[R_rtu[1]])
                T.op("dve", f_tt(d4[:, :, 0, :], t4[:, :, 0, :], u4[:, :, 0, :], ALU.subtract), r=R_rtu, w=dst_regs)
                T.op("dve", f_tt(d4[:, :, 1, :], t4[:, :, 1, :], u4[:, :, 1, :], ALU.add), r=R_rtu, w=dst_regs)

            R_wbf = [Reg(f"wbf{i}") for i in range(NCHUNK)]
            CH_N = [4096, 4096, 8 * 260] + [8 * 384] * 8 + [2048] * 8 + [4096] * 18
            CH_P = [128] * 11 + [64] * 8 + [128] * 18
            sched = [0, 1, 2] + [v for oc in range(8) for v in (3 + oc, 11 + oc)] + list(range(19, 37))
            ring_state = {"issued": 0, "got": 0}
            total_gets = 8 * NCHUNK
            def ring_issue():
                k = ring_state["issued"]
                if k >= total_gets: return
                c = sched[k % NCHUNK]; b = k % NRING
                T.dma("sp", ring[b][0:CH_P[c], 0:CH_N[c]], wbf_d[c, 0:CH_P[c], 0:CH_N[c]], R_ring[b], r=[R_wbf[c]], w=[R_ring[b]])
                ring_state["issued"] += 1
            def ring_get(expect):
                k = ring_state["got"]
                assert sched[k % NCHUNK] == expect, (k, expect)
                while ring_state["issued"] < min(k + NRING, total_gets):
                    ring_issue()
                ring_state["got"] += 1
                b = k % NRING
                return ring[b], R_ring[b]

            def convert(src_dram_ap, npart, nelem, ncols, goff, dst_ap, dst_regs, half):
                stg = BIG[0:npart, half * 4096: half * 4096 + nelem]
                sregs = ISC[half * 8:(half + 1) * 8]
                T.dma("pool", stg, src_dram_ap, sregs[0], w=sregs)
                if goff is None:
                    T.op("act", f_act(dst_ap, stg, AF.Copy), r=sregs, w=dst_regs)
                else:
                    for kc in range(8):
                        T.op("dve", f_ts(dst_ap[:, kc * ncols:(kc + 1) * ncols], stg[:, kc * ncols:(kc + 1) * ncols],
                                          gvec[:, goff + kc:goff + kc + 1], None, ALU.mult), r=sregs + [R_misc], w=dst_regs)
            _ckpt(1)
            convert(wk_d[:, :], 128, 8 * 448, 448, 0, wk_sb[:].rearrange("p k c -> p (k c)"), [R_wk], 0)
            wmem_b = MSK[:, 4096:8192]
            convert(wmem_d[:, :], 128, 4096, 512, 16, wmem_b, MSKR[8:16], 1)

            for mc in range(2):
                T.dma("sp", xo[:], memc[mc * 128:(mc + 1) * 128, :], R_rl[0], w=R_rl)
                norm_T(xo[:], R_rl, hTo[:], [R_hTo], 0)
                T.op("pe", f_seq([f_mm(PB[1][:, :], hTo[:, kc, :], wmem_b[:, kc * 512:(kc + 1) * 512], kc == 0, kc == 7) for kc in range(8)]),
                     r=[R_hTo] + MSKR[8:16], w=[R_PB[1]])
                ti = rot("tok", 2)
                T.op("act", f_act(tok_b[ti][:, 0:256], PB[1][:, 0:256], AF.Copy), r=[R_PB[1]], w=[R_tok[ti]])
                T.op("act", f_act(mv[:, mc, :], PB[1][:, 256:512], AF.Copy), r=[R_PB[1]], w=[R_mv])
                pv = pbf(2)
                T.op("pe", f_seq([f_tr(pv[:, jj * 128:(jj + 1) * 128], tok_b[ti][:, jj * 128:(jj + 1) * 128], ident[:]) for jj in range(2)]),
                     r=[R_tok[ti], R_const], w=[R_PB[2]])
                T.op("act", f_act(mkT[:, :, mc * 128:(mc + 1) * 128], pv[:, 0:256].rearrange("p (j m) -> p j m", j=2), AF.Copy),
                     r=[R_PB[2]], w=[R_mkT])

            _ckpt(2)
            for c in range(NCHUNK):
                half = c % 2
                goff = 0 if c < 11 else (8 if 21 <= c < 29 else None)
                ncols = {0: 512, 1: 512, 2: 260}.get(c, 384 if c < 11 else 512)
                npart = CH_P[c]; nelem = CH_N[c]
                dstb = MSK[0:npart, half * 4096: half * 4096 + nelem]
                dregs = MSKR[half * 8:(half + 1) * 8]
                convert(wch_d[c, 0:npart, 0:nelem], npart, nelem, ncols, goff, dstb, dregs, half)
                T.dma("pool", wbf_d[c, 0:npart, 0:nelem], dstb, dregs[0], r=dregs, w=[R_wbf[c]])

            def kside(pos, x_ap, x_regs, hT_ap, hT_regs, cb, stage="AB"):
                kidx = 0 if pos == 63 else pos + 1
                rg = (pos + 1) % 16
                if "A" in stage:
                    norm_T(x_ap, x_regs, hT_ap, hT_regs, 0)
                if "B" in stage or "M" in stage:
                    T.op("pe", f_seq([f_mm(PB[1][:, 0:448], hT_ap[:, kc, :], wk_sb[:, kc, :], kc == 0, kc == 7) for kc in range(8)]),
                         r=hT_regs + [R_wk], w=[R_PB[1]])
                if "B" not in stage and "R" not in stage:
                    return
                ti = rot("tok", 2)
                rope_apply(PB[1][:, 0:256], [R_PB[1]], tok_b[ti][:, 0:256], [R_tok[ti]], 4, cb)
                _ckpt(3.3)
                T.op("dve", f_cp(vS[:, rg, :], PB[1][:, 256:384]), r=[R_PB[1]], w=[R_vS[rg]])
                T.op("dve", f_cp(vD[:, kidx, 0:64], PB[1][:, 384:448]), r=[R_PB[1]], w=[R_vD[kidx]])
                _ckpt(3.4)
                pv = pbf(2)
                T.op("pe", f_seq([f_tr(pv[:, jj * 128:(jj + 1) * 128], tok_b[ti][:, jj * 128:(jj + 1) * 128], ident[:]) for jj in range(2)]),
                     r=[R_tok[ti], R_const], w=[R_PB[2]])
                _ckpt(3.5)
                T.op("act", f_act(kST[:, rg, :], pv[:, 0:128], AF.Copy), r=[R_PB[2]], w=[R_kST[rg]])
                T.op("act", f_act(kDI[:, kidx * 128:(kidx + 1) * 128], pv[:, 128:256], AF.Copy), r=[R_PB[2]], w=[R_kDI[kidx]])

            _ckpt(3)
            rope_tables(63, 1)
            T.dma("sp", xo[:], xc[63 * 128:64 * 128, :], R_rl[0], w=R_rl)
            kside(63, xo[:], R_rl, hTo[:], [R_hTo], 0)

            def finish_attn(bO, bD, j, h0, es_ap):
                di = rot("dn", 1)
                d_ = dn[di][0:64, :]
                if es_ap is not None:
                    T.op("dve", f_tt(d_.rearrange("p (h t) -> p h t", h=4), PB[bD][0:64, :].rearrange("p (h t) -> p h t", h=4), es_ap, ALU.add),
                         r=[R_PB[bD], R_es], w=[R_dn[di]])
                    T.op("act", f_act(d_, d_, AF.Ln), r=[R_dn[di]], w=[R_dn[di]])
                else:
                    T.op("act", f_act(d_, PB[bD][0:64, :], AF.Ln), r=[R_PB[bD]], w=[R_dn[di]])
                T.op("act", f_act(d_, d_, AF.Exp, scale=-1.0), r=[R_dn[di]], w=[R_dn[di]])
                T.op("dve", f_tt(oTg[:, h0:h0 + 4, j * 128:(j + 1) * 128], PB[bO][0:64, :].rearrange("p (h t) -> p h t", h=4),
                                 d_.rearrange("p (h t) -> p h t", h=4), ALU.mult), r=[R_PB[bO], R_dn[di]], w=[R_oTg[j]])

            for g in range(8):
                T.mark(f"g{g} kside")
                npos = 8 if g < 7 else 7
                rope_tables(8 * g, 8)
                def kblock(pl, stage):
                    pos = 8 * g + pl
                    if pl % 2 == 0:
                        j = pl // 2
                        if "A" in stage:
                            T.dma("sp", xg[:, j, :], xc[pos * 128:(pos + 1) * 128, :], R_xg[j], w=[R_xg[j]])
                        kside(pos, xg[:, j, :], [R_xg[j]], hTg[:, :, j * 128:(j + 1) * 128], [R_hTg[j]], pl, stage)
                    else:
                        if "A" in stage:
                            T.dma("sp", xo[:], xc[pos * 128:(pos + 1) * 128, :], R_rl[0], w=R_rl)
                        kside(pos, xo[:], R_rl, hTo[:], [R_hTo], pl, stage)
                kblock(0, "A")
                for pl in range(npos):
                    kblock(pl, "M")
                    if pl + 1 < npos:
                        kblock(pl + 1, "A")
                    kblock(pl, "R")

                _ckpt(10 * (g + 1) + 1)
                T.mark(f"g{g} qproj")
                units = [(c, j) for c in range(3) for j in range(4)]
                qW = {}
                def q_mm(u):
                    c, j = units[u]
                    if j == 0:
                        W, RW = ring_get(c)
                        qW[c] = (W, RW)
                    W, RW = qW[c]
                    ncols = (512, 512, 260)[c]
                    Wv = W[:, 0:8 * ncols].rearrange("p (k c) -> p k c", k=8)
                    pb = 3 + (u % 2)
                    T.op("pe", f_seq([f_mm(PB[pb][:, 0:ncols], hTg[:, kc, j * 128:(j + 1) * 128], Wv[:, kc, :], kc == 0, kc == 7) for kc in range(8)]),
                         r=[R_hTg[j], RW], w=[R_PB[pb]])
                def q_post(u):
                    c, j = units[u]
                    pb = 3 + (u % 2); pt = 5 + (u % 2)
                    ti = rot("tok", 2)
                    if c < 2:
                        rope_apply(PB[pb][:, 0:512], [R_PB[pb]], tok_b[ti][:, 0:512], [R_tok[ti]], 8, 2 * j)
                        pv = pbf(pt)
                        T.op("pe", f_seq([f_tr(pv[:, q * 128:(q + 1) * 128], tok_b[ti][:, q * 128:(q + 1) * 128], ident[:]) for q in range(4)]),
                             r=[R_tok[ti], R_const], w=[R_PB[pt]])
                        dst = qsT if c == 0 else qdiT
                        T.op("act", f_act(dst[:, j, :], pv[:, 0:512], AF.Copy), r=[R_PB[pt]], w=[(R_qs if c == 0 else R_qdi)[j]])
                    else:
                        T.op("act", f_act(tok_b[ti][:, 0:256], PB[pb][:, 0:256], AF.Copy), r=[R_PB[pb]], w=[R_tok[ti]])
                        T.op("act", f_act(wiT[:, j, :], PB[pb][:, 256:260], AF.Copy, scale=1.0 / 16.0), r=[R_PB[pb]], w=[R_wi[j]])
                        pv = pbf(pt)
                        T.op("pe", f_seq([f_tr(pv[:, q * 128:(q + 1) * 128], tok_b[ti][:, q * 128:(q + 1) * 128], ident[:]) for q in range(2)]),
                             r=[R_tok[ti], R_const], w=[R_PB[pt]])
                        T.op("act", f_act(qmT[:, j, :], pv[:, 0:256], AF.Copy), r=[R_PB[pt]], w=[R_qm[j]])
                q_mm(0)
                for u in range(len(units)):
                    if u + 1 < len(units):
                        q_mm(u + 1)
                    q_post(u)

                _ckpt(10 * (g + 1) + 2)
                T.mark(f"g{g} attn")
                def slot_vars(j):
                    i = 4 * g + j
                    nkb = 2 * i + 2
                    return i, slice(j * 128, (j + 1) * 128), nkb, (nkb + 3) // 4, nkb * 128
                def fin_gen(bO, bD, j, h0, es_ap):
                    di = rot("dn", 1)
                    d_ = dn[di][0:64, :]
                    if es_ap is not None:
                        T.op("dve", f_tt(d_.rearrange("p (h t) -> p h t", h=4), PB[bD][0:64, :].rearrange("p (h t) -> p h t", h=4), es_ap, ALU.add),
                             r=[R_PB[bD], R_es], w=[R_dn[di]])
                        yield
                        T.op("act", f_act(d_, d_, AF.Ln), r=[R_dn[di]], w=[R_dn[di]])
                    else:
                        T.op("act", f_act(d_, PB[bD][0:64, :], AF.Ln), r=[R_PB[bD]], w=[R_dn[di]])
                    T.op("act", f_act(d_, d_, AF.Exp, scale=-1.0), r=[R_dn[di]], w=[R_dn[di]])
                    yield
                    T.op("dve", f_tt(oTg[:, h0:h0 + 4, j * 128:(j + 1) * 128], PB[bO][0:64, :].rearrange("p (h t) -> p h t", h=4),
                                     d_.rearrange("p (h t) -> p h t", h=4), ALU.mult), r=[R_PB[bO], R_dn[di]], w=[R_oTg[j]])
                    yield
                def swa_mem_gen(j):
                    i, tok, nkb, nch, n = slot_vars(j)
                    rp = (2 * i) % 16; rc = (2 * i + 1) % 16
                    for kv in range(2):
                        pr = slice(kv * 64, (kv + 1) * 64)
                        bS = (2, 3) if kv == 0 else (4, 5)
                        bO, bD = (0, 1) if kv == 0 else (6, 7)
                        Es_ = []
                        for which, rr_ in enumerate((rp, rc)):
                            b = bS[which]
                            T.op("pe", f_mm(PB[b][:, :], kST[pr, rr_, :], qsT[pr, j, :]), r=[R_kST[rr_], R_qs[j]], w=[R_PB[b]])
                            ei = rot("E", 5)
                            T.op("act", f_act(Et[ei][:], PB[b][:, :], AF.Exp, scale=0.125), r=[R_PB[b]], w=[R_E[ei]])
                            Es_.append((ei, rr_, which))
                        yield
                        Ps = []
                        for ei, rr_, which in Es_:
                            pi_ = rot("P", 2)
                            msk = (prev0 if i == 0 else tri_prev) if which == 0 else tri_cur
                            T.op("dve", f_tt(Pt[pi_][:].rearrange("p (h t) -> p h t", h=4), Et[ei][:].rearrange("p (h t) -> p h t", h=4),
                                             msk.unsqueeze(1).to_broadcast([128, 4, 128]), ALU.mult), r=[R_E[ei], R_const], w=[R_P[pi_]])
                            Ps.append((pi_, rr_))
                        T.op("pe", f_seq([f_mm(PB[bO][0:64, :], vS[:, rr_, pr], Pt[pi_][:], w_ == 0, w_ == 1) for w_, (pi_, rr_) in enumerate(Ps)]
                                         + [f_mm(PB[bD][0:64, :], ones_b[:], Pt[pi_][:], w_ == 0, w_ == 1) for w_, (pi_, rr_) in enumerate(Ps)]),
                             r=[R_P[p_] for p_, _ in Ps] + [R_vS[r_] for _, r_ in Ps] + [R_const], w=[R_PB[bO], R_PB[bD]])
                        yield
                        yield from fin_gen(bO, bD, j, 4 * kv, es[:, 4 * kv:4 * kv + 4].unsqueeze(2).to_broadcast([64, 4, 128]))
                    Es = [rot("E", 5), rot("E", 5)]
                    fns = []
                    for e in range(2):
                        pr = slice(e * 64, (e + 1) * 64)
                        for mc in range(2):
                            for jj in range(2):
                                fns.append(f_mm(PB[2 + 2 * mc + e][:, jj * 128:(jj + 1) * 128], mkT[pr, jj, mc * 128:(mc + 1) * 128],
                                                qmT[pr, j, jj * 128:(jj + 1) * 128]))
                    T.op("pe", f_seq(fns), r=[R_mkT, R_qm[j]], w=[R_PB[2], R_PB[3], R_PB[4], R_PB[5]])
                    for mc in range(2):
                        E4 = Et[Es[mc]][:].rearrange("p (jj e t) -> p jj e t", jj=2, e=2)
                        for e in range(2):
                            b = 2 + 2 * mc + e
                            T.op("act", f_act(E4[:, :, e, :], PB[b][:, 0:256].rearrange("p (jj t) -> p jj t", jj=2), AF.Exp, scale=0.125),
                                 r=[R_PB[b]], w=[R_E[Es[mc]]])
                    yield
                    fns = []
                    for hh in range(4):
                        for mc in range(2):
                            fns.append(f_mm(PB[0][0:64, hh * 128:(hh + 1) * 128], mv[:, mc, hh * 64:(hh + 1) * 64], Et[Es[mc]][:, hh * 128:(hh + 1) * 128], mc == 0, mc == 1))
                    for mc in range(2):
                        fns.append(f_mm(PB[1][0:64, :], ones_b[:], Et[Es[mc]][:], mc == 0, mc == 1))
                    T.op("pe", f_seq(fns), r=[R_E[e_] for e_ in Es] + [R_mv, R_const], w=[R_PB[0], R_PB[1]])
                    yield
                    yield from fin_gen(0, 1, j, 12, None)
                def swa_mem(j):
                    for _ in swa_mem_gen(j):
                        pass
                def dsa_index(j):
                    i, tok, nkb, nch, n = slot_vars(j)
                    for c in range(nch):
                        kb0 = 4 * c; nb = min(4, nkb - kb0); w_ = nb * 128
                        cols = slice(kb0 * 128, kb0 * 128 + w_)
                        bb = 4 * (c % 2)
                        T.op("pe", f_seq([f_mm(PB[bb + hh][:, 0:w_], qdiT[64:128, j, hh * 128:(hh + 1) * 128], kDI[64:128, cols]) for hh in range(4)]),
                             r=[R_qdi[j]] + R_kDI[kb0:kb0 + nb], w=[R_PB[bb + hh] for hh in range(4)])
                        for hh in range(4):
                            ri = rot("rl", 2)
                            T.op("act", f_act(rl[ri][:, 0:w_], PB[bb + hh][:, 0:w_], AF.Relu), r=[R_PB[bb + hh]], w=[R_rl[ri]])
                            if hh == 0:
                                T.op("dve", f_ts(BIG[:, cols], rl[ri][:, 0:w_], wiT[:, j, 0:1], None, ALU.mult), r=[R_rl[ri], R_wi[j]], w=[ISC[c]])
                            else:
                                T.op("dve", f_stt(BIG[:, cols], rl[ri][:, 0:w_], wiT[:, j, hh:hh + 1], BIG[:, cols], ALU.mult, ALU.add),
                                     r=[R_rl[ri], R_wi[j], ISC[c]], w=[ISC[c]])
                def dsa_bisect(j, pipe=None):
                    i, tok, nkb, nch, n = slot_vars(j)
                    _ckpt(12.3)
                    IR = ISC[0:nch]; MR = MSKR[0:nch]
                    cd = nch if nch < 2 else max(1, int(round(0.42 * nch)))
                    nd = min(n, cd * 512); na = n - nd
                    IRd, MRd, IRa, MRa = ISC[0:cd], MSKR[0:cd], ISC[cd:nch], MSKR[cd:nch]
                    amax = bis[:, 0:1]; wtot = bis[:, 1:2]; lo = bis[:, 2:3]; mid = bis[:, 3:4]; cn = bis[:, 4:5]; dl = bis[:, 5:6]
                    sa = bis[:, 6:7]; tt_ = bis[:, 7:8]
                    wks = bis[:, 8:8 + NITER]
                    T.op("dve", lambda h, a=amax, b=BIG[:, 0:n]: h.tensor_reduce(out=a, in_=b, axis=AX.X, op=ALU.max, apply_absolute_value=True),
                         r=IR, w=[R_lo])
                    T.op("pool", f_tt(BIG[:, 0:128], BIG[:, 0:128], p63bias, ALU.add), r=[ISC[0], R_cmf, R_lo], w=[ISC[0]])
                    dcol = slice((nkb - 1) * 128, nkb * 128)
                    T.op("pool", f_tt(BIG[:, dcol], BIG[:, dcol], dbias, ALU.add), r=[ISC[nch - 1], R_cmf, R_lo], w=[ISC[nch - 1]])
                    T.op("dve", f_ts(wtot, amax, 2.0002, 2e-6, ALU.mult, ALU.add), r=[R_lo], w=[R_lo])
                    T.op("dve", f_ts(lo, amax, -1.0001, -1e-6, ALU.mult, ALU.add), r=[R_lo], w=[R_lo])
                    T.op("dve", f_ts(wks, pw2[:], wtot, None, ALU.mult), r=[R_lo, R_const], w=[R_lo])
                    thrc = TOPK - na / 2.0
                    for k in range(NITER):
                        T.op("dve", f_tt(mid, lo, wks[:, k:k + 1], ALU.add), r=[R_lo], w=[R_mid])
                        if na > 0:
                            T.op("act", f_act(MSK[:, nd:n], BIG[:, nd:n], AF.Sign, bias=mid, scale=-1.0, accum=sa), r=IRa + [R_mid], w=MRa + [R_ca])
                        T.op("dve", f_ts(MSK[:, 0:nd], BIG[:, 0:nd], mid, 0.0, ALU.is_ge, ALU.add, accum=cn), r=IRd + [R_mid], w=MRd + [R_cd])
                        if na > 0:
                            T.op("dve", f_stt(tt_, sa, -0.5, cn, ALU.mult, ALU.add), r=[R_ca, R_cd], w=[R_t])
                            T.op("dve", f_stt(dl, tt_, thrc, wks[:, k:k + 1], ALU.is_ge, ALU.mult), r=[R_t, R_lo], w=[R_t])
                        else:
                            T.op("dve", f_stt(dl, cn, TOPK, wks[:, k:k + 1], ALU.is_ge, ALU.mult), r=[R_cd, R_lo], w=[R_t])
                        T.op("dve", f_tt(lo, lo, dl, ALU.add), r=[R_t], w=[R_lo])
                        if pipe is not None:
                            next(pipe, None)
                    T.op("dve", f_ts(MSK[:, 0:n], BIG[:, 0:n], lo, None, ALU.is_ge), r=IR + [R_lo], w=MR)
                def dsa_attn(j):
                    i, tok, nkb, nch, n = slot_vars(j)
                    _ckpt(12.4)
                    LA = 3
                    st_ = {}
                    T.op("dve", f_cp(qd0[0:64, :], qdiT[0:64, j, :]), r=[R_qdi[j]], w=[R_qd0])
                    def prep(c):
                        kb_ = 4 * c
                        nb = min(4, nkb - kb_)
                        mi = c % 2
                        pv = pbf(2)
                        T.op("pe", f_seq([f_tr(pv[:, q2 * 128:(q2 + 1) * 128], MSK[:, (kb_ + q2) * 128:(kb_ + q2 + 1) * 128], ident[:]) for q2 in range(nb)]),
                             r=[MSKR[c], R_const], w=[R_PB[2]])
                        T.op("act", f_act(mT[mi][:, 0:nb * 128], pv[:, 0:nb * 128], AF.Identity, scale=30000.0, bias=-30000.0), r=[R_PB[2]], w=[R_mT[mi]])
                    prep(0)
                    def front(kb):
                        c, q = kb // 4, kb % 4
                        if q == 0 and c + 1 < nch:
                            prep(c + 1)
                        mi = c % 2
                        b = 3 + (kb % 5)
                        T.op("pe", f_seq([f_mm(PB[b][:, :], kDI[:, kb * 128:(kb + 1) * 128], qd0[:, :], True, False)]
                                         + [f_mm(PB[b][:, hh * 128:(hh + 1) * 128], ident[:], mT[mi][:, q * 128:(q + 1) * 128], False, hh == 3) for hh in range(4)]),
                             r=[R_kDI[kb], R_qd0, R_mT[mi], R_const], w=[R_PB[b]])
                        ei = rot("E", 5)
                        T.op("act", f_act(Et[ei][:], PB[b][:, :], AF.Exp, scale=0.125), r=[R_PB[b]], w=[R_E[ei]])
                        st_[kb] = ei
                    def back(kb):
                        ei = st_.pop(kb)
                        T.op("pe", f_mm(PB[0][:, :], vD[:, kb, :], Et[ei][:], kb == 0, kb == nkb - 1),
                             r=[R_E[ei], R_vD[kb]], w=[R_PB[0]])
                    for t in range(nkb + LA):
                        if t < nkb: front(t)
                        if t - LA >= 0: back(t - LA)
                    T.op("act", f_act(dn[0][64:128, :], PB[0][64:128, :], AF.Copy), r=[R_PB[0]], w=[R_dn[0]])
                    T.op("pe", f_mm(PB[1][0:64, :], identf[:, 64:128], dn[0][:, :]), r=[R_dn[0], R_const], w=[R_PB[1]])
                    finish_attn(0, 1, j, 8, None)

                swa_mem(0)
                for j in range(4):
                    T.mark(f"g{g} s{j} idx")
                    dsa_index(j)
                    T.mark(f"g{g} s{j} bis")
                    pipe = swa_mem_gen(j + 1) if j < 3 else None
                    dsa_bisect(j, pipe)
                    if pipe is not None:
                        for _ in pipe:
                            pass
                    T.mark(f"g{g} s{j} dattn")
                    dsa_attn(j)

                _ckpt(10 * (g + 1) + 3)
                T.mark(f"g{g} pass2")
                RQ = R_qs + R_qdi
                for oc in range(8):
                    G, RG = ring_get(3 + oc)
                    Gv = G[:, 0:8 * 384].rearrange("p (k c) -> p k c", k=8)
                    for jb in range(3):
                        T.op("pe", f_seq([f_mm(PB[jb][:, :], Gv[:, kc, jb * 128:(jb + 1) * 128], hTg[:, kc, :], kc == 0, kc == 7) for kc in range(8)]),
                             r=R_hTg + [RG], w=[R_PB[jb]])
                        T.op("act", f_act(gsb[jb][:], PB[jb][:, :], AF.Sigmoid, bias=bgate[:, oc * 3 + jb:oc * 3 + jb + 1], scale=1.0),
                             r=[R_PB[jb], R_misc], w=[R_gsb[jb]])
                    Pw, RP = ring_get(11 + oc)
                    Pv = Pw[0:64, 0:2048].rearrange("p (h c) -> p h c", h=16)
                    for jb, (h0, nh) in enumerate(((0, 8), (8, 4), (12, 4))):
                        T.op("pe", f_seq([f_mm(PB[3 + jb][:, :], Pv[:, h0 + hh, :], oTg[:, h0 + hh, :], hh == 0, hh == nh - 1) for hh in range(nh)]),
                             r=R_oTg + [RP], w=[R_PB[3 + jb]])
                    T.op("dve", f_tt(mg[0][:], PB[3][:, :], gsb[0][:], ALU.mult), r=[R_PB[3], R_gsb[0]], w=[R_mg[0]])
                    T.op("dve", f_tt(mg[1][:], PB[4][:, :], gsb[1][:], ALU.mult), r=[R_PB[4], R_gsb[1]], w=[R_mg[1]])
                    T.op("dve", f_tt(mg[0][:], mg[0][:], mg[1][:], ALU.add), r=[R_mg[0], R_mg[1]], w=[R_mg[0]])
                    T.op("dve", f_tt(mg[1][:], PB[5][:, :], gsb[2][:], ALU.mult), r=[R_PB[5], R_gsb[2]], w=[R_mg[1]])
                    T.op("dve", f_tt(mergedT[:, oc, :], mg[0][:], mg[1][:], ALU.add), r=[R_mg[0], R_mg[1]], w=RQ)

                _ckpt(10 * (g + 1) + 4)
                T.mark(f"g{g} wout")
                for cc in range(2):
                    W, RW = ring_get(19 + cc)
                    Wv = W[:, :].rearrange("p (k c) -> p k c", k=8)
                    for j in range(4):
                        pb = 6 + (j % 2)
                        T.op("pe", f_seq([f_mm(PB[pb][:, :], mergedT[:, kc, j * 128:(j + 1) * 128], Wv[:, kc, :], kc == 0, kc == 7) for kc in range(8)]),
                             r=RQ + [RW], w=[R_PB[pb]])
                        T.op("dve", f_tt(xg[:, j, cc * 512:(cc + 1) * 512], PB[pb][:, :], xg[:, j, cc * 512:(cc + 1) * 512], ALU.add),
                             r=[R_PB[pb], R_xg[j]], w=[R_xg[j]])

                _ckpt(10 * (g + 1) + 5)
                T.mark(f"g{g} mlp")
                for j in range(4):
                    norm_T(xg[:, j, :], [R_xg[j]], hTg[:, :, j * 128:(j + 1) * 128], [R_hTg[j]], j % 2)
                for fb in range(8):
                    W, RW = ring_get(21 + fb)
                    Wv = W[:, :].rearrange("p (k c) -> p k c", k=8)
                    for fs in range(4):
                        fc = fb * 4 + fs
                        pb = 2 + (fc % 4)
                        T.op("pe", f_seq([f_mm(PB[pb][:, :], Wv[:, kc, fs * 128:(fs + 1) * 128], hTg[:, kc, :], kc == 0, kc == 7) for kc in range(8)]),
                             r=R_hTg + [RW], w=[R_PB[pb]])
                        ri = rot("rl", 2)
                        T.op("act", f_act(rl[ri][:], PB[pb][:, :], AF.Relu), r=[R_PB[pb]], w=[R_rl[ri]])
                        T.op("dve", f_tt(hidT[:, fc, :], rl[ri][:], rl[ri][:], ALU.mult), r=[R_rl[ri]], w=[ISC[fc // 2]])
                for cc in range(2):
                    for ks in range(4):
                        W, RW = ring_get(29 + cc * 4 + ks)
                        Wv = W[:, :].rearrange("p (k c) -> p k c", k=8)
                        for j in range(4):
                            pb = 4 + j
                            T.op("pe", f_seq([f_mm(PB[pb][:, :], hidT[:, ks * 8 + fl, j * 128:(j + 1) * 128], Wv[:, fl, :],
                                                   ks == 0 and fl == 0, ks == 3 and fl == 7) for fl in range(8)]),
                                 r=ISC[ks * 4:(ks + 1) * 4] + [RW], w=[R_PB[pb]])
                    for j in range(4):
                        pb = 4 + j
                        T.op("dve", f_tt(xg[:, j, cc * 512:(cc + 1) * 512], PB[pb][:, :], xg[:, j, cc * 512:(cc + 1) * 512], ALU.add),
                             r=[R_PB[pb], R_xg[j]], w=[R_xg[j]])
                _ckpt(10 * (g + 1) + 6)
                T.mark(f"g{g} final")
                for j in range(4):
                    i = 4 * g + j
                    si = rot("stat", 4); s = stat[:, si * 4:(si + 1) * 4]; rs = R_stat[si]
                    hi = rot("hn", 1)
                    T.op("act", f_act(hn[hi][:], xg[:, j, :], AF.Square, accum=s[:, 0:1]), r=[R_xg[j]], w=[R_hn[hi], rs])
                    T.op("act", f_act(s[:, 1:2], s[:, 0:1], AF.Ln, scale=1.0 / 1024.0, bias=1e-6), r=[rs], w=[rs])
                    T.op("act", f_act(s[:, 2:3], s[:, 1:2], AF.Exp, scale=-0.5), r=[rs], w=[rs])
                    T.op("dve", f_stt(xg[:, j, :], xg[:, j, :], s[:, 2:3], gfin[:], ALU.mult, ALU.mult), r=[R_xg[j], rs, R_misc], w=[R_xg[j]])
                    T.dma("sp", out_d[i * 128:(i + 1) * 128, :], xg[:, j, :], R_xg[j], r=[R_xg[j]])
        try:
            emit()
        except _Stop:
            T.dma("sp", out_d[0:128, :], xo[:], R_rl[0], r=R_rl)
            T.wait_all("sp", R_rl + R_ring)
            T.wait_all("pool", [MSKR[0], MSKR[8], ISC[0], ISC[8]])
        T.wait_all("sp", R_xg)
        with nc.Block() as block:
            T.replay(block)
    return nc


OFF = dict(q_s=0, k_s=512, v_s=640, q_d=768, k_d=1024, v_d=1088, q_i=1152, k_i=1408, w_i=1472, q_m=1476, gates=1732)


def _chunked(w, cols):
    sub = w[:, cols]
    n = sub.shape[1]
    return np.ascontiguousarray(sub.reshape(8, 128, n).transpose(1, 0, 2).reshape(128, 8 * n))


def _prep_weights(w_in, w_proj_swa, w_proj_dsa, w_proj_mem, w_out, w_mlp_in, w_mlp_out):
    wch = np.zeros((NCHUNK, 128, 4096), np.float32)
    r64 = np.arange(64)
    qs_cols = np.concatenate([OFF["q_s"] + h * 64 + r64 for h in (0, 4, 1, 5, 2, 6, 3, 7)])
    qdi_cols = np.concatenate([np.concatenate([OFF["q_d"] + h * 64 + r64, OFF["q_i"] + h * 64 + r64]) for h in range(4)])
    qm_cols = np.concatenate([OFF["q_m"] + np.arange(256), OFF["w_i"] + np.arange(4)])
    wch[0] = _chunked(w_in, qs_cols)
    wch[1] = _chunked(w_in, qdi_cols)
    wch[2, :, 0:8 * 260] = _chunked(w_in, qm_cols)
    for oc in range(8):
        gc = np.concatenate([OFF["gates"] + jb * 1024 + oc * 128 + np.arange(128) for jb in range(3)])
        wch[3 + oc, :, 0:8 * 384] = _chunked(w_in, gc)
        wp = np.concatenate([w_proj_swa.reshape(8, 64, 1024), w_proj_dsa.reshape(4, 64, 1024), w_proj_mem.reshape(4, 64, 1024)], 0)
        wch[11 + oc, 0:64, 0:2048] = wp[:, :, oc * 128:(oc + 1) * 128].transpose(1, 0, 2).reshape(64, 2048)
    for cc in range(2):
        wch[19 + cc] = _chunked(w_out, cc * 512 + np.arange(512))
    for fb in range(8):
        wch[21 + fb] = _chunked(w_mlp_in, fb * 512 + np.arange(512))
    for cc in range(2):
        for ks in range(4):
            blk = w_mlp_out[ks * 1024:(ks + 1) * 1024, cc * 512:(cc + 1) * 512]
            wch[29 + cc * 4 + ks] = blk.reshape(8, 128, 512).transpose(1, 0, 2).reshape(128, 4096)
    k_cols = np.concatenate([OFF["k_s"] + np.arange(128), OFF["k_d"] + r64, OFF["k_i"] + r64, OFF["v_s"] + np.arange(128), OFF["v_d"] + r64])
    wk = _chunked(w_in, k_cols)
    return wch, wk


_NC_CACHE = {}


def kernel(x, mem, positions, g_mix, w_in, b_gate, sinks, g_mem, w_mem_kv, w_proj_swa, w_proj_dsa,
           w_proj_mem, w_out, g_mlp, w_mlp_in, w_mlp_out, g_final):
    f = lambda a: np.asarray(a, dtype=np.float32)
    x = f(x); mem = f(mem); positions = np.asarray(positions, dtype=np.int32)
    wch, wk = _prep_weights(f(w_in)[0], f(w_proj_swa)[0], f(w_proj_dsa)[0], f(w_proj_mem)[0], f(w_out)[0], f(w_mlp_in)[0], f(w_mlp_out)[0])
    wmem = _chunked(f(w_mem_kv)[0], np.arange(512))
    tr = lambda v: np.ascontiguousarray(f(v).reshape(8, 128).T)
    gvec = np.concatenate([tr(g_mix[0]), tr(g_mlp[0]), tr(g_mem[0])], 1)
    gfin = np.ascontiguousarray(np.broadcast_to(f(g_final)[None, :], (128, 1024)))
    bg = f(b_gate)[0].reshape(3, 8, 128)
    bgate = np.ascontiguousarray(bg.transpose(2, 1, 0).reshape(128, 24))
    sinkr = np.ascontiguousarray(np.broadcast_to(f(sinks)[0][None, :], (64, 8)))
    half = 32
    invf = np.power(np.float32(10000.0), -np.arange(half, dtype=np.float32) / np.float32(half)).astype(np.float32)
    invf = np.ascontiguousarray(np.broadcast_to(invf[None, :], (128, 32)))
    s_ = np.arange(128)[:, None]; t_ = np.arange(128)[None, :]
    tri_cur = (t_ >= s_).astype(np.float32)
    tri_prev = (s_ > t_).astype(np.float32)
    dbias = np.where(t_ <= s_, 0.0, NEG).astype(np.float32)
    in_maps = []
    for c in range(8):
        b, par = c // 2, c % 2
        order = np.arange(64) if par == 0 else np.concatenate([np.arange(1, 64), [0]])
        xb = x[b].reshape(64, 128, 1024)[order].reshape(64 * 128, 1024)
        pb = positions[b].reshape(64, 128)[order]
        posc = np.ascontiguousarray(pb.T)
        prev0 = tri_prev * float(par)
        p63 = np.full((128, 128), 0.0 if par == 1 else NEG, np.float32)
        cmask = np.ascontiguousarray(np.stack([tri_cur, tri_prev, prev0, dbias, p63], 1).reshape(128, 5 * 128).astype(np.float32))
        in_maps.append(dict(xc=np.ascontiguousarray(xb), posc=posc, memc=np.ascontiguousarray(mem[b]), cmask=cmask, invf=invf,
                            wk=wk, wch=wch, wmem=wmem, gvec=gvec, gfin=gfin, bgate=bgate, sinkr=sinkr))
    if "nc" not in _NC_CACHE:
        _NC_CACHE["nc"] = build_program()
    nc = _NC_CACHE["nc"]
    res = run_bass_kernel_spmd(nc, in_maps, core_ids=list(range(8)))
    out = np.zeros((4, 64, 128, 1024), np.float32)
    for c in range(8):
        b, par = c // 2, c % 2
        o = np.asarray(res.results[c]["out"]).reshape(32, 128, 1024)
        out[b, par::2] = o
    return out.reshape(4, 8192, 1024)
```

```python
import math
import numpy as np
from contextlib import ExitStack
import concourse.bass as bass
import concourse.mybir as mybir
from concourse.bass_utils import run_bass_kernel_spmd

F32 = mybir.dt.float32; BF16 = mybir.dt.bfloat16; I32 = mybir.dt.int32
ALU = mybir.AluOpType; AF = mybir.ActivationFunctionType; AX = mybir.AxisListType

STAGE = 99
class _Stop(Exception):
    pass
def _ckpt(n):
    if STAGE <= n:
        raise _Stop()
NITER = 14
NEG = -1.0e30
NCHUNK = 37
TOPK = 256.0


class Reg:
    __slots__ = ("name", "w", "r", "dsem", "dcnt")
    def __init__(self, name):
        self.name = name; self.w = None; self.r = {}; self.dsem = None; self.dcnt = 0


class Eng:
    def __init__(self, name):
        self.name = name; self.q = []; self.sem = None; self.cnt = 0; self.seen = {}


class Trk:
    def __init__(self, nc, stack, nsem):
        self.nc = nc
        self.sems = [stack.enter_context(nc.semaphore(f"s{i}")) for i in range(nsem)]
        self.si = 0
        self.E = {n: Eng(n) for n in ("pe", "act", "dve", "pool", "sp")}
        for e in self.E.values():
            e.sem = self.newsem()
    def newsem(self):
        s = self.sems[self.si]; self.si += 1; return s
    def _deps(self, eng, reads, writes):
        deps = {}
        def add(ev, kind):
            if ev is None: return
            sem, val, src = ev
            if src is eng and eng.name == "pe":
                return
            k = id(sem)
            if eng.seen.get(k, 0) >= val: return
            if k not in deps or deps[k][1] < val: deps[k] = (sem, val)
        for r in reads: add(r.w, "raw")
        for w in writes:
            add(w.w, "waw")
            for ev in w.r.values(): add(ev, "war")
        out = list(deps.values())
        for sem, val in out: eng.seen[id(sem)] = val
        return out
    def op(self, en, fn, r=(), w=()):
        eng = self.E[en]
        waits = self._deps(eng, r, w)
        eng.cnt += 1
        ev = (eng.sem, eng.cnt, eng)
        eng.q.append((waits, fn, (eng.sem, 1)))
        for x in r: x.r[en] = ev
        for x in w: x.w = ev; x.r = {}
    def dma(self, en, out_ap, in_ap, slot, r=(), w=()):
        eng = self.E[en]
        waits = self._deps(eng, r, w)
        if slot.dsem is None: slot.dsem = self.newsem()
        slot.dcnt += 16
        ev = (slot.dsem, slot.dcnt, None)
        eng.q.append((waits, lambda h: h.dma_start(out=out_ap, in_=in_ap), (slot.dsem, 16)))
        for x in r: x.r["dma%d" % id(slot)] = ev
        for x in w: x.w = ev; x.r = {}
    def mark(self, label):
        self.E["pe"].q.append(([], None, label))
    def wait_all(self, en, regs):
        eng = self.E[en]
        waits = self._deps(eng, [], regs)
        eng.q.append((waits, None, None))
    def replay(self, block):
        def mk(en):
            q = self.E[en].q
            def body(h):
                for waits, fn, inc in q:
                    for sem, val in waits: h.wait_ge(sem, val)
                    if fn is None:
                        if isinstance(inc, str): MARKS.append((inc, PE_CNT[0]))
                        continue
                    ins = fn(h)
                    ins.then_inc(inc[0], inc[1])
            return body
        block.tensor(mk("pe")); block.scalar(mk("act")); block.vector(mk("dve"))
        block.gpsimd(mk("pool")); block.sync(mk("sp"))


PE_CNT = [0]
MARKS = []
def f_mm(out, lhsT, rhs, start=True, stop=True):
    def f(h):
        PE_CNT[0] += 1
        return h.matmul(out, lhsT=lhsT, rhs=rhs, start=start, stop=stop)
    return f
def f_tr(out, in_, ident):
    def f(h):
        PE_CNT[0] += 1
        return h.transpose(out=out, in_=in_, identity=ident)
    return f
def f_seq(fns):
    def f(h):
        ins = None
        for fn in fns: ins = fn(h)
        return ins
    return f
def f_act(out, in_, func, bias=None, scale=None, accum=None):
    kw = {}
    if bias is not None: kw["bias"] = bias
    if scale is not None: kw["scale"] = scale
    if accum is not None: kw["accum_out"] = accum
    return lambda h: h.activation(out=out, in_=in_, func=func, **kw)
def f_ts(out, in0, s1, s2=None, op0=ALU.mult, op1=None, accum=None):
    kw = {}
    if op1 is not None: kw["op1"] = op1
    if accum is not None: kw["accum_out"] = accum
    return lambda h: h.tensor_scalar(out=out, in0=in0, scalar1=s1, scalar2=s2, op0=op0, **kw)
def f_tt(out, in0, in1, op):
    return lambda h: h.tensor_tensor(out=out, in0=in0, in1=in1, op=op)
def f_stt(out, in0, scalar, in1, op0, op1):
    return lambda h: h.scalar_tensor_tensor(out=out, in0=in0, scalar=scalar, in1=in1, op0=op0, op1=op1)
def f_cp(out, in_):
    return lambda h: h.tensor_copy(out=out, in_=in_)
def f_memset(ap, v):
    return lambda h: h.memset(ap, v)


def build_program():
    nc = bass.Bass("TRN2", target_bir_lowering=False)
    dt_in = lambda n, s, d=F32: nc.dram_tensor(n, s, d, kind="ExternalInput").ap()
    xc = dt_in("xc", [64 * 128, 1024])
    posc = dt_in("posc", [128, 64], I32)
    memc = dt_in("memc", [256, 1024])
    cmask = dt_in("cmask", [128, 5 * 128])
    invf_d = dt_in("invf", [128, 32])
    wk_d = dt_in("wk", [128, 8 * 448])
    wch_d = dt_in("wch", [NCHUNK, 128, 4096])
    wmem_d = dt_in("wmem", [128, 4096])
    gvec_d = dt_in("gvec", [128, 24])
    gfin_d = dt_in("gfin", [128, 1024])
    bgate_d = dt_in("bgate", [128, 24])
    sinkr_d = dt_in("sinkr", [64, 8])
    out_d = nc.dram_tensor("out", [32 * 128, 1024], F32, kind="ExternalOutput").ap()
    wbf_d = nc.dram_tensor("wbf", [NCHUNK, 128, 4096], BF16, kind="Internal").ap()

    with ExitStack() as st:
        T = Trk(nc, st, 48)
        def sb(name, shape, dt):
            return st.enter_context(nc.sbuf_tensor(name, shape, dt))
        BIG = sb("BIG", [128, 8192], F32)
        MSK = sb("MSK", [128, 8192], BF16)
        ISC = [Reg(f"isc{c}") for c in range(16)]
        MSKR = [Reg(f"msk{c}") for c in range(16)]
        hidT = BIG[:].bitcast(BF16).rearrange("p (f t) -> p f t", t=512)
        wk_sb = sb("wk_sb", [128, 8, 448], BF16); R_wk = Reg("wk")
        kDI = sb("kDI", [128, 8192], BF16); R_kDI = [Reg(f"kDI{i}") for i in range(64)]
        vD = sb("vD", [128, 64, 128], BF16); R_vD = [Reg(f"vD{i}") for i in range(64)]
        kST = sb("kST", [128, 16, 128], BF16); R_kST = [Reg(f"kST{i}") for i in range(16)]
        vS = sb("vS", [128, 16, 128], BF16); R_vS = [Reg(f"vS{i}") for i in range(16)]
        mkT = sb("mkT", [128, 2, 256], BF16); R_mkT = Reg("mkT")
        mv = sb("mv", [128, 2, 256], BF16); R_mv = Reg("mv")
        xg = sb("xg", [128, 4, 1024], F32); R_xg = [Reg(f"xg{j}") for j in range(4)]
        hTg = sb("hTg", [128, 8, 512], BF16); R_hTg = [Reg(f"hTg{j}") for j in range(4)]
        hTo = sb("hTo", [128, 8, 128], BF16); R_hTo = Reg("hTo")
        oTg = sb("oTg", [64, 16, 512], BF16); R_oTg = [Reg(f"oTg{j}") for j in range(4)]
        QM = sb("QM", [128, 4096], BF16)
        qsT = QM[:, 0:2048].rearrange("p (s c) -> p s c", s=4)
        qdiT = QM[:, 2048:4096].rearrange("p (s c) -> p s c", s=4)
        mergedT = QM[:].rearrange("p (k t) -> p k t", k=8)
        R_qs = [Reg(f"qs{j}") for j in range(4)]; R_qdi = [Reg(f"qdi{j}") for j in range(4)]
        qmT = sb("qmT", [128, 4, 256], BF16); R_qm = [Reg(f"qm{j}") for j in range(4)]
        wiT = sb("wiT", [128, 4, 4], F32); R_wi = [Reg(f"wi{j}") for j in range(4)]
        NRING = 2
        ring = [sb(f"ring{i}", [128, 4096], BF16) for i in range(NRING)]
        R_ring = [Reg(f"ring{i}") for i in range(NRING)]
        ident = sb("ident", [128, 128], BF16); identf = sb("identf", [128, 128], F32); onesf = sb("onesf", [128, 128], F32)
        ones_b = sb("ones_b", [128, 64], BF16)
        qd0 = sb("qd0", [128, 512], BF16); R_qd0 = Reg("qd0")
        R_const = Reg("const")
        cm_f = sb("cm_f", [128, 5, 128], F32)
        cm_b = sb("cm_b", [128, 3, 128], BF16)
        invf = sb("invf_sb", [128, 32], F32)
        gvec = sb("gvec_sb", [128, 24], F32)
        gfin = sb("gfin_sb", [128, 1024], F32)
        bgate = sb("bgate_sb", [128, 24], F32)
        es = sb("es_sb", [64, 8], F32)
        pw2 = sb("pw2", [128, NITER], F32)
        posi = sb("posi", [128, 64], I32); posf = sb("posf", [128, 64], F32)
        cosT = sb("cosT", [128, 8, 32], F32); sinT = sb("sinT", [128, 8, 32], F32); R_cs = Reg("cs")
        rtmp = [sb(f"rtmp{i}", [128, 256], F32) for i in range(3)]; rtmpi = sb("rtmpi", [128, 256], I32); R_rt = Reg("rt")
        stat = sb("stat", [128, 16], F32); R_stat = [Reg(f"stat{i}") for i in range(4)]
        hn = [sb(f"hn{i}", [128, 1024], BF16) for i in range(1)]; R_hn = [Reg(f"hn{i}") for i in range(1)]
        tok_b = [sb(f"tok_b{i}", [128, 512], BF16) for i in range(2)]; R_tok = [Reg(f"tok{i}") for i in range(2)]
        Et = [sb(f"Et{i}", [128, 512], BF16) for i in range(5)]; R_E = [Reg(f"E{i}") for i in range(5)]
        Pt = [sb(f"Pt{i}", [128, 512], BF16) for i in range(2)]; R_P = [Reg(f"P{i}") for i in range(2)]
        RLX = sb("RLX", [128, 1024], F32); rl = [RLX[:, 0:512], RLX[:, 512:1024]]; R_rl = [Reg(f"rl{i}") for i in range(2)]
        xo = RLX; R_xo = R_rl[0]
        mT = [sb(f"mT{i}", [128, 512], BF16) for i in range(2)]; R_mT = [Reg(f"mT{i}") for i in range(2)]
        dn = [sb(f"dn{i}", [128, 512], F32) for i in range(1)]; R_dn = [Reg(f"dn{i}") for i in range(1)]
        mg = rl; R_mg = R_rl
        RTU = sb("RTU", [128, 1024], F32); rope_t = RTU[:, 0:512]; rope_u = RTU[:, 512:1024]; R_rtu = [Reg("rt_"), Reg("ru_")]
        gsb = [sb(f"gsb{i}", [128, 512], BF16) for i in range(3)]; R_gsb = [Reg(f"gsb{i}") for i in range(3)]
        bis = sb("bis", [128, 8 + NITER], F32); R_lo = Reg("lo"); R_mid = Reg("mid"); R_cd = Reg("cd"); R_ca = Reg("ca"); R_t = Reg("t")
        PB = [st.enter_context(nc.psum_tensor(f"pb{i}", [128, 512], F32)) for i in range(8)]
        R_PB = [Reg(f"pb{i}") for i in range(8)]
        def pbf(i):
            return PB[i][:].bitcast(BF16)

        cnt = {"ring": 0, "stat": 0, "hn": 0, "tok": 0, "E": 0, "P": 0, "rl": 0, "mT": 0, "dn": 0, "mg": 0}
        def rot(name, n):
            i = cnt[name] % n; cnt[name] += 1; return i

        R_cmf = Reg("cmf"); R_misc = Reg("misc")
        T.dma("sp", cm_f[:].rearrange("p a b -> p (a b)"), cmask[:, :], R_cmf, w=[R_cmf])
        for (dst, src) in ((invf, invf_d), (gvec, gvec_d), (gfin, gfin_d), (bgate, bgate_d)):
            T.dma("sp", dst[:], src[:, :], R_misc, w=[R_misc])
            T.wait_all("sp", [R_misc])
        R_es = Reg("es")
        T.dma("sp", es[:], sinkr_d[:, :], R_es, w=[R_es])
        R_pos = Reg("pos")
        T.dma("sp", posi[:], posc[:, :], R_pos, w=[R_pos])
        T.op("pool", f_memset(onesf[:], 1.0), w=[R_const])
        T.op("pool", f_memset(identf[:], 0.0), w=[R_const])
        T.op("pool", lambda h: h.affine_select(out=identf[:], in_=onesf[:], pattern=[[-1, 128]], compare_op=ALU.is_equal,
                                                fill=0.0, base=0, channel_multiplier=1), r=[R_const], w=[R_const])
        T.op("dve", f_cp(ident[:], identf[:]), r=[R_const], w=[R_const])
        T.op("dve", f_memset(ones_b[:], 1.0), w=[R_const])
        T.op("pool", f_memset(vD[:].rearrange("p a b -> p (a b)"), 1.0), w=R_vD)
        T.op("dve", f_memset(dn[0][:], 0.0), w=[R_dn[0]])
        T.op("dve", f_memset(qd0[:], 0.0), w=[R_qd0])
        for k in range(NITER):
            T.op("dve", f_memset(pw2[:, k:k + 1], 2.0 ** (-(k + 1))), w=[R_const])
        T.op("dve", f_cp(cm_b[:], cm_f[:, 0:3, :]), r=[R_cmf], w=[R_const])
        T.op("act", f_act(es[:], es[:], AF.Exp), r=[R_es], w=[R_es])
        T.op("dve", f_cp(posf[:], posi[:]), r=[R_pos], w=[R_pos])
        tri_cur = cm_b[:, 0, :]; tri_prev = cm_b[:, 1, :]; prev0 = cm_b[:, 2, :]
        dbias = cm_f[:, 3, :]; p63bias = cm_f[:, 4, :]

        def emit():
            def norm_T(x_ap, x_regs, dstT, dst_regs, pbank):
                si = rot("stat", 4); s = stat[:, si * 4:(si + 1) * 4]; rs = R_stat[si]
                hi = rot("hn", 1); h_ = hn[hi]; rh = R_hn[hi]
                T.op("act", f_act(h_[:], x_ap, AF.Square, accum=s[:, 0:1]), r=x_regs, w=[rh, rs])
                T.op("act", f_act(s[:, 1:2], s[:, 0:1], AF.Ln, scale=1.0 / 1024.0, bias=1e-6), r=[rs], w=[rs])
                T.op("act", f_act(s[:, 2:3], s[:, 1:2], AF.Exp, scale=-0.5), r=[rs], w=[rs])
                T.op("dve", f_ts(h_[:], x_ap, s[:, 2:3], None, ALU.mult), r=x_regs + [rs], w=[rh])
                pv = pbf(pbank)
                T.op("pe", f_seq([f_tr(pv[:, kc * 128:(kc + 1) * 128], h_[:, kc * 128:(kc + 1) * 128], ident[:]) for kc in range(8)]),
                     r=[rh, R_const], w=[R_PB[pbank]])
                T.op("act", f_act(dstT, pv[:, :].rearrange("p (k t) -> p k t", k=8), AF.Copy), r=[R_PB[pbank]], w=dst_regs)
                return s, rs

            def rope_tables(blk0, nb):
                n = nb * 32
                a0 = rtmp[0][:, 0:n]; a1 = rtmp[1][:, 0:n]; a2 = rtmp[2][:, 0:n]; ai = rtmpi[:, 0:n]
                v3 = lambda a: a.rearrange("p (b f) -> p b f", f=32)
                T.op("dve", f_tt(v3(a0), posf[:, blk0:blk0 + nb].unsqueeze(2).to_broadcast([128, nb, 32]),
                                 invf[:].unsqueeze(1).to_broadcast([128, nb, 32]), ALU.mult), r=[R_pos, R_misc], w=[R_rt])
                T.op("dve", f_ts(ai, a0, 1.0 / (2 * math.pi), None, ALU.mult), r=[R_rt], w=[R_rt])
                T.op("dve", f_cp(a1, ai), r=[R_rt], w=[R_rt])
                T.op("dve", f_stt(a2, a1, -6.28125, a0, ALU.mult, ALU.add), r=[R_rt], w=[R_rt])
                T.op("dve", f_stt(a2, a1, -0.0019353072, a2, ALU.mult, ALU.add), r=[R_rt], w=[R_rt])
                T.op("dve", f_ts(a0, a2, -3.1415925, 3.1415925, ALU.max, ALU.min), r=[R_rt], w=[R_rt])
                T.op("act", f_act(sinT[:, 0:nb, :].rearrange("p b f -> p (b f)"), a0, AF.Sin), r=[R_rt], w=[R_cs])
                T.op("dve", f_ts(a1, a2, math.pi / 2, None, ALU.add), r=[R_rt], w=[R_rt])
                T.op("dve", f_ts(a0, a1, math.pi, -2 * math.pi, ALU.is_gt, ALU.mult), r=[R_rt, R_cs], w=[R_rt])
                T.op("dve", f_tt(a0, a0, a1, ALU.add), r=[R_rt], w=[R_rt])
                T.op("dve", f_ts(a0, a0, -3.1415925, 3.1415925, ALU.max, ALU.min), r=[R_rt], w=[R_rt])
                T.op("act", f_act(cosT[:, 0:nb, :].rearrange("p b f -> p (b f)"), a0, AF.Sin), r=[R_rt], w=[R_cs])

            def rope_apply(src, src_regs, dst, dst_regs, H, cb):
                n = H * 64
                t_ = rope_t[:, 0:n]; u_ = rope_u[:, 0:n]
                cosb = cosT[:, cb:cb + 1, :]; sinb = sinT[:, cb:cb + 1, :]
                T.op("dve", f_tt(t_.rearrange("p (a f) -> p a f", f=32), src.rearrange("p (a f) -> p a f", f=32),
                                 cosb.to_broadcast([128, 2 * H, 32]), ALU.mult), r=src_regs + [R_cs], w=[R_rtu[0]])
                s4 = src.rearrange("p (h e f) -> p h e f", e=2, f=32)
                u4 = u_.rearrange("p (h e f) -> p h e f", e=2, f=32)
                t4 = t_.rearrange("p (h e f) -> p h e f", e=2, f=32)
                d4 = dst.rearrange("p (h e f) -> p h e f", e=2, f=32)
                sb_ = sinb.to_broadcast([128, H, 32])
                T.op("dve", f_tt(u4[:, :, 0, :], s4[:, :, 1, :], sb_, ALU.mult), r=src_regs + [R_cs], w=[R_rtu[1]])
                T.op("dve", f_tt(u4[:, :, 1, :], s4[:, :, 0, :], sb_, ALU.mult), r=src_regs + [R_cs], w=[R_rtu[1]])
                T.op("dve", f_tt(d4[:, :, 0, :], t4[:, :, 0, :], u4[:, :, 0, :], ALU.subtract), r=R_rtu, w=dst_regs)
                T.op("dve", f_tt(d4[:, :, 1, :], t4[:, :, 1, :], u4[:, :, 1, :], ALU.add), r=R_rtu, w=dst_regs)

            R_wbf = [Reg(f"wbf{i}") for i in range(NCHUNK)]
            CH_N = [4096, 4096, 8 * 260] + [8 * 384] * 8 + [2048] * 8 + [4096] * 18
            CH_P = [128] * 11 + [64] * 8 + [128] * 18
            sched = [0, 1, 2] + [v for oc in range(8) for v in (3 + oc, 11 + oc)] + list(range(19, 37))
            ring_state = {"issued": 0, "got": 0}
            total_gets = 8 * NCHUNK
            def ring_issue():
                k = ring_state["issued"]
                if k >= total_gets: return
                c = sched[k % NCHUNK]; b = k % NRING
                T.dma("sp", ring[b][0:CH_P[c], 0:CH_N[c]], wbf_d[c, 0:CH_P[c], 0:CH_N[c]], R_ring[b], r=[R_wbf[c]], w=[R_ring[b]])
                ring_state["issued"] += 1
            def ring_get(expect):
                k = ring_state["got"]
                assert sched[k % NCHUNK] == expect, (k, expect)
                while ring_state["issued"] < min(k + NRING, total_gets):
                    ring_issue()
                ring_state["got"] += 1
                b = k % NRING
                return ring[b], R_ring[b]

            def convert(src_dram_ap, npart, nelem, ncols, goff, dst_ap, dst_regs, half):
                stg = BIG[0:npart, half * 4096: half * 4096 + nelem]
                sregs = ISC[half * 8:(half + 1) * 8]
                T.dma("pool", stg, src_dram_ap, sregs[0], w=sregs)
                if goff is None:
                    T.op("act", f_act(dst_ap, stg, AF.Copy), r=sregs, w=dst_regs)
                else:
                    for kc in range(8):
                        T.op("dve", f_ts(dst_ap[:, kc * ncols:(kc + 1) * ncols], stg[:, kc * ncols:(kc + 1) * ncols],
                                          gvec[:, goff + kc:goff + kc + 1], None, ALU.mult), r=sregs + [R_misc], w=dst_regs)
            _ckpt(1)
            convert(wk_d[:, :], 128, 8 * 448, 448, 0, wk_sb[:].rearrange("p k c -> p (k c)"), [R_wk], 0)
            wmem_b = MSK[:, 4096:8192]
            convert(wmem_d[:, :], 128, 4096, 512, 16, wmem_b, MSKR[8:16], 1)

            for mc in range(2):
                T.dma("sp", xo[:], memc[mc * 128:(mc + 1) * 128, :], R_rl[0], w=R_rl)
                norm_T(xo[:], R_rl, hTo[:], [R_hTo], 0)
                T.op("pe", f_seq([f_mm(PB[1][:, :], hTo[:, kc, :], wmem_b[:, kc * 512:(kc + 1) * 512], kc == 0, kc == 7) for kc in range(8)]),
                     r=[R_hTo] + MSKR[8:16], w=[R_PB[1]])
                ti = rot("tok", 2)
                T.op("act", f_act(tok_b[ti][:, 0:256], PB[1][:, 0:256], AF.Copy), r=[R_PB[1]], w=[R_tok[ti]])
                T.op("act", f_act(mv[:, mc, :], PB[1][:, 256:512], AF.Copy), r=[R_PB[1]], w=[R_mv])
                pv = pbf(2)
                T.op("pe", f_seq([f_tr(pv[:, jj * 128:(jj + 1) * 128], tok_b[ti][:, jj * 128:(jj + 1) * 128], ident[:]) for jj in range(2)]),
                     r=[R_tok[ti], R_const], w=[R_PB[2]])
                T.op("act", f_act(mkT[:, :, mc * 128:(mc + 1) * 128], pv[:, 0:256].rearrange("p (j m) -> p j m", j=2), AF.Copy),
                     r=[R_PB[2]], w=[R_mkT])

            _ckpt(2)
            for c in range(NCHUNK):
                half = c % 2
                goff = 0 if c < 11 else (8 if 21 <= c < 29 else None)
                ncols = {0: 512, 1: 512, 2: 260}.get(c, 384 if c < 11 else 512)
                npart = CH_P[c]; nelem = CH_N[c]
                dstb = MSK[0:npart, half * 4096: half * 4096 + nelem]
                dregs = MSKR[half * 8:(half + 1) * 8]
                convert(wch_d[c, 0:npart, 0:nelem], npart, nelem, ncols, goff, dstb, dregs, half)
                T.dma("pool", wbf_d[c, 0:npart, 0:nelem], dstb, dregs[0], r=dregs, w=[R_wbf[c]])

            def kside(pos, x_ap, x_regs, hT_ap, hT_regs, cb, stage="AB"):
                kidx = 0 if pos == 63 else pos + 1
                rg = (pos + 1) % 16
                if "A" in stage:
                    norm_T(x_ap, x_regs, hT_ap, hT_regs, 0)
                if "B" in stage or "M" in stage:
                    T.op("pe", f_seq([f_mm(PB[1][:, 0:448], hT_ap[:, kc, :], wk_sb[:, kc, :], kc == 0, kc == 7) for kc in range(8)]),
                         r=hT_regs + [R_wk], w=[R_PB[1]])
                if "B" not in stage and "R" not in stage:
                    return
                ti = rot("tok", 2)
                rope_apply(PB[1][:, 0:256], [R_PB[1]], tok_b[ti][:, 0:256], [R_tok[ti]], 4, cb)
                _ckpt(3.3)
                T.op("dve", f_cp(vS[:, rg, :], PB[1][:, 256:384]), r=[R_PB[1]], w=[R_vS[rg]])
                T.op("dve", f_cp(vD[:, kidx, 0:64], PB[1][:, 384:448]), r=[R_PB[1]], w=[R_vD[kidx]])
                _ckpt(3.4)
                pv = pbf(2)
                T.op("pe", f_seq([f_tr(pv[:, jj * 128:(jj + 1) * 128], tok_b[ti][:, jj * 128:(jj + 1) * 128], ident[:]) for jj in range(2)]),
                     r=[R_tok[ti], R_const], w=[R_PB[2]])
                _ckpt(3.5)
                T.op("act", f_act(kST[:, rg, :], pv[:, 0:128], AF.Copy), r=[R_PB[2]], w=[R_kST[rg]])
                T.op("act", f_act(kDI[:, kidx * 128:(kidx + 1) * 128], pv[:, 128:256], AF.Copy), r=[R_PB[2]], w=[R_kDI[kidx]])

            _ckpt(3)
            rope_tables(63, 1)
            T.dma("sp", xo[:], xc[63 * 128:64 * 128, :], R_rl[0], w=R_rl)
            kside(63, xo[:], R_rl, hTo[:], [R_hTo], 0)

            def finish_attn(bO, bD, j, h0, es_ap):
                di = rot("dn", 1)
                d_ = dn[di][0:64, :]
                if es_ap is not None:
                    T.op("dve", f_tt(d_.rearrange("p (h t) -> p h t", h=4), PB[bD][0:64, :].rearrange("p (h t) -> p h t", h=4), es_ap, ALU.add),
                         r=[R_PB[bD], R_es], w=[R_dn[di]])
                    T.op("act", f_act(d_, d_, AF.Ln), r=[R_dn[di]], w=[R_dn[di]])
                else:
                    T.op("act", f_act(d_, PB[bD][0:64, :], AF.Ln), r=[R_PB[bD]], w=[R_dn[di]])
                T.op("act", f_act(d_, d_, AF.Exp, scale=-1.0), r=[R_dn[di]], w=[R_dn[di]])
                T.op("dve", f_tt(oTg[:, h0:h0 + 4, j * 128:(j + 1) * 128], PB[bO][0:64, :].rearrange("p (h t) -> p h t", h=4),
                                 d_.rearrange("p (h t) -> p h t", h=4), ALU.mult), r=[R_PB[bO], R_dn[di]], w=[R_oTg[j]])

            for g in range(8):
                T.mark(f"g{g} kside")
                npos = 8 if g < 7 else 7
                rope_tables(8 * g, 8)
                def kblock(pl, stage):
                    pos = 8 * g + pl
                    if pl % 2 == 0:
                        j = pl // 2
                        if "A" in stage:
                            T.dma("sp", xg[:, j, :], xc[pos * 128:(pos + 1) * 128, :], R_xg[j], w=[R_xg[j]])
                        kside(pos, xg[:, j, :], [R_xg[j]], hTg[:, :, j * 128:(j + 1) * 128], [R_hTg[j]], pl, stage)
                    else:
                        if "A" in stage:
                            T.dma("sp", xo[:], xc[pos * 128:(pos + 1) * 128, :], R_rl[0], w=R_rl)
                        kside(pos, xo[:], R_rl, hTo[:], [R_hTo], pl, stage)
                kblock(0, "A")
                for pl in range(npos):
                    kblock(pl, "M")
                    if pl + 1 < npos:
                        kblock(pl + 1, "A")
                    kblock(pl, "R")

                _ckpt(10 * (g + 1) + 1)
                T.mark(f"g{g} qproj")
                units = [(c, j) for c in range(3) for j in range(4)]
                qW = {}
                def q_mm(u):
                    c, j = units[u]
                    if j == 0:
                        W, RW = ring_get(c)
                        qW[c] = (W, RW)
                    W, RW = qW[c]
                    ncols = (512, 512, 260)[c]
                    Wv = W[:, 0:8 * ncols].rearrange("p (k c) -> p k c", k=8)
                    pb = 3 + (u % 2)
                    T.op("pe", f_seq([f_mm(PB[pb][:, 0:ncols], hTg[:, kc, j * 128:(j + 1) * 128], Wv[:, kc, :], kc == 0, kc == 7) for kc in range(8)]),
                         r=[R_hTg[j], RW], w=[R_PB[pb]])
                def q_post(u):
                    c, j = units[u]
                    pb = 3 + (u % 2); pt = 5 + (u % 2)
                    ti = rot("tok", 2)
                    if c < 2:
                        rope_apply(PB[pb][:, 0:512], [R_PB[pb]], tok_b[ti][:, 0:512], [R_tok[ti]], 8, 2 * j)
                        pv = pbf(pt)
                        T.op("pe", f_seq([f_tr(pv[:, q * 128:(q + 1) * 128], tok_b[ti][:, q * 128:(q + 1) * 128], ident[:]) for q in range(4)]),
                             r=[R_tok[ti], R_const], w=[R_PB[pt]])
                        dst = qsT if c == 0 else qdiT
                        T.op("act", f_act(dst[:, j, :], pv[:, 0:512], AF.Copy), r=[R_PB[pt]], w=[(R_qs if c == 0 else R_qdi)[j]])
                    else:
                        T.op("act", f_act(tok_b[ti][:, 0:256], PB[pb][:, 0:256], AF.Copy), r=[R_PB[pb]], w=[R_tok[ti]])
                        T.op("act", f_act(wiT[:, j, :], PB[pb][:, 256:260], AF.Copy, scale=1.0 / 16.0), r=[R_PB[pb]], w=[R_wi[j]])
                        pv = pbf(pt)
                        T.op("pe", f_seq([f_tr(pv[:, q * 128:(q + 1) * 128], tok_b[ti][:, q * 128:(q + 1) * 128], ident[:]) for q in range(2)]),
                             r=[R_tok[ti], R_const], w=[R_PB[pt]])
                        T.op("act", f_act(qmT[:, j, :], pv[:, 0:256], AF.Copy), r=[R_PB[pt]], w=[R_qm[j]])
                q_mm(0)
                for u in range(len(units)):
                    if u + 1 < len(units):
                        q_mm(u + 1)
                    q_post(u)

                _ckpt(10 * (g + 1) + 2)
                T.mark(f"g{g} attn")
                def slot_vars(j):
                    i = 4 * g + j
                    nkb = 2 * i + 2
                    return i, slice(j * 128, (j + 1) * 128), nkb, (nkb + 3) // 4, nkb * 128
                def fin_gen(bO, bD, j, h0, es_ap):
                    di = rot("dn", 1)
                    d_ = dn[di][0:64, :]
                    if es_ap is not None:
                        T.op("dve", f_tt(d_.rearrange("p (h t) -> p h t", h=4), PB[bD][0:64, :].rearrange("p (h t) -> p h t", h=4), es_ap, ALU.add),
                             r=[R_PB[bD], R_es], w=[R_dn[di]])
                        yield
                        T.op("act", f_act(d_, d_, AF.Ln), r=[R_dn[di]], w=[R_dn[di]])
                    else:
                        T.op("act", f_act(d_, PB[bD][0:64, :], AF.Ln), r=[R_PB[bD]], w=[R_dn[di]])
                    T.op("act", f_act(d_, d_, AF.Exp, scale=-1.0), r=[R_dn[di]], w=[R_dn[di]])
                    yield
                    T.op("dve", f_tt(oTg[:, h0:h0 + 4, j * 128:(j + 1) * 128], PB[bO][0:64, :].rearrange("p (h t) -> p h t", h=4),
                                     d_.rearrange("p (h t) -> p h t", h=4), ALU.mult), r=[R_PB[bO], R_dn[di]], w=[R_oTg[j]])
                    yield
                def swa_mem_gen(j):
                    i, tok, nkb, nch, n = slot_vars(j)
                    rp = (2 * i) % 16; rc = (2 * i + 1) % 16
                    for kv in range(2):
                        pr = slice(kv * 64, (kv + 1) * 64)
                        bS = (2, 3) if kv == 0 else (4, 5)
                        bO, bD = (0, 1) if kv == 0 else (6, 7)
                        Es_ = []
                        for which, rr_ in enumerate((rp, rc)):
                            b = bS[which]
                            T.op("pe", f_mm(PB[b][:, :], kST[pr, rr_, :], qsT[pr, j, :]), r=[R_kST[rr_], R_qs[j]], w=[R_PB[b]])
                            ei = rot("E", 5)
                            T.op("act", f_act(Et[ei][:], PB[b][:, :], AF.Exp, scale=0.125), r=[R_PB[b]], w=[R_E[ei]])
                            Es_.append((ei, rr_, which))
                        yield
                        Ps = []
                        for ei, rr_, which in Es_:
                            pi_ = rot("P", 2)
                            msk = (prev0 if i == 0 else tri_prev) if which == 0 else tri_cur
                            T.op("dve", f_tt(Pt[pi_][:].rearrange("p (h t) -> p h t", h=4), Et[ei][:].rearrange("p (h t) -> p h t", h=4),
                                             msk.unsqueeze(1).to_broadcast([128, 4, 128]), ALU.mult), r=[R_E[ei], R_const], w=[R_P[pi_]])
                            Ps.append((pi_, rr_))
                        T.op("pe", f_seq([f_mm(PB[bO][0:64, :], vS[:, rr_, pr], Pt[pi_][:], w_ == 0, w_ == 1) for w_, (pi_, rr_) in enumerate(Ps)]
                                         + [f_mm(PB[bD][0:64, :], ones_b[:], Pt[pi_][:], w_ == 0, w_ == 1) for w_, (pi_, rr_) in enumerate(Ps)]),
                             r=[R_P[p_] for p_, _ in Ps] + [R_vS[r_] for _, r_ in Ps] + [R_const], w=[R_PB[bO], R_PB[bD]])
                        yield
                        yield from fin_gen(bO, bD, j, 4 * kv, es[:, 4 * kv:4 * kv + 4].unsqueeze(2).to_broadcast([64, 4, 128]))
                    Es = [rot("E", 5), rot("E", 5)]
                    fns = []
                    for e in range(2):
                        pr = slice(e * 64, (e + 1) * 64)
                        for mc in range(2):
                            for jj in range(2):
                                fns.append(f_mm(PB[2 + 2 * mc + e][:, jj * 128:(jj + 1) * 128], mkT[pr, jj, mc * 128:(mc + 1) * 128],
                                                qmT[pr, j, jj * 128:(jj + 1) * 128]))
                    T.op("pe", f_seq(fns), r=[R_mkT, R_qm[j]], w=[R_PB[2], R_PB[3], R_PB[4], R_PB[5]])
                    for mc in range(2):
                        E4 = Et[Es[mc]][:].rearrange("p (jj e t) -> p jj e t", jj=2, e=2)
                        for e in range(2):
                            b = 2 + 2 * mc + e
                            T.op("act", f_act(E4[:, :, e, :], PB[b][:, 0:256].rearrange("p (jj t) -> p jj t", jj=2), AF.Exp, scale=0.125),
                                 r=[R_PB[b]], w=[R_E[Es[mc]]])
                    yield
                    fns = []
                    for hh in range(4):
                        for mc in range(2):
                            fns.append(f_mm(PB[0][0:64, hh * 128:(hh + 1) * 128], mv[:, mc, hh * 64:(hh + 1) * 64], Et[Es[mc]][:, hh * 128:(hh + 1) * 128], mc == 0, mc == 1))
                    for mc in range(2):
                        fns.append(f_mm(PB[1][0:64, :], ones_b[:], Et[Es[mc]][:], mc == 0, mc == 1))
                    T.op("pe", f_seq(fns), r=[R_E[e_] for e_ in Es] + [R_mv, R_const], w=[R_PB[0], R_PB[1]])
                    yield
                    yield from fin_gen(0, 1, j, 12, None)
                def swa_mem(j):
                    for _ in swa_mem_gen(j):
                        pass
                def dsa_index_gen(j, inter):
                    i, tok, nkb, nch, n = slot_vars(j)
                    hu = 0
                    for c in range(nch):
                        kb0 = 4 * c; nb = min(4, nkb - kb0); w_ = nb * 128
                        cols = slice(kb0 * 128, kb0 * 128 + w_)
                        for hh in range(4):
                            bk = (5 + hu % 3) if inter else (4 * (c % 2) + hh)
                            hu += 1
                            T.op("pe", f_mm(PB[bk][:, 0:w_], qdiT[64:128, j, hh * 128:(hh + 1) * 128], kDI[64:128, cols]),
                                 r=[R_qdi[j]] + R_kDI[kb0:kb0 + nb], w=[R_PB[bk]])
                            ri = rot("rl", 2)
                            T.op("act", f_act(rl[ri][:, 0:w_], PB[bk][:, 0:w_], AF.Relu), r=[R_PB[bk]], w=[R_rl[ri]])
                            if hh == 0:
                                T.op("dve", f_ts(BIG[:, cols], rl[ri][:, 0:w_], wiT[:, j, 0:1], None, ALU.mult), r=[R_rl[ri], R_wi[j]], w=[ISC[c]])
                            else:
                                T.op("dve", f_stt(BIG[:, cols], rl[ri][:, 0:w_], wiT[:, j, hh:hh + 1], BIG[:, cols], ALU.mult, ALU.add),
                                     r=[R_rl[ri], R_wi[j], ISC[c]], w=[ISC[c]])
                            yield
                def dsa_index(j):
                    for _ in dsa_index_gen(j, False):
                        pass
                def dsa_bisect(j, pipe=None):
                    i, tok, nkb, nch, n = slot_vars(j)
                    _ckpt(12.3)
                    IR = ISC[0:nch]; MR = MSKR[0:nch]
                    cd = nch if nch < 2 else max(1, int(round(0.42 * nch)))
                    nd = min(n, cd * 512); na = n - nd
                    IRd, MRd, IRa, MRa = ISC[0:cd], MSKR[0:cd], ISC[cd:nch], MSKR[cd:nch]
                    amax = bis[:, 0:1]; wtot = bis[:, 1:2]; lo = bis[:, 2:3]; mid = bis[:, 3:4]; cn = bis[:, 4:5]; dl = bis[:, 5:6]
                    sa = bis[:, 6:7]; tt_ = bis[:, 7:8]
                    wks = bis[:, 8:8 + NITER]
                    T.op("dve", lambda h, a=amax, b=BIG[:, 0:n]: h.tensor_reduce(out=a, in_=b, axis=AX.X, op=ALU.max, apply_absolute_value=True),
                         r=IR, w=[R_lo])
                    T.op("pool", f_tt(BIG[:, 0:128], BIG[:, 0:128], p63bias, ALU.add), r=[ISC[0], R_cmf, R_lo], w=[ISC[0]])
                    dcol = slice((nkb - 1) * 128, nkb * 128)
                    T.op("pool", f_tt(BIG[:, dcol], BIG[:, dcol], dbias, ALU.add), r=[ISC[nch - 1], R_cmf, R_lo], w=[ISC[nch - 1]])
                    T.op("dve", f_ts(wtot, amax, 2.0002, 2e-6, ALU.mult, ALU.add), r=[R_lo], w=[R_lo])
                    T.op("dve", f_ts(lo, amax, -1.0001, -1e-6, ALU.mult, ALU.add), r=[R_lo], w=[R_lo])
                    T.op("dve", f_ts(wks, pw2[:], wtot, None, ALU.mult), r=[R_lo, R_const], w=[R_lo])
                    thrc = TOPK - na / 2.0
                    for k in range(NITER):
                        T.op("dve", f_tt(mid, lo, wks[:, k:k + 1], ALU.add), r=[R_lo], w=[R_mid])
                        if na > 0:
                            T.op("act", f_act(MSK[:, nd:n], BIG[:, nd:n], AF.Sign, bias=mid, scale=-1.0, accum=sa), r=IRa + [R_mid], w=MRa + [R_ca])
                        T.op("dve", f_ts(MSK[:, 0:nd], BIG[:, 0:nd], mid, 0.0, ALU.is_ge, ALU.add, accum=cn), r=IRd + [R_mid], w=MRd + [R_cd])
                        if na > 0:
                            T.op("dve", f_stt(tt_, sa, -0.5, cn, ALU.mult, ALU.add), r=[R_ca, R_cd], w=[R_t])
                            T.op("dve", f_stt(dl, tt_, thrc, wks[:, k:k + 1], ALU.is_ge, ALU.mult), r=[R_t, R_lo], w=[R_t])
                        else:
                            T.op("dve", f_stt(dl, cn, TOPK, wks[:, k:k + 1], ALU.is_ge, ALU.mult), r=[R_cd, R_lo], w=[R_t])
                        T.op("dve", f_tt(lo, lo, dl, ALU.add), r=[R_t], w=[R_lo])
                        if pipe is not None:
                            next(pipe, None)
                    T.op("dve", f_ts(MSK[:, 0:n], BIG[:, 0:n], lo, None, ALU.is_ge), r=IR + [R_lo], w=MR)
                def dsa_attn(j, pipe=None):
                    i, tok, nkb, nch, n = slot_vars(j)
                    inter = pipe is not None
                    _ckpt(12.4)
                    LA = 2 if inter else 3
                    st_ = {}
                    T.op("dve", f_cp(qd0[0:64, :], qdiT[0:64, j, :]), r=[R_qdi[j]], w=[R_qd0])
                    def prep(c):
                        kb_ = 4 * c
                        nb = min(4, nkb - kb_)
                        mi = c % 2
                        pv = pbf(1)
                        T.op("pe", f_seq([f_tr(pv[:, q2 * 128:(q2 + 1) * 128], MSK[:, (kb_ + q2) * 128:(kb_ + q2 + 1) * 128], ident[:]) for q2 in range(nb)]),
                             r=[MSKR[c], R_const], w=[R_PB[1]])
                        T.op("act", f_act(mT[mi][:, 0:nb * 128], pv[:, 0:nb * 128], AF.Identity, scale=30000.0, bias=-30000.0), r=[R_PB[1]], w=[R_mT[mi]])
                    prep(0)
                    def front(kb):
                        c, q = kb // 4, kb % 4
                        if q == 0 and c + 1 < nch:
                            prep(c + 1)
                        mi = c % 2
                        b = (2 + kb % 3) if inter else (2 + kb % 6)
                        T.op("pe", f_seq([f_mm(PB[b][:, :], kDI[:, kb * 128:(kb + 1) * 128], qd0[:, :], True, False)]
                                         + [f_mm(PB[b][:, hh * 128:(hh + 1) * 128], ident[:], mT[mi][:, q * 128:(q + 1) * 128], False, hh == 3) for hh in range(4)]),
                             r=[R_kDI[kb], R_qd0, R_mT[mi], R_const], w=[R_PB[b]])
                        ei = rot("E", 5)
                        T.op("act", f_act(Et[ei][:], PB[b][:, :], AF.Exp, scale=0.125), r=[R_PB[b]], w=[R_E[ei]])
                        st_[kb] = ei
                    def back(kb):
                        ei = st_.pop(kb)
                        T.op("pe", f_mm(PB[0][:, :], vD[:, kb, :], Et[ei][:], kb == 0, kb == nkb - 1),
                             r=[R_E[ei], R_vD[kb]], w=[R_PB[0]])
                    for t in range(nkb + LA):
                        if t < nkb: front(t)
                        if t - LA >= 0: back(t - LA)
                        if inter:
                            next(pipe, None)
                    if inter:
                        for _ in pipe:
                            pass
                    T.op("act", f_act(dn[0][64:128, :], PB[0][64:128, :], AF.Copy), r=[R_PB[0]], w=[R_dn[0]])
                    T.op("pe", f_mm(PB[1][0:64, :], identf[:, 64:128], dn[0][:, :]), r=[R_dn[0], R_const], w=[R_PB[1]])
                    finish_attn(0, 1, j, 8, None)

                swa_mem(0)
                T.mark(f"g{g} s0 idx")
                dsa_index(0)
                for j in range(4):
                    T.mark(f"g{g} s{j} bis")
                    pipe = swa_mem_gen(j + 1) if j < 3 else None
                    dsa_bisect(j, pipe)
                    if pipe is not None:
                        for _ in pipe:
                            pass
                    T.mark(f"g{g} s{j} dattn")
                    dsa_attn(j, dsa_index_gen(j + 1, True) if j < 3 else None)

                _ckpt(10 * (g + 1) + 3)
                T.mark(f"g{g} pass2")
                RQ = R_qs + R_qdi
                for oc in range(8):
                    G, RG = ring_get(3 + oc)
                    Gv = G[:, 0:8 * 384].rearrange("p (k c) -> p k c", k=8)
                    for jb in range(3):
                        T.op("pe", f_seq([f_mm(PB[jb][:, :], Gv[:, kc, jb * 128:(jb + 1) * 128], hTg[:, kc, :], kc == 0, kc == 7) for kc in range(8)]),
                             r=R_hTg + [RG], w=[R_PB[jb]])
                        T.op("act", f_act(gsb[jb][:], PB[jb][:, :], AF.Sigmoid, bias=bgate[:, oc * 3 + jb:oc * 3 + jb + 1], scale=1.0),
                             r=[R_PB[jb], R_misc], w=[R_gsb[jb]])
                    Pw, RP = ring_get(11 + oc)
                    Pv = Pw[0:64, 0:2048].rearrange("p (h c) -> p h c", h=16)
                    for jb, (h0, nh) in enumerate(((0, 8), (8, 4), (12, 4))):
                        T.op("pe", f_seq([f_mm(PB[3 + jb][:, :], Pv[:, h0 + hh, :], oTg[:, h0 + hh, :], hh == 0, hh == nh - 1) for hh in range(nh)]),
                             r=R_oTg + [RP], w=[R_PB[3 + jb]])
                    T.op("dve", f_tt(mg[0][:], PB[3][:, :], gsb[0][:], ALU.mult), r=[R_PB[3], R_gsb[0]], w=[R_mg[0]])
                    T.op("dve", f_tt(mg[1][:], PB[4][:, :], gsb[1][:], ALU.mult), r=[R_PB[4], R_gsb[1]], w=[R_mg[1]])
                    T.op("dve", f_tt(mg[0][:], mg[0][:], mg[1][:], ALU.add), r=[R_mg[0], R_mg[1]], w=[R_mg[0]])
                    T.op("dve", f_tt(mg[1][:], PB[5][:, :], gsb[2][:], ALU.mult), r=[R_PB[5], R_gsb[2]], w=[R_mg[1]])
                    T.op("dve", f_tt(mergedT[:, oc, :], mg[0][:], mg[1][:], ALU.add), r=[R_mg[0], R_mg[1]], w=RQ)

                _ckpt(10 * (g + 1) + 4)
                T.mark(f"g{g} wout")
                for cc in range(2):
                    W, RW = ring_get(19 + cc)
                    Wv = W[:, :].rearrange("p (k c) -> p k c", k=8)
                    for j in range(4):
                        pb = 6 + (j % 2)
                        T.op("pe", f_seq([f_mm(PB[pb][:, :], mergedT[:, kc, j * 128:(j + 1) * 128], Wv[:, kc, :], kc == 0, kc == 7) for kc in range(8)]),
                             r=RQ + [RW], w=[R_PB[pb]])
                        T.op("dve", f_tt(xg[:, j, cc * 512:(cc + 1) * 512], PB[pb][:, :], xg[:, j, cc * 512:(cc + 1) * 512], ALU.add),
                             r=[R_PB[pb], R_xg[j]], w=[R_xg[j]])

                _ckpt(10 * (g + 1) + 5)
                T.mark(f"g{g} mlp")
                for j in range(4):
                    norm_T(xg[:, j, :], [R_xg[j]], hTg[:, :, j * 128:(j + 1) * 128], [R_hTg[j]], j % 2)
                for fb in range(8):
                    W, RW = ring_get(21 + fb)
                    Wv = W[:, :].rearrange("p (k c) -> p k c", k=8)
                    for fs in range(4):
                        fc = fb * 4 + fs
                        pb = 2 + (fc % 4)
                        T.op("pe", f_seq([f_mm(PB[pb][:, :], Wv[:, kc, fs * 128:(fs + 1) * 128], hTg[:, kc, :], kc == 0, kc == 7) for kc in range(8)]),
                             r=R_hTg + [RW], w=[R_PB[pb]])
                        ri = rot("rl", 2)
                        T.op("act", f_act(rl[ri][:], PB[pb][:, :], AF.Relu), r=[R_PB[pb]], w=[R_rl[ri]])
                        T.op("dve", f_tt(hidT[:, fc, :], rl[ri][:], rl[ri][:], ALU.mult), r=[R_rl[ri]], w=[ISC[fc // 2]])
                for cc in range(2):
                    for ks in range(4):
                        W, RW = ring_get(29 + cc * 4 + ks)
                        Wv = W[:, :].rearrange("p (k c) -> p k c", k=8)
                        for j in range(4):
                            pb = 4 + j
                            T.op("pe", f_seq([f_mm(PB[pb][:, :], hidT[:, ks * 8 + fl, j * 128:(j + 1) * 128], Wv[:, fl, :],
                                                   ks == 0 and fl == 0, ks == 3 and fl == 7) for fl in range(8)]),
                                 r=ISC[ks * 4:(ks + 1) * 4] + [RW], w=[R_PB[pb]])
                    for j in range(4):
                        pb = 4 + j
                        T.op("dve", f_tt(xg[:, j, cc * 512:(cc + 1) * 512], PB[pb][:, :], xg[:, j, cc * 512:(cc + 1) * 512], ALU.add),
                             r=[R_PB[pb], R_xg[j]], w=[R_xg[j]])
                _ckpt(10 * (g + 1) + 6)
                T.mark(f"g{g} final")
                for j in range(4):
                    i = 4 * g + j
                    si = rot("stat", 4); s = stat[:, si * 4:(si + 1) * 4]; rs = R_stat[si]
                    hi = rot("hn", 1)
                    T.op("act", f_act(hn[hi][:], xg[:, j, :], AF.Square, accum=s[:, 0:1]), r=[R_xg[j]], w=[R_hn[hi], rs])
                    T.op("act", f_act(s[:, 1:2], s[:, 0:1], AF.Ln, scale=1.0 / 1024.0, bias=1e-6), r=[rs], w=[rs])
                    T.op("act", f_act(s[:, 2:3], s[:, 1:2], AF.Exp, scale=-0.5), r=[rs], w=[rs])
                    T.op("dve", f_stt(xg[:, j, :], xg[:, j, :], s[:, 2:3], gfin[:], ALU.mult, ALU.mult), r=[R_xg[j], rs, R_misc], w=[R_xg[j]])
                    T.dma("sp", out_d[i * 128:(i + 1) * 128, :], xg[:, j, :], R_xg[j], r=[R_xg[j]])
        try:
            emit()
        except _Stop:
            T.dma("sp", out_d[0:128, :], xo[:], R_rl[0], r=R_rl)
            T.wait_all("sp", R_rl + R_ring)
            T.wait_all("pool", [MSKR[0], MSKR[8], ISC[0], ISC[8]])
        T.wait_all("sp", R_xg)
        with nc.Block() as block:
            T.replay(block)
    return nc


OFF = dict(q_s=0, k_s=512, v_s=640, q_d=768, k_d=1024, v_d=1088, q_i=1152, k_i=1408, w_i=1472, q_m=1476, gates=1732)


def _chunked(w, cols):
    sub = w[:, cols]
    n = sub.shape[1]
    return np.ascontiguousarray(sub.reshape(8, 128, n).transpose(1, 0, 2).reshape(128, 8 * n))


def _prep_weights(w_in, w_proj_swa, w_proj_dsa, w_proj_mem, w_out, w_mlp_in, w_mlp_out):
    wch = np.zeros((NCHUNK, 128, 4096), np.float32)
    r64 = np.arange(64)
    qs_cols = np.concatenate([OFF["q_s"] + h * 64 + r64 for h in (0, 4, 1, 5, 2, 6, 3, 7)])
    qdi_cols = np.concatenate([np.concatenate([OFF["q_d"] + h * 64 + r64, OFF["q_i"] + h * 64 + r64]) for h in range(4)])
    qm_cols = np.concatenate([OFF["q_m"] + np.arange(256), OFF["w_i"] + np.arange(4)])
    wch[0] = _chunked(w_in, qs_cols)
    wch[1] = _chunked(w_in, qdi_cols)
    wch[2, :, 0:8 * 260] = _chunked(w_in, qm_cols)
    for oc in range(8):
        gc = np.concatenate([OFF["gates"] + jb * 1024 + oc * 128 + np.arange(128) for jb in range(3)])
        wch[3 + oc, :, 0:8 * 384] = _chunked(w_in, gc)
        wp = np.concatenate([w_proj_swa.reshape(8, 64, 1024), w_proj_dsa.reshape(4, 64, 1024), w_proj_mem.reshape(4, 64, 1024)], 0)
        wch[11 + oc, 0:64, 0:2048] = wp[:, :, oc * 128:(oc + 1) * 128].transpose(1, 0, 2).reshape(64, 2048)
    for cc in range(2):
        wch[19 + cc] = _chunked(w_out, cc * 512 + np.arange(512))
    for fb in range(8):
        wch[21 + fb] = _chunked(w_mlp_in, fb * 512 + np.arange(512))
    for cc in range(2):
        for ks in range(4):
            blk = w_mlp_out[ks * 1024:(ks + 1) * 1024, cc * 512:(cc + 1) * 512]
            wch[29 + cc * 4 + ks] = blk.reshape(8, 128, 512).transpose(1, 0, 2).reshape(128, 4096)
    k_cols = np.concatenate([OFF["k_s"] + np.arange(128), OFF["k_d"] + r64, OFF["k_i"] + r64, OFF["v_s"] + np.arange(128), OFF["v_d"] + r64])
    wk = _chunked(w_in, k_cols)
    return wch, wk


_NC_CACHE = {}


def kernel(x, mem, positions, g_mix, w_in, b_gate, sinks, g_mem, w_mem_kv, w_proj_swa, w_proj_dsa,
           w_proj_mem, w_out, g_mlp, w_mlp_in, w_mlp_out, g_final):
    f = lambda a: np.asarray(a, dtype=np.float32)
    x = f(x); mem = f(mem); positions = np.asarray(positions, dtype=np.int32)
    wch, wk = _prep_weights(f(w_in)[0], f(w_proj_swa)[0], f(w_proj_dsa)[0], f(w_proj_mem)[0], f(w_out)[0], f(w_mlp_in)[0], f(w_mlp_out)[0])
    wmem = _chunked(f(w_mem_kv)[0], np.arange(512))
    tr = lambda v: np.ascontiguousarray(f(v).reshape(8, 128).T)
    gvec = np.concatenate([tr(g_mix[0]), tr(g_mlp[0]), tr(g_mem[0])], 1)
    gfin = np.ascontiguousarray(np.broadcast_to(f(g_final)[None, :], (128, 1024)))
    bg = f(b_gate)[0].reshape(3, 8, 128)
    bgate = np.ascontiguousarray(bg.transpose(2, 1, 0).reshape(128, 24))
    sinkr = np.ascontiguousarray(np.broadcast_to(f(sinks)[0][None, :], (64, 8)))
    half = 32
    invf = np.power(np.float32(10000.0), -np.arange(half, dtype=np.float32) / np.float32(half)).astype(np.float32)
    invf = np.ascontiguousarray(np.broadcast_to(invf[None, :], (128, 32)))
    s_ = np.arange(128)[:, None]; t_ = np.arange(128)[None, :]
    tri_cur = (t_ >= s_).astype(np.float32)
    tri_prev = (s_ > t_).astype(np.float32)
    dbias = np.where(t_ <= s_, 0.0, NEG).astype(np.float32)
    in_maps = []
    for c in range(8):
        b, par = c // 2, c % 2
        order = np.arange(64) if par == 0 else np.concatenate([np.arange(1, 64), [0]])
        xb = x[b].reshape(64, 128, 1024)[order].reshape(64 * 128, 1024)
        pb = positions[b].reshape(64, 128)[order]
        posc = np.ascontiguousarray(pb.T)
        prev0 = tri_prev * float(par)
        p63 = np.full((128, 128), 0.0 if par == 1 else NEG, np.float32)
        cmask = np.ascontiguousarray(np.stack([tri_cur, tri_prev, prev0, dbias, p63], 1).reshape(128, 5 * 128).astype(np.float32))
        in_maps.append(dict(xc=np.ascontiguousarray(xb), posc=posc, memc=np.ascontiguousarray(mem[b]), cmask=cmask, invf=invf,
                            wk=wk, wch=wch, wmem=wmem, gvec=gvec, gfin=gfin, bgate=bgate, sinkr=sinkr))
    if "nc" not in _NC_CACHE:
        _NC_CACHE["nc"] = build_program()
    nc = _NC_CACHE["nc"]
    res = run_bass_kernel_spmd(nc, in_maps, core_ids=list(range(8)))
    out = np.zeros((4, 64, 128, 1024), np.float32)
    for c in range(8):
        b, par = c // 2, c % 2
        o = np.asarray(res.results[c]["out"]).reshape(32, 128, 1024)
        out[b, par::2] = o
    return out.reshape(4, 8192, 1024)
```

```python
import math
import numpy as np
from contextlib import ExitStack
import concourse.bass as bass
import concourse.mybir as mybir
from concourse.bass_utils import run_bass_kernel_spmd

F32 = mybir.dt.float32; BF16 = mybir.dt.bfloat16; I32 = mybir.dt.int32
ALU = mybir.AluOpType; AF = mybir.ActivationFunctionType; AX = mybir.AxisListType

STAGE = 99
class _Stop(Exception):
    pass
def _ckpt(n):
    if STAGE <= n:
        raise _Stop()
NITER = 14
NEG = -1.0e30
NCHUNK = 37
TOPK = 256.0


class Reg:
    __slots__ = ("name", "w", "r", "dsem", "dcnt")
    def __init__(self, name):
        self.name = name; self.w = None; self.r = {}; self.dsem = None; self.dcnt = 0


class Eng:
    def __init__(self, name):
        self.name = name; self.q = []; self.sem = None; self.cnt = 0; self.seen = {}


class Trk:
    def __init__(self, nc, stack, nsem):
        self.nc = nc
        self.sems = [stack.enter_context(nc.semaphore(f"s{i}")) for i in range(nsem)]
        self.si = 0
        self.E = {n: Eng(n) for n in ("pe", "act", "dve", "pool", "sp")}
        for e in self.E.values():
            e.sem = self.newsem()
    def newsem(self):
        s = self.sems[self.si]; self.si += 1; return s
    def _deps(self, eng, reads, writes):
        deps = {}
        def add(ev, kind):
            if ev is None: return
            sem, val, src = ev
            if src is eng and eng.name == "pe":
                return
            k = id(sem)
            if eng.seen.get(k, 0) >= val: return
            if k not in deps or deps[k][1] < val: deps[k] = (sem, val)
        for r in reads: add(r.w, "raw")
        for w in writes:
            add(w.w, "waw")
            for ev in w.r.values(): add(ev, "war")
        out = list(deps.values())
        for sem, val in out: eng.seen[id(sem)] = val
        return out
    def op(self, en, fn, r=(), w=()):
        eng = self.E[en]
        waits = self._deps(eng, r, w)
        eng.cnt += 1
        ev = (eng.sem, eng.cnt, eng)
        eng.q.append((waits, fn, (eng.sem, 1)))
        for x in r: x.r[en] = ev
        for x in w: x.w = ev; x.r = {}
    def dma(self, en, out_ap, in_ap, slot, r=(), w=()):
        eng = self.E[en]
        waits = self._deps(eng, r, w)
        if slot.dsem is None: slot.dsem = self.newsem()
        slot.dcnt += 16
        ev = (slot.dsem, slot.dcnt, None)
        eng.q.append((waits, lambda h: h.dma_start(out=out_ap, in_=in_ap), (slot.dsem, 16)))
        for x in r: x.r["dma%d" % id(slot)] = ev
        for x in w: x.w = ev; x.r = {}
    def mark(self, label):
        self.E["pe"].q.append(([], None, label))
    def wait_all(self, en, regs):
        eng = self.E[en]
        waits = self._deps(eng, [], regs)
        eng.q.append((waits, None, None))
    def replay(self, block):
        def mk(en):
            q = self.E[en].q
            def body(h):
                for waits, fn, inc in q:
                    for sem, val in waits: h.wait_ge(sem, val)
                    if fn is None:
                        if isinstance(inc, str): MARKS.append((inc, PE_CNT[0]))
                        continue
                    ins = fn(h)
                    ins.then_inc(inc[0], inc[1])
            return body
        block.tensor(mk("pe")); block.scalar(mk("act")); block.vector(mk("dve"))
        block.gpsimd(mk("pool")); block.sync(mk("sp"))


PE_CNT = [0]
MARKS = []
def f_mm(out, lhsT, rhs, start=True, stop=True):
    def f(h):
        PE_CNT[0] += 1
        return h.matmul(out, lhsT=lhsT, rhs=rhs, start=start, stop=stop)
    return f
def f_tr(out, in_, ident):
    def f(h):
        PE_CNT[0] += 1
        return h.transpose(out=out, in_=in_, identity=ident)
    return f
def f_seq(fns):
    def f(h):
        ins = None
        for fn in fns: ins = fn(h)
        return ins
    return f
def f_act(out, in_, func, bias=None, scale=None, accum=None):
    kw = {}
    if bias is not None: kw["bias"] = bias
    if scale is not None: kw["scale"] = scale
    if accum is not None: kw["accum_out"] = accum
    return lambda h: h.activation(out=out, in_=in_, func=func, **kw)
def f_ts(out, in0, s1, s2=None, op0=ALU.mult, op1=None, accum=None):
    kw = {}
    if op1 is not None: kw["op1"] = op1
    if accum is not None: kw["accum_out"] = accum
    return lambda h: h.tensor_scalar(out=out, in0=in0, scalar1=s1, scalar2=s2, op0=op0, **kw)
def f_tt(out, in0, in1, op):
    return lambda h: h.tensor_tensor(out=out, in0=in0, in1=in1, op=op)
def f_stt(out, in0, scalar, in1, op0, op1):
    return lambda h: h.scalar_tensor_tensor(out=out, in0=in0, scalar=scalar, in1=in1, op0=op0, op1=op1)
def f_cp(out, in_):
    return lambda h: h.tensor_copy(out=out, in_=in_)
def f_memset(ap, v):
    return lambda h: h.memset(ap, v)


def build_program():
    nc = bass.Bass("TRN2", target_bir_lowering=False)
    dt_in = lambda n, s, d=F32: nc.dram_tensor(n, s, d, kind="ExternalInput").ap()
    xc = dt_in("xc", [64 * 128, 1024])
    posc = dt_in("posc", [128, 64], I32)
    memc = dt_in("memc", [256, 1024])
    cmask = dt_in("cmask", [128, 5 * 128])
    invf_d = dt_in("invf", [128, 32])
    wk_d = dt_in("wk", [128, 8 * 448])
    wch_d = dt_in("wch", [NCHUNK, 128, 4096])
    wmem_d = dt_in("wmem", [128, 4096])
    gvec_d = dt_in("gvec", [128, 24])
    gfin_d = dt_in("gfin", [128, 1024])
    bgate_d = dt_in("bgate", [128, 24])
    sinkr_d = dt_in("sinkr", [64, 8])
    out_d = nc.dram_tensor("out", [32 * 128, 1024], F32, kind="ExternalOutput").ap()
    wbf_d = nc.dram_tensor("wbf", [NCHUNK, 128, 4096], BF16, kind="Internal").ap()

    with ExitStack() as st:
        T = Trk(nc, st, 48)
        def sb(name, shape, dt):
            return st.enter_context(nc.sbuf_tensor(name, shape, dt))
        BIG = sb("BIG", [128, 8192], F32)
        MSK = sb("MSK", [128, 8192], BF16)
        ISC = [Reg(f"isc{c}") for c in range(16)]
        MSKR = [Reg(f"msk{c}") for c in range(16)]
        hidT = BIG[:].bitcast(BF16).rearrange("p (f t) -> p f t", t=512)
        wk_sb = sb("wk_sb", [128, 8, 448], BF16); R_wk = Reg("wk")
        kDI = sb("kDI", [128, 8192], BF16); R_kDI = [Reg(f"kDI{i}") for i in range(64)]
        vD = sb("vD", [128, 64, 128], BF16); R_vD = [Reg(f"vD{i}") for i in range(64)]
        kST = sb("kST", [128, 16, 128], BF16); R_kST = [Reg(f"kST{i}") for i in range(16)]
        vS = sb("vS", [128, 16, 128], BF16); R_vS = [Reg(f"vS{i}") for i in range(16)]
        mkT = sb("mkT", [128, 2, 256], BF16); R_mkT = Reg("mkT")
        mv = sb("mv", [128, 2, 256], BF16); R_mv = Reg("mv")
        xg = sb("xg", [128, 4, 1024], F32); R_xg = [Reg(f"xg{j}") for j in range(4)]
        hTg = sb("hTg", [128, 8, 512], BF16); R_hTg = [Reg(f"hTg{j}") for j in range(4)]
        hTo = sb("hTo", [128, 8, 128], BF16); R_hTo = Reg("hTo")
        oTg = sb("oTg", [64, 16, 512], BF16); R_oTg = [Reg(f"oTg{j}") for j in range(4)]
        QM = sb("QM", [128, 4096], BF16)
        qsT = QM[:, 0:2048].rearrange("p (s c) -> p s c", s=4)
        qdiT = QM[:, 2048:4096].rearrange("p (s c) -> p s c", s=4)
        mergedT = QM[:].rearrange("p (k t) -> p k t", k=8)
        R_qs = [Reg(f"qs{j}") for j in range(4)]; R_qdi = [Reg(f"qdi{j}") for j in range(4)]
        qmT = sb("qmT", [128, 4, 256], BF16); R_qm = [Reg(f"qm{j}") for j in range(4)]
        wiT = sb("wiT", [128, 4, 4], F32); R_wi = [Reg(f"wi{j}") for j in range(4)]
        NRING = 2
        ring = [sb(f"ring{i}", [128, 4096], BF16) for i in range(NRING)]
        R_ring = [Reg(f"ring{i}") for i in range(NRING)]
        ident = sb("ident", [128, 128], BF16); identf = sb("identf", [128, 128], F32); onesf = sb("onesf", [128, 128], F32)
        ones_b = sb("ones_b", [128, 64], BF16)
        qd0 = sb("qd0", [128, 512], BF16); R_qd0 = Reg("qd0")
        R_const = Reg("const")
        cm_f = sb("cm_f", [128, 5, 128], F32)
        cm_b = sb("cm_b", [128, 3, 128], BF16)
        invf = sb("invf_sb", [128, 32], F32)
        gvec = sb("gvec_sb", [128, 24], F32)
        gfin = sb("gfin_sb", [128, 1024], F32)
        bgate = sb("bgate_sb", [128, 24], F32)
        es = sb("es_sb", [64, 8], F32)
        pw2 = sb("pw2", [128, NITER], F32)
        posi = sb("posi", [128, 64], I32); posf = sb("posf", [128, 64], F32)
        cosT = sb("cosT", [128, 8, 32], F32); sinT = sb("sinT", [128, 8, 32], F32); R_cs = Reg("cs")
        rtmp = [sb(f"rtmp{i}", [128, 256], F32) for i in range(3)]; rtmpi = sb("rtmpi", [128, 256], I32); R_rt = Reg("rt")
        stat = sb("stat", [128, 16], F32); R_stat = [Reg(f"stat{i}") for i in range(4)]
        hn = [sb(f"hn{i}", [128, 1024], BF16) for i in range(1)]; R_hn = [Reg(f"hn{i}") for i in range(1)]
        tok_b = [sb(f"tok_b{i}", [128, 512], BF16) for i in range(2)]; R_tok = [Reg(f"tok{i}") for i in range(2)]
        Et = [sb(f"Et{i}", [128, 512], BF16) for i in range(5)]; R_E = [Reg(f"E{i}") for i in range(5)]
        Pt = [sb(f"Pt{i}", [128, 512], BF16) for i in range(2)]; R_P = [Reg(f"P{i}") for i in range(2)]
        RLX = sb("RLX", [128, 1024], F32); rl = [RLX[:, 0:512], RLX[:, 512:1024]]; R_rl = [Reg(f"rl{i}") for i in range(2)]
        xo = RLX; R_xo = R_rl[0]
        mT = [sb(f"mT{i}", [128, 512], BF16) for i in range(2)]; R_mT = [Reg(f"mT{i}") for i in range(2)]
        dn = [sb(f"dn{i}", [128, 512], F32) for i in range(1)]; R_dn = [Reg(f"dn{i}") for i in range(1)]
        mg = rl; R_mg = R_rl
        RTU = sb("RTU", [128, 1024], F32); rope_t = RTU[:, 0:512]; rope_u = RTU[:, 512:1024]; R_rtu = [Reg("rt_"), Reg("ru_")]
        gsb = [sb(f"gsb{i}", [128, 512], BF16) for i in range(3)]; R_gsb = [Reg(f"gsb{i}") for i in range(3)]
        bis = sb("bis", [128, 8 + NITER], F32); R_lo = Reg("lo"); R_mid = Reg("mid"); R_cd = Reg("cd"); R_ca = Reg("ca"); R_t = Reg("t")
        PB = [st.enter_context(nc.psum_tensor(f"pb{i}", [128, 512], F32)) for i in range(8)]
        R_PB = [Reg(f"pb{i}") for i in range(8)]
        def pbf(i):
            return PB[i][:].bitcast(BF16)

        cnt = {"ring": 0, "stat": 0, "hn": 0, "tok": 0, "E": 0, "P": 0, "rl": 0, "mT": 0, "dn": 0, "mg": 0}
        def rot(name, n):
            i = cnt[name] % n; cnt[name] += 1; return i

        R_cmf = Reg("cmf"); R_misc = Reg("misc")
        T.dma("sp", cm_f[:].rearrange("p a b -> p (a b)"), cmask[:, :], R_cmf, w=[R_cmf])
        for (dst, src) in ((invf, invf_d), (gvec, gvec_d), (gfin, gfin_d), (bgate, bgate_d)):
            T.dma("sp", dst[:], src[:, :], R_misc, w=[R_misc])
            T.wait_all("sp", [R_misc])
        R_es = Reg("es")
        T.dma("sp", es[:], sinkr_d[:, :], R_es, w=[R_es])
        R_pos = Reg("pos")
        T.dma("sp", posi[:], posc[:, :], R_pos, w=[R_pos])
        T.op("pool", f_memset(onesf[:], 1.0), w=[R_const])
        T.op("pool", f_memset(identf[:], 0.0), w=[R_const])
        T.op("pool", lambda h: h.affine_select(out=identf[:], in_=onesf[:], pattern=[[-1, 128]], compare_op=ALU.is_equal,
                                                fill=0.0, base=0, channel_multiplier=1), r=[R_const], w=[R_const])
        T.op("dve", f_cp(ident[:], identf[:]), r=[R_const], w=[R_const])
        T.op("dve", f_memset(ones_b[:], 1.0), w=[R_const])
        T.op("pool", f_memset(vD[:].rearrange("p a b -> p (a b)"), 1.0), w=R_vD)
        T.op("dve", f_memset(dn[0][:], 0.0), w=[R_dn[0]])
        T.op("dve", f_memset(qd0[:], 0.0), w=[R_qd0])
        for k in range(NITER):
            T.op("dve", f_memset(pw2[:, k:k + 1], 2.0 ** (-(k + 1))), w=[R_const])
        T.op("dve", f_cp(cm_b[:], cm_f[:, 0:3, :]), r=[R_cmf], w=[R_const])
        T.op("act", f_act(es[:], es[:], AF.Exp), r=[R_es], w=[R_es])
        T.op("dve", f_cp(posf[:], posi[:]), r=[R_pos], w=[R_pos])
        tri_cur = cm_b[:, 0, :]; tri_prev = cm_b[:, 1, :]; prev0 = cm_b[:, 2, :]
        dbias = cm_f[:, 3, :]; p63bias = cm_f[:, 4, :]

        def emit():
            def norm_T(x_ap, x_regs, dstT, dst_regs, pbank):
                si = rot("stat", 4); s = stat[:, si * 4:(si + 1) * 4]; rs = R_stat[si]
                hi = rot("hn", 1); h_ = hn[hi]; rh = R_hn[hi]
                T.op("act", f_act(h_[:], x_ap, AF.Square, accum=s[:, 0:1]), r=x_regs, w=[rh, rs])
                T.op("act", f_act(s[:, 1:2], s[:, 0:1], AF.Ln, scale=1.0 / 1024.0, bias=1e-6), r=[rs], w=[rs])
                T.op("act", f_act(s[:, 2:3], s[:, 1:2], AF.Exp, scale=-0.5), r=[rs], w=[rs])
                T.op("dve", f_ts(h_[:], x_ap, s[:, 2:3], None, ALU.mult), r=x_regs + [rs], w=[rh])
                pv = pbf(pbank)
                T.op("pe", f_seq([f_tr(pv[:, kc * 128:(kc + 1) * 128], h_[:, kc * 128:(kc + 1) * 128], ident[:]) for kc in range(8)]),
                     r=[rh, R_const], w=[R_PB[pbank]])
                T.op("act", f_act(dstT, pv[:, :].rearrange("p (k t) -> p k t", k=8), AF.Copy), r=[R_PB[pbank]], w=dst_regs)
                return s, rs

            def rope_tables(blk0, nb):
                n = nb * 32
                a0 = rtmp[0][:, 0:n]; a1 = rtmp[1][:, 0:n]; a2 = rtmp[2][:, 0:n]; ai = rtmpi[:, 0:n]
                v3 = lambda a: a.rearrange("p (b f) -> p b f", f=32)
                T.op("dve", f_tt(v3(a0), posf[:, blk0:blk0 + nb].unsqueeze(2).to_broadcast([128, nb, 32]),
                                 invf[:].unsqueeze(1).to_broadcast([128, nb, 32]), ALU.mult), r=[R_pos, R_misc], w=[R_rt])
                T.op("dve", f_ts(ai, a0, 1.0 / (2 * math.pi), None, ALU.mult), r=[R_rt], w=[R_rt])
                T.op("dve", f_cp(a1, ai), r=[R_rt], w=[R_rt])
                T.op("dve", f_stt(a2, a1, -6.28125, a0, ALU.mult, ALU.add), r=[R_rt], w=[R_rt])
                T.op("dve", f_stt(a2, a1, -0.0019353072, a2, ALU.mult, ALU.add), r=[R_rt], w=[R_rt])
                T.op("dve", f_ts(a0, a2, -3.1415925, 3.1415925, ALU.max, ALU.min), r=[R_rt], w=[R_rt])
                T.op("act", f_act(sinT[:, 0:nb, :].rearrange("p b f -> p (b f)"), a0, AF.Sin), r=[R_rt], w=[R_cs])
                T.op("dve", f_ts(a1, a2, math.pi / 2, None, ALU.add), r=[R_rt], w=[R_rt])
                T.op("dve", f_ts(a0, a1, math.pi, -2 * math.pi, ALU.is_gt, ALU.mult), r=[R_rt, R_cs], w=[R_rt])
                T.op("dve", f_tt(a0, a0, a1, ALU.add), r=[R_rt], w=[R_rt])
                T.op("dve", f_ts(a0, a0, -3.1415925, 3.1415925, ALU.max, ALU.min), r=[R_rt], w=[R_rt])
                T.op("act", f_act(cosT[:, 0:nb, :].rearrange("p b f -> p (b f)"), a0, AF.Sin), r=[R_rt], w=[R_cs])

            def rope_apply(src, src_regs, dst, dst_regs, H, cb):
                n = H * 64
                t_ = rope_t[:, 0:n]; u_ = rope_u[:, 0:n]
                cosb = cosT[:, cb:cb + 1, :]; sinb = sinT[:, cb:cb + 1, :]
                T.op("dve", f_tt(t_.rearrange("p (a f) -> p a f", f=32), src.rearrange("p (a f) -> p a f", f=32),
                                 cosb.to_broadcast([128, 2 * H, 32]), ALU.mult), r=src_regs + [R_cs], w=[R_rtu[0]])
                s4 = src.rearrange("p (h e f) -> p h e f", e=2, f=32)
                u4 = u_.rearrange("p (h e f) -> p h e f", e=2, f=32)
                t4 = t_.rearrange("p (h e f) -> p h e f", e=2, f=32)
                d4 = dst.rearrange("p (h e f) -> p h e f", e=2, f=32)
                sb_ = sinb.to_broadcast([128, H, 32])
                T.op("dve", f_tt(u4[:, :, 0, :], s4[:, :, 1, :], sb_, ALU.mult), r=src_regs + [R_cs], w=[R_rtu[1]])
                T.op("dve", f_tt(u4[:, :, 1, :], s4[:, :, 0, :], sb_, ALU.mult), r=src_regs + [R_cs], w=[R_rtu[1]])
                T.op("dve", f_tt(d4[:, :, 0, :], t4[:, :, 0, :], u4[:, :, 0, :], ALU.subtract), r=R_rtu, w=dst_regs)
                T.op("dve", f_tt(d4[:, :, 1, :], t4[:, :, 1, :], u4[:, :, 1, :], ALU.add), r=R_rtu, w=dst_regs)

            R_wbf = [Reg(f"wbf{i}") for i in range(NCHUNK)]
            CH_N = [4096, 4096, 8 * 260] + [8 * 384] * 8 + [2048] * 8 + [4096] * 18
            CH_P = [128] * 11 + [64] * 8 + [128] * 18
            sched = [0, 1, 2] + [v for oc in range(8) for v in (3 + oc, 11 + oc)] + list(range(19, 37))
            ring_state = {"issued": 0, "got": 0}
            total_gets = 8 * NCHUNK
            def ring_issue():
                k = ring_state["issued"]
                if k >= total_gets: return
                c = sched[k % NCHUNK]; b = k % NRING
                T.dma("sp", ring[b][0:CH_P[c], 0:CH_N[c]], wbf_d[c, 0:CH_P[c], 0:CH_N[c]], R_ring[b], r=[R_wbf[c]], w=[R_ring[b]])
                ring_state["issued"] += 1
            def ring_get(expect):
                k = ring_state["got"]
                assert sched[k % NCHUNK] == expect, (k, expect)
                while ring_state["issued"] < min(k + NRING, total_gets):
                    ring_issue()
                ring_state["got"] += 1
                b = k % NRING
                return ring[b], R_ring[b]

            def convert(src_dram_ap, npart, nelem, ncols, goff, dst_ap, dst_regs, half):
                stg = BIG[0:npart, half * 4096: half * 4096 + nelem]
                sregs = ISC[half * 8:(half + 1) * 8]
                T.dma("pool", stg, src_dram_ap, sregs[0], w=sregs)
                if goff is None:
                    T.op("act", f_act(dst_ap, stg, AF.Copy), r=sregs, w=dst_regs)
                else:
                    for kc in range(8):
                        T.op("dve", f_ts(dst_ap[:, kc * ncols:(kc + 1) * ncols], stg[:, kc * ncols:(kc + 1) * ncols],
                                          gvec[:, goff + kc:goff + kc + 1], None, ALU.mult), r=sregs + [R_misc], w=dst_regs)
            _ckpt(1)
            convert(wk_d[:, :], 128, 8 * 448, 448, 0, wk_sb[:].rearrange("p k c -> p (k c)"), [R_wk], 0)
            wmem_b = MSK[:, 4096:8192]
            convert(wmem_d[:, :], 128, 4096, 512, 16, wmem_b, MSKR[8:16], 1)

            for mc in range(2):
                T.dma("sp", xo[:], memc[mc * 128:(mc + 1) * 128, :], R_rl[0], w=R_rl)
                norm_T(xo[:], R_rl, hTo[:], [R_hTo], 0)
                T.op("pe", f_seq([f_mm(PB[1][:, :], hTo[:, kc, :], wmem_b[:, kc * 512:(kc + 1) * 512], kc == 0, kc == 7) for kc in range(8)]),
                     r=[R_hTo] + MSKR[8:16], w=[R_PB[1]])
                ti = rot("tok", 2)
                T.op("act", f_act(tok_b[ti][:, 0:256], PB[1][:, 0:256], AF.Copy), r=[R_PB[1]], w=[R_tok[ti]])
                T.op("act", f_act(mv[:, mc, :], PB[1][:, 256:512], AF.Copy), r=[R_PB[1]], w=[R_mv])
                pv = pbf(2)
                T.op("pe", f_seq([f_tr(pv[:, jj * 128:(jj + 1) * 128], tok_b[ti][:, jj * 128:(jj + 1) * 128], ident[:]) for jj in range(2)]),
                     r=[R_tok[ti], R_const], w=[R_PB[2]])
                T.op("act", f_act(mkT[:, :, mc * 128:(mc + 1) * 128], pv[:, 0:256].rearrange("p (j m) -> p j m", j=2), AF.Copy),
                     r=[R_PB[2]], w=[R_mkT])

            _ckpt(2)
            for c in range(NCHUNK):
                half = c % 2
                goff = 0 if c < 11 else (8 if 21 <= c < 29 else None)
                ncols = {0: 512, 1: 512, 2: 260}.get(c, 384 if c < 11 else 512)
                npart = CH_P[c]; nelem = CH_N[c]
                dstb = MSK[0:npart, half * 4096: half * 4096 + nelem]
                dregs = MSKR[half * 8:(half + 1) * 8]
                convert(wch_d[c, 0:npart, 0:nelem], npart, nelem, ncols, goff, dstb, dregs, half)
                T.dma("pool", wbf_d[c, 0:npart, 0:nelem], dstb, dregs[0], r=dregs, w=[R_wbf[c]])

            def kside(pos, x_ap, x_regs, hT_ap, hT_regs, cb, stage="AB"):
                kidx = 0 if pos == 63 else pos + 1
                rg = (pos + 1) % 16
                if "A" in stage:
                    norm_T(x_ap, x_regs, hT_ap, hT_regs, 0)
                if "B" in stage or "M" in stage:
                    T.op("pe", f_seq([f_mm(PB[1][:, 0:448], hT_ap[:, kc, :], wk_sb[:, kc, :], kc == 0, kc == 7) for kc in range(8)]),
                         r=hT_regs + [R_wk], w=[R_PB[1]])
                if "B" not in stage and "R" not in stage:
                    return
                ti = rot("tok", 2)
                rope_apply(PB[1][:, 0:256], [R_PB[1]], tok_b[ti][:, 0:256], [R_tok[ti]], 4, cb)
                _ckpt(3.3)
                T.op("dve", f_cp(vS[:, rg, :], PB[1][:, 256:384]), r=[R_PB[1]], w=[R_vS[rg]])
                T.op("dve", f_cp(vD[:, kidx, 0:64], PB[1][:, 384:448]), r=[R_PB[1]], w=[R_vD[kidx]])
                _ckpt(3.4)
                pv = pbf(2)
                T.op("pe", f_seq([f_tr(pv[:, jj * 128:(jj + 1) * 128], tok_b[ti][:, jj * 128:(jj + 1) * 128], ident[:]) for jj in range(2)]),
                     r=[R_tok[ti], R_const], w=[R_PB[2]])
                _ckpt(3.5)
                T.op("act", f_act(kST[:, rg, :], pv[:, 0:128], AF.Copy), r=[R_PB[2]], w=[R_kST[rg]])
                T.op("act", f_act(kDI[:, kidx * 128:(kidx + 1) * 128], pv[:, 128:256], AF.Copy), r=[R_PB[2]], w=[R_kDI[kidx]])

            _ckpt(3)
            rope_tables(63, 1)
            T.dma("sp", xo[:], xc[63 * 128:64 * 128, :], R_rl[0], w=R_rl)
            kside(63, xo[:], R_rl, hTo[:], [R_hTo], 0)

            def finish_attn(bO, bD, j, h0, es_ap):
                di = rot("dn", 1)
                d_ = dn[di][0:64, :]
                if es_ap is not None:
                    T.op("dve", f_tt(d_.rearrange("p (h t) -> p h t", h=4), PB[bD][0:64, :].rearrange("p (h t) -> p h t", h=4), es_ap, ALU.add),
                         r=[R_PB[bD], R_es], w=[R_dn[di]])
                    T.op("act", f_act(d_, d_, AF.Ln), r=[R_dn[di]], w=[R_dn[di]])
                else:
                    T.op("act", f_act(d_, PB[bD][0:64, :], AF.Ln), r=[R_PB[bD]], w=[R_dn[di]])
                T.op("act", f_act(d_, d_, AF.Exp, scale=-1.0), r=[R_dn[di]], w=[R_dn[di]])
                T.op("dve", f_tt(oTg[:, h0:h0 + 4, j * 128:(j + 1) * 128], PB[bO][0:64, :].rearrange("p (h t) -> p h t", h=4),
                                 d_.rearrange("p (h t) -> p h t", h=4), ALU.mult), r=[R_PB[bO], R_dn[di]], w=[R_oTg[j]])

            for g in range(8):
                T.mark(f"g{g} kside")
                npos = 8 if g < 7 else 7
                rope_tables(8 * g, 8)
                def kblock(pl, stage):
                    pos = 8 * g + pl
                    if pl % 2 == 0:
                        j = pl // 2
                        if "A" in stage:
                            T.dma("sp", xg[:, j, :], xc[pos * 128:(pos + 1) * 128, :], R_xg[j], w=[R_xg[j]])
                        kside(pos, xg[:, j, :], [R_xg[j]], hTg[:, :, j * 128:(j + 1) * 128], [R_hTg[j]], pl, stage)
                    else:
                        if "A" in stage:
                            T.dma("sp", xo[:], xc[pos * 128:(pos + 1) * 128, :], R_rl[0], w=R_rl)
                        kside(pos, xo[:], R_rl, hTo[:], [R_hTo], pl, stage)
                kblock(0, "A")
                for pl in range(npos):
                    kblock(pl, "M")
                    if pl + 1 < npos:
                        kblock(pl + 1, "A")
                    kblock(pl, "R")

                _ckpt(10 * (g + 1) + 1)
                T.mark(f"g{g} qproj")
                units = [(c, j) for c in range(3) for j in range(4)]
                qW = {}
                def q_mm(u):
                    c, j = units[u]
                    if j == 0:
                        W, RW = ring_get(c)
                        qW[c] = (W, RW)
                    W, RW = qW[c]
                    ncols = (512, 512, 260)[c]
                    Wv = W[:, 0:8 * ncols].rearrange("p (k c) -> p k c", k=8)
                    pb = 3 + (u % 2)
                    T.op("pe", f_seq([f_mm(PB[pb][:, 0:ncols], hTg[:, kc, j * 128:(j + 1) * 128], Wv[:, kc, :], kc == 0, kc == 7) for kc in range(8)]),
                         r=[R_hTg[j], RW], w=[R_PB[pb]])
                def q_post(u):
                    c, j = units[u]
                    pb = 3 + (u % 2); pt = 5 + (u % 2)
                    ti = rot("tok", 2)
                    if c < 2:
                        rope_apply(PB[pb][:, 0:512], [R_PB[pb]], tok_b[ti][:, 0:512], [R_tok[ti]], 8, 2 * j)
                        pv = pbf(pt)
                        T.op("pe", f_seq([f_tr(pv[:, q * 128:(q + 1) * 128], tok_b[ti][:, q * 128:(q + 1) * 128], ident[:]) for q in range(4)]),
                             r=[R_tok[ti], R_const], w=[R_PB[pt]])
                        dst = qsT if c == 0 else qdiT
                        T.op("act", f_act(dst[:, j, :], pv[:, 0:512], AF.Copy), r=[R_PB[pt]], w=[(R_qs if c == 0 else R_qdi)[j]])
                    else:
                        T.op("act", f_act(tok_b[ti][:, 0:256], PB[pb][:, 0:256], AF.Copy), r=[R_PB[pb]], w=[R_tok[ti]])
                        T.op("act", f_act(wiT[:, j, :], PB[pb][:, 256:260], AF.Copy, scale=1.0 / 16.0), r=[R_PB[pb]], w=[R_wi[j]])
                        pv = pbf(pt)
                        T.op("pe", f_seq([f_tr(pv[:, q * 128:(q + 1) * 128], tok_b[ti][:, q * 128:(q + 1) * 128], ident[:]) for q in range(2)]),
                             r=[R_tok[ti], R_const], w=[R_PB[pt]])
                        T.op("act", f_act(qmT[:, j, :], pv[:, 0:256], AF.Copy), r=[R_PB[pt]], w=[R_qm[j]])
                q_mm(0)
                for u in range(len(units)):
                    if u + 1 < len(units):
                        q_mm(u + 1)
                    q_post(u)

                _ckpt(10 * (g + 1) + 2)
                T.mark(f"g{g} attn")
                def slot_vars(j):
                    i = 4 * g + j
                    nkb = 2 * i + 2
                    return i, slice(j * 128, (j + 1) * 128), nkb, (nkb + 3) // 4, nkb * 128
                def fin_gen(bO, bD, j, h0, es_ap):
                    di = rot("dn", 1)
                    d_ = dn[di][0:64, :]
                    if es_ap is not None:
                        T.op("dve", f_tt(d_.rearrange("p (h t) -> p h t", h=4), PB[bD][0:64, :].rearrange("p (h t) -> p h t", h=4), es_ap, ALU.add),
                             r=[R_PB[bD], R_es], w=[R_dn[di]])
                        yield
                        T.op("act", f_act(d_, d_, AF.Ln), r=[R_dn[di]], w=[R_dn[di]])
                    else:
                        T.op("act", f_act(d_, PB[bD][0:64, :], AF.Ln), r=[R_PB[bD]], w=[R_dn[di]])
                    T.op("act", f_act(d_, d_, AF.Exp, scale=-1.0), r=[R_dn[di]], w=[R_dn[di]])
                    yield
                    T.op("dve", f_tt(oTg[:, h0:h0 + 4, j * 128:(j + 1) * 128], PB[bO][0:64, :].rearrange("p (h t) -> p h t", h=4),
                                     d_.rearrange("p (h t) -> p h t", h=4), ALU.mult), r=[R_PB[bO], R_dn[di]], w=[R_oTg[j]])
                    yield
                def swa_mem_gen(j):
                    i, tok, nkb, nch, n = slot_vars(j)
                    rp = (2 * i) % 16; rc = (2 * i + 1) % 16
                    for kv in range(2):
                        pr = slice(kv * 64, (kv + 1) * 64)
                        bS = (2, 3) if kv == 0 else (4, 5)
                        bO, bD = (0, 1) if kv == 0 else (6, 7)
                        Es_ = []
                        for which, rr_ in enumerate((rp, rc)):
                            b = bS[which]
                            T.op("pe", f_mm(PB[b][:, :], kST[pr, rr_, :], qsT[pr, j, :]), r=[R_kST[rr_], R_qs[j]], w=[R_PB[b]])
                            ei = rot("E", 5)
                            T.op("act", f_act(Et[ei][:], PB[b][:, :], AF.Exp, scale=0.125), r=[R_PB[b]], w=[R_E[ei]])
                            Es_.append((ei, rr_, which))
                        yield
                        Ps = []
                        for ei, rr_, which in Es_:
                            pi_ = rot("P", 2)
                            msk = (prev0 if i == 0 else tri_prev) if which == 0 else tri_cur
                            T.op("dve", f_tt(Pt[pi_][:].rearrange("p (h t) -> p h t", h=4), Et[ei][:].rearrange("p (h t) -> p h t", h=4),
                                             msk.unsqueeze(1).to_broadcast([128, 4, 128]), ALU.mult), r=[R_E[ei], R_const], w=[R_P[pi_]])
                            Ps.append((pi_, rr_))
                        T.op("pe", f_seq([f_mm(PB[bO][0:64, :], vS[:, rr_, pr], Pt[pi_][:], w_ == 0, w_ == 1) for w_, (pi_, rr_) in enumerate(Ps)]
                                         + [f_mm(PB[bD][0:64, :], ones_b[:], Pt[pi_][:], w_ == 0, w_ == 1) for w_, (pi_, rr_) in enumerate(Ps)]),
                             r=[R_P[p_] for p_, _ in Ps] + [R_vS[r_] for _, r_ in Ps] + [R_const], w=[R_PB[bO], R_PB[bD]])
                        yield
                        yield from fin_gen(bO, bD, j, 4 * kv, es[:, 4 * kv:4 * kv + 4].unsqueeze(2).to_broadcast([64, 4, 128]))
                    Es = [rot("E", 5), rot("E", 5)]
                    fns = []
                    for e in range(2):
                        pr = slice(e * 64, (e + 1) * 64)
                        for mc in range(2):
                            for jj in range(2):
                                fns.append(f_mm(PB[2 + 2 * mc + e][:, jj * 128:(jj + 1) * 128], mkT[pr, jj, mc * 128:(mc + 1) * 128],
                                                qmT[pr, j, jj * 128:(jj + 1) * 128]))
                    T.op("pe", f_seq(fns), r=[R_mkT, R_qm[j]], w=[R_PB[2], R_PB[3], R_PB[4], R_PB[5]])
                    for mc in range(2):
                        E4 = Et[Es[mc]][:].rearrange("p (jj e t) -> p jj e t", jj=2, e=2)
                        for e in range(2):
                            b = 2 + 2 * mc + e
                            T.op("act", f_act(E4[:, :, e, :], PB[b][:, 0:256].rearrange("p (jj t) -> p jj t", jj=2), AF.Exp, scale=0.125),
                                 r=[R_PB[b]], w=[R_E[Es[mc]]])
                    yield
                    fns = []
                    for hh in range(4):
                        for mc in range(2):
                            fns.append(f_mm(PB[0][0:64, hh * 128:(hh + 1) * 128], mv[:, mc, hh * 64:(hh + 1) * 64], Et[Es[mc]][:, hh * 128:(hh + 1) * 128], mc == 0, mc == 1))
                    for mc in range(2):
                        fns.append(f_mm(PB[1][0:64, :], ones_b[:], Et[Es[mc]][:], mc == 0, mc == 1))
                    T.op("pe", f_seq(fns), r=[R_E[e_] for e_ in Es] + [R_mv, R_const], w=[R_PB[0], R_PB[1]])
                    yield
                    yield from fin_gen(0, 1, j, 12, None)
                def swa_mem(j):
                    for _ in swa_mem_gen(j):
                        pass
                def dsa_index_gen(j, inter):
                    i, tok, nkb, nch, n = slot_vars(j)
                    hu = 0
                    for c in range(nch):
                        kb0 = 4 * c; nb = min(4, nkb - kb0); w_ = nb * 128
                        cols = slice(kb0 * 128, kb0 * 128 + w_)
                        for hh in range(4):
                            bk = (5 + hu % 3) if inter else (4 * (c % 2) + hh)
                            hu += 1
                            T.op("pe", f_mm(PB[bk][:, 0:w_], qdiT[64:128, j, hh * 128:(hh + 1) * 128], kDI[64:128, cols]),
                                 r=[R_qdi[j]] + R_kDI[kb0:kb0 + nb], w=[R_PB[bk]])
                            ri = rot("rl", 2)
                            T.op("act", f_act(rl[ri][:, 0:w_], PB[bk][:, 0:w_], AF.Relu), r=[R_PB[bk]], w=[R_rl[ri]])
                            if hh == 0:
                                T.op("dve", f_ts(BIG[:, cols], rl[ri][:, 0:w_], wiT[:, j, 0:1], None, ALU.mult), r=[R_rl[ri], R_wi[j]], w=[ISC[c]])
                            else:
                                T.op("dve", f_stt(BIG[:, cols], rl[ri][:, 0:w_], wiT[:, j, hh:hh + 1], BIG[:, cols], ALU.mult, ALU.add),
                                     r=[R_rl[ri], R_wi[j], ISC[c]], w=[ISC[c]])
                            yield
                def dsa_index(j):
                    for _ in dsa_index_gen(j, False):
                        pass
                def dsa_bisect(j, pipe=None):
                    i, tok, nkb, nch, n = slot_vars(j)
                    _ckpt(12.3)
                    IR = ISC[0:nch]; MR = MSKR[0:nch]
                    cd = nch if nch < 2 else max(1, int(round(0.47 * nch)))
                    nd = min(n, cd * 512); na = n - nd
                    IRd, MRd, IRa, MRa = ISC[0:cd], MSKR[0:cd], ISC[cd:nch], MSKR[cd:nch]
                    amax = bis[:, 0:1]; wtot = bis[:, 1:2]; lo = bis[:, 2:3]; mid = bis[:, 3:4]; cn = bis[:, 4:5]; dl = bis[:, 5:6]
                    sa = bis[:, 6:7]; tt_ = bis[:, 7:8]
                    wks = bis[:, 8:8 + NITER]
                    T.op("dve", lambda h, a=amax, b=BIG[:, 0:n]: h.tensor_reduce(out=a, in_=b, axis=AX.X, op=ALU.max, apply_absolute_value=True),
                         r=IR, w=[R_lo])
                    T.op("pool", f_tt(BIG[:, 0:128], BIG[:, 0:128], p63bias, ALU.add), r=[ISC[0], R_cmf, R_lo], w=[ISC[0]])
                    dcol = slice((nkb - 1) * 128, nkb * 128)
                    T.op("pool", f_tt(BIG[:, dcol], BIG[:, dcol], dbias, ALU.add), r=[ISC[nch - 1], R_cmf, R_lo], w=[ISC[nch - 1]])
                    T.op("dve", f_ts(wtot, amax, 2.0002, 2e-6, ALU.mult, ALU.add), r=[R_lo], w=[R_lo])
                    T.op("dve", f_ts(lo, amax, -1.0001, -1e-6, ALU.mult, ALU.add), r=[R_lo], w=[R_lo])
                    T.op("dve", f_ts(wks, pw2[:], wtot, None, ALU.mult), r=[R_lo, R_const], w=[R_lo])
                    thrc = TOPK - na / 2.0
                    for k in range(NITER):
                        T.op("dve", f_tt(mid, lo, wks[:, k:k + 1], ALU.add), r=[R_lo], w=[R_mid])
                        if na > 0:
                            T.op("act", f_act(MSK[:, nd:n], BIG[:, nd:n], AF.Sign, bias=mid, scale=-1.0, accum=sa), r=IRa + [R_mid], w=MRa + [R_ca])
                        T.op("dve", f_ts(MSK[:, 0:nd], BIG[:, 0:nd], mid, 0.0, ALU.is_ge, ALU.add, accum=cn), r=IRd + [R_mid], w=MRd + [R_cd])
                        if pipe is not None:
                            next(pipe, None)
                        if na > 0:
                            T.op("dve", f_stt(tt_, sa, -0.5, cn, ALU.mult, ALU.add), r=[R_ca, R_cd], w=[R_t])
                            T.op("dve", f_stt(dl, tt_, thrc, wks[:, k:k + 1], ALU.is_ge, ALU.mult), r=[R_t, R_lo], w=[R_t])
                        else:
                            T.op("dve", f_stt(dl, cn, TOPK, wks[:, k:k + 1], ALU.is_ge, ALU.mult), r=[R_cd, R_lo], w=[R_t])
                        T.op("dve", f_tt(lo, lo, dl, ALU.add), r=[R_t], w=[R_lo])
                    T.op("dve", f_ts(MSK[:, 0:n], BIG[:, 0:n], lo, None, ALU.is_ge), r=IR + [R_lo], w=MR)
                def dsa_attn(j, pipe=None):
                    i, tok, nkb, nch, n = slot_vars(j)
                    inter = pipe is not None
                    _ckpt(12.4)
                    LA = 2 if inter else 3
                    st_ = {}
                    T.op("dve", f_cp(qd0[0:64, :], qdiT[0:64, j, :]), r=[R_qdi[j]], w=[R_qd0])
                    def prep(c):
                        kb_ = 4 * c
                        nb = min(4, nkb - kb_)
                        mi = c % 2
                        pv = pbf(1)
                        T.op("pe", f_seq([f_tr(pv[:, q2 * 128:(q2 + 1) * 128], MSK[:, (kb_ + q2) * 128:(kb_ + q2 + 1) * 128], ident[:]) for q2 in range(nb)]),
                             r=[MSKR[c], R_const], w=[R_PB[1]])
                        T.op("act", f_act(mT[mi][:, 0:nb * 128], pv[:, 0:nb * 128], AF.Identity, scale=30000.0, bias=-30000.0), r=[R_PB[1]], w=[R_mT[mi]])
                    prep(0)
                    def front(kb):
                        c, q = kb // 4, kb % 4
                        if q == 0 and c + 1 < nch:
                            prep(c + 1)
                        mi = c % 2
                        b = (2 + kb % 3) if inter else (2 + kb % 6)
                        T.op("pe", f_seq([f_mm(PB[b][:, :], kDI[:, kb * 128:(kb + 1) * 128], qd0[:, :], True, False)]
                                         + [f_mm(PB[b][:, hh * 128:(hh + 1) * 128], ident[:], mT[mi][:, q * 128:(q + 1) * 128], False, hh == 3) for hh in range(4)]),
                             r=[R_kDI[kb], R_qd0, R_mT[mi], R_const], w=[R_PB[b]])
                        ei = rot("E", 5)
                        T.op("act", f_act(Et[ei][:], PB[b][:, :], AF.Exp, scale=0.125), r=[R_PB[b]], w=[R_E[ei]])
                        st_[kb] = ei
                    def back(kb):
                        ei = st_.pop(kb)
                        T.op("pe", f_mm(PB[0][:, :], vD[:, kb, :], Et[ei][:], kb == 0, kb == nkb - 1),
                             r=[R_E[ei], R_vD[kb]], w=[R_PB[0]])
                    for t in range(nkb + LA):
                        if t < nkb: front(t)
                        if t - LA >= 0: back(t - LA)
                        if inter:
                            next(pipe, None)
                    if inter:
                        for _ in pipe:
                            pass
                    T.op("act", f_act(dn[0][64:128, :], PB[0][64:128, :], AF.Copy), r=[R_PB[0]], w=[R_dn[0]])
                    T.op("pe", f_mm(PB[1][0:64, :], identf[:, 64:128], dn[0][:, :]), r=[R_dn[0], R_const], w=[R_PB[1]])
                    finish_attn(0, 1, j, 8, None)

                swa_mem(0)
                T.mark(f"g{g} s0 idx")
                dsa_index(0)
                for j in range(4):
                    T.mark(f"g{g} s{j} bis")
                    pipe = swa_mem_gen(j + 1) if j < 3 else None
                    dsa_bisect(j, pipe)
                    if pipe is not None:
                        for _ in pipe:
                            pass
                    T.mark(f"g{g} s{j} dattn")
                    dsa_attn(j, dsa_index_gen(j + 1, True) if j < 3 else None)

                _ckpt(10 * (g + 1) + 3)
                T.mark(f"g{g} pass2")
                RQ = R_qs + R_qdi
                for oc in range(8):
                    G, RG = ring_get(3 + oc)
                    Gv = G[:, 0:8 * 384].rearrange("p (k c) -> p k c", k=8)
                    for jb in range(3):
                        T.op("pe", f_seq([f_mm(PB[jb][:, :], Gv[:, kc, jb * 128:(jb + 1) * 128], hTg[:, kc, :], kc == 0, kc == 7) for kc in range(8)]),
                             r=R_hTg + [RG], w=[R_PB[jb]])
                        T.op("act", f_act(gsb[jb][:], PB[jb][:, :], AF.Sigmoid, bias=bgate[:, oc * 3 + jb:oc * 3 + jb + 1], scale=1.0),
                             r=[R_PB[jb], R_misc], w=[R_gsb[jb]])
                    Pw, RP = ring_get(11 + oc)
                    Pv = Pw[0:64, 0:2048].rearrange("p (h c) -> p h c", h=16)
                    for jb, (h0, nh) in enumerate(((0, 8), (8, 4), (12, 4))):
                        T.op("pe", f_seq([f_mm(PB[3 + jb][:, :], Pv[:, h0 + hh, :], oTg[:, h0 + hh, :], hh == 0, hh == nh - 1) for hh in range(nh)]),
                             r=R_oTg + [RP], w=[R_PB[3 + jb]])
                    T.op("dve", f_tt(mg[0][:], PB[3][:, :], gsb[0][:], ALU.mult), r=[R_PB[3], R_gsb[0]], w=[R_mg[0]])
                    T.op("dve", f_tt(mg[1][:], PB[4][:, :], gsb[1][:], ALU.mult), r=[R_PB[4], R_gsb[1]], w=[R_mg[1]])
                    T.op("dve", f_tt(mg[0][:], mg[0][:], mg[1][:], ALU.add), r=[R_mg[0], R_mg[1]], w=[R_mg[0]])
                    T.op("dve", f_tt(mg[1][:], PB[5][:, :], gsb[2][:], ALU.mult), r=[R_PB[5], R_gsb[2]], w=[R_mg[1]])
                    T.op("dve", f_tt(mergedT[:, oc, :], mg[0][:], mg[1][:], ALU.add), r=[R_mg[0], R_mg[1]], w=RQ)

                _ckpt(10 * (g + 1) + 4)
                T.mark(f"g{g} wout")
                for cc in range(2):
                    W, RW = ring_get(19 + cc)
                    Wv = W[:, :].rearrange("p (k c) -> p k c", k=8)
                    for j in range(4):
                        pb = 6 + (j % 2)
                        T.op("pe", f_seq([f_mm(PB[pb][:, :], mergedT[:, kc, j * 128:(j + 1) * 128], Wv[:, kc, :], kc == 0, kc == 7) for kc in range(8)]),
                             r=RQ + [RW], w=[R_PB[pb]])
                        T.op("dve", f_tt(xg[:, j, cc * 512:(cc + 1) * 512], PB[pb][:, :], xg[:, j, cc * 512:(cc + 1) * 512], ALU.add),
                             r=[R_PB[pb], R_xg[j]], w=[R_xg[j]])

                _ckpt(10 * (g + 1) + 5)
                T.mark(f"g{g} mlp")
                for j in range(4):
                    norm_T(xg[:, j, :], [R_xg[j]], hTg[:, :, j * 128:(j + 1) * 128], [R_hTg[j]], j % 2)
                for fb in range(8):
                    W, RW = ring_get(21 + fb)
                    Wv = W[:, :].rearrange("p (k c) -> p k c", k=8)
                    for fs in range(4):
                        fc = fb * 4 + fs
                        pb = 2 + (fc % 4)
                        T.op("pe", f_seq([f_mm(PB[pb][:, :], Wv[:, kc, fs * 128:(fs + 1) * 128], hTg[:, kc, :], kc == 0, kc == 7) for kc in range(8)]),
                             r=R_hTg + [RW], w=[R_PB[pb]])
                        ri = rot("rl", 2)
                        T.op("act", f_act(rl[ri][:], PB[pb][:, :], AF.Relu), r=[R_PB[pb]], w=[R_rl[ri]])
                        T.op("dve", f_tt(hidT[:, fc, :], rl[ri][:], rl[ri][:], ALU.mult), r=[R_rl[ri]], w=[ISC[fc // 2]])
                for cc in range(2):
                    for ks in range(4):
                        W, RW = ring_get(29 + cc * 4 + ks)
                        Wv = W[:, :].rearrange("p (k c) -> p k c", k=8)
                        for j in range(4):
                            pb = 4 + j
                            T.op("pe", f_seq([f_mm(PB[pb][:, :], hidT[:, ks * 8 + fl, j * 128:(j + 1) * 128], Wv[:, fl, :],
                                                   ks == 0 and fl == 0, ks == 3 and fl == 7) for fl in range(8)]),
                                 r=ISC[ks * 4:(ks + 1) * 4] + [RW], w=[R_PB[pb]])
                    for j in range(4):
                        pb = 4 + j
                        T.op("dve", f_tt(xg[:, j, cc * 512:(cc + 1) * 512], PB[pb][:, :], xg[:, j, cc * 512:(cc + 1) * 512], ALU.add),
                             r=[R_PB[pb], R_xg[j]], w=[R_xg[j]])
                _ckpt(10 * (g + 1) + 6)
                T.mark(f"g{g} final")
                for j in range(4):
                    i = 4 * g + j
                    si = rot("stat", 4); s = stat[:, si * 4:(si + 1) * 4]; rs = R_stat[si]
                    hi = rot("hn", 1)
                    T.op("act", f_act(hn[hi][:], xg[:, j, :], AF.Square, accum=s[:, 0:1]), r=[R_xg[j]], w=[R_hn[hi], rs])
                    T.op("act", f_act(s[:, 1:2], s[:, 0:1], AF.Ln, scale=1.0 / 1024.0, bias=1e-6), r=[rs], w=[rs])
                    T.op("act", f_act(s[:, 2:3], s[:, 1:2], AF.Exp, scale=-0.5), r=[rs], w=[rs])
                    T.op("dve", f_stt(xg[:, j, :], xg[:, j, :], s[:, 2:3], gfin[:], ALU.mult, ALU.mult), r=[R_xg[j], rs, R_misc], w=[R_xg[j]])
                    T.dma("sp", out_d[i * 128:(i + 1) * 128, :], xg[:, j, :], R_xg[j], r=[R_xg[j]])
        try:
            emit()
        except _Stop:
            T.dma("sp", out_d[0:128, :], xo[:], R_rl[0], r=R_rl)
            T.wait_all("sp", R_rl + R_ring)
            T.wait_all("pool", [MSKR[0], MSKR[8], ISC[0], ISC[8]])
        T.wait_all("sp", R_xg)
        with nc.Block() as block:
            T.replay(block)
    return nc


OFF = dict(q_s=0, k_s=512, v_s=640, q_d=768, k_d=1024, v_d=1088, q_i=1152, k_i=1408, w_i=1472, q_m=1476, gates=1732)


def _chunked(w, cols):
    sub = w[:, cols]
    n = sub.shape[1]
    return np.ascontiguousarray(sub.reshape(8, 128, n).transpose(1, 0, 2).reshape(128, 8 * n))


def _prep_weights(w_in, w_proj_swa, w_proj_dsa, w_proj_mem, w_out, w_mlp_in, w_mlp_out):
    wch = np.zeros((NCHUNK, 128, 4096), np.float32)
    r64 = np.arange(64)
    qs_cols = np.concatenate([OFF["q_s"] + h * 64 + r64 for h in (0, 4, 1, 5, 2, 6, 3, 7)])
    qdi_cols = np.concatenate([np.concatenate([OFF["q_d"] + h * 64 + r64, OFF["q_i"] + h * 64 + r64]) for h in range(4)])
    qm_cols = np.concatenate([OFF["q_m"] + np.arange(256), OFF["w_i"] + np.arange(4)])
    wch[0] = _chunked(w_in, qs_cols)
    wch[1] = _chunked(w_in, qdi_cols)
    wch[2, :, 0:8 * 260] = _chunked(w_in, qm_cols)
    for oc in range(8):
        gc = np.concatenate([OFF["gates"] + jb * 1024 + oc * 128 + np.arange(128) for jb in range(3)])
        wch[3 + oc, :, 0:8 * 384] = _chunked(w_in, gc)
        wp = np.concatenate([w_proj_swa.reshape(8, 64, 1024), w_proj_dsa.reshape(4, 64, 1024), w_proj_mem.reshape(4, 64, 1024)], 0)
        wch[11 + oc, 0:64, 0:2048] = wp[:, :, oc * 128:(oc + 1) * 128].transpose(1, 0, 2).reshape(64, 2048)
    for cc in range(2):
        wch[19 + cc] = _chunked(w_out, cc * 512 + np.arange(512))
    for fb in range(8):
        wch[21 + fb] = _chunked(w_mlp_in, fb * 512 + np.arange(512))
    for cc in range(2):
        for ks in range(4):
            blk = w_mlp_out[ks * 1024:(ks + 1) * 1024, cc * 512:(cc + 1) * 512]
            wch[29 + cc * 4 + ks] = blk.reshape(8, 128, 512).transpose(1, 0, 2).reshape(128, 4096)
    k_cols = np.concatenate([OFF["k_s"] + np.arange(128), OFF["k_d"] + r64, OFF["k_i"] + r64, OFF["v_s"] + np.arange(128), OFF["v_d"] + r64])
    wk = _chunked(w_in, k_cols)
    return wch, wk


_NC_CACHE = {}


def kernel(x, mem, positions, g_mix, w_in, b_gate, sinks, g_mem, w_mem_kv, w_proj_swa, w_proj_dsa,
           w_proj_mem, w_out, g_mlp, w_mlp_in, w_mlp_out, g_final):
    f = lambda a: np.asarray(a, dtype=np.float32)
    x = f(x); mem = f(mem); positions = np.asarray(positions, dtype=np.int32)
    wch, wk = _prep_weights(f(w_in)[0], f(w_proj_swa)[0], f(w_proj_dsa)[0], f(w_proj_mem)[0], f(w_out)[0], f(w_mlp_in)[0], f(w_mlp_out)[0])
    wmem = _chunked(f(w_mem_kv)[0], np.arange(512))
    tr = lambda v: np.ascontiguousarray(f(v).reshape(8, 128).T)
    gvec = np.concatenate([tr(g_mix[0]), tr(g_mlp[0]), tr(g_mem[0])], 1)
    gfin = np.ascontiguousarray(np.broadcast_to(f(g_final)[None, :], (128, 1024)))
    bg = f(b_gate)[0].reshape(3, 8, 128)
    bgate = np.ascontiguousarray(bg.transpose(2, 1, 0).reshape(128, 24))
    sinkr = np.ascontiguousarray(np.broadcast_to(f(sinks)[0][None, :], (64, 8)))
    half = 32
    invf = np.power(np.float32(10000.0), -np.arange(half, dtype=np.float32) / np.float32(half)).astype(np.float32)
    invf = np.ascontiguousarray(np.broadcast_to(invf[None, :], (128, 32)))
    s_ = np.arange(128)[:, None]; t_ = np.arange(128)[None, :]
    tri_cur = (t_ >= s_).astype(np.float32)
    tri_prev = (s_ > t_).astype(np.float32)
    dbias = np.where(t_ <= s_, 0.0, NEG).astype(np.float32)
    in_maps = []
    for c in range(8):
        b, par = c // 2, c % 2
        order = np.arange(64) if par == 0 else np.concatenate([np.arange(1, 64), [0]])
        xb = x[b].reshape(64, 128, 1024)[order].reshape(64 * 128, 1024)
        pb = positions[b].reshape(64, 128)[order]
        posc = np.ascontiguousarray(pb.T)
        prev0 = tri_prev * float(par)
        p63 = np.full((128, 128), 0.0 if par == 1 else NEG, np.float32)
        cmask = np.ascontiguousarray(np.stack([tri_cur, tri_prev, prev0, dbias, p63], 1).reshape(128, 5 * 128).astype(np.float32))
        in_maps.append(dict(xc=np.ascontiguousarray(xb), posc=posc, memc=np.ascontiguousarray(mem[b]), cmask=cmask, invf=invf,
                            wk=wk, wch=wch, wmem=wmem, gvec=gvec, gfin=gfin, bgate=bgate, sinkr=sinkr))
    if "nc" not in _NC_CACHE:
        _NC_CACHE["nc"] = build_program()
    nc = _NC_CACHE["nc"]
    res = run_bass_kernel_spmd(nc, in_maps, core_ids=list(range(8)))
    out = np.zeros((4, 64, 128, 1024), np.float32)
    for c in range(8):
        b, par = c // 2, c % 2
        o = np.asarray(res.results[c]["out"]).reshape(32, 128, 1024)
        out[b, par::2] = o
    return out.reshape(4, 8192, 1024)
```
